# Optimizing a Trainium2 kernel written in Bass

```python
import math
import jax
import jax.numpy as jnp
from jax import lax
import numpy as np

D_MODEL = 1024
BATCH = 4
SEQ = 8192
DEPTH = 2

GRID_W = 64
CTX_LEN = 256
BRANCH_W = 512
N_BRANCH = 3
RWKV_HEADS = 8
RWKV_HEAD = BRANCH_W // RWKV_HEADS
LORA_W = 64
LORA_A = 64
LORA_G = 128
RWKV_COLS = 3 * BRANCH_W + LORA_W + LORA_A + LORA_G
RWKV_GN_EPS = 64e-5
NA_HEADS = 8
NA_HEAD = BRANCH_W // NA_HEADS
NA_KH = 8
NA_KW = 16
NA_COLS = 3 * BRANCH_W
S5_GROUP = 16
S5_GROUPS = BRANCH_W // S5_GROUP
S5_STATE = 64
GATE_COLS = N_BRANCH * D_MODEL
N_IN = RWKV_COLS + NA_COLS + BRANCH_W + GATE_COLS
N_EXPERTS = 32
TOP_K = 4
D_EXPERT = D_MODEL
SWIGLU_LIMIT = 7.0
SWIGLU_ALPHA = 1.702
MOE_BLOCK = 128
NORM_EPS = 1e-6

kernel_name = 'hybrid_rwkv7_natten_s5_moe_dit'


def rmsnorm(x, g):
    xf = x.astype(jnp.float32)
    y = xf * lax.rsqrt(jnp.mean(xf * xf, axis=-1, keepdims=True) + NORM_EPS)
    return (y * g.astype(jnp.float32)).astype(x.dtype)


def token_shift(y, mu_prev, mu_next):
    zero = jnp.zeros_like(y[:, :1])
    prev = jnp.concatenate([zero, y[:, :-1]], axis=1)
    nxt = jnp.concatenate([y[:, 1:], zero], axis=1)
    return y + mu_prev * (prev - y) + mu_next * (nxt - y)


def rwkv_prepare(p, lp):
    z = token_shift(p, lp['rwkv_mu_prev'], lp['rwkv_mu_next']).astype(jnp.float32)
    b, t = z.shape[:2]
    heads = lambda a: a.reshape(b, t, RWKV_HEADS, RWKV_HEAD)
    o = [0, BRANCH_W, 2 * BRANCH_W, 3 * BRANCH_W, 3 * BRANCH_W + LORA_W,
         3 * BRANCH_W + LORA_W + LORA_A, RWKV_COLS]
    r, k, v, wd, ad, gd = [z[..., o[j]:o[j + 1]] for j in range(6)]
    kk = heads(k * lp['rwkv_k_k'])
    kk = kk / jnp.maximum(jnp.sqrt(jnp.sum(kk * kk, -1, keepdims=True)), 1e-12)
    g = jax.nn.sigmoid(gd) @ lp['rwkv_g2']
    dirs = []
    for d in range(2):
        w = -jax.nn.softplus(-(lp['rwkv_w0'][d] + jnp.tanh(wd) @ lp['rwkv_w2'][d])) - 0.5
        a = jax.nn.sigmoid(lp['rwkv_a0'][d] + ad @ lp['rwkv_a2'][d])
        k_d = k * (1.0 + (a - 1.0) * lp['rwkv_k_a'])
        dirs.append((heads(jnp.exp(-jnp.exp(w))), heads(a), heads(k_d)))
    return heads(r), heads(v), kk, g, dirs


def rwkv_scan(prep, d, s0):
    r, v, kk, _, dirs = prep
    decay, a, k = dirs[d]

    def step(s, inp):
        r_t, w_t, kk_t, a_t, k_t, v_t = inp
        sa = jnp.einsum('bhvk,bhk->bhv', s, -kk_t)
        s = (s * w_t[:, :, None, :] + sa[..., None] * (kk_t * a_t)[:, :, None, :]
             + v_t[..., None] * k_t[:, :, None, :])
        return s, jnp.einsum('bhvk,bhk->bhv', s, r_t)

    xs = tuple(jnp.moveaxis(u, 1, 0) for u in (r, decay, kk, a, k, v))
    s_last, ys = lax.scan(step, s0, xs, reverse=(d == 1))
    return jnp.moveaxis(ys, 0, 1), s_last


def rwkv_readout(ys, prep, lp):
    r, v, _, g, dirs = prep
    b, t = r.shape[:2]
    y = ys[0] + ys[1]
    mu = jnp.mean(y, -1, keepdims=True)
    var = jnp.mean(jnp.square(y - mu), -1, keepdims=True)
    yn = ((y - mu) * lax.rsqrt(var + RWKV_GN_EPS)).reshape(b, t, BRANCH_W)
    bonus = sum(jnp.sum(r * dirs[d][2] * lp['rwkv_r_k'], -1, keepdims=True) for d in range(2)) * v
    return (yn * lp['rwkv_ln_w'] + lp['rwkv_ln_b'] + bonus.reshape(b, t, BRANCH_W)) * g


def rwkv_branch(pc, pl, lp, ctx_out):
    prep_c = rwkv_prepare(pc, lp)
    prep_l = rwkv_prepare(pl, lp)
    s0 = jnp.zeros((pl.shape[0], RWKV_HEADS, RWKV_HEAD, RWKV_HEAD), jnp.float32)
    ys_c, ys_l = [], []
    for d in range(2):
        y_c, s_ctx = rwkv_scan(prep_c, d, s0)
        y_l, _ = rwkv_scan(prep_l, d, s_ctx)
        ys_c.append(y_c)
        ys_l.append(y_l)
    out_c = rwkv_readout(ys_c, prep_c, lp) if ctx_out else None
    return out_c, rwkv_readout(ys_l, prep_l, lp)


def na_branch(pc, pl, rpb, ctx_out):
    b, s = pl.shape[:2]
    l = pc.shape[1]
    rows = s // GRID_W
    kh = min(NA_KH, rows)
    scale = NA_HEAD ** -0.5

    def split_heads(p):
        return [u.reshape(u.shape[0], u.shape[1], NA_HEADS, NA_HEAD) for u in jnp.split(p, 3, axis=-1)]

    qc, kc, vc = split_heads(pc)
    ql, kl, vl = split_heads(pl)
    kg = kl.reshape(b, rows, GRID_W, NA_HEADS, NA_HEAD)
    vg = vl.reshape(b, rows, GRID_W, NA_HEADS, NA_HEAD)
    q_rows = jnp.moveaxis(ql.reshape(b, rows, GRID_W, NA_HEADS, NA_HEAD), 1, 0)
    cols = np.arange(GRID_W)
    col_start = np.clip(cols - NA_KW // 2, 0, GRID_W - NA_KW)
    col_idx = col_start[:, None] + np.arange(NA_KW)[None, :]
    col_off = col_idx - cols[:, None] + NA_KW - 1

    def row_block(args):
        q_r, r = args
        rs = jnp.clip(r - kh // 2, 0, rows - kh)
        kb = lax.dynamic_slice_in_dim(kg, rs, kh, axis=1)[:, :, col_idx]
        vb = lax.dynamic_slice_in_dim(vg, rs, kh, axis=1)[:, :, col_idx]
        row_off = rs + jnp.arange(kh) - r + NA_KH - 1
        bias = rpb[:, row_off[None, :, None], col_off[:, None, :]]
        s_loc = jnp.einsum('bqhd,biqjhd->bhqij', q_r, kb) * scale + bias[None]
        s_ctx = jnp.einsum('bqhd,blhd->bhql', q_r, kc) * scale
        sc = jnp.concatenate([s_loc.reshape(b, NA_HEADS, GRID_W, kh * NA_KW), s_ctx], axis=-1)
        pr = jax.nn.softmax(sc.astype(jnp.float32), axis=-1)
        p_loc = pr[..., :kh * NA_KW].reshape(b, NA_HEADS, GRID_W, kh, NA_KW)
        p_ctx = pr[..., kh * NA_KW:]
        return (jnp.einsum('bhqij,biqjhd->bqhd', p_loc, vb)
                + jnp.einsum('bhql,blhd->bqhd', p_ctx, vc))

    o_rows = lax.map(row_block, (q_rows, jnp.arange(rows)))
    out_l = jnp.moveaxis(o_rows, 0, 1).reshape(b, s, BRANCH_W)
    out_c = None
    if ctx_out:
        p_c = jax.nn.softmax((jnp.einsum('bqhd,bkhd->bhqk', qc, kc) * scale).astype(jnp.float32), axis=-1)
        out_c = jnp.einsum('bhqk,bkhd->bqhd', p_c, vc).reshape(b, l, BRANCH_W)
    return out_c, out_l


def s5_discretize(lam_re, lam_im, log_dt, b_re, b_im):
    lam_re, lam_im, b_re, b_im = (u.astype(jnp.float32) for u in (lam_re, lam_im, b_re, b_im))
    dt = jnp.exp(log_dt.astype(jnp.float32))[:, None]
    mag = jnp.exp(lam_re * dt)
    ar = mag * jnp.cos(lam_im * dt)
    ai = mag * jnp.sin(lam_im * dt)
    den = lam_re * lam_re + lam_im * lam_im
    cr = ((ar - 1.0) * lam_re + ai * lam_im) / den
    ci = (ai * lam_re - (ar - 1.0) * lam_im) / den
    bbr = cr[..., None] * b_re - ci[..., None] * b_im
    bbi = cr[..., None] * b_im + ci[..., None] * b_re
    return ar, ai, bbr, bbi


def s5_combine(e1, e2):
    ar1, ai1, br1, bi1 = e1
    ar2, ai2, br2, bi2 = e2
    return (ar1 * ar2 - ai1 * ai2, ar1 * ai2 + ai1 * ar2,
            ar2 * br1 - ai2 * bi1 + br2, ar2 * bi1 + ai2 * br1 + bi2)


def s5_scan(u, disc, c_re, c_im, x0r, x0i, reverse):
    ar, ai, bbr, bbi = disc
    ut = jnp.moveaxis(u, 1, 0)
    if reverse:
        ut = ut[::-1]
    br = jnp.einsum('gpc,tbgc->tbgp', bbr, ut).at[0].add(ar * x0r - ai * x0i)
    bi = jnp.einsum('gpc,tbgc->tbgp', bbi, ut).at[0].add(ar * x0i + ai * x0r)
    shape = (ut.shape[0], 1) + ar.shape
    _, _, xr, xi = lax.associative_scan(
        s5_combine, (jnp.broadcast_to(ar, shape), jnp.broadcast_to(ai, shape), br, bi), axis=0)
    y = jnp.einsum('gcp,tbgp->tbgc', c_re, xr) - jnp.einsum('gcp,tbgp->tbgc', c_im, xi)
    if reverse:
        y = y[::-1]
    return jnp.moveaxis(y, 0, 1), xr[-1], xi[-1]


def s5_branch(uc, ul, lp, ctx_out):
    def groups(u):
        return u.astype(jnp.float32).reshape(u.shape[0], u.shape[1], S5_GROUPS, S5_GROUP)

    uc, ul = groups(uc), groups(ul)
    zero = jnp.zeros((ul.shape[0], S5_GROUPS, S5_STATE), jnp.float32)
    yc, yl = 0.0, 0.0
    for d in range(2):
        disc = s5_discretize(lp['s5_lambda_re'][d], lp['s5_lambda_im'][d], lp['s5_log_dt'][d],
                             lp['s5_b_re'][d], lp['s5_b_im'][d])
        y_c, xr, xi = s5_scan(uc, disc, lp['s5_c_re'][d], lp['s5_c_im'][d], zero, zero, d == 1)
        y_l, _, _ = s5_scan(ul, disc, lp['s5_c_re'][d], lp['s5_c_im'][d], xr, xi, d == 1)
        yc, yl = yc + y_c, yl + y_l

    def readout(y, u):
        bb, t = u.shape[:2]
        y = (y + u * lp['s5_d'].reshape(S5_GROUPS, S5_GROUP)).reshape(bb, t, BRANCH_W)
        y = jax.nn.gelu(y)
        return y * jax.nn.sigmoid(y @ lp['s5_glu_w'] + lp['s5_glu_b'])

    return (readout(yc, uc) if ctx_out else None), readout(yl, ul)


def mixer(hc, hl, lp, ctx_out):
    pc = hc @ lp['w_in']
    pl = hl @ lp['w_in']
    o1 = RWKV_COLS
    o2 = o1 + NA_COLS
    o3 = o2 + BRANCH_W
    a_c, a_l = rwkv_branch(pc[..., :o1], pl[..., :o1], lp, ctx_out)
    n_c, n_l = na_branch(pc[..., o1:o2], pl[..., o1:o2], lp['na_rpb'], ctx_out)
    s_c, s_l = s5_branch(pc[..., o2:o3], pl[..., o2:o3], lp, ctx_out)

    def merge(p, outs):
        y = 0.0
        for j in range(N_BRANCH):
            gate = jax.nn.sigmoid(p[..., o3 + j * D_MODEL:o3 + (j + 1) * D_MODEL])
            y = y + gate * (outs[j] @ lp['w_branch'][j])
        return y @ lp['w_out']

    out_c = merge(pc, (a_c, n_c, s_c)) if ctx_out else None
    return out_c, merge(pl, (a_l, n_l, s_l))


def moe(h, router_w, router_b, gu_w, gu_b, dn_w, dn_b):
    n, d = h.shape
    logits = (h @ router_w + router_b).astype(jnp.float32)
    top_val, top_idx = lax.top_k(logits, TOP_K)
    weights = jax.nn.softmax(top_val, axis=-1).reshape(-1)
    e_flat = top_idx.reshape(-1)
    n_assign = n * TOP_K
    order = jnp.argsort(e_flat)
    e_sorted = e_flat[order]
    counts = jnp.bincount(e_flat, length=N_EXPERTS)
    group_start = jnp.cumsum(counts) - counts
    padded = (counts + MOE_BLOCK - 1) // MOE_BLOCK * MOE_BLOCK
    pad_end = jnp.cumsum(padded)
    pad_start = pad_end - padded
    dest = pad_start[e_sorted] + jnp.arange(n_assign) - group_start[e_sorted]
    n_blocks = -(-n_assign // MOE_BLOCK) + N_EXPERTS
    n_slots = n_blocks * MOE_BLOCK
    slot_tok = jnp.full((n_slots,), n, jnp.int32).at[dest].set((order // TOP_K).astype(jnp.int32))
    slot_w = jnp.zeros((n_slots,), jnp.float32).at[dest].set(weights[order])
    block_expert = jnp.minimum(
        jnp.searchsorted(pad_end, jnp.arange(n_blocks) * MOE_BLOCK, side='right'), N_EXPERTS - 1)
    h_pad = jnp.concatenate([h, jnp.zeros((1, d), h.dtype)], axis=0)

    def expert_block(args):
        tok, e = args
        xb = h_pad[tok]
        gu = xb @ gu_w[e] + gu_b[e]
        glu, lin = jnp.split(gu, 2, axis=-1)
        glu = jnp.minimum(glu, SWIGLU_LIMIT)
        lin = jnp.clip(lin, -SWIGLU_LIMIT, SWIGLU_LIMIT)
        act = glu * jax.nn.sigmoid(SWIGLU_ALPHA * glu) * (lin + 1.0)
        return act @ dn_w[e] + dn_b[e]

    yb = lax.map(expert_block, (slot_tok.reshape(n_blocks, MOE_BLOCK), block_expert))
    yb = yb.reshape(n_slots, d)
    y = jnp.zeros((n + 1, d), yb.dtype).at[slot_tok].add(yb * slot_w[:, None].astype(yb.dtype))
    return y[:n]


def setup_inputs(seed: int = 0) -> dict:
    key = jax.random.key(seed)
    keys = iter(jax.random.split(key, 64))

    def nrm(shape, scale):
        return jax.random.normal(next(keys), shape, jnp.float32) * scale

    def uni(shape, lo, hi):
        return jax.random.uniform(next(keys), shape, jnp.float32, lo, hi)

    D, L, E, F = D_MODEL, DEPTH, N_EXPERTS, D_EXPERT
    G, P, C = S5_GROUPS, S5_STATE, S5_GROUP
    return {
        'x': nrm((BATCH, SEQ, D), 1.0),
        'c': nrm((BATCH, D), 1.0),
        'ctx': nrm((BATCH, CTX_LEN, D), 1.0),
        'c_ctx': nrm((D,), 1.0),
        'ada_w': nrm((L, D, 6 * D), 0.5 * D ** -0.5),
        'ada_b': nrm((L, 6 * D), 0.02),
        'norm1_g': 1.0 + nrm((L, D), 0.02),
        'norm2_g': 1.0 + nrm((L, D), 0.02),
        'w_in': nrm((L, D, N_IN), D ** -0.5),
        'rwkv_mu_prev': uni((L, RWKV_COLS), 0.0, 0.5),
        'rwkv_mu_next': uni((L, RWKV_COLS), 0.0, 0.5),
        'rwkv_w0': uni((L, 2, BRANCH_W), -6.0, -1.0),
        'rwkv_w2': nrm((L, 2, LORA_W, BRANCH_W), 0.1 * LORA_W ** -0.5),
        'rwkv_a0': nrm((L, 2, BRANCH_W), 0.1),
        'rwkv_a2': nrm((L, 2, LORA_A, BRANCH_W), 0.5 * LORA_A ** -0.5),
        'rwkv_g2': nrm((L, LORA_G, BRANCH_W), LORA_G ** -0.5),
        'rwkv_k_k': 0.85 + nrm((L, BRANCH_W), 0.02),
        'rwkv_k_a': 1.0 + nrm((L, BRANCH_W), 0.02),
        'rwkv_r_k': nrm((L, RWKV_HEADS, RWKV_HEAD), 0.1),
        'rwkv_ln_w': 1.0 + nrm((L, BRANCH_W), 0.02),
        'rwkv_ln_b': nrm((L, BRANCH_W), 0.02),
        'na_rpb': nrm((L, NA_HEADS, 2 * NA_KH - 1, 2 * NA_KW - 1), 0.1),
        's5_lambda_re': -0.5 + nrm((L, 2, G, P), 0.01),
        's5_lambda_im': jnp.pi * jnp.arange(P, dtype=jnp.float32) + nrm((L, 2, G, P), 0.01),
        's5_log_dt': uni((L, 2, G), math.log(1e-3), math.log(1e-1)),
        's5_b_re': nrm((L, 2, G, P, C), (2 * C) ** -0.5),
        's5_b_im': nrm((L, 2, G, P, C), (2 * C) ** -0.5),
        's5_c_re': nrm((L, 2, G, C, P), P ** -0.5),
        's5_c_im': nrm((L, 2, G, C, P), P ** -0.5),
        's5_d': nrm((L, BRANCH_W), 1.0),
        's5_glu_w': nrm((L, BRANCH_W, BRANCH_W), BRANCH_W ** -0.5),
        's5_glu_b': nrm((L, BRANCH_W), 0.02),
        'w_branch': nrm((L, N_BRANCH, BRANCH_W, D), BRANCH_W ** -0.5),
        'w_out': nrm((L, D, D), D ** -0.5),
        'router_w': nrm((L, D, E), D ** -0.5),
        'router_b': nrm((L, E), 0.01),
        'expert_gu_w': nrm((L, E, D, 2 * F), D ** -0.5),
        'expert_gu_b': nrm((L, E, 2 * F), 0.01),
        'expert_dn_w': nrm((L, E, F, D), F ** -0.5),
        'expert_dn_b': nrm((L, E, D), 0.01),
        'final_g': 1.0 + nrm((D,), 0.02),
    }


def reference(x, c, ctx, c_ctx, ada_w, ada_b, norm1_g, norm2_g, w_in, rwkv_mu_prev, rwkv_mu_next,
              rwkv_w0, rwkv_w2, rwkv_a0, rwkv_a2, rwkv_g2, rwkv_k_k, rwkv_k_a, rwkv_r_k, rwkv_ln_w,
              rwkv_ln_b, na_rpb, s5_lambda_re, s5_lambda_im, s5_log_dt, s5_b_re, s5_b_im, s5_c_re,
              s5_c_im, s5_d, s5_glu_w, s5_glu_b, w_branch, w_out, router_w, router_b, expert_gu_w,
              expert_gu_b, expert_dn_w, expert_dn_b, final_g):
    xl, xc = x, ctx
    c_act = jax.nn.silu(c)
    c_ctx_act = jax.nn.silu(c_ctx)
    for i in range(DEPTH):
        last = i == DEPTH - 1
        lp = {
            'w_in': w_in[i], 'rwkv_mu_prev': rwkv_mu_prev[i], 'rwkv_mu_next': rwkv_mu_next[i],
            'rwkv_w0': rwkv_w0[i], 'rwkv_w2': rwkv_w2[i], 'rwkv_a0': rwkv_a0[i], 'rwkv_a2': rwkv_a2[i],
            'rwkv_g2': rwkv_g2[i], 'rwkv_k_k': rwkv_k_k[i], 'rwkv_k_a': rwkv_k_a[i],
            'rwkv_r_k': rwkv_r_k[i], 'rwkv_ln_w': rwkv_ln_w[i], 'rwkv_ln_b': rwkv_ln_b[i],
            'na_rpb': na_rpb[i],
            's5_lambda_re': s5_lambda_re[i], 's5_lambda_im': s5_lambda_im[i], 's5_log_dt': s5_log_dt[i],
            's5_b_re': s5_b_re[i], 's5_b_im': s5_b_im[i], 's5_c_re': s5_c_re[i], 's5_c_im': s5_c_im[i],
            's5_d': s5_d[i], 's5_glu_w': s5_glu_w[i], 's5_glu_b': s5_glu_b[i],
            'w_branch': w_branch[i], 'w_out': w_out[i],
        }
        m_l = jnp.split((c_act @ ada_w[i] + ada_b[i])[:, None, :], 6, axis=-1)
        m_c = jnp.split(c_ctx_act @ ada_w[i] + ada_b[i], 6, axis=-1)
        hl = rmsnorm(xl, norm1_g[i]) * (1.0 + m_l[1]) + m_l[0]
        hc = rmsnorm(xc, norm1_g[i]) * (1.0 + m_c[1]) + m_c[0]
        oc, ol = mixer(hc, hl, lp, not last)
        xl = xl + m_l[2] * ol
        hl = rmsnorm(xl, norm2_g[i]) * (1.0 + m_l[4]) + m_l[3]
        moe_args = (router_w[i], router_b[i], expert_gu_w[i], expert_gu_b[i], expert_dn_w[i], expert_dn_b[i])
        if last:
            b, s, d = hl.shape
            xl = xl + m_l[5] * moe(hl.reshape(b * s, d), *moe_args).reshape(b, s, d)
        else:
            xc = xc + m_c[2] * oc
            hc = rmsnorm(xc, norm2_g[i]) * (1.0 + m_c[4]) + m_c[3]
            b, l, d = hc.shape
            s = hl.shape[1]
            y = moe(jnp.concatenate([hc, hl], axis=1).reshape(b * (l + s), d), *moe_args).reshape(b, l + s, d)
            xc = xc + m_c[5] * y[:, :l]
            xl = xl + m_l[5] * y[:, l:]
    return rmsnorm(xl, final_g)
```

```python
import numpy as np
from contextlib import ExitStack
import concourse.bass as bass
import concourse.mybir as mybir
from concourse.bass_utils import run_bass_kernel_spmd

F32 = mybir.dt.float32
BF16 = mybir.dt.bfloat16
I32 = mybir.dt.int32
ALU = mybir.AluOpType
AF = mybir.ActivationFunctionType
AX = mybir.AxisListType

D = 1024
NCTX = 256
SEQ = 8192
T = NCTX + SEQ
TT = T + NCTX
NIN = 6912
DEPTH = 2
KT = D // 128
O_R, O_K, O_V, O_WD, O_AD, O_GD = 0, 512, 1024, 1536, 1600, 1664
O_NQ, O_NK, O_NV = 1792, 2304, 2816
O_S5 = 3328
O_G = 3840


NOSYNC_SAME = ('pe',)


class Prog:
    ENGS = ('pe', 'dve', 'act', 'pool', 'sp')

    def __init__(self, nc, es, n_dma_sems=10):
        self.nc = nc
        self.lists = {e: [] for e in self.ENGS}
        self.sems = {e: es.enter_context(nc.semaphore('s_' + e)) for e in self.ENGS}
        self.cnt = {e: 0 for e in self.ENGS}
        self.seen = {e: {} for e in self.ENGS}
        self.lastw = {}
        self.readers = {}
        self.dma_sems, self.dma_cnt, self.dma_rr = {}, {}, {}
        for q in ('sp', 'act', 'pool'):
            self.dma_sems[q] = [es.enter_context(nc.semaphore('d_%s%d' % (q, i))) for i in range(n_dma_sems)]
            self.dma_cnt[q] = [0] * n_dma_sems
            self.dma_rr[q] = 0
        self.semobj = dict(self.sems)
        for q in self.dma_sems:
            for i, s in enumerate(self.dma_sems[q]):
                self.semobj[(q, i)] = s

    def _deps(self, eng, reads, writes):
        evs = []
        for k in reads:
            if k in self.lastw:
                evs.append(self.lastw[k])
        for k in writes:
            if k in self.lastw:
                evs.append(self.lastw[k])
            evs.extend(self.readers.get(k, ()))
        waits = {}
        for (sk, v) in evs:
            if sk == eng and eng in NOSYNC_SAME:
                continue
            if self.seen[eng].get(sk, 0) >= v:
                continue
            if waits.get(sk, 0) < v:
                waits[sk] = v
        for sk, v in waits.items():
            self.seen[eng][sk] = v
        return list(waits.items())

    def _commit(self, ev, reads, writes):
        for k in writes:
            self.lastw[k] = ev
            self.readers[k] = []
        for k in reads:
            self.readers.setdefault(k, []).append(ev)

    def op(self, eng, fn, reads=(), writes=()):
        waits = self._deps(eng, reads, writes)
        self.cnt[eng] += 1
        ev = (eng, self.cnt[eng])
        self.lists[eng].append((waits, fn, eng, 1))
        self._commit(ev, reads, writes)
        return ev

    def dma(self, q, out, in_, reads=(), writes=(), **kw):
        i = self.dma_rr[q]
        self.dma_rr[q] = (i + 1) % len(self.dma_sems[q])
        sk = (q, i)
        waits = self._deps(q, reads, writes)
        prev = self.dma_cnt[q][i]
        if prev > 0 and self.seen[q].get(sk, 0) < prev:
            waits.append((sk, prev))
            self.seen[q][sk] = prev
        self.dma_cnt[q][i] += 16
        ev = (sk, self.dma_cnt[q][i])
        fn = lambda e, out=out, in_=in_, kw=kw: e.dma_start(out=out, in_=in_, **kw)
        self.lists[q].append((waits, fn, sk, 16))
        self._commit(ev, reads, writes)
        return ev

    def dma_fn(self, q, fn, reads=(), writes=()):
        i = self.dma_rr[q]
        self.dma_rr[q] = (i + 1) % len(self.dma_sems[q])
        sk = (q, i)
        waits = self._deps(q, reads, writes)
        prev = self.dma_cnt[q][i]
        if prev > 0 and self.seen[q].get(sk, 0) < prev:
            waits.append((sk, prev))
            self.seen[q][sk] = prev
        self.dma_cnt[q][i] += 16
        ev = (sk, self.dma_cnt[q][i])
        self.lists[q].append((waits, fn, sk, 16))
        self._commit(ev, reads, writes)
        return ev

    def barrier(self):
        evs = []
        for e in self.ENGS:
            if self.cnt[e] > 0:
                evs.append((e, self.cnt[e]))
        for q in self.dma_sems:
            for i, c in enumerate(self.dma_cnt[q]):
                if c > 0:
                    evs.append(((q, i), c))
        for e in self.ENGS:
            waits = []
            for (sk, v) in evs:
                if sk == e:
                    continue
                if self.seen[e].get(sk, 0) < v:
                    waits.append((sk, v))
                    self.seen[e][sk] = v
            if waits:
                self.lists[e].append((waits, None, None, 0))

    def wait_all(self, eng='sp'):
        waits = []
        for e in self.ENGS:
            if e != eng and self.cnt[e] > 0:
                waits.append((e, self.cnt[e]))
        for q in self.dma_sems:
            for i, c in enumerate(self.dma_cnt[q]):
                if c > 0:
                    waits.append(((q, i), c))
        self.lists[eng].append((waits, None, None, 0))

    def emit(self):
        nc = self.nc
        names = {'pe': 'tensor', 'dve': 'vector', 'act': 'scalar', 'pool': 'gpsimd', 'sp': 'sync'}
        with nc.Block() as block:
            for e in self.ENGS:
                lst = self.lists[e]

                def body(engobj, lst=lst):
                    for (waits, fn, sk, inc) in lst:
                        for (wk, v) in waits:
                            engobj.wait_ge(self.semobj[wk], v)
                        if fn is not None:
                            ins = fn(engobj)
                            ins.then_inc(self.semobj[sk], inc)
                getattr(block, names[e])(body)


class Ctx:
    pass


_UNIQ = [0]


def mk_alloc(nc, es):
    _UNIQ[0] += 1
    sfx = "_u%d" % _UNIQ[0]
    sb = lambda name, shape, dt: es.enter_context(nc.sbuf_tensor(name + sfx, shape, dt))
    ps = lambda name, shape, dt: es.enter_context(nc.psum_tensor(name + sfx, shape, dt))
    return sb, ps


def declare_io(nc, stage):
    g = Ctx()
    def inp(name, shape, dt=F32):
        t = nc.dram_tensor(name, list(shape), dt, kind="ExternalInput").ap()
        setattr(g, name, t)
        return t
    def scr(name, shape, dt=F32):
        t = nc.dram_tensor(name, list(shape), dt, kind="Internal").ap()
        setattr(g, name, t)
        return t
    g.inp, g.scr = inp, scr
    inp("xin", [T, D])
    inp("cvec", [128, 2 * KT])
    inp("ada_w", [DEPTH, D, 6 * D])
    inp("ada_b", [DEPTH, 6 * D])
    inp("norm1_g", [DEPTH, 128, KT])
    inp("norm2_g", [DEPTH, 128, KT])
    inp("w_in", [DEPTH, D, NIN])
    inp("c_ident", [128, 128])
    inp("c_ones", [128, 128])
    scr("XR", [T, D])
    scr("MOD", [DEPTH, 2, 6 * D])
    scr("RWT", [1792, TT])
    scr("UB", [32, 8, 16, TT // 8])
    scr("NAQT", [512, T], BF16)
    scr("NAKT", [512, T], BF16)
    scr("NAV", [T, 512], BF16)
    scr("GT", [3072, T], BF16)
    inp("na_tab", [DEPTH, 5, 8, 576, 128])
    scr("NAO", [T, 512], BF16)
    inp("w_branch", [DEPTH, 3, 512, D])
    inp("w_out", [DEPTH, D, D])
    inp("c_tri", [128, 128]); inp("c_iota", [128, NE])
    inp("router_w", [DEPTH, D, NE]); inp("router_b", [DEPTH, NE])
    inp("norm2_row", [DEPTH, D]); inp("final_row", [1, D])
    inp("expert_gu_w", [DEPTH, NE, D, 2 * D]); inp("expert_dn_w", [DEPTH, NE, D, D])
    inp("gu_b", [DEPTH, NE, 128, 16]); inp("expert_dn_b", [DEPTH, NE, D])
    inp("c_ii", [128, 128])
    inp("c_bo", [128, 128]); inp("c_hi", [128, 2]); inp("c_masks", [128, 8, 128])
    inp("rw_mup", [DEPTH, 128, 14]); inp("rw_mun", [DEPTH, 128, 14]); inp("rw_pv", [DEPTH, 128, 3, 4]); inp("rw_wa0", [DEPTH, 128, 16])
    inp("rwkv_w2", [DEPTH, 2, 64, 512]); inp("rwkv_a2", [DEPTH, 2, 64, 512]); inp("rwkv_g2", [DEPTH, 128, 512])
    inp("rwkv_ln_w", [DEPTH, 512]); inp("rwkv_ln_b", [DEPTH, 512])
    scr("YR", [T, 512]); scr("BON", [T, 8])
    for nm in ("s5_lr", "s5_li", "s5_ldt"):
        inp(nm, [DEPTH, 128, 64])
    for nm in ("s5_br", "s5_bi", "s5_cr", "s5_ci"):
        inp(nm, [DEPTH, 128, 64, 16])
    inp("s5_dv", [DEPTH, 128, 32])
    inp("s5_glu_w", [DEPTH, 512, 512]); inp("s5_glu_bp", [DEPTH, 128, 4])
    scr("YB", [32, 8, 16, T // 8])
    scr("XS", [NE * CAP, D], BF16); scr("YS", [NE * CAP, D], BF16)
    if stage in ("merge", "moe"):
        inp("AO", [T, 512], BF16); inp("SO", [T, 512], BF16)
    else:
        scr("AO", [T, 512], BF16); scr("SO", [T, 512], BF16)
    return g


def phase0(nc, P, g, es0):
    with ExitStack() as es:
        sb, ps = mk_alloc(nc, es)
        cv = sb("p0_cv", [128, 2 * KT], F32)
        cs = sb("p0_cs", [128, 2 * KT], F32)
        aw = [sb("p0_aw%d" % i, [128, KT, 512], F32) for i in range(2)]
        ab = sb("p0_ab", [1, 6 * D], F32)
        row = [sb("p0_row%d" % i, [1, 6 * D], F32) for i in range(2)]
        pr = [ps("p0_pr%d" % i, [1, 512], F32) for i in range(2)]
        P.dma('sp', cv[:], g.cvec[:, :], writes=['p0_cv'])
        P.op('act', lambda e: e.activation(out=cs[:], in_=cv[:], func=AF.Silu), reads=['p0_cv'], writes=['p0_cs'])
        for l in range(DEPTH):
            P.dma('sp', ab[:], g.ada_b[l:l + 1, :], reads=[], writes=['p0_ab'])
            for cch in range(12):
                a = aw[cch % 2]
                ak = 'p0_aw%d' % (cch % 2)
                P.dma('sp', a[:], g.ada_w[l, :, cch * 512:(cch + 1) * 512].rearrange("(k p) n -> p k n", p=128),
                      writes=[ak])
                for v in range(2):
                    for k in range(KT):
                        P.op('pe', lambda e, v=v, k=k, a=a: e.matmul(pr[v][:], lhsT=cs[:, v * KT + k:v * KT + k + 1],
                                                                    rhs=a[:, k, :], start=(k == 0), stop=(k == KT - 1)),
                             reads=['p0_cs', ak], writes=['p0_pr%d' % v])
                    P.op('dve', lambda e, v=v, cch=cch: e.tensor_tensor(out=row[v][:, cch * 512:(cch + 1) * 512],
                                                                        in0=pr[v][:], in1=ab[:, cch * 512:(cch + 1) * 512],
                                                                        op=ALU.add),
                         reads=['p0_pr%d' % v, 'p0_ab'], writes=['p0_row%d' % v])
            for v in range(2):
                P.dma('sp', g.MOD[l, v:v + 1, :], row[v][:], reads=['p0_row%d' % v], writes=['MOD'])


def load_mod_pp(nc, P, g, l, es, sb, tag):
    m = sb(tag + "_modpp", [128, 2, 6, KT], F32)
    for v in range(2):
        for j in range(6):
            P.dma('sp', m[:, v, j, :], g.MOD[l, v, j * D:(j + 1) * D].rearrange("(k p) -> p k", p=128),
                  reads=['MOD'], writes=[tag + '_modpp'], allow_slow_non_contiguous=True)
    return m


def phase1(nc, P, g, l, x_src):
    with ExitStack() as es:
        sb, ps = mk_alloc(nc, es)
        wb = sb("p1_w", [128, KT, NIN], BF16)
        for k in range(KT):
            for c0 in range(0, NIN, 1728):
                P.dma('pool', wb[:, k, c0:c0 + 1728], g.w_in[l, k * 128:(k + 1) * 128, c0:c0 + 1728], writes=['p1_w'])
        ident = sb("p1_ident", [128, 128], BF16)
        P.dma('pool', ident[:], g.c_ident[:, :], writes=['p1_ident'])
        m = load_mod_pp(nc, P, g, l, es, sb, "p1")
        g1 = sb("p1_g1", [128, KT], F32)
        P.dma('sp', g1[:], g.norm1_g[l, :, :], writes=['p1_g1'])
        A1 = sb("p1_A1", [128, 2, KT], F32)
        for v in range(2):
            P.op('dve', lambda e, v=v: e.scalar_tensor_tensor(out=A1[:, v, :], in0=m[:, v, 1, :], scalar=1.0, in1=g1[:],
                                                              op0=ALU.add, op1=ALU.mult),
                 reads=['p1_modpp', 'p1_g1'], writes=['p1_A1'])
        xt = [sb("p1_xt%d" % i, [128, D], F32) for i in range(2)]
        junk = sb("p1_junk", [128, D], F32)
        xb = [sb("p1_xb%d" % i, [128, D], BF16) for i in range(2)]
        ss = sb("p1_ss", [128, 4], F32)
        hT = [sb("p1_hT%d" % i, [128, KT, 512], BF16) for i in range(2)]
        ptr = [ps("p1_ptr%d" % i, [128, KT, 128], BF16) for i in range(2)]
        pp = [ps("p1_pp%d" % i, [128, 512], F32) for i in range(4)]
        ev = [sb("p1_ev%d" % i, [128, 512], F32) for i in range(4)]
        evb = [sb("p1_evb%d" % i, [128, 512], BF16) for i in range(4)]
        evu = [sb("p1_evu%d" % i, [128, 8, 64], F32) for i in range(2)]
        nblk = (T + 511) // 512
        ntile = T // 128
        cnt = 0
        for b in range(nblk):
            tiles = [t for t in range(4 * b, min(4 * b + 4, ntile))]
            nt = len(tiles)
            ntok = nt * 128
            h = hT[b % 2]
            hk = 'p1_hT%d' % (b % 2)
            for ti, t in enumerate(tiles):
                v = 1 if t < 2 else 0
                x_ = xt[t % 2]; xk = 'p1_xt%d' % (t % 2)
                xb_ = xb[t % 2]; xbk = 'p1_xb%d' % (t % 2)
                pt_ = ptr[t % 2]; ptk = 'p1_ptr%d' % (t % 2)
                sc = ss[:, (t % 2) * 2:(t % 2) * 2 + 1]
                sck = 'p1_ss%d' % (t % 2)
                P.dma('sp', x_[:], x_src[t * 128:(t + 1) * 128, :], writes=[xk])
                P.op('act', lambda e, x_=x_, sc=sc: e.activation(out=junk[:], in_=x_[:], func=AF.Square, accum_out=sc),
                     reads=[xk], writes=['p1_junk', sck])
                P.op('dve', lambda e, sc=sc: e.tensor_scalar(out=sc, in0=sc, scalar1=1.0 / D, scalar2=1e-6,
                                                             op0=ALU.mult, op1=ALU.add), reads=[sck], writes=[sck])
                P.op('act', lambda e, sc=sc: e.activation(out=sc, in_=sc, func=AF.Sqrt), reads=[sck], writes=[sck])
                P.op('dve', lambda e, sc=sc: e.reciprocal(out=sc, in_=sc), reads=[sck], writes=[sck])
                P.op('dve', lambda e, x_=x_, xb_=xb_, sc=sc: e.tensor_scalar(out=xb_[:], in0=x_[:], scalar1=sc, scalar2=None,
                                                                            op0=ALU.mult), reads=[xk, sck], writes=[xbk])
                for k in range(KT):
                    P.op('pe', lambda e, k=k, xb_=xb_, pt_=pt_: e.transpose(out=pt_[:, k, :], in_=xb_[:, k * 128:(k + 1) * 128],
                                                                           identity=ident[:]),
                         reads=[xbk, 'p1_ident'], writes=[ptk])
                hs = h[:, :, ti * 128:(ti + 1) * 128]
                P.op('dve', lambda e, hs=hs, pt_=pt_, v=v: e.tensor_tensor(out=hs, in0=pt_[:],
                                                                           in1=A1[:, v, :].unsqueeze(2).to_broadcast([128, KT, 128]),
                                                                           op=ALU.mult),
                     reads=[ptk, 'p1_A1'], writes=[hk])
                P.op('pool', lambda e, hs=hs, v=v: e.tensor_tensor(out=hs, in0=hs,
                                                                   in1=m[:, v, 0, :].unsqueeze(2).to_broadcast([128, KT, 128]),
                                                                   op=ALU.add),
                     reads=[hk, 'p1_modpp'], writes=[hk])
            chunks = [c for c in range(NIN // 128) if not (O_NV <= c * 128 < O_NV + 512)]
            for c in chunks:
                col = c * 128
                pi = cnt % 4; cnt += 1
                pk = 'p1_pp%d' % pi
                for k in range(KT):
                    P.op('pe', lambda e, k=k, col=col, pi=pi, ntok=ntok, h=h: e.matmul(
                        pp[pi][:, :ntok], lhsT=wb[:, k, col:col + 128], rhs=h[:, k, :ntok],
                        start=(k == 0), stop=(k == KT - 1)),
                         reads=['p1_w', hk], writes=[pk])
                t0 = b * 512
                if col < O_NQ:
                    eng = 'act' if c % 2 == 0 else 'dve'
                    evk = 'p1_ev%d' % pi
                    if eng == 'act':
                        P.op('act', lambda e, pi=pi, ntok=ntok: e.copy(out=ev[pi][:, :ntok], in_=pp[pi][:, :ntok]),
                             reads=[pk], writes=[evk])
                    else:
                        P.op('dve', lambda e, pi=pi, ntok=ntok: e.tensor_copy(out=ev[pi][:, :ntok], in_=pp[pi][:, :ntok]),
                             reads=[pk], writes=[evk])
                    P.dma('sp', g.RWT[col:col + 128, t0:t0 + ntok], ev[pi][:, :ntok], reads=[evk], writes=['RWT'])
                    if b == 0:
                        P.dma('sp', g.RWT[col:col + 128, T:T + NCTX], ev[pi][:, :NCTX], reads=[evk], writes=['RWT'])
                elif col < O_S5:
                    evk = 'p1_evb%d' % pi
                    P.op('act', lambda e, pi=pi, ntok=ntok: e.copy(out=evb[pi][:, :ntok], in_=pp[pi][:, :ntok]),
                         reads=[pk], writes=[evk])
                    dst = g.NAQT if col < O_NK else g.NAKT
                    r0 = col - (O_NQ if col < O_NK else O_NK)
                    P.dma('sp', dst[r0:r0 + 128, t0:t0 + ntok], evb[pi][:, :ntok], reads=[evk], writes=['NAQK'])
                elif col < O_G:
                    ui = cnt % 2
                    evk = 'p1_evu%d' % ui
                    nj = ntok // 8
                    P.op('dve', lambda e, pi=pi, ui=ui, ntok=ntok, nj=nj: e.tensor_copy(
                        out=evu[ui][:, :, :nj], in_=pp[pi][:, :ntok].rearrange("p (j i) -> p i j", i=8)),
                         reads=[pk], writes=[evk])
                    g0 = (col - O_S5) // 16
                    j0 = t0 // 8
                    for i in range(8):
                        for gg in range(8):
                            P.dma('sp', g.UB[g0 + gg, i, :, j0:j0 + nj], evu[ui][gg * 16:(gg + 1) * 16, i, :nj],
                                  reads=[evk], writes=['UB'])
                        if b == 0:
                            for gg in range(8):
                                P.dma('sp', g.UB[g0 + gg, i, :, T // 8:T // 8 + 32],
                                      evu[ui][gg * 16:(gg + 1) * 16, i, :32], reads=[evk], writes=['UB'])
                else:
                    evk = 'p1_evb%d' % pi
                    P.op('act', lambda e, pi=pi, ntok=ntok: e.activation(out=evb[pi][:, :ntok], in_=pp[pi][:, :ntok],
                                                                         func=AF.Sigmoid),
                         reads=[pk], writes=[evk])
                    r0 = col - O_G
                    P.dma('sp', g.GT[r0:r0 + 128, t0:t0 + ntok], evb[pi][:, :ntok], reads=[evk], writes=['GT'])
            for ti, t in enumerate(tiles):
                pi = cnt % 4; cnt += 1
                pk = 'p1_pp%d' % pi
                for k in range(KT):
                    P.op('pe', lambda e, k=k, pi=pi, ti=ti, h=h: e.matmul(
                        pp[pi][:, :], lhsT=h[:, k, ti * 128:(ti + 1) * 128], rhs=wb[:, k, O_NV:O_NV + 512],
                        start=(k == 0), stop=(k == KT - 1)),
                         reads=['p1_w', hk], writes=[pk])
                evk = 'p1_evb%d' % pi
                P.op('dve', lambda e, pi=pi: e.tensor_copy(out=evb[pi][:], in_=pp[pi][:]), reads=[pk], writes=[evk])
                P.dma('sp', g.NAV[t * 128:(t + 1) * 128, :], evb[pi][:], reads=[evk], writes=['NAV'])


def build(stage="p1", dbg=()):
    nc = bass.Bass("TRN2", target_bir_lowering=False)
    g = declare_io(nc, stage)
    outs = {}
    for name, shape, dt in dbg:
        outs[name] = nc.dram_tensor("o_" + name, list(shape), dt, kind="ExternalOutput").ap()
    if stage == "moe":
        g.dbg_slots = nc.dram_tensor("o_slots", [128, T // 128, 4], U32, kind="ExternalOutput").ap()
        g.dbg_wts = nc.dram_tensor("o_wts", [128, T // 128, 4], F32, kind="ExternalOutput").ap()
    with ExitStack() as es:
        P = Prog(nc, es)
        phase0(nc, P, g, es)
        P.barrier()
        phase1(nc, P, g, 0, g.xin)
        P.barrier()
        if stage in ("rwkv",):
            phase_rwkv(nc, P, g, 0)
            P.barrier()
        if stage in ("s5",):
            phase_s5(nc, P, g, 0, True)
            P.barrier()
            phase_s5_readout(nc, P, g, 0)
            P.barrier()
        if stage in ("na", "merge", "moe"):
            phase_na(nc, P, g, 0, True)
            P.barrier()
        if stage in ("merge", "moe"):
            phase_merge(nc, P, g, 0, g.xin, True)
            P.barrier()
        if stage in ("moe",):
            phase_moe(nc, P, g, 0, True)
            P.barrier()
        for name, shape, dt in dbg:
            src = getattr(g, name)
            P.dma('sp', outs[name], src, reads=[name, 'RWT', 'UB', 'NAQK', 'NAV', 'GT', 'MOD', 'NAO', 'SO', 'AO'] + [('XR', t) for t in range(T // 128)] + [('YS', r) for r in range(0, NE * CAP, 128)] + [('YB', gi) for gi in range(32)], writes=['o_' + name])
        P.wait_all('sp')
        P.emit()
    return nc


def host_inputs(inputs, b):
    f = lambda a: np.ascontiguousarray(a, dtype=np.float32)
    d = {}
    d["xin"] = f(np.concatenate([inputs["ctx"][b], inputs["x"][b]], axis=0))
    cl = inputs["c"][b].reshape(KT, 128).T
    cc = inputs["c_ctx"].reshape(KT, 128).T
    d["cvec"] = f(np.concatenate([cl, cc], axis=1))
    d["ada_w"] = f(inputs["ada_w"])
    d["ada_b"] = f(inputs["ada_b"])
    d["norm1_g"] = f(inputs["norm1_g"].reshape(DEPTH, KT, 128).transpose(0, 2, 1))
    d["norm2_g"] = f(inputs["norm2_g"].reshape(DEPTH, KT, 128).transpose(0, 2, 1))
    d["w_in"] = f(inputs["w_in"])
    d["c_ident"] = np.eye(128, dtype=np.float32)
    d["c_ones"] = np.ones((128, 128), dtype=np.float32)
    d["w_branch"] = f(inputs["w_branch"]); d["w_out"] = f(inputs["w_out"])
    d["c_tri"] = np.triu(np.ones((128, 128), np.float32), 1)
    d["c_iota"] = np.tile(np.arange(NE, dtype=np.float32)[None, :], (128, 1))
    d["router_w"] = f(inputs["router_w"]); d["router_b"] = f(inputs["router_b"])
    d["norm2_row"] = f(inputs["norm2_g"]); d["final_row"] = f(inputs["final_g"].reshape(1, D))
    d["expert_gu_w"] = f(inputs["expert_gu_w"]); d["expert_dn_w"] = f(inputs["expert_dn_w"])
    d["gu_b"] = f(inputs["expert_gu_b"].reshape(DEPTH, NE, 16, 128).transpose(0, 1, 3, 2))
    d["expert_dn_b"] = f(inputs["expert_dn_b"])
    ii = np.zeros((128, 128), np.float32)
    for k in range(128):
        ii[k, k % 64] = 1.0; ii[k, 64 + k % 64] = 1.0
    d["c_ii"] = ii
    def pdup(a):
        a = np.moveaxis(a.reshape((DEPTH, 64, 64) + a.shape[4:]), 2, 1)
        return f(np.concatenate([a, a], axis=1))
    d["s5_lr"] = pdup(inputs["s5_lambda_re"]); d["s5_li"] = pdup(inputs["s5_lambda_im"])
    d["s5_ldt"] = f(np.tile(inputs["s5_log_dt"].reshape(DEPTH, 1, 64), (1, 128, 1)))
    d["s5_br"] = pdup(inputs["s5_b_re"]); d["s5_bi"] = pdup(inputs["s5_b_im"])
    d["s5_cr"] = pdup(np.swapaxes(inputs["s5_c_re"], 3, 4)); d["s5_ci"] = pdup(np.swapaxes(inputs["s5_c_im"], 3, 4))
    dv = inputs["s5_d"].reshape(DEPTH, 32, 16)
    d["s5_dv"] = f(np.tile(np.transpose(dv, (0, 2, 1))[:, None, :, :], (1, 8, 1, 1)).reshape(DEPTH, 128, 32))
    bo = np.zeros((128, 128), np.float32); bo[:64, :64] = 1; bo[64:, 64:] = 1
    d["c_bo"] = bo
    hi_ = np.zeros((128, 2), np.float32); hi_[:64, 0] = 1; hi_[64:, 1] = 1
    d["c_hi"] = hi_
    ii_, jj_ = np.meshgrid(np.arange(128), np.arange(128), indexing="ij")
    bd_ = (ii_ // 32) == (jj_ // 32)
    d["c_masks"] = f(np.stack([(jj_ < ii_), (jj_ > ii_), (jj_ <= ii_), (jj_ >= ii_),
                               (jj_ < ii_) & bd_, (jj_ > ii_) & bd_, (jj_ < ii_) & ~bd_, (jj_ > ii_) & ~bd_], axis=1).astype(np.float32))
    pm = lambda a, n: f(a.reshape(DEPTH, n, 128).transpose(0, 2, 1))
    d["rw_mup"] = pm(inputs["rwkv_mu_prev"], 14); d["rw_mun"] = pm(inputs["rwkv_mu_next"], 14)
    d["rw_pv"] = f(np.stack([pm(inputs["rwkv_k_k"], 4), pm(inputs["rwkv_k_a"], 4), pm(inputs["rwkv_r_k"].reshape(DEPTH, 512), 4)], axis=2))
    w0 = inputs["rwkv_w0"].reshape(DEPTH, 2, 4, 128).transpose(0, 3, 1, 2)
    a0 = inputs["rwkv_a0"].reshape(DEPTH, 2, 4, 128).transpose(0, 3, 1, 2)
    d["rw_wa0"] = f(np.stack([w0, a0], axis=2).reshape(DEPTH, 128, 16))
    for nm in ("rwkv_w2", "rwkv_a2", "rwkv_g2", "rwkv_ln_w", "rwkv_ln_b"):
        d[nm] = f(inputs[nm])
    d["s5_glu_w"] = f(inputs["s5_glu_w"])
    d["s5_glu_bp"] = f(inputs["s5_glu_b"].reshape(DEPTH, 4, 128).transpose(0, 2, 1))
    d["na_tab"] = np.stack([na_tables(inputs["na_rpb"][l]) for l in range(DEPTH)])
    return d


def na_tables(rpb_l):
    out = np.full((5, 8, 576, 128), -30000.0, np.float32)
    for ti, m in enumerate([0, 1, 30, 62, 63]):
        kb = min(max(2 * m - 4, 0), 119)
        qi = np.arange(128); r = 2 * m + qi // 64; c = qi % 64
        rs = np.clip(r - 4, 0, 120); cs = np.clip(c - 8, 0, 48)
        ki = np.arange(576); kr = kb + ki // 64; kc = ki % 64
        ok = ((kr[:, None] >= rs[None, :]) & (kr[:, None] < rs[None, :] + 8) &
              (kc[:, None] >= cs[None, :]) & (kc[:, None] < cs[None, :] + 16))
        ro = np.clip(kr[:, None] - r[None, :] + 7, 0, 14); co = np.clip(kc[:, None] - c[None, :] + 15, 0, 30)
        for h in range(8):
            b = rpb_l[h][ro, co]
            out[ti, h] = np.where(ok, b, -30000.0)
    return out


def phase_na(nc, P, g, l, ctx_out):
    with ExitStack() as es:
        sb, ps = mk_alloc(nc, es)
        NTL = T // 128
        kT = sb("na_kT", [128, T], BF16)
        qT = sb("na_qT", [128, T], BF16)
        v0 = sb("na_v0", [128, NTL, 2, 65], BF16)
        v1 = sb("na_v1", [128, NTL, 2, 65], BF16)
        tbf = sb("na_tbf", [128, 5, 128], F32)
        eb = sb("na_eb", [128, 2, 5, 5, 128], BF16)
        et = [sb("na_et%d" % i, [128, 7, 128], BF16) for i in range(2)]
        ob = sb("na_ob", [128, NTL, 128], BF16)
        rc = sb("na_rc", [128, 2], F32)
        pst = [ps("na_pst%d" % i, [128, 8, 128], F32) for i in range(2)]
        po = [ps("na_po%d" % i, [128, 65], F32) for i in range(2)]
        P.op('pool', lambda e: e.memset(v0[:], 1.0), writes=['na_v0'])
        P.op('pool', lambda e: e.memset(v1[:], 1.0), writes=['na_v1'])
        P.op('pool', lambda e: e.memset(eb[:], 0.0), writes=['na_eb'])
        u = 0
        for hp in range(4):
            P.dma('sp', kT[:], g.NAKT[hp * 128:(hp + 1) * 128, :], reads=['NAQK'], writes=['na_kT'])
            P.dma('sp', qT[:], g.NAQT[hp * 128:(hp + 1) * 128, :], reads=['NAQK'], writes=['na_qT'])
            for hh in range(2):
                P.dma('sp', v1[0:64, NTL - 1, hh, 0:64], g.NAV[T - 64:T, hp * 128 + hh * 64:hp * 128 + hh * 64 + 64],
                      reads=['NAV'], writes=['na_v1'])
                P.dma('sp', v0[:, :, hh, 0:64],
                      g.NAV[:, hp * 128 + hh * 64:hp * 128 + hh * 64 + 64].rearrange("(n p) d -> p n d", p=128),
                      reads=['NAV'], writes=['na_v0'])
                P.dma('sp', v1[:, 0:NTL - 1, hh, 0:64],
                      g.NAV[64:T - 64, hp * 128 + hh * 64:hp * 128 + hh * 64 + 64].rearrange("(n p) d -> p n d", p=128),
                      reads=['NAV'], writes=['na_v1'])
                for tb in range(5):
                    h = hp * 2 + hh
                    for blk in range(5):
                        nk = 128 if blk < 4 else 64
                        P.dma('sp', tbf[:nk, blk, :], g.na_tab[l, tb, h, blk * 128:blk * 128 + nk, :],
                              writes=['na_tbf'])
                    P.op('act', lambda e, hh=hh, tb=tb: e.activation(out=eb[:, hh, tb, 0:4, :], in_=tbf[:, 0:4, :], func=AF.Exp),
                         reads=['na_tbf'], writes=['na_eb'])
                    P.op('act', lambda e, hh=hh, tb=tb: e.activation(out=eb[0:64, hh, tb, 4, :], in_=tbf[0:64, 4, :], func=AF.Exp),
                         reads=['na_tbf'], writes=['na_eb'])
            units = []
            if ctx_out:
                units += [('c', 0), ('c', 1)]
            units += [('l', m) for m in range(64)]
            for (kind, m) in units:
                for hh in range(2):
                    pr = slice(hh * 64, hh * 64 + 64)
                    ui = u % 2; u += 1
                    pk, ek, ok = 'na_pst%d' % ui, 'na_et%d' % ui, 'na_po%d' % ui
                    if kind == 'c':
                        q0 = m * 128
                        blocks = [(0, 128, None), (128, 128, None)]
                        tb = None
                    else:
                        q0 = NCTX + m * 128
                        kb = min(max(2 * m - 4, 0), 119)
                        k0 = NCTX + kb * 64
                        blocks = [(k0 + 128 * j, 128 if j < 4 else 64, j) for j in range(5)] + [(0, 128, None), (128, 128, None)]
                        tb = {0: 0, 1: 1, 62: 3, 63: 4}.get(m, 2)
                    nb = len(blocks)
                    for j, (ks, nk, tj) in enumerate(blocks):
                        P.op('pe', lambda e, ui=ui, j=j, ks=ks, nk=nk, pr=pr, q0=q0: e.matmul(
                            pst[ui][:nk, j, :], lhsT=kT[pr, ks:ks + nk], rhs=qT[pr, q0:q0 + 128], start=True, stop=True),
                             reads=['na_kT', 'na_qT'], writes=[pk])
                    if kind == 'l':
                        P.op('act', lambda e, ui=ui: e.activation(out=et[ui][:, 0:4, :], in_=pst[ui][:, 0:4, :], func=AF.Exp, scale=0.125),
                             reads=[pk], writes=[ek])
                        P.op('act', lambda e, ui=ui: e.activation(out=et[ui][0:64, 4, :], in_=pst[ui][0:64, 4, :], func=AF.Exp, scale=0.125),
                             reads=[pk], writes=[ek])
                        P.op('act', lambda e, ui=ui: e.activation(out=et[ui][:, 5:7, :], in_=pst[ui][:, 5:7, :], func=AF.Exp, scale=0.125),
                             reads=[pk], writes=[ek])
                        P.op('dve', lambda e, ui=ui, hh=hh, tb=tb: e.tensor_tensor(out=et[ui][:, 0:4, :], in0=et[ui][:, 0:4, :],
                                                                                  in1=eb[:, hh, tb, 0:4, :], op=ALU.mult),
                             reads=[ek, 'na_eb'], writes=[ek])
                        P.op('dve', lambda e, ui=ui, hh=hh, tb=tb: e.tensor_tensor(out=et[ui][0:64, 4, :], in0=et[ui][0:64, 4, :],
                                                                                  in1=eb[0:64, hh, tb, 4, :], op=ALU.mult),
                             reads=[ek, 'na_eb'], writes=[ek])
                    else:
                        P.op('act', lambda e, ui=ui: e.activation(out=et[ui][:, 0:2, :], in_=pst[ui][:, 0:2, :], func=AF.Exp, scale=0.125),
                             reads=[pk], writes=[ek])
                    for j, (ks, nk, tj) in enumerate(blocks):
                        if ks % 128 == 0:
                            vv = v0[:nk, ks // 128, hh, :]
                        else:
                            vv = v1[:nk, (ks - 64) // 128, hh, :]
                        P.op('pe', lambda e, ui=ui, j=j, nk=nk, vv=vv, nb=nb: e.matmul(
                            po[ui][:, :], lhsT=et[ui][:nk, j, :], rhs=vv, start=(j == 0), stop=(j == nb - 1)),
                             reads=[ek, 'na_v0', 'na_v1'], writes=[ok])
                    rk = 'na_rc%d' % ui
                    P.op('dve', lambda e, ui=ui: e.reciprocal(out=rc[:, ui:ui + 1], in_=po[ui][:, 64:65]), reads=[ok], writes=[rk])
                    P.op('dve', lambda e, ui=ui, q0=q0, hh=hh: e.tensor_scalar(out=ob[:, q0 // 128, hh * 64:hh * 64 + 64], in0=po[ui][:, 0:64],
                                                                             scalar1=rc[:, ui:ui + 1], scalar2=None, op0=ALU.mult),
                         reads=[ok, rk], writes=['na_ob'])
            t_lo = 0 if ctx_out else 2
            P.dma('sp', g.NAO[t_lo * 128:T, hp * 128:(hp + 1) * 128].rearrange("(n p) d -> p n d", p=128), ob[:, t_lo:, :],
                  reads=['na_ob'], writes=['NAO'])


def phase_merge(nc, P, g, l, x_src, ctx_out):
    with ExitStack() as es:
        sb, ps = mk_alloc(nc, es)
        wbr = sb("mg_wbr", [128, 3, 4, D], BF16)
        wo = sb("mg_wo", [128, KT, D], BF16)
        ident = sb("mg_ident", [128, 128], BF16)
        P.dma('pool', ident[:], g.c_ident[:, :], writes=['mg_ident'])
        for j in range(3):
            P.dma('pool', wbr[:, j, :, :], g.w_branch[l, j, :, :].rearrange("(k p) n -> p k n", p=128), writes=['mg_wbr'])
        P.dma('pool', wo[:], g.w_out[l, :, :].rearrange("(k p) n -> p k n", p=128), writes=['mg_wo'])
        g1bc = sb("mg_g1bc", [128, 2, D], F32)
        for v in range(2):
            P.dma('sp', g1bc[:, v, :], g.MOD[l, v, 2 * D:3 * D].partition_broadcast(128), reads=['MOD'], writes=['mg_g1bc'])
        bt = [sb("mg_bt%d" % i, [128, 512], BF16) for i in range(3)]
        bT = sb("mg_bT", [128, 3, 4, 512], BF16)
        gt = [sb("mg_gt%d" % i, [128, 512], BF16) for i in range(3)]
        tmp = [sb("mg_tmp%d" % i, [128, 512], F32) for i in range(2)]
        acc = sb("mg_acc", [128, 512], F32)
        ymT = sb("mg_ymT", [128, KT, 512], BF16)
        xt = [sb("mg_xt%d" % i, [128, D], F32) for i in range(2)]
        xo = [sb("mg_xo%d" % i, [128, D], F32) for i in range(2)]
        ptr = [ps("mg_ptr%d" % i, [128, 4, 128], BF16) for i in range(2)]
        pm = [ps("mg_pm%d" % i, [128, 512], F32) for i in range(2)]
        po = [ps("mg_po%d" % i, [128, 512], F32) for i in range(2)]
        srcs = [g.AO, g.NAO, g.SO]
        ntile = T // 128
        nblk = (ntile + 3) // 4
        cn = 0
        for b in range(nblk):
            tiles = [t for t in range(4 * b, min(4 * b + 4, ntile))]
            if not ctx_out:
                tiles = [t for t in tiles if t >= 2]
            if not tiles:
                continue
            tA = tiles[0]
            ntok = len(tiles) * 128
            c0 = tA * 128
            for ti, t in enumerate(tiles):
                for j in range(3):
                    P.dma('sp', bt[j][:], srcs[j][t * 128:(t + 1) * 128, :], reads=['AO', 'NAO', 'SO'], writes=['mg_bt%d' % j])
                    pi = cn % 2; cn += 1
                    for k in range(4):
                        P.op('pe', lambda e, j=j, k=k, pi=pi: e.transpose(out=ptr[pi][:, k, :], in_=bt[j][:, k * 128:(k + 1) * 128], identity=ident[:]),
                             reads=['mg_bt%d' % j, 'mg_ident'], writes=['mg_ptr%d' % pi])
                    P.op('act', lambda e, j=j, ti=ti, pi=pi: e.copy(out=bT[:, j, :, ti * 128:(ti + 1) * 128], in_=ptr[pi][:]),
                         reads=['mg_ptr%d' % pi], writes=['mg_bT'])
            for dc in range(KT):
                for j in range(3):
                    pi = cn % 2; cn += 1
                    P.dma('sp', gt[j][:, :ntok], g.GT[j * D + dc * 128:j * D + (dc + 1) * 128, c0:c0 + ntok], reads=['GT'], writes=['mg_gt%d' % j])
                    for k in range(4):
                        P.op('pe', lambda e, j=j, k=k, pi=pi, dc=dc, ntok=ntok: e.matmul(
                            pm[pi][:, :ntok], lhsT=wbr[:, j, k, dc * 128:(dc + 1) * 128], rhs=bT[:, j, k, :ntok], start=(k == 0), stop=(k == 3)),
                             reads=['mg_wbr', 'mg_bT'], writes=['mg_pm%d' % pi])
                    if j == 0:
                        P.op('dve', lambda e, pi=pi, j=j, ntok=ntok: e.tensor_tensor(out=acc[:, :ntok], in0=pm[pi][:, :ntok], in1=gt[j][:, :ntok], op=ALU.mult),
                             reads=['mg_pm%d' % pi, 'mg_gt%d' % j], writes=['mg_acc'])
                    else:
                        tk = j - 1
                        P.op('dve', lambda e, pi=pi, j=j, tk=tk, ntok=ntok: e.tensor_tensor(out=tmp[tk][:, :ntok], in0=pm[pi][:, :ntok], in1=gt[j][:, :ntok], op=ALU.mult),
                             reads=['mg_pm%d' % pi, 'mg_gt%d' % j], writes=['mg_tmp%d' % tk])
                        if j == 1:
                            P.op('pool', lambda e, tk=tk, ntok=ntok: e.tensor_tensor(out=acc[:, :ntok], in0=acc[:, :ntok], in1=tmp[tk][:, :ntok], op=ALU.add),
                                 reads=['mg_tmp%d' % tk, 'mg_acc'], writes=['mg_acc'])
                        else:
                            P.op('pool', lambda e, tk=tk, ntok=ntok, dc=dc: e.tensor_tensor(out=ymT[:, dc, :ntok], in0=acc[:, :ntok], in1=tmp[tk][:, :ntok], op=ALU.add),
                                 reads=['mg_tmp%d' % tk, 'mg_acc'], writes=['mg_ymT'])
            for ti, t in enumerate(tiles):
                v = 1 if t < 2 else 0
                xi = t % 2
                P.dma('sp', xt[xi][:], x_src[t * 128:(t + 1) * 128, :], reads=[('XR', t)], writes=['mg_xt%d' % xi])
                for hf in range(2):
                    pi = cn % 2; cn += 1
                    for k in range(KT):
                        P.op('pe', lambda e, k=k, pi=pi, ti=ti, hf=hf: e.matmul(
                            po[pi][:, :], lhsT=ymT[:, k, ti * 128:(ti + 1) * 128], rhs=wo[:, k, hf * 512:(hf + 1) * 512], start=(k == 0), stop=(k == KT - 1)),
                             reads=['mg_ymT', 'mg_wo'], writes=['mg_po%d' % pi])
                    P.op('dve', lambda e, pi=pi, xi=xi, hf=hf, v=v: e.tensor_tensor(out=xo[xi][:, hf * 512:(hf + 1) * 512], in0=po[pi][:, :],
                                                                                   in1=g1bc[:, v, hf * 512:(hf + 1) * 512], op=ALU.mult),
                         reads=['mg_po%d' % pi, 'mg_g1bc'], writes=['mg_xo%d' % xi])
                P.op('pool', lambda e, xi=xi: e.tensor_tensor(out=xo[xi][:], in0=xo[xi][:], in1=xt[xi][:], op=ALU.add),
                     reads=['mg_xo%d' % xi, 'mg_xt%d' % xi], writes=['mg_xo%d' % xi])
                P.dma('sp', g.XR[t * 128:(t + 1) * 128, :], xo[xi][:], reads=['mg_xo%d' % xi], writes=[('XR', t)])


CAP = 3072
NE = 32
U32 = mybir.dt.uint32


def phase_moe(nc, P, g, l, with_ctx):
    ntile = T // 128
    tiles = list(range(0 if with_ctx else 2, ntile))
    with ExitStack() as es:
        sb, ps = mk_alloc(nc, es)
        ident = sb("mo_ident", [128, 128], BF16)
        tri = sb("mo_tri", [128, 128], BF16)
        ones = sb("mo_ones", [128, 128], BF16)
        iota = sb("mo_iota", [128, NE], F32)
        P.dma('pool', ident[:], g.c_ident[:, :], writes=['mo_ident'])
        P.dma('pool', tri[:], g.c_tri[:, :], writes=['mo_tri'])
        P.dma('pool', ones[:], g.c_ones[:, :], writes=['mo_ones'])
        P.dma('sp', iota[:], g.c_iota[:, :], writes=['mo_iota'])
        rw = sb("mo_rw", [128, KT, NE], BF16)
        P.dma('pool', rw[:], g.router_w[l, :, :].rearrange("(k p) e -> p k e", p=128), writes=['mo_rw'])
        rb = sb("mo_rb", [128, NE], F32)
        P.dma('sp', rb[:], g.router_b[l, :].partition_broadcast(128), writes=['mo_rb'])
        A2 = sb("mo_A2", [128, 2, D], F32)
        S2 = sb("mo_S2", [128, 2, D], F32)
        G2 = sb("mo_G2", [128, 2, D], F32)
        g2 = sb("mo_g2", [128, D], F32)
        P.dma('sp', g2[:], g.norm2_row[l, :].partition_broadcast(128), writes=['mo_g2'])
        for v in range(2):
            P.dma('sp', A2[:, v, :], g.MOD[l, v, 4 * D:5 * D].partition_broadcast(128), reads=['MOD'], writes=['mo_A2'])
            P.dma('sp', S2[:, v, :], g.MOD[l, v, 3 * D:4 * D].partition_broadcast(128), reads=['MOD'], writes=['mo_S2'])
            P.dma('sp', G2[:, v, :], g.MOD[l, v, 5 * D:6 * D].partition_broadcast(128), reads=['MOD'], writes=['mo_G2'])
            P.op('dve', lambda e, v=v: e.scalar_tensor_tensor(out=A2[:, v, :], in0=A2[:, v, :], scalar=1.0, in1=g2[:], op0=ALU.add, op1=ALU.mult),
                 reads=['mo_A2', 'mo_g2'], writes=['mo_A2'])
        slots = sb("mo_slots", [128, ntile, 4], U32)
        wts = sb("mo_wts", [128, ntile, 4], F32)
        base = sb("mo_base", [128, NE], F32)
        P.op('pool', lambda e: e.memset(base[:], 0.0), writes=['mo_base'])
        xt = [sb("mo_xt%d" % i, [128, D], F32) for i in range(2)]
        junk = sb("mo_junk", [128, D], F32)
        hb = [sb("mo_hb%d" % i, [128, D], BF16) for i in range(2)]
        hT = sb("mo_hT", [128, KT, 128], BF16)
        sm = sb("mo_sm", [128, 16], F32)
        lg = sb("mo_lg", [128, NE], F32)
        mx = sb("mo_mx", [128, 8], F32)
        mi = sb("mo_mi", [128, 8], U32)
        mif = sb("mo_mif", [128, 8], F32)
        ex = sb("mo_ex", [128, 8], F32)
        sel = sb("mo_sel", [128, NE], BF16)
        oh = sb("mo_oh", [128, 4, NE], F32)
        pos = sb("mo_pos", [128, NE], F32)
        pk = sb("mo_pk", [128, 4], F32)
        slf = sb("mo_slf", [128, 4], F32)
        ptr = ps("mo_ptr", [128, KT, 128], BF16)
        plg = ps("mo_plg", [128, 3 * NE], F32)
        for t in tiles:
            v = 1 if t < 2 else 0
            xi = t % 2
            xk, hk = 'mo_xt%d' % xi, 'mo_hb%d' % xi
            P.dma('sp', xt[xi][:], g.XR[t * 128:(t + 1) * 128, :], reads=[('XR', t)], writes=[xk])
            P.op('act', lambda e, xi=xi: e.activation(out=junk[:], in_=xt[xi][:], func=AF.Square, accum_out=sm[:, 0:1]),
                 reads=[xk], writes=['mo_junk', 'mo_sm'])
            P.op('dve', lambda e: e.tensor_scalar(out=sm[:, 0:1], in0=sm[:, 0:1], scalar1=1.0 / D, scalar2=1e-6, op0=ALU.mult, op1=ALU.add),
                 reads=['mo_sm'], writes=['mo_sm'])
            P.op('act', lambda e: e.activation(out=sm[:, 0:1], in_=sm[:, 0:1], func=AF.Sqrt), reads=['mo_sm'], writes=['mo_sm'])
            P.op('dve', lambda e: e.reciprocal(out=sm[:, 0:1], in_=sm[:, 0:1]), reads=['mo_sm'], writes=['mo_sm'])
            P.op('dve', lambda e, xi=xi, v=v: e.scalar_tensor_tensor(out=junk[:], in0=xt[xi][:], scalar=sm[:, 0:1], in1=A2[:, v, :], op0=ALU.mult, op1=ALU.mult),
                 reads=[xk, 'mo_sm', 'mo_A2'], writes=['mo_junk'])
            P.op('pool', lambda e, xi=xi, v=v: e.tensor_tensor(out=hb[xi][:], in0=junk[:], in1=S2[:, v, :], op=ALU.add),
                 reads=['mo_junk', 'mo_S2'], writes=[hk])
            for k in range(KT):
                P.op('pe', lambda e, k=k, xi=xi: e.transpose(out=ptr[:, k, :], in_=hb[xi][:, k * 128:(k + 1) * 128], identity=ident[:]),
                     reads=[hk, 'mo_ident'], writes=['mo_ptr'])
            P.op('act', lambda e: e.copy(out=hT[:], in_=ptr[:]), reads=['mo_ptr'], writes=['mo_hT'])
            for k in range(KT):
                P.op('pe', lambda e, k=k: e.matmul(plg[:, 0:NE], lhsT=hT[:, k, :], rhs=rw[:, k, :], start=(k == 0), stop=(k == KT - 1)),
                     reads=['mo_hT', 'mo_rw'], writes=['mo_plg'])
            P.op('dve', lambda e: e.tensor_tensor(out=lg[:], in0=plg[:, 0:NE], in1=rb[:], op=ALU.add), reads=['mo_plg', 'mo_rb'], writes=['mo_lg'])
            P.op('dve', lambda e: e.max(out=mx[:], in_=lg[:]), reads=['mo_lg'], writes=['mo_mx'])
            P.op('dve', lambda e: e.max_index(out=mi[:], in_max=mx[:], in_values=lg[:]), reads=['mo_lg', 'mo_mx'], writes=['mo_mi'])
            P.op('dve', lambda e: e.tensor_copy(out=mif[:], in_=mi[:]), reads=['mo_mi'], writes=['mo_mif'])
            P.op('dve', lambda e: e.tensor_scalar(out=ex[:, 0:4], in0=mx[:, 0:4], scalar1=mx[:, 0:1], scalar2=None, op0=ALU.subtract),
                 reads=['mo_mx'], writes=['mo_ex'])
            P.op('act', lambda e: e.activation(out=ex[:, 0:4], in_=ex[:, 0:4], func=AF.Exp, accum_out=sm[:, 1:2]), reads=['mo_ex'], writes=['mo_ex', 'mo_sm'])
            P.op('dve', lambda e: e.reciprocal(out=sm[:, 1:2], in_=sm[:, 1:2]), reads=['mo_sm'], writes=['mo_sm'])
            P.op('dve', lambda e, t=t: e.tensor_scalar(out=wts[:, t, :], in0=ex[:, 0:4], scalar1=sm[:, 1:2], scalar2=None, op0=ALU.mult),
                 reads=['mo_ex', 'mo_sm'], writes=['mo_wts'])
            for k in range(4):
                P.op('dve', lambda e, k=k: e.tensor_scalar(out=oh[:, k, :], in0=iota[:], scalar1=mif[:, k:k + 1], scalar2=None, op0=ALU.is_equal),
                     reads=['mo_mif', 'mo_iota'], writes=['mo_oh'])
            P.op('dve', lambda e: e.tensor_tensor(out=pos[:], in0=oh[:, 0, :], in1=oh[:, 1, :], op=ALU.add), reads=['mo_oh'], writes=['mo_pos'])
            P.op('dve', lambda e: e.tensor_tensor(out=pos[:], in0=pos[:], in1=oh[:, 2, :], op=ALU.add), reads=['mo_oh', 'mo_pos'], writes=['mo_pos'])
            P.op('dve', lambda e: e.tensor_tensor(out=sel[:], in0=pos[:], in1=oh[:, 3, :], op=ALU.add), reads=['mo_oh', 'mo_pos'], writes=['mo_sel'])
            P.op('pe', lambda e: e.matmul(plg[:, NE:2 * NE], lhsT=tri[:], rhs=sel[:], start=True, stop=True), reads=['mo_tri', 'mo_sel'], writes=['mo_plg2'])
            P.op('pe', lambda e: e.matmul(plg[:, 2 * NE:3 * NE], lhsT=ones[:], rhs=sel[:], start=True, stop=True), reads=['mo_ones', 'mo_sel'], writes=['mo_plg2'])
            P.op('dve', lambda e: e.tensor_tensor(out=pos[:], in0=plg[:, NE:2 * NE], in1=base[:], op=ALU.add), reads=['mo_plg2', 'mo_base'], writes=['mo_pos'])
            P.op('dve', lambda e: e.tensor_tensor(out=base[:], in0=plg[:, 2 * NE:3 * NE], in1=base[:], op=ALU.add), reads=['mo_plg2', 'mo_base'], writes=['mo_base'])
            for k in range(4):
                P.op('dve', lambda e, k=k: e.tensor_tensor(out=oh[:, k, :], in0=oh[:, k, :], in1=pos[:], op=ALU.mult), reads=['mo_oh', 'mo_pos'], writes=['mo_oh'])
            P.op('dve', lambda e: e.tensor_reduce(out=pk[:], in_=oh[:], axis=AX.X, op=ALU.add), reads=['mo_oh'], writes=['mo_pk'])
            P.op('dve', lambda e: e.scalar_tensor_tensor(out=slf[:], in0=mif[:, 0:4], scalar=float(CAP), in1=pk[:], op0=ALU.mult, op1=ALU.add),
                 reads=['mo_mif', 'mo_pk'], writes=['mo_slf'])
            P.op('dve', lambda e, t=t: e.tensor_copy(out=slots[:, t, :], in_=slf[:]), reads=['mo_slf'], writes=['mo_slots'])
            for k in range(4):
                fn = lambda e, t=t, k=k, xi=xi: e.indirect_dma_start(
                    out=g.XS[:, :], out_offset=bass.IndirectOffsetOnAxis(ap=slots[:, t, k:k + 1], axis=0),
                    in_=hb[xi][:], in_offset=None)
                P.dma_fn('pool', fn, reads=['mo_slots', hk], writes=[('XS', t, k)])
        P.barrier()
        gu = sb("mo_gu", [128, KT, 2 * D], BF16)
        dn = sb("mo_dn", [128, KT, D], BF16)
        gub = sb("mo_gub", [128, 16], F32)
        dnb = sb("mo_dnb", [128, D], F32)
        xs = [sb("mo_xs%d" % i, [128, D], BF16) for i in range(2)]
        XeT = sb("mo_XeT", [128, KT, 512], BF16)
        actT = sb("mo_actT", [128, KT, 512], BF16)
        tg = [sb("mo_tg%d" % i, [128, 512], F32) for i in range(2)]
        tsg = [sb("mo_tsg%d" % i, [128, 512], F32) for i in range(2)]
        tl = [sb("mo_tl%d" % i, [128, 512], F32) for i in range(2)]
        ysb = [sb("mo_ysb%d" % i, [128, D], BF16) for i in range(2)]
        pg = ps("mo_pg", [128, 512], F32)
        pl = ps("mo_pl", [128, 512], F32)
        pdn = [ps("mo_pdn%d" % i, [128, 512], F32) for i in range(2)]
        cn = 0
        for e_ in range(NE):
            for k in range(KT):
                P.dma('pool', gu[:, k, :], g.expert_gu_w[l, e_, k * 128:(k + 1) * 128, :], writes=['mo_gu'])
            P.dma('pool', dn[:], g.expert_dn_w[l, e_, :, :].rearrange("(k p) n -> p k n", p=128), writes=['mo_dn'])
            P.dma('sp', gub[:], g.gu_b[l, e_, :, :], writes=['mo_gub'])
            P.dma('sp', dnb[:], g.expert_dn_b[l, e_, :].partition_broadcast(128), writes=['mo_dnb'])
            for (s0, ns) in [(i * 512, 512) for i in range(CAP // 512)]:
                nst = ns // 128
                for st in range(nst):
                    xi = cn % 2; cn += 1
                    r0 = e_ * CAP + s0 + st * 128
                    P.dma('sp', xs[xi][:], g.XS[r0:r0 + 128, :], writes=['mo_xs%d' % xi])
                    for k in range(KT):
                        P.op('pe', lambda e, k=k, xi=xi: e.transpose(out=ptr[:, k, :], in_=xs[xi][:, k * 128:(k + 1) * 128], identity=ident[:]),
                             reads=['mo_xs%d' % xi, 'mo_ident'], writes=['mo_ptr'])
                    P.op('act', lambda e, st=st: e.copy(out=XeT[:, :, st * 128:(st + 1) * 128], in_=ptr[:]), reads=['mo_ptr'], writes=['mo_XeT'])
                for fc in range(KT):
                    i2 = fc % 2
                    for k in range(KT):
                        P.op('pe', lambda e, k=k, fc=fc, ns=ns: e.matmul(pg[:, :ns], lhsT=gu[:, k, fc * 128:(fc + 1) * 128], rhs=XeT[:, k, :ns],
                                                                     start=(k == 0), stop=(k == KT - 1)), reads=['mo_gu', 'mo_XeT'], writes=['mo_pg'])
                    for k in range(KT):
                        P.op('pe', lambda e, k=k, fc=fc, ns=ns: e.matmul(pl[:, :ns], lhsT=gu[:, k, D + fc * 128:D + (fc + 1) * 128], rhs=XeT[:, k, :ns],
                                                                     start=(k == 0), stop=(k == KT - 1)), reads=['mo_gu', 'mo_XeT'], writes=['mo_pl'])
                    P.op('dve', lambda e, fc=fc, i2=i2, ns=ns: e.tensor_scalar(out=tg[i2][:, :ns], in0=pg[:, :ns], scalar1=gub[:, fc:fc + 1], scalar2=7.0,
                                                                            op0=ALU.add, op1=ALU.min), reads=['mo_pg', 'mo_gub'], writes=['mo_tg%d' % i2])
                    P.op('act', lambda e, i2=i2, ns=ns: e.activation(out=tsg[i2][:, :ns], in_=tg[i2][:, :ns], func=AF.Sigmoid, scale=1.702),
                         reads=['mo_tg%d' % i2], writes=['mo_tsg%d' % i2])
                    P.op('dve', lambda e, fc=fc, i2=i2, ns=ns: e.tensor_scalar(out=tl[i2][:, :ns], in0=pl[:, :ns], scalar1=gub[:, 8 + fc:9 + fc], scalar2=7.0,
                                                                            op0=ALU.add, op1=ALU.min), reads=['mo_pl', 'mo_gub'], writes=['mo_tl%d' % i2])
                    P.op('pool', lambda e, i2=i2, ns=ns: e.tensor_scalar(out=tl[i2][:, :ns], in0=tl[i2][:, :ns], scalar1=-7.0, scalar2=1.0,
                                                                      op0=ALU.max, op1=ALU.add), reads=['mo_tl%d' % i2], writes=['mo_tl%d' % i2])
                    P.op('pool', lambda e, i2=i2, ns=ns: e.tensor_tensor(out=tg[i2][:, :ns], in0=tg[i2][:, :ns], in1=tsg[i2][:, :ns], op=ALU.mult),
                         reads=['mo_tg%d' % i2, 'mo_tsg%d' % i2], writes=['mo_tg%d' % i2])
                    P.op('pool', lambda e, i2=i2, fc=fc, ns=ns: e.tensor_tensor(out=actT[:, fc, :ns], in0=tg[i2][:, :ns], in1=tl[i2][:, :ns], op=ALU.mult),
                         reads=['mo_tg%d' % i2, 'mo_tl%d' % i2], writes=['mo_actT'])
                for st in range(nst):
                    yi = cn % 2; cn += 1
                    for hf in range(2):
                        for k in range(KT):
                            P.op('pe', lambda e, k=k, st=st, hf=hf: e.matmul(pdn[hf][:, :], lhsT=actT[:, k, st * 128:(st + 1) * 128], rhs=dn[:, k, hf * 512:(hf + 1) * 512],
                                                                             start=(k == 0), stop=(k == KT - 1)), reads=['mo_actT', 'mo_dn'], writes=['mo_pdn%d' % hf])
                        P.op('dve', lambda e, yi=yi, hf=hf: e.tensor_tensor(out=ysb[yi][:, hf * 512:(hf + 1) * 512], in0=pdn[hf][:, :], in1=dnb[:, hf * 512:(hf + 1) * 512], op=ALU.add),
                             reads=['mo_pdn%d' % hf, 'mo_dnb'], writes=['mo_ysb%d' % yi])
                    r0 = e_ * CAP + s0 + st * 128
                    P.dma('sp', g.YS[r0:r0 + 128, :], ysb[yi][:], reads=['mo_ysb%d' % yi], writes=[('YS', r0)])
        P.barrier()
        if getattr(g, "dbg_slots", None) is not None:
            P.dma('sp', g.dbg_slots, slots[:], reads=['mo_slots'], writes=['dbg_slots'])
            P.dma('sp', g.dbg_wts, wts[:], reads=['mo_wts'], writes=['dbg_wts'])
        yk = [sb("mo_yk%d" % i, [128, D], BF16) for i in range(4)]
        acc = [sb("mo_acc%d" % i, [128, D], F32) for i in range(2)]
        for t in tiles:
            v = 1 if t < 2 else 0
            ai = t % 2
            for k in range(4):
                fn = lambda e, t=t, k=k: e.indirect_dma_start(
                    out=yk[k][:], out_offset=None, in_=g.YS[:, :],
                    in_offset=bass.IndirectOffsetOnAxis(ap=slots[:, t, k:k + 1], axis=0))
                P.dma_fn('pool', fn, reads=['mo_slots'], writes=['mo_yk%d' % k])
            P.dma('sp', xt[ai][:], g.XR[t * 128:(t + 1) * 128, :], reads=[('XR', t)], writes=['mo_xt%d' % ai])
            P.op('dve', lambda e, t=t, ai=ai: e.tensor_scalar(out=acc[ai][:], in0=yk[0][:], scalar1=wts[:, t, 0:1], scalar2=None, op0=ALU.mult),
                 reads=['mo_yk0', 'mo_wts'], writes=['mo_acc%d' % ai])
            for k in range(1, 4):
                eng = 'dve'
                P.op(eng, lambda e, t=t, k=k, ai=ai: e.scalar_tensor_tensor(out=acc[ai][:], in0=yk[k][:], scalar=wts[:, t, k:k + 1], in1=acc[ai][:], op0=ALU.mult, op1=ALU.add),
                     reads=['mo_yk%d' % k, 'mo_wts', 'mo_acc%d' % ai], writes=['mo_acc%d' % ai])
            P.op('dve', lambda e, ai=ai, v=v: e.tensor_tensor(out=acc[ai][:], in0=acc[ai][:], in1=G2[:, v, :], op=ALU.mult), reads=['mo_acc%d' % ai, 'mo_G2'], writes=['mo_acc%d' % ai])
            P.op('pool', lambda e, ai=ai: e.tensor_tensor(out=acc[ai][:], in0=acc[ai][:], in1=xt[ai][:], op=ALU.add), reads=['mo_acc%d' % ai, 'mo_xt%d' % ai], writes=['mo_acc%d' % ai])
            P.dma('sp', g.XR[t * 128:(t + 1) * 128, :], acc[ai][:], reads=['mo_acc%d' % ai], writes=[('XR', t)])


def phase_final(nc, P, g, out_ap):
    with ExitStack() as es:
        sb, ps = mk_alloc(nc, es)
        fg = sb("fn_g", [128, D], F32)
        P.dma('sp', fg[:], g.final_row[0, :].partition_broadcast(128), writes=['fn_g'])
        xt = [sb("fn_xt%d" % i, [128, D], F32) for i in range(2)]
        yo = [sb("fn_yo%d" % i, [128, D], F32) for i in range(2)]
        junk = sb("fn_junk", [128, D], F32)
        sm = sb("fn_sm", [128, 2], F32)
        for t in range(2, T // 128):
            i = t % 2
            P.dma('sp', xt[i][:], g.XR[t * 128:(t + 1) * 128, :], reads=[('XR', t)], writes=['fn_xt%d' % i])
            P.op('act', lambda e, i=i: e.activation(out=junk[:], in_=xt[i][:], func=AF.Square, accum_out=sm[:, i:i + 1]), reads=['fn_xt%d' % i], writes=['fn_junk', 'fn_sm%d' % i])
            P.op('dve', lambda e, i=i: e.tensor_scalar(out=sm[:, i:i + 1], in0=sm[:, i:i + 1], scalar1=1.0 / D, scalar2=1e-6, op0=ALU.mult, op1=ALU.add), reads=['fn_sm%d' % i], writes=['fn_sm%d' % i])
            P.op('act', lambda e, i=i: e.activation(out=sm[:, i:i + 1], in_=sm[:, i:i + 1], func=AF.Sqrt), reads=['fn_sm%d' % i], writes=['fn_sm%d' % i])
            P.op('dve', lambda e, i=i: e.reciprocal(out=sm[:, i:i + 1], in_=sm[:, i:i + 1]), reads=['fn_sm%d' % i], writes=['fn_sm%d' % i])
            P.op('dve', lambda e, i=i: e.scalar_tensor_tensor(out=yo[i][:], in0=xt[i][:], scalar=sm[:, i:i + 1], in1=fg[:], op0=ALU.mult, op1=ALU.mult),
                 reads=['fn_xt%d' % i, 'fn_sm%d' % i, 'fn_g'], writes=['fn_yo%d' % i])
            P.dma('sp', out_ap[(t - 2) * 128:(t - 1) * 128, :], yo[i][:], reads=['fn_yo%d' % i], writes=[('out', t)])


def build_full():
    nc = bass.Bass("TRN2", target_bir_lowering=False)
    g = declare_io(nc, "full")
    out = nc.dram_tensor("out", [SEQ, D], F32, kind="ExternalOutput").ap()
    with ExitStack() as es:
        P = Prog(nc, es)
        phase0(nc, P, g, es)
        P.barrier()
        for l in range(DEPTH):
            last = (l == DEPTH - 1)
            x_src = g.xin if l == 0 else g.XR
            phase1(nc, P, g, l, x_src)
            P.barrier()
            phase_rwkv(nc, P, g, l)
            P.barrier()
            phase_s5(nc, P, g, l, not last)
            P.barrier()
            phase_s5_readout(nc, P, g, l)
            P.barrier()
            phase_na(nc, P, g, l, not last)
            P.barrier()
            phase_merge(nc, P, g, l, x_src, not last)
            P.barrier()
            phase_moe(nc, P, g, l, not last)
            P.barrier()
        phase_final(nc, P, g, out)
        P.wait_all('sp')
        P.emit()
    return nc


def kernel(**inputs):
    inputs = {k: np.asarray(v) for k, v in inputs.items()}
    nc = build_full()
    shared = None
    in_maps = []
    for core in range(8):
        b = core % 4
        d = host_inputs(inputs, b) if shared is None else dict(shared)
        if shared is None:
            shared = dict(d)
        else:
            f = lambda a: np.ascontiguousarray(a, dtype=np.float32)
            d["xin"] = f(np.concatenate([inputs["ctx"][b], inputs["x"][b]], axis=0))
            cl = inputs["c"][b].reshape(KT, 128).T
            cc = inputs["c_ctx"].reshape(KT, 128).T
            d["cvec"] = f(np.concatenate([cl, cc], axis=1))
        in_maps.append(d)
    res = run_bass_kernel_spmd(nc, in_maps, core_ids=list(range(8)))
    out = np.stack([np.asarray(res.results[b]["out"], dtype=np.float32) for b in range(4)], axis=0)
    return out


NJ = T // 8
PI = float(np.pi)


def sl_(start, count, step):
    if step > 0:
        return slice(start, start + step * (count - 1) + 1, step)
    stop = start + step * (count - 1) - 1
    return slice(start, stop if stop >= 0 else None, step)


def phase_s5(nc, P, g, l, ctx_out):
    with ExitStack() as es:
        sb, ps = mk_alloc(nc, es)
        TAUS = list(range(9)) + [64, 256]
        NTAU = len(TAUS)
        LR = sb("s5_LR", [128, 64], F32); LI = sb("s5_LI", [128, 64], F32); DT = sb("s5_DT", [128, 64], F32)
        P.dma('sp', LR[:], g.s5_lr[l], writes=['s5_LR']); P.dma('sp', LI[:], g.s5_li[l], writes=['s5_LI'])
        P.dma('sp', DT[:], g.s5_ldt[l], writes=['s5_DT'])
        P.op('act', lambda e: e.activation(out=DT[:], in_=DT[:], func=AF.Exp), reads=['s5_DT'], writes=['s5_DT'])
        RD = sb("s5_RD", [128, 64], F32); IDt = sb("s5_ID", [128, 64], F32)
        P.op('dve', lambda e: e.tensor_tensor(out=RD[:], in0=LR[:], in1=DT[:], op=ALU.mult), reads=['s5_LR', 's5_DT'], writes=['s5_RD'])
        P.op('dve', lambda e: e.tensor_tensor(out=IDt[:], in0=LI[:], in1=DT[:], op=ALU.mult), reads=['s5_LI', 's5_DT'], writes=['s5_ID'])
        AR = sb("s5_AR", [128, NTAU, 64], F32); AI = sb("s5_AI", [128, NTAU, 64], F32); NAI = sb("s5_NAI", [128, NTAU, 64], F32)
        tmp = sb("s5_tmp", [128, 64], F32); tmp2 = sb("s5_tmp2", [128, 64], F32); mag = sb("s5_mag", [128, 64], F32)
        ki = sb("s5_ki", [128, 64], I32)
        for ti, tau in enumerate(TAUS):
            P.op('act', lambda e, tau=tau: e.activation(out=mag[:], in_=RD[:], func=AF.Exp, scale=float(tau)), reads=['s5_RD'], writes=['s5_mag'])
            for which, shift in (("sin", PI), ("cos", 1.5 * PI)):
                P.op('dve', lambda e, tau=tau, shift=shift: e.tensor_scalar(out=tmp[:], in0=IDt[:], scalar1=float(tau), scalar2=shift - PI, op0=ALU.mult, op1=ALU.add),
                     reads=['s5_ID'], writes=['s5_tmp'])
                P.op('dve', lambda e: e.tensor_scalar(out=tmp2[:], in0=tmp[:], scalar1=1.0 / (2 * PI), scalar2=None, op0=ALU.mult), reads=['s5_tmp'], writes=['s5_tmp2'])
                P.op('dve', lambda e: e.tensor_copy(out=ki[:], in_=tmp2[:]), reads=['s5_tmp2'], writes=['s5_ki'])
                P.op('dve', lambda e: e.tensor_copy(out=tmp2[:], in_=ki[:]), reads=['s5_ki'], writes=['s5_tmp2'])
                P.op('dve', lambda e: e.scalar_tensor_tensor(out=tmp[:], in0=tmp2[:], scalar=-2 * PI, in1=tmp[:], op0=ALU.mult, op1=ALU.add), reads=['s5_tmp2', 's5_tmp'], writes=['s5_tmp'])
                P.op('dve', lambda e: e.tensor_scalar(out=tmp2[:], in0=tmp[:], scalar1=PI, scalar2=-2 * PI, op0=ALU.is_gt, op1=ALU.mult), reads=['s5_tmp'], writes=['s5_tmp2'])
                P.op('dve', lambda e: e.tensor_tensor(out=tmp[:], in0=tmp[:], in1=tmp2[:], op=ALU.add), reads=['s5_tmp', 's5_tmp2'], writes=['s5_tmp'])
                P.op('dve', lambda e: e.tensor_scalar(out=tmp2[:], in0=tmp[:], scalar1=-PI, scalar2=2 * PI, op0=ALU.is_lt, op1=ALU.mult), reads=['s5_tmp'], writes=['s5_tmp2'])
                P.op('dve', lambda e: e.tensor_tensor(out=tmp[:], in0=tmp[:], in1=tmp2[:], op=ALU.add), reads=['s5_tmp', 's5_tmp2'], writes=['s5_tmp'])
                P.op('act', lambda e: e.activation(out=tmp2[:], in_=tmp[:], func=AF.Sin), reads=['s5_tmp'], writes=['s5_tmp2'])
                dst = AI if which == "sin" else AR
                P.op('dve', lambda e, dst=dst, ti=ti: e.tensor_tensor(out=dst[:, ti, :], in0=tmp2[:], in1=mag[:], op=ALU.mult),
                     reads=['s5_tmp2', 's5_mag'], writes=['s5_A'])
        P.op('dve', lambda e: e.tensor_scalar(out=NAI[:], in0=AI[:], scalar1=-1.0, scalar2=None, op0=ALU.mult), reads=['s5_A'], writes=['s5_NAI'])
        S1 = sb("s5_S1", [128, NTAU, 64], F32); S2 = sb("s5_S2", [128, NTAU, 64], F32)
        T1 = sb("s5_T1", [128, NTAU, 64], F32); T2 = sb("s5_T2", [128, NTAU, 64], F32)
        NAR = sb("s5_NAR", [128, NTAU, 64], F32)
        P.op('dve', lambda e: e.tensor_scalar(out=NAR[:], in0=AR[:], scalar1=-1.0, scalar2=None, op0=ALU.mult), reads=['s5_A'], writes=['s5_NAR'])
        T3 = sb("s5_T3", [128, NTAU, 64], F32)
        for (dstt, top, bot) in ((S1, AR, NAI), (S2, NAI, NAR), (T1, AR, AI), (T2, NAI, AR), (T3, AI, AR)):
            P.op('pool', lambda e, dstt=dstt, top=top: e.tensor_copy(out=dstt[0:64], in_=top[0:64]), reads=['s5_A', 's5_NAI', 's5_NAR'], writes=['s5_ST'])
            P.op('pool', lambda e, dstt=dstt, bot=bot: e.tensor_copy(out=dstt[64:128], in_=bot[64:128]), reads=['s5_A', 's5_NAI', 's5_NAR'], writes=['s5_ST'])
        den = sb("s5_den", [128, 64], F32); cr = sb("s5_cr", [128, 64], F32); ci = sb("s5_ci", [128, 64], F32); am1 = sb("s5_am1", [128, 64], F32)
        P.op('dve', lambda e: e.tensor_tensor(out=den[:], in0=LR[:], in1=LR[:], op=ALU.mult), reads=['s5_LR'], writes=['s5_den'])
        P.op('dve', lambda e: e.tensor_tensor(out=tmp[:], in0=LI[:], in1=LI[:], op=ALU.mult), reads=['s5_LI'], writes=['s5_tmp'])
        P.op('dve', lambda e: e.tensor_tensor(out=den[:], in0=den[:], in1=tmp[:], op=ALU.add), reads=['s5_den', 's5_tmp'], writes=['s5_den'])
        P.op('dve', lambda e: e.reciprocal(out=den[:], in_=den[:]), reads=['s5_den'], writes=['s5_den'])
        P.op('dve', lambda e: e.tensor_scalar(out=am1[:], in0=AR[:, 1, :], scalar1=-1.0, scalar2=None, op0=ALU.add), reads=['s5_A'], writes=['s5_am1'])
        P.op('dve', lambda e: e.tensor_tensor(out=cr[:], in0=am1[:], in1=LR[:], op=ALU.mult), reads=['s5_am1', 's5_LR'], writes=['s5_cr'])
        P.op('dve', lambda e: e.tensor_tensor(out=tmp[:], in0=AI[:, 1, :], in1=LI[:], op=ALU.mult), reads=['s5_A', 's5_LI'], writes=['s5_tmp'])
        P.op('dve', lambda e: e.tensor_tensor(out=cr[:], in0=cr[:], in1=tmp[:], op=ALU.add), reads=['s5_cr', 's5_tmp'], writes=['s5_cr'])
        P.op('dve', lambda e: e.tensor_tensor(out=cr[:], in0=cr[:], in1=den[:], op=ALU.mult), reads=['s5_cr', 's5_den'], writes=['s5_cr'])
        P.op('dve', lambda e: e.tensor_tensor(out=ci[:], in0=AI[:, 1, :], in1=LR[:], op=ALU.mult), reads=['s5_A', 's5_LR'], writes=['s5_ci'])
        P.op('dve', lambda e: e.tensor_tensor(out=tmp[:], in0=am1[:], in1=LI[:], op=ALU.mult), reads=['s5_am1', 's5_LI'], writes=['s5_tmp'])
        P.op('dve', lambda e: e.tensor_tensor(out=ci[:], in0=ci[:], in1=tmp[:], op=ALU.subtract), reads=['s5_ci', 's5_tmp'], writes=['s5_ci'])
        P.op('dve', lambda e: e.tensor_tensor(out=ci[:], in0=ci[:], in1=den[:], op=ALU.mult), reads=['s5_ci', 's5_den'], writes=['s5_ci'])
        BR = sb("s5_BR", [128, 64, 16], F32); BI = sb("s5_BI", [128, 64, 16], F32)
        CR = sb("s5_CR", [128, 64, 16], F32); CI = sb("s5_CI", [128, 64, 16], F32)
        P.dma('sp', BR[:], g.s5_br[l], writes=['s5_BR']); P.dma('sp', BI[:], g.s5_bi[l], writes=['s5_BI'])
        P.dma('sp', CR[:], g.s5_cr[l], writes=['s5_CR']); P.dma('sp', CI[:], g.s5_ci[l], writes=['s5_CI'])
        BBR = sb("s5_BBR", [128, 64, 16], F32); BBI = sb("s5_BBI", [128, 64, 16], F32); big = sb("s5_big", [128, 64, 16], F32)
        bc = lambda t2: t2[:].unsqueeze(2).to_broadcast([128, 64, 16])
        P.op('dve', lambda e: e.tensor_tensor(out=BBR[:], in0=BR[:], in1=bc(cr), op=ALU.mult), reads=['s5_BR', 's5_cr'], writes=['s5_BBR'])
        P.op('dve', lambda e: e.tensor_tensor(out=big[:], in0=BI[:], in1=bc(ci), op=ALU.mult), reads=['s5_BI', 's5_ci'], writes=['s5_big'])
        P.op('dve', lambda e: e.tensor_tensor(out=BBR[:], in0=BBR[:], in1=big[:], op=ALU.subtract), reads=['s5_BBR', 's5_big'], writes=['s5_BBR'])
        P.op('dve', lambda e: e.tensor_tensor(out=BBI[:], in0=BI[:], in1=bc(cr), op=ALU.mult), reads=['s5_BI', 's5_cr'], writes=['s5_BBI'])
        P.op('dve', lambda e: e.tensor_tensor(out=big[:], in0=BR[:], in1=bc(ci), op=ALU.mult), reads=['s5_BR', 's5_ci', 's5_BBR'], writes=['s5_big'])
        P.op('dve', lambda e: e.tensor_tensor(out=BBI[:], in0=BBI[:], in1=big[:], op=ALU.add), reads=['s5_BBI', 's5_big'], writes=['s5_BBI'])
        CA = sb("s5_CA", [128, 9, 64, 16], F32); GG = sb("s5_GG", [128, 8, 64, 16], F32)
        bct = lambda tb, ti: tb[:, ti, :].unsqueeze(2).to_broadcast([128, 64, 16])
        for ti in range(9):
            P.op('dve', lambda e, ti=ti: e.tensor_tensor(out=CA[:, ti], in0=CR[:], in1=bct(S1, ti), op=ALU.mult), reads=['s5_CR', 's5_ST'], writes=['s5_CA'])
            P.op('pool', lambda e, ti=ti: e.tensor_tensor(out=big[:], in0=CI[:], in1=bct(S2, ti), op=ALU.mult), reads=['s5_CI', 's5_ST', 's5_BBI', 's5_CA'], writes=['s5_big'])
            P.op('dve', lambda e, ti=ti: e.tensor_tensor(out=CA[:, ti], in0=CA[:, ti], in1=big[:], op=ALU.add), reads=['s5_big', 's5_CA'], writes=['s5_CA'])
        for ti in range(8):
            P.op('dve', lambda e, ti=ti: e.tensor_tensor(out=GG[:, ti], in0=BBR[:], in1=bct(T1, ti), op=ALU.mult), reads=['s5_BBR', 's5_ST'], writes=['s5_GG'])
            P.op('pool', lambda e, ti=ti: e.tensor_tensor(out=big[:], in0=BBI[:], in1=bct(T2, ti), op=ALU.mult), reads=['s5_BBI', 's5_ST', 's5_CA', 's5_GG'], writes=['s5_big'])
            P.op('dve', lambda e, ti=ti: e.tensor_tensor(out=GG[:, ti], in0=GG[:, ti], in1=big[:], op=ALU.add), reads=['s5_big', 's5_GG'], writes=['s5_GG'])
        identf = sb("s5_identf", [128, 128], F32); identb = sb("s5_identb", [128, 128], BF16); II = sb("s5_II", [128, 128], F32)
        DV = sb("s5_DV", [128, 32], F32)
        P.dma('sp', identf[:], g.c_ident[:, :], writes=['s5_identf']); P.dma('pool', identb[:], g.c_ident[:, :], writes=['s5_identb'])
        P.dma('sp', II[:], g.c_ii[:, :], writes=['s5_II']); P.dma('sp', DV[:], g.s5_dv[l], writes=['s5_DV'])
        BP = sb("s5_BP", [128, 15, 16], F32); CP = sb("s5_CP", [128, 15, 16], F32)
        P.op('pool', lambda e: e.memset(BP[:], 0.0), writes=['s5_BP']); P.op('pool', lambda e: e.memset(CP[:], 0.0), writes=['s5_CP'])
        Mi = [sb("s5_Mi%d" % i, [128, 128], BF16) for i in range(2)]
        MV = [sb("s5_MV%d" % i, [128, 128], BF16) for i in range(2)]
        MY = [sb("s5_MY%d" % i, [128, 8, 16], F32) for i in range(2)]
        GT_ = [sb("s5_GTt%d" % i, [128, 8, 16], F32) for i in range(2)]
        Am = [sb("s5_Am%d" % i, [128, 3, 128], F32) for i in range(2)]
        U = [sb("s5_U%d" % i, [128, NJ + 32], BF16) for i in range(2)]
        Z0 = sb("s5_Z0", [128, NJ], F32); Z1 = sb("s5_Z1", [128, 132], F32); Z2 = sb("s5_Z2", [128, 33], F32)
        acc = [sb("s5_acc%d" % i, [128, 132], F32) for i in range(2)]
        P3 = sb("s5_P3", [128, 34], F32); P2 = sb("s5_P2", [128, 132], F32); P1 = sb("s5_P1", [128, NJ], F32)
        Y = [sb("s5_Y%d" % i, [128, NJ], F32) for i in range(2)]
        Ys = [sb("s5_Ys%d" % i, [128, NJ], F32) for i in range(2)]
        pm = [ps("s5_pm%d" % i, [128, 128], F32) for i in range(2)]
        pz = [ps("s5_pz%d" % i, [128, 352], F32) for i in range(3)]
        pv = ps("s5_pv", [128, 128], F32)
        pt_ = ps("s5_pt", [128, 4], F32)
        it = 0
        for gi in range(32):
            for d in range(2):
                dg = d * 32 + gi
                b = it % 2; it += 1
                kb = '_%d' % b
                P.op('dve', lambda e, dg=dg: e.tensor_copy(out=BP[:, 7, :], in_=GG[:, 0, dg, :]), reads=['s5_GG'], writes=['s5_BP'])
                if d == 0:
                    P.op('dve', lambda e, dg=dg: e.tensor_copy(out=CP[:, 7:15, :], in_=CA[:, 0:8, dg, :]), reads=['s5_CA'], writes=['s5_CP'])
                    P.op('pool', lambda e: e.memset(CP[:, 0:7, :], 0.0), writes=['s5_CP'])
                else:
                    P.op('dve', lambda e, dg=dg: e.tensor_copy(out=CP[:, 0:8, :], in_=CA[:, 7::-1, dg, :]), reads=['s5_CA'], writes=['s5_CP'])
                    P.op('pool', lambda e: e.memset(CP[:, 8:15, :], 0.0), writes=['s5_CP'])
                for s in range(8):
                    P.op('pe', lambda e, s=s, b=b: e.matmul(pm[b][:, :], lhsT=BP[:, 7 - s:15 - s, :], rhs=CP[:, 7 - s:15 - s, :], start=(s == 0), stop=(s == 7)),
                         reads=['s5_BP', 's5_CP'], writes=['s5_pm' + kb])
                if d == 0:
                    P.op('dve', lambda e, b=b, gi=gi: e.scalar_tensor_tensor(out=Mi[b][:], in0=identf[:], scalar=DV[:, gi:gi + 1], in1=pm[b][:], op0=ALU.mult, op1=ALU.add),
                         reads=['s5_pm' + kb, 's5_identf', 's5_DV'], writes=['s5_Mi' + kb])
                else:
                    P.op('dve', lambda e, b=b: e.tensor_copy(out=Mi[b][:], in_=pm[b][:]), reads=['s5_pm' + kb], writes=['s5_Mi' + kb])
                if d == 0:
                    P.op('dve', lambda e, b=b, dg=dg: e.tensor_copy(out=GT_[b][:], in_=GG[:, 7::-1, dg, :]), reads=['s5_GG'], writes=['s5_GTt' + kb])
                else:
                    P.op('dve', lambda e, b=b, dg=dg: e.tensor_copy(out=GT_[b][:], in_=GG[:, 0:8, dg, :]), reads=['s5_GG'], writes=['s5_GTt' + kb])
                P.op('pe', lambda e, b=b: e.matmul(pv[:, :], lhsT=GT_[b][:], rhs=identf[:], start=True, stop=True), reads=['s5_GTt' + kb, 's5_identf'], writes=['s5_pv'])
                P.op('act', lambda e, b=b: e.copy(out=MV[b][:], in_=pv[:]), reads=['s5_pv'], writes=['s5_MV' + kb])
                if d == 0:
                    P.op('pool', lambda e, b=b, dg=dg: e.tensor_copy(out=MY[b][:], in_=CA[:, 1:9, dg, :]), reads=['s5_CA'], writes=['s5_MY' + kb])
                else:
                    P.op('pool', lambda e, b=b, dg=dg: e.tensor_copy(out=MY[b][:], in_=CA[:, 8:0:-1, dg, :]), reads=['s5_CA'], writes=['s5_MY' + kb])
                for li, ti in enumerate((8, 9, 10)):
                    P.op('dve', lambda e, b=b, li=li, ti=ti, dg=dg: e.tensor_scalar(out=Am[b][:, li, 0:64], in0=II[:, 0:64], scalar1=S1[:, ti, dg:dg + 1], scalar2=None, op0=ALU.mult),
                         reads=['s5_II', 's5_ST'], writes=['s5_Am' + kb])
                    P.op('dve', lambda e, b=b, li=li, ti=ti, dg=dg: e.tensor_scalar(out=Am[b][:, li, 64:128], in0=II[:, 64:128], scalar1=T3[:, ti, dg:dg + 1], scalar2=None, op0=ALU.mult),
                         reads=['s5_II', 's5_ST'], writes=['s5_Am' + kb])
                j0 = 0 if d == 0 else 32
                P.dma('pool', U[b][:, 0:NJ], g.UB[gi, :, :, j0:j0 + NJ].rearrange("i c j -> (i c) j"), reads=['UB'], writes=['s5_U' + kb])
                Uv = (lambda c0, n, b=b: U[b][:, c0:c0 + n]) if d == 0 else (lambda c0, n, b=b: U[b][:, sl_(NJ - 1 - c0, n, -1)])
                for pc in range(3):
                    P.op('pe', lambda e, b=b, pc=pc, Uv=Uv: e.matmul(pz[pc][:, :], lhsT=MV[b][:], rhs=Uv(pc * 352, 352), start=True, stop=True),
                         reads=['s5_MV' + kb, 's5_U' + kb], writes=['s5_pz%d' % pc])
                    eng = 'act' if pc == 1 else 'dve'
                    if eng == 'act':
                        P.op('act', lambda e, pc=pc: e.copy(out=Z0[:, pc * 352:(pc + 1) * 352], in_=pz[pc][:, :]), reads=['s5_pz%d' % pc], writes=['s5_Z0'])
                    else:
                        P.op('dve', lambda e, pc=pc: e.tensor_copy(out=Z0[:, pc * 352:(pc + 1) * 352], in_=pz[pc][:, :]), reads=['s5_pz%d' % pc], writes=['s5_Z0'])

                def horner(src, R, M, A, dst, tag):
                    cur = None
                    for n in range(1, R):
                        prev = src[:, sl_(0, M, R)] if n == 1 else cur
                        prevk = tag if n == 1 else 's5_acc%d' % ((n - 1) % 2)
                        last = (n == R - 1)
                        o = dst if last else acc[n % 2][:, 0:M]
                        ok = ('s5_dst' + tag) if last else 's5_acc%d' % (n % 2)
                        P.op('pe', lambda e, prev=prev, A=A, M=M: e.matmul(pz[0][:, 0:M], lhsT=A, rhs=prev, start=True, stop=False),
                             reads=['s5_Am' + kb, prevk], writes=['s5_pz0'])
                        P.op('pe', lambda e, n=n, M=M, R=R, src=src: e.matmul(pz[0][:, 0:M], lhsT=identf[:], rhs=src[:, sl_(n, M, R)], start=False, stop=True),
                             reads=['s5_identf', tag], writes=['s5_pz0'])
                        P.op('dve', lambda e, o=o, M=M: e.tensor_copy(out=o, in_=pz[0][:, 0:M]), reads=['s5_pz0'], writes=[ok])
                        cur = o
                horner(Z0, 8, 132, Am[b][:, 0, :], Z1[:, :], 's5_Z0')
                horner(Z1, 4, 33, Am[b][:, 1, :], Z2[:, :], 's5_dsts5_Z0')
                P.op('pool', lambda e: e.memset(P3[:, 0:1], 0.0), writes=['s5_P3'])
                for q in range(32):
                    P.op('pe', lambda e, q=q, b=b: e.matmul(pt_[:, 0:1], lhsT=Am[b][:, 2, :], rhs=P3[:, q:q + 1], start=True, stop=False),
                         reads=['s5_Am' + kb, 's5_P3'], writes=['s5_pt'])
                    P.op('pe', lambda e, q=q: e.matmul(pt_[:, 0:1], lhsT=identf[:], rhs=Z2[:, q:q + 1], start=False, stop=True),
                         reads=['s5_identf', 's5_dsts5_dsts5_Z0'], writes=['s5_pt'])
                    P.op('dve', lambda e, q=q: e.tensor_copy(out=P3[:, q + 1:q + 2], in_=pt_[:, 0:1]), reads=['s5_pt'], writes=['s5_P3'])

                def expand(Pc, Zs, R, M, A, Pf, pck, zk, pfk):
                    P.op('pool', lambda e, Pf=Pf, Pc=Pc, R=R, M=M: e.tensor_copy(out=Pf[:, sl_(0, M, R)], in_=Pc[:, 0:M]), reads=[pck], writes=[pfk])
                    for n in range(R - 1):
                        P.op('pe', lambda e, n=n, Pf=Pf, A=A, R=R, M=M: e.matmul(pz[1][:, 0:M], lhsT=A, rhs=Pf[:, sl_(n, M, R)], start=True, stop=False),
                             reads=['s5_Am' + kb, pfk], writes=['s5_pz1'])
                        P.op('pe', lambda e, n=n, Zs=Zs, R=R, M=M: e.matmul(pz[1][:, 0:M], lhsT=identf[:], rhs=Zs[:, sl_(n, M, R)], start=False, stop=True),
                             reads=['s5_identf', zk], writes=['s5_pz1'])
                        P.op('act', lambda e, n=n, Pf=Pf, R=R, M=M: e.copy(out=Pf[:, sl_(n + 1, M, R)], in_=pz[1][:, 0:M]), reads=['s5_pz1'], writes=[pfk])
                expand(P3, Z1, 4, 33, Am[b][:, 1, :], P2, 's5_P3', 's5_dsts5_Z0', 's5_P2')
                expand(P2, Z0, 8, 132, Am[b][:, 0, :], P1, 's5_P2', 's5_Z0', 's5_P1')
                for pc in range(3):
                    c0 = pc * 352
                    Pv = P1[:, c0:c0 + 352] if d == 0 else P1[:, sl_(NJ - 1 - c0, 352, -1)]
                    P.op('pe', lambda e, b=b, pc=pc, Pv=Pv: e.matmul(pz[pc][:, :], lhsT=MY[b][:], rhs=Pv, start=True, stop=False),
                         reads=['s5_MY' + kb, 's5_P1'], writes=['s5_pz%d' % pc])
                    P.op('pe', lambda e, b=b, pc=pc, c0=c0: e.matmul(pz[pc][:, :], lhsT=Mi[b][:], rhs=U[b][:, c0:c0 + 352], start=False, stop=True),
                         reads=['s5_Mi' + kb, 's5_U' + kb], writes=['s5_pz%d' % pc])
                    if d == 0:
                        P.op('act', lambda e, pc=pc, c0=c0: e.copy(out=Y[0][:, c0:c0 + 352], in_=pz[pc][:, :]), reads=['s5_pz%d' % pc], writes=['s5_Y0'])
                    else:
                        P.op('dve', lambda e, pc=pc, c0=c0: e.tensor_copy(out=Y[1][:, c0:c0 + 352], in_=pz[pc][:, :]), reads=['s5_pz%d' % pc], writes=['s5_Y1'])
                if d == 1:
                    yi = gi % 2
                    P.op('pool', lambda e, yi=yi: e.tensor_tensor(out=Ys[yi][:, 32:NJ], in0=Y[0][:, 32:NJ], in1=Y[1][:, 0:NJ - 32], op=ALU.add),
                         reads=['s5_Y0', 's5_Y1'], writes=['s5_Ys%d' % yi])
                    P.op('pool', lambda e, yi=yi: e.tensor_tensor(out=Ys[yi][:, 0:32], in0=Y[0][:, 0:32], in1=Y[1][:, NJ - 32:NJ], op=ALU.add),
                         reads=['s5_Y0', 's5_Y1'], writes=['s5_Ys%d' % yi])
                    P.dma('sp', g.YB[gi].rearrange("i c j -> (i c) j"), Ys[yi][:], reads=['s5_Ys%d' % yi], writes=[('YB', gi)])


def phase_s5_readout(nc, P, g, l):
    with ExitStack() as es:
        sb, ps = mk_alloc(nc, es)
        gw = sb("sr_gw", [128, 4, 512], BF16)
        P.dma('pool', gw[:], g.s5_glu_w[l, :, :].rearrange("(k p) n -> p k n", p=128), writes=['sr_gw'])
        gb = sb("sr_gb", [128, 4], F32)
        P.dma('sp', gb[:], g.s5_glu_bp[l], writes=['sr_gb'])
        ident = sb("sr_ident", [128, 128], BF16)
        P.dma('pool', ident[:], g.c_ident[:, :], writes=['sr_ident'])
        yT = [sb("sr_yT%d" % i, [128, 8, 64], F32) for i in range(2)]
        xo = sb("sr_xo", [128, 512], F32); sq = sb("sr_sq", [128, 512], F32); sg = sb("sr_sg", [128, 512], F32)
        glf = sb("sr_glf", [128, 4, 512], F32); glb = sb("sr_glb", [128, 4, 512], BF16)
        soT = sb("sr_soT", [128, 4, 512], BF16)
        so = [sb("sr_so%d" % i, [128, 512], BF16) for i in range(2)]
        pg = [ps("sr_pg%d" % i, [128, 512], F32) for i in range(2)]
        ptr = [ps("sr_ptr%d" % i, [128, 4, 128], BF16) for i in range(2)]
        nblk = (T + 511) // 512
        cn = 0
        for b in range(nblk):
            ntok = min(512, T - b * 512)
            nj = ntok // 8
            j0 = b * 64
            for cc in range(4):
                yi = cn % 2; cn += 1
                for gg in range(8):
                    gi = cc * 8 + gg
                    P.dma('sp', yT[yi][gg * 16:(gg + 1) * 16, :, :nj], g.YB[gi, :, :, j0:j0 + nj].rearrange("i c j -> c i j"),
                          reads=[('YB', gi)], writes=['sr_yT%d' % yi])
                P.op('dve', lambda e, yi=yi, nj=nj, ntok=ntok: e.tensor_copy(out=xo[:, :ntok].rearrange("p (j i) -> p j i", i=8),
                                                                            in_=yT[yi][:, :, :nj].rearrange("p i j -> p j i")),
                     reads=['sr_yT%d' % yi], writes=['sr_xo'])
                P.op('act', lambda e, ntok=ntok: e.activation(out=sq[:, :ntok], in_=xo[:, :ntok], func=AF.Square), reads=['sr_xo'], writes=['sr_sq'])
                P.op('dve', lambda e, ntok=ntok: e.tensor_scalar(out=sq[:, :ntok], in0=sq[:, :ntok], scalar1=0.044715, scalar2=1.0, op0=ALU.mult, op1=ALU.add),
                     reads=['sr_sq'], writes=['sr_sq'])
                P.op('dve', lambda e, ntok=ntok: e.tensor_tensor(out=sq[:, :ntok], in0=sq[:, :ntok], in1=xo[:, :ntok], op=ALU.mult), reads=['sr_sq', 'sr_xo'], writes=['sr_sq'])
                P.op('act', lambda e, ntok=ntok: e.activation(out=sg[:, :ntok], in_=sq[:, :ntok], func=AF.Sigmoid, scale=1.5957691216), reads=['sr_sq'], writes=['sr_sg'])
                P.op('dve', lambda e, cc=cc, ntok=ntok: e.tensor_tensor(out=glf[:, cc, :ntok], in0=xo[:, :ntok], in1=sg[:, :ntok], op=ALU.mult),
                     reads=['sr_xo', 'sr_sg'], writes=['sr_glf'])
                P.op('pool', lambda e, cc=cc, ntok=ntok: e.tensor_copy(out=glb[:, cc, :ntok], in_=glf[:, cc, :ntok]), reads=['sr_glf'], writes=['sr_glb'])
            for oc in range(4):
                pi = cn % 2; cn += 1
                for k in range(4):
                    P.op('pe', lambda e, k=k, oc=oc, pi=pi, ntok=ntok: e.matmul(pg[pi][:, :ntok], lhsT=gw[:, k, oc * 128:(oc + 1) * 128], rhs=glb[:, k, :ntok],
                                                                           start=(k == 0), stop=(k == 3)), reads=['sr_gw', 'sr_glb'], writes=['sr_pg%d' % pi])
                P.op('act', lambda e, oc=oc, pi=pi, ntok=ntok: e.activation(out=sg[:, :ntok], in_=pg[pi][:, :ntok], func=AF.Sigmoid, bias=gb[:, oc:oc + 1]),
                     reads=['sr_pg%d' % pi, 'sr_gb'], writes=['sr_sg'])
                P.op('dve', lambda e, oc=oc, ntok=ntok: e.tensor_tensor(out=soT[:, oc, :ntok], in0=glf[:, oc, :ntok], in1=sg[:, :ntok], op=ALU.mult),
                     reads=['sr_glf', 'sr_sg'], writes=['sr_soT'])
            for ti in range(ntok // 128):
                pi = cn % 2; cn += 1
                for oc in range(4):
                    P.op('pe', lambda e, oc=oc, pi=pi, ti=ti: e.transpose(out=ptr[pi][:, oc, :], in_=soT[:, oc, ti * 128:(ti + 1) * 128], identity=ident[:]),
                         reads=['sr_soT', 'sr_ident'], writes=['sr_ptr%d' % pi])
                P.op('act', lambda e, pi=pi: e.copy(out=so[pi][:].rearrange("p (a b) -> p a b", a=4), in_=ptr[pi][:]), reads=['sr_ptr%d' % pi], writes=['sr_so%d' % pi])
                t = b * 4 + ti
                P.dma('sp', g.SO[t * 128:(t + 1) * 128, :], so[pi][:], reads=['sr_so%d' % pi], writes=['SO'])


import os as _os
RW_DBG_CHUNKS = int(_os.environ.get('RW_DBG_CHUNKS', '0'))
RW_DBG_STOP = int(_os.environ.get('RW_DBG_STOP', '99'))
CDEC = 0.6065306597126334


def phase_rwkv(nc, P, g, l):
    with ExitStack() as es:
        sb, ps = mk_alloc(nc, es)
        MUP = sb("rw_MUP", [128, 14], F32); MUN = sb("rw_MUN", [128, 14], F32); C0 = sb("rw_C0", [128, 14], F32)
        P.dma('sp', MUP[:], g.rw_mup[l], writes=['rw_MUP']); P.dma('sp', MUN[:], g.rw_mun[l], writes=['rw_MUN'])
        P.op('dve', lambda e: e.tensor_tensor(out=C0[:], in0=MUP[:], in1=MUN[:], op=ALU.add), reads=['rw_MUP', 'rw_MUN'], writes=['rw_C0'])
        P.op('dve', lambda e: e.tensor_scalar(out=C0[:], in0=C0[:], scalar1=-1.0, scalar2=1.0, op0=ALU.mult, op1=ALU.add), reads=['rw_C0'], writes=['rw_C0'])
        PV = sb("rw_PV", [128, 4, 4], F32)
        P.dma('sp', PV[:, 0:3, :], g.rw_pv[l], writes=['rw_PV'])
        P.op('dve', lambda e: e.tensor_scalar(out=PV[:, 3, :], in0=PV[:, 1, :], scalar1=-1.0, scalar2=1.0, op0=ALU.mult, op1=ALU.add), reads=['rw_PV'], writes=['rw_PV'])
        WA0 = sb("rw_WA0", [128, 2, 2, 4], F32)
        P.dma('sp', WA0[:].rearrange("p a d h -> p (a d h)"), g.rw_wa0[l], writes=['rw_WA0'])
        LW = sb("rw_LW", [128, 2, 512], BF16)
        for d in range(2):
            P.dma('pool', LW[0:64, d, :], g.rwkv_w2[l, d, :, :], writes=['rw_LW'])
            P.dma('pool', LW[64:128, d, :], g.rwkv_a2[l, d, :, :], writes=['rw_LW'])
        G2 = sb("rw_G2", [128, 512], BF16)
        P.dma('pool', G2[:], g.rwkv_g2[l, :, :], writes=['rw_G2'])
        LNW = sb("rw_LNW", [128, 512], F32); LNB = sb("rw_LNB", [128, 512], F32)
        P.dma('sp', LNW[:], g.rwkv_ln_w[l, :].partition_broadcast(128), writes=['rw_LNW'])
        P.dma('sp', LNB[:], g.rwkv_ln_b[l, :].partition_broadcast(128), writes=['rw_LNB'])
        BO = sb("rw_BO", [128, 128], F32); HI = sb("rw_HI", [128, 2], F32)
        P.dma('sp', BO[:], g.c_bo[:, :], writes=['rw_BO']); P.dma('sp', HI[:], g.c_hi[:, :], writes=['rw_HI'])
        identb = sb("rw_identb", [128, 128], BF16); identf = sb("rw_identf", [128, 128], F32)
        P.dma('pool', identb[:], g.c_ident[:, :], writes=['rw_identb']); P.dma('sp', identf[:], g.c_ident[:, :], writes=['rw_identf'])
        MK = sb("rw_MK", [128, 8, 128], F32)
        P.dma('sp', MK[:], g.c_masks[:, :, :], writes=['rw_MK'])
        RS = sb("rw_RS", [128, 4, 128], F32)
        P.op('pool', lambda e: e.memset(RS[:], 1.0), writes=['rw_RS'])
        P.op('pool', lambda e: e.memset(RS[:, :, 0:1], 0.0), writes=['rw_RS'])
        W4 = [128, 4, 128]
        zraw = [sb("rw_zraw%d" % i, [128, 14, 130], F32) for i in range(2)]
        zT = sb("rw_zT", [128, 14, 128], F32); zt2 = sb("rw_zt2", [128, 14, 128], F32)
        tws = sb("rw_tws", [128, 128], BF16); sgd = sb("rw_sgd", [128, 128], BF16)
        gsb = sb("rw_gsb", [128, 512], F32)
        kk = sb("rw_kk", W4, F32); t1 = sb("rw_t1", W4, F32); t2 = sb("rw_t2", W4, F32)
        sig = sb("rw_sig", W4, F32); cs = sb("rw_cs", W4, F32); ex = sb("rw_ex", W4, F32)
        gi_ = sb("rw_gi", W4, F32); ge_ = sb("rw_ge", W4, F32); d4 = sb("rw_d4", W4, F32)
        e1 = sb("rw_e1", W4, F32); e2 = sb("rw_e2", W4, F32); e3 = sb("rw_e3", W4, F32); e4 = sb("rw_e4", W4, F32)
        av = sb("rw_av", W4, F32); kd = sb("rw_kd", W4, F32); ka_ = sb("rw_ka", W4, F32)
        tot = sb("rw_tot", [128, 4], F32); gamL = sb("rw_gamL", [128, 4], F32)
        ART = sb("rw_ART", [128, 4, 2, 128], BF16); BKT = sb("rw_BKT", [128, 4, 2, 128], BF16)
        TR3 = sb("rw_TR3", [128, 4, 3, 128], BF16); VKB = sb("rw_VKB", [128, 3, 512], BF16)
        bon = sb("rw_bon", [128, 8], F32)
        Sf = sb("rw_Sf", [128, 4, 64], F32); Sb = sb("rw_Sb", [128, 8, 64], BF16)
        BKZ = sb("rw_BKZ", [128, 8, 2, 128], BF16)
        P.op('pool', lambda e: e.memset(BKZ[:], 0.0), writes=['rw_BKZ'])
        XD = [sb("rw_XD%d" % i, [128, 2, 8, 128], BF16) for i in range(2)]
        XO = sb("rw_XO", [128, 8, 128], BF16)
        DD = [sb("rw_DD%d" % i, [128, 2, 8, 128], BF16) for i in range(2)]
        Mb = sb("rw_Mb", [128, 8, 128], BF16); Nb = sb("rw_Nb", [128, 8, 128], BF16); M2b = sb("rw_M2b", [128, 8, 128], BF16); Ssb = sb("rw_Ssb", [128, 8, 128], BF16)
        Ttb = sb("rw_Ttb", [128, 8, 128], BF16)
        ArbT = sb("rw_ArbT", [128, 8, 128], BF16); A3T = sb("rw_A3T", [128, 8, 2, 128], BF16)
        Wb = sb("rw_Wb", [128, 8, 64], BF16); Ub = sb("rw_Ub", [128, 8, 64], BF16)
        Yo = [sb("rw_Yo%d" % i, [128, 512], F32) for i in range(2)]
        y0 = sb("rw_y0", [128, 512], F32); b0 = sb("rw_b0", [128, 8], F32)
        st = sb("rw_st", [128, 4, 8], F32)
        ysq = sb("rw_ysq", [128, 512], F32); ao = [sb("rw_ao%d" % i, [128, 512], BF16) for i in range(2)]
        ptr = ps("rw_ptr", [128, 2, 3, 128], BF16)
        PL = ps("rw_PL", [128, 4, 128], F32)
        PA_ = ps("rw_PA", [128, 2, 256], F32); pA = [PA_[:, 0, :], PA_[:, 1, :]]
        N1 = ps("rw_N1", [128, 4, 128], F32); N2 = ps("rw_N2", [128, 4, 128], F32); N3 = ps("rw_N3", [128, 4, 128], F32)
        PW = ps("rw_PW", [128, 8, 64], F32)
        pY = ps("rw_pY", [128, 512], F32)
        seg_start = {0, 2, 66}; seg_end = {1, 65, 67}
        b14 = lambda t_: t_[:].unsqueeze(2).to_broadcast([128, 14, 128])
        b4 = lambda ap_: ap_.unsqueeze(2).to_broadcast(W4)
        cnt = 0
        for d in range(2):
            P.op('pool', lambda e: e.memset(Sf[:], 0.0), writes=['rw_Sf'])
            P.op('pool', lambda e: e.memset(Sb[:], 0.0), writes=['rw_Sb'])
            chunks = list(range(0, 66)) if d == 0 else [67, 66] + list(range(65, 1, -1))
            if RW_DBG_CHUNKS:
                chunks = chunks[:RW_DBG_CHUNKS]
            m_strict = 0 if d == 0 else 1
            m_T = (1, 3) if d == 0 else (0, 2)
            GI, GE = (cs, ex) if d == 0 else (gi_, ge_)
            gik, gek = ('rw_cs', 'rw_ex') if d == 0 else ('rw_gi', 'rw_ge')
            for c in chunks:
                tt = c if c <= 65 else c - 66
                zi = cnt % 2; cnt += 1
                zr = zraw[zi]; zk = 'rw_zraw%d' % zi
                s0 = c * 128
                lo = s0 - 1 if c not in seg_start else s0
                hi = s0 + 129 if c not in seg_end else s0 + 128
                if c in seg_start:
                    P.op('pool', lambda e, zr=zr: e.memset(zr[:, :, 0:1], 0.0), writes=[zk])
                if c in seg_end:
                    P.op('pool', lambda e, zr=zr: e.memset(zr[:, :, 129:130], 0.0), writes=[zk])
                P.dma('sp', zr[:, :, lo - s0 + 1:hi - s0 + 1], g.RWT[:, lo:hi].rearrange("(r p) t -> p r t", p=128), reads=['RWT'], writes=[zk])
                P.op('dve', lambda e, zr=zr: e.tensor_tensor(out=zT[:], in0=zr[:, :, 1:129], in1=b14(C0), op=ALU.mult), reads=[zk, 'rw_C0'], writes=['rw_zT'])
                P.op('pool', lambda e, zr=zr: e.tensor_tensor(out=zt2[:], in0=zr[:, :, 0:128], in1=b14(MUP), op=ALU.mult), reads=[zk, 'rw_MUP'], writes=['rw_zt2'])
                P.op('dve', lambda e: e.tensor_tensor(out=zT[:], in0=zT[:], in1=zt2[:], op=ALU.add), reads=['rw_zT', 'rw_zt2'], writes=['rw_zT'])
                P.op('pool', lambda e, zr=zr: e.tensor_tensor(out=zt2[:], in0=zr[:, :, 2:130], in1=b14(MUN), op=ALU.mult), reads=[zk, 'rw_MUN', 'rw_zT'], writes=['rw_zt2'])
                P.op('dve', lambda e: e.tensor_tensor(out=zT[:], in0=zT[:], in1=zt2[:], op=ALU.add), reads=['rw_zT', 'rw_zt2'], writes=['rw_zT'])
                r4 = zT[:, 0:4, :]; k4 = zT[:, 4:8, :]; v4 = zT[:, 8:12, :]
                if RW_DBG_STOP <= 1:
                    continue
                P.op('act', lambda e: e.activation(out=tws[0:64, :], in_=zT[0:64, 12, :], func=AF.Tanh), reads=['rw_zT'], writes=['rw_tws'])
                P.op('pool', lambda e: e.tensor_copy(out=tws[64:128, :], in_=zT[64:128, 12, :]), reads=['rw_zT'], writes=['rw_tws'])
                if d == 1:
                    P.op('act', lambda e: e.activation(out=sgd[:], in_=zT[:, 13, :], func=AF.Sigmoid), reads=['rw_zT'], writes=['rw_sgd'])
                    P.op('pe', lambda e: e.matmul(PL[:].rearrange("p a b -> p (a b)"), lhsT=sgd[:], rhs=G2[:], start=True, stop=True), reads=['rw_sgd', 'rw_G2'], writes=['rw_PL'])
                    P.op('act', lambda e: e.copy(out=gsb[:], in_=PL[:].rearrange("p a b -> p (a b)")), reads=['rw_PL'], writes=['rw_gsb'])
                P.op('dve', lambda e: e.tensor_tensor(out=kk[:], in0=k4, in1=b4(PV[:, 0, :]), op=ALU.mult), reads=['rw_zT', 'rw_PV'], writes=['rw_kk'])
                P.op('act', lambda e: e.activation(out=t1[:], in_=kk[:], func=AF.Square), reads=['rw_kk'], writes=['rw_t1'])
                P.op('pe', lambda e: e.matmul(PL[:].rearrange("p a b -> p (a b)"), lhsT=BO[:], rhs=t1[:].rearrange("p a b -> p (a b)"), start=True, stop=True), reads=['rw_BO', 'rw_t1'], writes=['rw_PL'])
                P.op('act', lambda e: e.activation(out=t1[:], in_=PL[:], func=AF.Sqrt), reads=['rw_PL'], writes=['rw_t1'])
                P.op('dve', lambda e: e.tensor_scalar(out=t1[:], in0=t1[:], scalar1=1e-12, scalar2=None, op0=ALU.max), reads=['rw_t1'], writes=['rw_t1'])
                P.op('dve', lambda e: e.reciprocal(out=t1[:], in_=t1[:]), reads=['rw_t1'], writes=['rw_t1'])
                P.op('dve', lambda e: e.tensor_tensor(out=kk[:], in0=kk[:], in1=t1[:], op=ALU.mult), reads=['rw_kk', 'rw_t1'], writes=['rw_kk'])
                for hp in range(4):
                    P.op('pe', lambda e, hp=hp, d=d: e.matmul(PL[:, hp, :], lhsT=LW[0:64, d, hp * 128:(hp + 1) * 128], rhs=tws[0:64, :], start=True, stop=True), reads=['rw_LW', 'rw_tws', 'rw_t1'], writes=['rw_PL'])
                P.op('dve', lambda e, d=d: e.tensor_tensor(out=sig[:], in0=PL[:], in1=b4(WA0[:, 0, d, :]), op=ALU.add), reads=['rw_PL', 'rw_WA0'], writes=['rw_sig'])
                P.op('act', lambda e: e.activation(out=sig[:], in_=sig[:], func=AF.Sigmoid), reads=['rw_sig'], writes=['rw_sig'])
                for hp in range(4):
                    P.op('pe', lambda e, hp=hp, d=d: e.matmul(PL[:, hp, :], lhsT=LW[64:128, d, hp * 128:(hp + 1) * 128], rhs=tws[64:128, :], start=True, stop=True), reads=['rw_LW', 'rw_tws', 'rw_sig'], writes=['rw_PL'])
                P.op('dve', lambda e, d=d: e.tensor_tensor(out=av[:], in0=PL[:], in1=b4(WA0[:, 1, d, :]), op=ALU.add), reads=['rw_PL', 'rw_WA0'], writes=['rw_av'])
                P.op('act', lambda e: e.activation(out=av[:], in_=av[:], func=AF.Sigmoid), reads=['rw_av'], writes=['rw_av'])
                if RW_DBG_STOP <= 2:
                    continue
                fl = lambda t_: t_[:].rearrange("p a b -> p (a b)")
                P.op('dve', lambda e: e.tensor_tensor_scan(out=fl(cs), data0=fl(RS), data1=fl(sig), initial=0.0, op0=ALU.mult, op1=ALU.add), reads=['rw_sig', 'rw_RS'], writes=['rw_cs'])
                P.op('pool', lambda e: e.tensor_copy(out=tot[:], in_=cs[:, :, 127]), reads=['rw_cs'], writes=['rw_tot'])
                P.op('act', lambda e: e.activation(out=gamL[:], in_=cs[:, :, 127], func=AF.Exp, scale=-CDEC), reads=['rw_cs'], writes=['rw_gamL'])
                P.op('dve', lambda e: e.tensor_tensor(out=ex[:], in0=cs[:], in1=sig[:], op=ALU.subtract), reads=['rw_cs', 'rw_sig'], writes=['rw_ex'])
                if d == 1:
                    P.op('dve', lambda e: e.tensor_tensor(out=gi_[:], in0=b4(tot[:, :]), in1=ex[:], op=ALU.subtract), reads=['rw_ex', 'rw_tot'], writes=['rw_gi'])
                    P.op('dve', lambda e: e.tensor_tensor(out=ge_[:], in0=b4(tot[:, :]), in1=cs[:], op=ALU.subtract), reads=['rw_cs', 'rw_tot'], writes=['rw_ge'])
                P.op('dve', lambda e, GI=GI: e.tensor_tensor(out=d4[:], in0=b4(tot[:, :]), in1=GI[:], op=ALU.subtract), reads=[gik, 'rw_tot'], writes=['rw_d4'])
                P.op('act', lambda e, GI=GI: e.activation(out=e1[:], in_=GI[:], func=AF.Exp, scale=-CDEC), reads=[gik], writes=['rw_e1'])
                P.op('act', lambda e, GI=GI: e.activation(out=e2[:], in_=GI[:], func=AF.Exp, scale=CDEC), reads=[gik], writes=['rw_e2'])
                P.op('act', lambda e, GE=GE: e.activation(out=e3[:], in_=GE[:], func=AF.Exp, scale=-CDEC), reads=[gek], writes=['rw_e3'])
                P.op('act', lambda e: e.activation(out=e4[:], in_=d4[:], func=AF.Exp, scale=-CDEC), reads=['rw_d4'], writes=['rw_e4'])
                if RW_DBG_STOP <= 3:
                    continue
                P.op('dve', lambda e: e.tensor_tensor(out=t2[:], in0=av[:], in1=b4(PV[:, 1, :]), op=ALU.mult), reads=['rw_av', 'rw_PV'], writes=['rw_t2'])
                P.op('dve', lambda e: e.tensor_tensor(out=t2[:], in0=t2[:], in1=b4(PV[:, 3, :]), op=ALU.add), reads=['rw_t2', 'rw_PV'], writes=['rw_t2'])
                P.op('dve', lambda e: e.tensor_tensor(out=kd[:], in0=k4, in1=t2[:], op=ALU.mult), reads=['rw_zT', 'rw_t2'], writes=['rw_kd'])
                P.op('pool', lambda e: e.tensor_tensor(out=ka_[:], in0=kk[:], in1=av[:], op=ALU.mult), reads=['rw_kk', 'rw_av'], writes=['rw_ka'])
                P.op('dve', lambda e: e.scalar_tensor_tensor(out=ART[:, :, 0, :], in0=kk[:], scalar=-1.0, in1=e3[:], op0=ALU.mult, op1=ALU.mult), reads=['rw_kk', 'rw_e3'], writes=['rw_ART'])
                P.op('pool', lambda e: e.tensor_tensor(out=ART[:, :, 1, :], in0=r4, in1=e1[:], op=ALU.mult), reads=['rw_zT', 'rw_e1'], writes=['rw_ART'])
                P.op('dve', lambda e: e.tensor_tensor(out=BKT[:, :, 0, :], in0=ka_[:], in1=e2[:], op=ALU.mult), reads=['rw_ka', 'rw_e2'], writes=['rw_BKT'])
                P.op('pool', lambda e: e.tensor_tensor(out=BKT[:, :, 1, :], in0=kd[:], in1=e2[:], op=ALU.mult), reads=['rw_kd', 'rw_e2'], writes=['rw_BKT'])
                P.op('act', lambda e: e.copy(out=TR3[:, :, 0, :], in_=v4), reads=['rw_zT'], writes=['rw_TR3'])
                P.op('dve', lambda e: e.tensor_tensor(out=TR3[:, :, 1, :], in0=kd[:], in1=e4[:], op=ALU.mult), reads=['rw_kd', 'rw_e4'], writes=['rw_TR3'])
                P.op('pool', lambda e: e.tensor_tensor(out=TR3[:, :, 2, :], in0=ka_[:], in1=e4[:], op=ALU.mult), reads=['rw_ka', 'rw_e4'], writes=['rw_TR3'])
                P.op('dve', lambda e: e.tensor_tensor(out=t2[:], in0=r4, in1=kd[:], op=ALU.mult), reads=['rw_zT', 'rw_kd', 'rw_t2'], writes=['rw_t2'])
                P.op('dve', lambda e: e.tensor_tensor(out=t2[:], in0=t2[:], in1=b4(PV[:, 2, :]), op=ALU.mult), reads=['rw_t2', 'rw_PV'], writes=['rw_t2'])
                for hp in range(4):
                    P.op('pe', lambda e, hp=hp: e.matmul(PL[:, 0, 2 * hp:2 * hp + 2], lhsT=t2[:, hp, :], rhs=HI[:], start=True, stop=True), reads=['rw_t2', 'rw_HI', 'rw_av'], writes=['rw_PL'])
                P.op('dve', lambda e: e.tensor_copy(out=bon[:], in_=PL[:, 0, 0:8]), reads=['rw_PL'], writes=['rw_bon'])
                for h2 in range(2):
                    for a_ in range(2):
                        hp = h2 * 2 + a_
                        for j in range(3):
                            P.op('pe', lambda e, hp=hp, a_=a_, j=j: e.transpose(out=ptr[:, a_, j, :], in_=TR3[:, hp, j, :], identity=identb[:]), reads=['rw_TR3', 'rw_identb'], writes=['rw_ptr'])
                    P.op('act', lambda e, h2=h2: e.copy(out=VKB[:, :, h2 * 256:(h2 + 1) * 256].rearrange("p j (a c) -> p j a c", a=2), in_=ptr.rearrange("p a j c -> p j a c")),
                         reads=['rw_ptr'], writes=['rw_VKB'])
                if RW_DBG_STOP <= 4:
                    continue
                hsl = lambda h: (h // 2, slice((h % 2) * 64, (h % 2) * 64 + 64))
                P.op('pool', lambda e: e.tensor_copy(out=BKZ[0:64, 0::2, :, :], in_=BKT[0:64, :, :, :]), reads=['rw_BKT'], writes=['rw_BKZ'])
                P.op('pool', lambda e: e.tensor_copy(out=BKZ[64:128, 1::2, :, :], in_=BKT[64:128, :, :, :]), reads=['rw_BKT'], writes=['rw_BKZ'])
                md_ = 4 if d == 0 else 5
                mo_ = 6 if d == 0 else 7
                mdT_ = 5 if d == 0 else 4
                for grp in range(2):
                    for j in range(4):
                        h = grp * 4 + j; hp, hr = hsl(h)
                        P.op('pe', lambda e, hp=hp, j=j, grp=grp: e.matmul(N1[:, j, :], lhsT=ART[:, hp, 0, :], rhs=BKZ[:, grp * 4 + j, 0, :], start=True, stop=True), reads=['rw_ART', 'rw_BKZ'], writes=['rw_N1'])
                    P.op('dve', lambda e, grp=grp, md_=md_: e.tensor_tensor(out=XD[0][:, 0, grp * 4:(grp + 1) * 4, :], in0=N1[:], in1=MK[:, md_:md_ + 1, :].to_broadcast([128, 4, 128]), op=ALU.mult),
                         reads=['rw_N1', 'rw_MK'], writes=['rw_XD0_g%d' % grp])
                    P.op('dve', lambda e, grp=grp, mo_=mo_: e.tensor_tensor(out=XO[:, grp * 4:(grp + 1) * 4, :], in0=N1[:], in1=MK[:, mo_:mo_ + 1, :].to_broadcast([128, 4, 128]), op=ALU.mult),
                         reads=['rw_N1', 'rw_MK'], writes=['rw_XO_g%d' % grp])
                for h in range(8):
                    hp, hr = hsl(h); ai = h % 2
                    P.op('pe', lambda e, hp=hp, hr=hr, ai=ai, h=h: e.matmul(pA[ai][:, :], lhsT=BKZ[:, h, 0, :], rhs=ART[:, hp, :, :], start=True, stop=True), reads=['rw_ART', 'rw_BKZ'], writes=['rw_pA%d' % ai])
                    P.op('dve', lambda e, ai=ai, h=h, mdT_=mdT_: e.tensor_tensor(out=XD[0][:, 1, h, :], in0=pA[ai][:, 0:128], in1=MK[:, mdT_, :], op=ALU.mult), reads=['rw_pA%d' % ai, 'rw_MK'], writes=['rw_XD0_g%d' % (h // 4)])
                    P.op('dve', lambda e, ai=ai, h=h, m_T=m_T: e.tensor_tensor(out=ArbT[:, h, :], in0=pA[ai][:, 128:256], in1=MK[:, m_T[1], :], op=ALU.mult), reads=['rw_pA%d' % ai, 'rw_MK'], writes=['rw_ArbT'])
                    P.op('pe', lambda e, hp=hp, hr=hr, ai=ai, h=h: e.matmul(pA[ai][:, :], lhsT=BKZ[:, h, 1, :], rhs=ART[:, hp, :, :], start=True, stop=True), reads=['rw_ART', 'rw_BKZ', 'rw_ArbT', 'rw_XD0_g%d' % (h // 4)], writes=['rw_pA%d' % ai])
                    P.op('dve', lambda e, ai=ai, h=h, m_T=m_T: e.tensor_tensor(out=A3T[:, h, 0, :], in0=pA[ai][:, 0:128], in1=MK[:, m_T[0], :], op=ALU.mult), reads=['rw_pA%d' % ai, 'rw_MK'], writes=['rw_A3T'])
                    P.op('dve', lambda e, ai=ai, h=h, m_T=m_T: e.tensor_tensor(out=A3T[:, h, 1, :], in0=pA[ai][:, 128:256], in1=MK[:, m_T[1], :], op=ALU.mult), reads=['rw_pA%d' % ai, 'rw_MK'], writes=['rw_A3T'])
                if RW_DBG_STOP <= 5:
                    continue
                idb4 = identb[:].unsqueeze(1).unsqueeze(1).to_broadcast([128, 2, 4, 128])
                for grp in range(2):
                    hs = slice(grp * 4, grp * 4 + 4)
                    gk = '_g%d' % grp
                    P.op('pool', lambda e, hs=hs: e.tensor_tensor(out=DD[0][:, :, hs, :], in0=XD[0][:, :, hs, :], in1=idb4, op=ALU.add), reads=['rw_XD0' + gk, 'rw_identb'], writes=['rw_DD0' + gk])
                for q_ in range(1, 5):
                    o = (q_ - 1) % 2; n = q_ % 2
                    xo_, xn_ = XD[o], XD[n]; do_, dn_ = DD[o], DD[n]
                    for grp in range(2):
                        hs = slice(grp * 4, grp * 4 + 4)
                        gk = '_g%d' % grp
                        for j in range(4):
                            h = grp * 4 + j
                            P.op('pe', lambda e, xo_=xo_, h=h, j=j: e.matmul(N1[:, j, :], lhsT=xo_[:, 1, h, :], rhs=xo_[:, 0, h, :], start=True, stop=True), reads=['rw_XD%d' % o + gk], writes=['rw_N1'])
                        for j in range(4):
                            h = grp * 4 + j
                            P.op('pe', lambda e, xo_=xo_, h=h, j=j: e.matmul(N2[:, j, :], lhsT=xo_[:, 0, h, :], rhs=xo_[:, 1, h, :], start=True, stop=True), reads=['rw_XD%d' % o + gk], writes=['rw_N2'])
                        P.op('act', lambda e, xn_=xn_, hs=hs: e.copy(out=xn_[:, 0, hs, :], in_=N1[:]), reads=['rw_N1'], writes=['rw_XD%d' % n + gk])
                        P.op('dve', lambda e, xn_=xn_, hs=hs: e.tensor_copy(out=xn_[:, 1, hs, :], in_=N2[:]), reads=['rw_N2'], writes=['rw_XD%d' % n + gk])
                        for j in range(4):
                            h = grp * 4 + j
                            P.op('pe', lambda e, xn_=xn_, do_=do_, h=h, j=j: e.matmul(N3[:, j, :], lhsT=do_[:, 1, h, :], rhs=xn_[:, 0, h, :], start=True, stop=True), reads=['rw_XD%d' % n + gk, 'rw_DD%d' % o + gk], writes=['rw_N3'])
                        for j in range(4):
                            h = grp * 4 + j
                            P.op('pe', lambda e, xn_=xn_, do_=do_, h=h, j=j: e.matmul(N1[:, j, :], lhsT=xn_[:, 0, h, :], rhs=do_[:, 1, h, :], start=True, stop=True), reads=['rw_XD%d' % n + gk, 'rw_DD%d' % o + gk], writes=['rw_N1'])
                        P.op('dve', lambda e, dn_=dn_, do_=do_, hs=hs: e.tensor_tensor(out=dn_[:, 0, hs, :], in0=N3[:], in1=do_[:, 0, hs, :], op=ALU.add), reads=['rw_N3', 'rw_DD%d' % o + gk], writes=['rw_DD%d' % n + gk])
                        P.op('dve', lambda e, dn_=dn_, do_=do_, hs=hs: e.tensor_tensor(out=dn_[:, 1, hs, :], in0=N1[:], in1=do_[:, 1, hs, :], op=ALU.add), reads=['rw_N1', 'rw_DD%d' % o + gk], writes=['rw_DD%d' % n + gk])
                Df = DD[0]
                for grp in range(2):
                    hs = slice(grp * 4, grp * 4 + 4)
                    gk = '_g%d' % grp
                    for j in range(4):
                        h = grp * 4 + j
                        P.op('pe', lambda e, h=h, j=j: e.matmul(N1[:, j, :], lhsT=XO[:, h, :], rhs=Df[:, 1, h, :], start=True, stop=True), reads=['rw_XO' + gk, 'rw_DD0' + gk], writes=['rw_N1'])
                    for j in range(4):
                        h = grp * 4 + j
                        P.op('pe', lambda e, h=h, j=j: e.matmul(N2[:, j, :], lhsT=Df[:, 1, h, :], rhs=XO[:, h, :], start=True, stop=True), reads=['rw_XO' + gk, 'rw_DD0' + gk], writes=['rw_N2'])
                    P.op('act', lambda e, hs=hs: e.copy(out=Mb[:, hs, :], in_=N1[:]), reads=['rw_N1'], writes=['rw_Mb' + gk])
                    P.op('dve', lambda e, hs=hs: e.tensor_copy(out=Nb[:, hs, :], in_=N2[:]), reads=['rw_N2'], writes=['rw_Nb' + gk])
                    for j in range(4):
                        h = grp * 4 + j
                        P.op('pe', lambda e, h=h, j=j: e.matmul(N3[:, j, :], lhsT=Nb[:, h, :], rhs=Mb[:, h, :], start=True, stop=True), reads=['rw_Nb' + gk, 'rw_Mb' + gk], writes=['rw_N3'])
                    P.op('act', lambda e, hs=hs: e.copy(out=M2b[:, hs, :], in_=N3[:]), reads=['rw_N3'], writes=['rw_M2b' + gk])
                    for j in range(4):
                        h = grp * 4 + j
                        P.op('pe', lambda e, h=h, j=j: e.matmul(N1[:, j, :], lhsT=Nb[:, h, :], rhs=M2b[:, h, :], start=True, stop=True), reads=['rw_Nb' + gk, 'rw_M2b' + gk], writes=['rw_N1'])
                    P.op('dve', lambda e, hs=hs: e.tensor_tensor(out=Ssb[:, hs, :], in0=N1[:], in1=Mb[:, hs, :], op=ALU.add), reads=['rw_N1', 'rw_Mb' + gk], writes=['rw_Ssb' + gk])
                    P.op('pool', lambda e, hs=hs: e.tensor_tensor(out=Ssb[:, hs, :], in0=Ssb[:, hs, :], in1=M2b[:, hs, :], op=ALU.add), reads=['rw_Ssb' + gk, 'rw_M2b' + gk], writes=['rw_Ssb' + gk])
                    for j in range(4):
                        h = grp * 4 + j
                        P.op('pe', lambda e, h=h, j=j: e.matmul(N2[:, j, :], lhsT=Df[:, 0, h, :], rhs=Ssb[:, h, :], start=True, stop=True), reads=['rw_DD0' + gk, 'rw_Ssb' + gk], writes=['rw_N2'])
                    P.op('dve', lambda e, hs=hs: e.tensor_tensor(out=Ttb[:, hs, :], in0=N2[:], in1=Df[:, 1, hs, :], op=ALU.add), reads=['rw_N2', 'rw_DD0' + gk], writes=['rw_Ttb'])
                for h in range(8):
                    hp, hr = hsl(h); hc = slice(h * 64, h * 64 + 64)
                    P.op('pe', lambda e, h=h, hc=hc: e.matmul(PW[:, h, :], lhsT=A3T[:, h, 0, :], rhs=VKB[:, 0, hc], start=True, stop=False), reads=['rw_A3T', 'rw_VKB'], writes=['rw_PW'])
                    P.op('pe', lambda e, h=h, hp=hp, hr=hr: e.matmul(PW[:, h, :], lhsT=ART[:, hp, 0, :], rhs=Sb[:, h, :], start=False, stop=True), reads=['rw_ART', 'rw_Sb'], writes=['rw_PW'])
                P.op('act', lambda e: e.copy(out=Wb[:], in_=PW[:]), reads=['rw_PW'], writes=['rw_Wb'])
                for h in range(8):
                    P.op('pe', lambda e, h=h: e.matmul(PW[:, h, :], lhsT=Ttb[:, h, :], rhs=Wb[:, h, :], start=True, stop=True), reads=['rw_Ttb', 'rw_Wb'], writes=['rw_PW'])
                P.op('act', lambda e: e.copy(out=Ub[:], in_=PW[:]), reads=['rw_PW'], writes=['rw_Ub'])
                for h in range(8):
                    hp, hr = hsl(h); hc = slice(h * 64, h * 64 + 64)
                    P.op('pe', lambda e, h=h, hc=hc: e.matmul(pY[:, hc], lhsT=A3T[:, h, 1, :], rhs=VKB[:, 0, hc], start=True, stop=False), reads=['rw_A3T', 'rw_VKB'], writes=['rw_pY'])
                    P.op('pe', lambda e, h=h, hc=hc: e.matmul(pY[:, hc], lhsT=ArbT[:, h, :], rhs=Ub[:, h, :], start=False, stop=False), reads=['rw_ArbT', 'rw_Ub'], writes=['rw_pY'])
                    P.op('pe', lambda e, h=h, hc=hc, hp=hp, hr=hr: e.matmul(pY[:, hc], lhsT=ART[:, hp, 1, :], rhs=Sb[:, h, :], start=False, stop=True), reads=['rw_ART', 'rw_Sb'], writes=['rw_pY'])
                for h in range(8):
                    hp, hr = hsl(h); hc = slice(h * 64, h * 64 + 64)
                    P.op('pe', lambda e, h=h, hp=hp, hc=hc: e.matmul(PW[:, h, :], lhsT=VKB[:, 1, hp * 128:(hp + 1) * 128], rhs=VKB[:, 0, hc], start=True, stop=False), reads=['rw_VKB', 'rw_Ub'], writes=['rw_PW'])
                    P.op('pe', lambda e, h=h, hp=hp: e.matmul(PW[:, h, :], lhsT=VKB[:, 2, hp * 128:(hp + 1) * 128], rhs=Ub[:, h, :], start=False, stop=True), reads=['rw_VKB', 'rw_Ub'], writes=['rw_PW'])
                PW4 = PW[:].rearrange("p (a b) v -> p a b v", b=2)
                for par in range(2):
                    rows = slice(par * 64, par * 64 + 64)
                    P.op('dve', lambda e, rows=rows: e.tensor_tensor(out=Sf[rows], in0=Sf[rows], in1=gamL[rows, :].unsqueeze(2).to_broadcast([64, 4, 64]), op=ALU.mult), reads=['rw_Sf', 'rw_gamL'], writes=['rw_Sf'])
                    P.op('dve', lambda e, rows=rows, par=par: e.tensor_tensor(out=Sf[rows], in0=Sf[rows], in1=PW4[rows, :, par, :], op=ALU.add), reads=['rw_Sf', 'rw_PW'], writes=['rw_Sf'])
                P.op('pool', lambda e: e.tensor_copy(out=Sb[0:64, 0::2, :], in_=Sf[0:64, :, :]), reads=['rw_Sf'], writes=['rw_Sb'])
                P.op('pool', lambda e: e.tensor_copy(out=Sb[64:128, 1::2, :], in_=Sf[64:128, :, :]), reads=['rw_Sf'], writes=['rw_Sb'])
                if RW_DBG_STOP <= 7:
                    continue
                yi = cnt % 2
                if d == 0:
                    P.op('act', lambda e, yi=yi: e.copy(out=Yo[yi][:], in_=pY[:, :]), reads=['rw_pY'], writes=['rw_Yo%d' % yi])
                    P.dma('sp', g.YR[tt * 128:(tt + 1) * 128, :], Yo[yi][:], reads=['rw_Yo%d' % yi], writes=[('YR', tt)])
                    P.dma('sp', g.BON[tt * 128:(tt + 1) * 128, :], bon[:], reads=['rw_bon'], writes=[('BON', tt)])
                else:
                    P.dma('sp', y0[:], g.YR[tt * 128:(tt + 1) * 128, :], reads=[('YR', tt)], writes=['rw_y0'])
                    P.dma('sp', b0[:], g.BON[tt * 128:(tt + 1) * 128, :], reads=[('BON', tt)], writes=['rw_b0'])
                    Y_ = Yo[yi]; yk_ = 'rw_Yo%d' % yi
                    P.op('dve', lambda e, Y_=Y_: e.tensor_tensor(out=Y_[:], in0=pY[:, :], in1=y0[:], op=ALU.add), reads=['rw_pY', 'rw_y0'], writes=[yk_])
                    P.op('pool', lambda e: e.tensor_tensor(out=b0[:], in0=b0[:], in1=bon[:], op=ALU.add), reads=['rw_b0', 'rw_bon'], writes=['rw_b0'])
                    Y3 = Y_[:].rearrange("p (h v) -> p h v", h=8)
                    P.op('dve', lambda e, Y3=Y3: e.tensor_reduce(out=st[:, 0, :], in_=Y3, axis=AX.X, op=ALU.add), reads=[yk_], writes=['rw_st'])
                    P.op('act', lambda e, Y_=Y_: e.activation(out=ysq[:], in_=Y_[:], func=AF.Square), reads=[yk_], writes=['rw_ysq'])
                    P.op('dve', lambda e: e.tensor_reduce(out=st[:, 1, :], in_=ysq[:].rearrange("p (h v) -> p h v", h=8), axis=AX.X, op=ALU.add), reads=['rw_ysq'], writes=['rw_st'])
                    P.op('dve', lambda e: e.tensor_scalar(out=st[:, 0, :], in0=st[:, 0, :], scalar1=1.0 / 64, scalar2=None, op0=ALU.mult), reads=['rw_st'], writes=['rw_st'])
                    P.op('dve', lambda e: e.tensor_tensor(out=st[:, 2, :], in0=st[:, 0, :], in1=st[:, 0, :], op=ALU.mult), reads=['rw_st'], writes=['rw_st'])
                    P.op('dve', lambda e: e.scalar_tensor_tensor(out=st[:, 1, :], in0=st[:, 1, :], scalar=1.0 / 64, in1=st[:, 2, :], op0=ALU.mult, op1=ALU.subtract), reads=['rw_st'], writes=['rw_st'])
                    P.op('dve', lambda e: e.tensor_scalar(out=st[:, 1, :], in0=st[:, 1, :], scalar1=64e-5, scalar2=None, op0=ALU.add), reads=['rw_st'], writes=['rw_st'])
                    P.op('act', lambda e: e.activation(out=st[:, 1, :], in_=st[:, 1, :], func=AF.Sqrt), reads=['rw_st'], writes=['rw_st'])
                    P.op('dve', lambda e: e.reciprocal(out=st[:, 1, :], in_=st[:, 1, :]), reads=['rw_st'], writes=['rw_st'])
                    bcs = lambda col: st[:, col, :].unsqueeze(2).to_broadcast([128, 8, 64])
                    P.op('dve', lambda e, Y3=Y3: e.tensor_tensor(out=Y3, in0=Y3, in1=bcs(0), op=ALU.subtract), reads=[yk_, 'rw_st'], writes=[yk_])
                    P.op('dve', lambda e, Y3=Y3: e.tensor_tensor(out=Y3, in0=Y3, in1=bcs(1), op=ALU.mult), reads=[yk_, 'rw_st'], writes=[yk_])
                    P.op('pool', lambda e, Y_=Y_: e.tensor_tensor(out=Y_[:], in0=Y_[:], in1=LNW[:], op=ALU.mult), reads=[yk_, 'rw_LNW'], writes=[yk_])
                    P.op('pool', lambda e, Y_=Y_: e.tensor_tensor(out=Y_[:], in0=Y_[:], in1=LNB[:], op=ALU.add), reads=[yk_, 'rw_LNB'], writes=[yk_])
                    P.op('dve', lambda e: e.tensor_tensor(out=ysq[:].rearrange("p (h v) -> p h v", h=8), in0=VKB[:, 0, :].rearrange("p (h v) -> p h v", h=8),
                                                        in1=b0[:].unsqueeze(2).to_broadcast([128, 8, 64]), op=ALU.mult), reads=['rw_VKB', 'rw_b0', 'rw_ysq'], writes=['rw_ysq'])
                    P.op('pool', lambda e, Y_=Y_: e.tensor_tensor(out=Y_[:], in0=Y_[:], in1=ysq[:], op=ALU.add), reads=[yk_, 'rw_ysq'], writes=[yk_])
                    P.op('dve', lambda e, Y_=Y_, yi=yi: e.tensor_tensor(out=ao[yi][:], in0=Y_[:], in1=gsb[:], op=ALU.mult), reads=[yk_, 'rw_gsb'], writes=['rw_ao%d' % yi])
                    P.dma('sp', g.AO[tt * 128:(tt + 1) * 128, :], ao[yi][:], reads=['rw_ao%d' % yi], writes=['AO'])
```

```python
import numpy as np
from contextlib import ExitStack
import concourse.bass as bass
import concourse.mybir as mybir
from concourse.bass_utils import run_bass_kernel_spmd

F32 = mybir.dt.float32
BF16 = mybir.dt.bfloat16
I32 = mybir.dt.int32
ALU = mybir.AluOpType
AF = mybir.ActivationFunctionType
AX = mybir.AxisListType

D = 1024
NCTX = 256
SEQ = 8192
T = NCTX + SEQ
TT = T + NCTX
NIN = 6912
DEPTH = 2
KT = D // 128
O_R, O_K, O_V, O_WD, O_AD, O_GD = 0, 512, 1024, 1536, 1600, 1664
O_NQ, O_NK, O_NV = 1792, 2304, 2816
O_S5 = 3328
O_G = 3840


NOSYNC_SAME = ('pe',)


class Prog:
    ENGS = ('pe', 'dve', 'act', 'pool', 'sp')

    def __init__(self, nc, es, n_dma_sems=10):
        self.nc = nc
        self.lists = {e: [] for e in self.ENGS}
        self.sems = {e: es.enter_context(nc.semaphore('s_' + e)) for e in self.ENGS}
        self.cnt = {e: 0 for e in self.ENGS}
        self.seen = {e: {} for e in self.ENGS}
        self.lastw = {}
        self.readers = {}
        self.dma_sems, self.dma_cnt, self.dma_rr = {}, {}, {}
        for q in ('sp', 'act', 'pool'):
            self.dma_sems[q] = [es.enter_context(nc.semaphore('d_%s%d' % (q, i))) for i in range(n_dma_sems)]
            self.dma_cnt[q] = [0] * n_dma_sems
            self.dma_rr[q] = 0
        self.semobj = dict(self.sems)
        for q in self.dma_sems:
            for i, s in enumerate(self.dma_sems[q]):
                self.semobj[(q, i)] = s

    def _deps(self, eng, reads, writes):
        evs = []
        for k in reads:
            if k in self.lastw:
                evs.append(self.lastw[k])
        for k in writes:
            if k in self.lastw:
                evs.append(self.lastw[k])
            evs.extend(self.readers.get(k, ()))
        waits = {}
        for (sk, v) in evs:
            if sk == eng and eng in NOSYNC_SAME:
                continue
            if self.seen[eng].get(sk, 0) >= v:
                continue
            if waits.get(sk, 0) < v:
                waits[sk] = v
        for sk, v in waits.items():
            self.seen[eng][sk] = v
        return list(waits.items())

    def _commit(self, ev, reads, writes):
        for k in writes:
            self.lastw[k] = ev
            self.readers[k] = []
        for k in reads:
            self.readers.setdefault(k, []).append(ev)

    def op(self, eng, fn, reads=(), writes=()):
        waits = self._deps(eng, reads, writes)
        self.cnt[eng] += 1
        ev = (eng, self.cnt[eng])
        self.lists[eng].append((waits, fn, eng, 1))
        self._commit(ev, reads, writes)
        return ev

    def dma(self, q, out, in_, reads=(), writes=(), **kw):
        i = self.dma_rr[q]
        self.dma_rr[q] = (i + 1) % len(self.dma_sems[q])
        sk = (q, i)
        waits = self._deps(q, reads, writes)
        prev = self.dma_cnt[q][i]
        if prev > 0 and self.seen[q].get(sk, 0) < prev:
            waits.append((sk, prev))
            self.seen[q][sk] = prev
        self.dma_cnt[q][i] += 16
        ev = (sk, self.dma_cnt[q][i])
        fn = lambda e, out=out, in_=in_, kw=kw: e.dma_start(out=out, in_=in_, **kw)
        self.lists[q].append((waits, fn, sk, 16))
        self._commit(ev, reads, writes)
        return ev

    def dma_fn(self, q, fn, reads=(), writes=()):
        i = self.dma_rr[q]
        self.dma_rr[q] = (i + 1) % len(self.dma_sems[q])
        sk = (q, i)
        waits = self._deps(q, reads, writes)
        prev = self.dma_cnt[q][i]
        if prev > 0 and self.seen[q].get(sk, 0) < prev:
            waits.append((sk, prev))
            self.seen[q][sk] = prev
        self.dma_cnt[q][i] += 16
        ev = (sk, self.dma_cnt[q][i])
        self.lists[q].append((waits, fn, sk, 16))
        self._commit(ev, reads, writes)
        return ev

    def barrier(self):
        evs = []
        for e in self.ENGS:
            if self.cnt[e] > 0:
                evs.append((e, self.cnt[e]))
        for q in self.dma_sems:
            for i, c in enumerate(self.dma_cnt[q]):
                if c > 0:
                    evs.append(((q, i), c))
        for e in self.ENGS:
            waits = []
            for (sk, v) in evs:
                if sk == e:
                    continue
                if self.seen[e].get(sk, 0) < v:
                    waits.append((sk, v))
                    self.seen[e][sk] = v
            if waits:
                self.lists[e].append((waits, None, None, 0))

    def wait_all(self, eng='sp'):
        waits = []
        for e in self.ENGS:
            if e != eng and self.cnt[e] > 0:
                waits.append((e, self.cnt[e]))
        for q in self.dma_sems:
            for i, c in enumerate(self.dma_cnt[q]):
                if c > 0:
                    waits.append(((q, i), c))
        self.lists[eng].append((waits, None, None, 0))

    def emit(self):
        nc = self.nc
        names = {'pe': 'tensor', 'dve': 'vector', 'act': 'scalar', 'pool': 'gpsimd', 'sp': 'sync'}
        with nc.Block() as block:
            for e in self.ENGS:
                lst = self.lists[e]

                def body(engobj, lst=lst):
                    for (waits, fn, sk, inc) in lst:
                        for (wk, v) in waits:
                            engobj.wait_ge(self.semobj[wk], v)
                        if fn is not None:
                            ins = fn(engobj)
                            ins.then_inc(self.semobj[sk], inc)
                getattr(block, names[e])(body)


class Ctx:
    pass


_UNIQ = [0]


def mk_alloc(nc, es):
    _UNIQ[0] += 1
    sfx = "_u%d" % _UNIQ[0]
    sb = lambda name, shape, dt: es.enter_context(nc.sbuf_tensor(name + sfx, shape, dt))
    ps = lambda name, shape, dt: es.enter_context(nc.psum_tensor(name + sfx, shape, dt))
    return sb, ps


def declare_io(nc, stage):
    g = Ctx()
    def inp(name, shape, dt=F32):
        t = nc.dram_tensor(name, list(shape), dt, kind="ExternalInput").ap()
        setattr(g, name, t)
        return t
    def scr(name, shape, dt=F32):
        t = nc.dram_tensor(name, list(shape), dt, kind="Internal").ap()
        setattr(g, name, t)
        return t
    g.inp, g.scr = inp, scr
    inp("xin", [T, D])
    inp("cvec", [128, 2 * KT])
    inp("ada_w", [DEPTH, D, 6 * D])
    inp("ada_b", [DEPTH, 6 * D])
    inp("norm1_g", [DEPTH, 128, KT])
    inp("norm2_g", [DEPTH, 128, KT])
    inp("w_in", [DEPTH, D, NIN])
    inp("c_ident", [128, 128])
    inp("c_ones", [128, 128])
    scr("XR", [T, D])
    scr("MOD", [DEPTH, 2, 6 * D])
    scr("RWT", [1792, TT])
    scr("UB", [32, 8, 16, TT // 8])
    scr("NAQT", [512, T], BF16)
    scr("NAKT", [512, T], BF16)
    scr("NAV", [T, 512], BF16)
    scr("GT", [3072, T], BF16)
    inp("na_tab", [DEPTH, 5, 8, 576, 128])
    scr("NAO", [T, 512], BF16)
    inp("w_branch", [DEPTH, 3, 512, D])
    inp("w_out", [DEPTH, D, D])
    inp("c_tri", [128, 128]); inp("c_iota", [128, NE])
    inp("router_w", [DEPTH, D, NE]); inp("router_b", [DEPTH, NE])
    inp("norm2_row", [DEPTH, D]); inp("final_row", [1, D])
    inp("expert_gu_w", [DEPTH, NE, D, 2 * D]); inp("expert_dn_w", [DEPTH, NE, D, D])
    inp("gu_b", [DEPTH, NE, 128, 16]); inp("expert_dn_b", [DEPTH, NE, D])
    inp("c_ii", [128, 128])
    inp("c_bo", [128, 128]); inp("c_hi", [128, 2]); inp("c_masks", [128, 8, 128])
    inp("rw_mup", [DEPTH, 128, 14]); inp("rw_mun", [DEPTH, 128, 14]); inp("rw_pv", [DEPTH, 128, 3, 4]); inp("rw_wa0", [DEPTH, 128, 16])
    inp("rwkv_w2", [DEPTH, 2, 64, 512]); inp("rwkv_a2", [DEPTH, 2, 64, 512]); inp("rwkv_g2", [DEPTH, 128, 512])
    inp("rwkv_ln_w", [DEPTH, 512]); inp("rwkv_ln_b", [DEPTH, 512])
    scr("YR", [T, 512]); scr("BON", [T, 8])
    for nm in ("s5_lr", "s5_li", "s5_ldt"):
        inp(nm, [DEPTH, 128, 64])
    for nm in ("s5_br", "s5_bi", "s5_cr", "s5_ci"):
        inp(nm, [DEPTH, 128, 64, 16])
    inp("s5_dv", [DEPTH, 128, 32])
    inp("s5_glu_w", [DEPTH, 512, 512]); inp("s5_glu_bp", [DEPTH, 128, 4])
    scr("YB", [32, 8, 16, T // 8])
    scr("XS", [NE * CAP, D], BF16); scr("YS", [NE * CAP, D], BF16)
    if stage in ("merge", "moe"):
        inp("AO", [T, 512], BF16); inp("SO", [T, 512], BF16)
    else:
        scr("AO", [T, 512], BF16); scr("SO", [T, 512], BF16)
    return g


def phase0(nc, P, g, es0):
    with ExitStack() as es:
        sb, ps = mk_alloc(nc, es)
        cv = sb("p0_cv", [128, 2 * KT], F32)
        cs = sb("p0_cs", [128, 2 * KT], F32)
        aw = [sb("p0_aw%d" % i, [128, KT, 512], F32) for i in range(2)]
        ab = sb("p0_ab", [1, 6 * D], F32)
        row = [sb("p0_row%d" % i, [1, 6 * D], F32) for i in range(2)]
        pr = [ps("p0_pr%d" % i, [1, 512], F32) for i in range(2)]
        P.dma('sp', cv[:], g.cvec[:, :], writes=['p0_cv'])
        P.op('act', lambda e: e.activation(out=cs[:], in_=cv[:], func=AF.Silu), reads=['p0_cv'], writes=['p0_cs'])
        for l in range(DEPTH):
            P.dma('sp', ab[:], g.ada_b[l:l + 1, :], reads=[], writes=['p0_ab'])
            for cch in range(12):
                a = aw[cch % 2]
                ak = 'p0_aw%d' % (cch % 2)
                P.dma('sp', a[:], g.ada_w[l, :, cch * 512:(cch + 1) * 512].rearrange("(k p) n -> p k n", p=128),
                      writes=[ak])
                for v in range(2):
                    for k in range(KT):
                        P.op('pe', lambda e, v=v, k=k, a=a: e.matmul(pr[v][:], lhsT=cs[:, v * KT + k:v * KT + k + 1],
                                                                    rhs=a[:, k, :], start=(k == 0), stop=(k == KT - 1)),
                             reads=['p0_cs', ak], writes=['p0_pr%d' % v])
                    P.op('dve', lambda e, v=v, cch=cch: e.tensor_tensor(out=row[v][:, cch * 512:(cch + 1) * 512],
                                                                        in0=pr[v][:], in1=ab[:, cch * 512:(cch + 1) * 512],
                                                                        op=ALU.add),
                         reads=['p0_pr%d' % v, 'p0_ab'], writes=['p0_row%d' % v])
            for v in range(2):
                P.dma('sp', g.MOD[l, v:v + 1, :], row[v][:], reads=['p0_row%d' % v], writes=['MOD'])


def load_mod_pp(nc, P, g, l, es, sb, tag):
    m = sb(tag + "_modpp", [128, 2, 6, KT], F32)
    for v in range(2):
        for j in range(6):
            P.dma('sp', m[:, v, j, :], g.MOD[l, v, j * D:(j + 1) * D].rearrange("(k p) -> p k", p=128),
                  reads=['MOD'], writes=[tag + '_modpp'], allow_slow_non_contiguous=True)
    return m


def phase1(nc, P, g, l, x_src):
    with ExitStack() as es:
        sb, ps = mk_alloc(nc, es)
        wb = sb("p1_w", [128, KT, NIN], BF16)
        for k in range(KT):
            for c0 in range(0, NIN, 1728):
                P.dma('pool', wb[:, k, c0:c0 + 1728], g.w_in[l, k * 128:(k + 1) * 128, c0:c0 + 1728], writes=['p1_w'])
        ident = sb("p1_ident", [128, 128], BF16)
        P.dma('pool', ident[:], g.c_ident[:, :], writes=['p1_ident'])
        m = load_mod_pp(nc, P, g, l, es, sb, "p1")
        g1 = sb("p1_g1", [128, KT], F32)
        P.dma('sp', g1[:], g.norm1_g[l, :, :], writes=['p1_g1'])
        A1 = sb("p1_A1", [128, 2, KT], F32)
        for v in range(2):
            P.op('dve', lambda e, v=v: e.scalar_tensor_tensor(out=A1[:, v, :], in0=m[:, v, 1, :], scalar=1.0, in1=g1[:],
                                                              op0=ALU.add, op1=ALU.mult),
                 reads=['p1_modpp', 'p1_g1'], writes=['p1_A1'])
        xt = [sb("p1_xt%d" % i, [128, D], F32) for i in range(2)]
        junk = sb("p1_junk", [128, D], F32)
        xb = [sb("p1_xb%d" % i, [128, D], BF16) for i in range(2)]
        ss = sb("p1_ss", [128, 4], F32)
        hT = [sb("p1_hT%d" % i, [128, KT, 512], BF16) for i in range(2)]
        ptr = [ps("p1_ptr%d" % i, [128, KT, 128], BF16) for i in range(2)]
        pp = [ps("p1_pp%d" % i, [128, 512], F32) for i in range(4)]
        ev = [sb("p1_ev%d" % i, [128, 512], F32) for i in range(4)]
        evb = [sb("p1_evb%d" % i, [128, 512], BF16) for i in range(4)]
        evu = [sb("p1_evu%d" % i, [128, 8, 64], F32) for i in range(2)]
        nblk = (T + 511) // 512
        ntile = T // 128
        cnt = 0
        for b in range(nblk):
            tiles = [t for t in range(4 * b, min(4 * b + 4, ntile))]
            nt = len(tiles)
            ntok = nt * 128
            h = hT[b % 2]
            hk = 'p1_hT%d' % (b % 2)
            for ti, t in enumerate(tiles):
                v = 1 if t < 2 else 0
                x_ = xt[t % 2]; xk = 'p1_xt%d' % (t % 2)
                xb_ = xb[t % 2]; xbk = 'p1_xb%d' % (t % 2)
                pt_ = ptr[t % 2]; ptk = 'p1_ptr%d' % (t % 2)
                sc = ss[:, (t % 2) * 2:(t % 2) * 2 + 1]
                sck = 'p1_ss%d' % (t % 2)
                P.dma('sp', x_[:], x_src[t * 128:(t + 1) * 128, :], writes=[xk])
                P.op('act', lambda e, x_=x_, sc=sc: e.activation(out=junk[:], in_=x_[:], func=AF.Square, accum_out=sc),
                     reads=[xk], writes=['p1_junk', sck])
                P.op('dve', lambda e, sc=sc: e.tensor_scalar(out=sc, in0=sc, scalar1=1.0 / D, scalar2=1e-6,
                                                             op0=ALU.mult, op1=ALU.add), reads=[sck], writes=[sck])
                P.op('act', lambda e, sc=sc: e.activation(out=sc, in_=sc, func=AF.Sqrt), reads=[sck], writes=[sck])
                P.op('dve', lambda e, sc=sc: e.reciprocal(out=sc, in_=sc), reads=[sck], writes=[sck])
                P.op('dve', lambda e, x_=x_, xb_=xb_, sc=sc: e.tensor_scalar(out=xb_[:], in0=x_[:], scalar1=sc, scalar2=None,
                                                                            op0=ALU.mult), reads=[xk, sck], writes=[xbk])
                for k in range(KT):
                    P.op('pe', lambda e, k=k, xb_=xb_, pt_=pt_: e.transpose(out=pt_[:, k, :], in_=xb_[:, k * 128:(k + 1) * 128],
                                                                           identity=ident[:]),
                         reads=[xbk, 'p1_ident'], writes=[ptk])
                hs = h[:, :, ti * 128:(ti + 1) * 128]
                P.op('dve', lambda e, hs=hs, pt_=pt_, v=v: e.tensor_tensor(out=hs, in0=pt_[:],
                                                                           in1=A1[:, v, :].unsqueeze(2).to_broadcast([128, KT, 128]),
                                                                           op=ALU.mult),
                     reads=[ptk, 'p1_A1'], writes=[hk])
                P.op('pool', lambda e, hs=hs, v=v: e.tensor_tensor(out=hs, in0=hs,
                                                                   in1=m[:, v, 0, :].unsqueeze(2).to_broadcast([128, KT, 128]),
                                                                   op=ALU.add),
                     reads=[hk, 'p1_modpp'], writes=[hk])
            chunks = [c for c in range(NIN // 128) if not (O_NV <= c * 128 < O_NV + 512)]
            for c in chunks:
                col = c * 128
                pi = cnt % 4; cnt += 1
                pk = 'p1_pp%d' % pi
                for k in range(KT):
                    P.op('pe', lambda e, k=k, col=col, pi=pi, ntok=ntok, h=h: e.matmul(
                        pp[pi][:, :ntok], lhsT=wb[:, k, col:col + 128], rhs=h[:, k, :ntok],
                        start=(k == 0), stop=(k == KT - 1)),
                         reads=['p1_w', hk], writes=[pk])
                t0 = b * 512
                if col < O_NQ:
                    eng = 'act' if c % 2 == 0 else 'dve'
                    evk = 'p1_ev%d' % pi
                    if eng == 'act':
                        P.op('act', lambda e, pi=pi, ntok=ntok: e.copy(out=ev[pi][:, :ntok], in_=pp[pi][:, :ntok]),
                             reads=[pk], writes=[evk])
                    else:
                        P.op('dve', lambda e, pi=pi, ntok=ntok: e.tensor_copy(out=ev[pi][:, :ntok], in_=pp[pi][:, :ntok]),
                             reads=[pk], writes=[evk])
                    P.dma('sp', g.RWT[col:col + 128, t0:t0 + ntok], ev[pi][:, :ntok], reads=[evk], writes=['RWT'])
                    if b == 0:
                        P.dma('sp', g.RWT[col:col + 128, T:T + NCTX], ev[pi][:, :NCTX], reads=[evk], writes=['RWT'])
                elif col < O_S5:
                    evk = 'p1_evb%d' % pi
                    P.op('act', lambda e, pi=pi, ntok=ntok: e.copy(out=evb[pi][:, :ntok], in_=pp[pi][:, :ntok]),
                         reads=[pk], writes=[evk])
                    dst = g.NAQT if col < O_NK else g.NAKT
                    r0 = col - (O_NQ if col < O_NK else O_NK)
                    P.dma('sp', dst[r0:r0 + 128, t0:t0 + ntok], evb[pi][:, :ntok], reads=[evk], writes=['NAQK'])
                elif col < O_G:
                    ui = cnt % 2
                    evk = 'p1_evu%d' % ui
                    nj = ntok // 8
                    P.op('dve', lambda e, pi=pi, ui=ui, ntok=ntok, nj=nj: e.tensor_copy(
                        out=evu[ui][:, :, :nj], in_=pp[pi][:, :ntok].rearrange("p (j i) -> p i j", i=8)),
                         reads=[pk], writes=[evk])
                    g0 = (col - O_S5) // 16
                    j0 = t0 // 8
                    for i in range(8):
                        for gg in range(8):
                            P.dma('sp', g.UB[g0 + gg, i, :, j0:j0 + nj], evu[ui][gg * 16:(gg + 1) * 16, i, :nj],
                                  reads=[evk], writes=['UB'])
                        if b == 0:
                            for gg in range(8):
                                P.dma('sp', g.UB[g0 + gg, i, :, T // 8:T // 8 + 32],
                                      evu[ui][gg * 16:(gg + 1) * 16, i, :32], reads=[evk], writes=['UB'])
                else:
                    evk = 'p1_evb%d' % pi
                    P.op('act', lambda e, pi=pi, ntok=ntok: e.activation(out=evb[pi][:, :ntok], in_=pp[pi][:, :ntok],
                                                                         func=AF.Sigmoid),
                         reads=[pk], writes=[evk])
                    r0 = col - O_G
                    P.dma('sp', g.GT[r0:r0 + 128, t0:t0 + ntok], evb[pi][:, :ntok], reads=[evk], writes=['GT'])
            for ti, t in enumerate(tiles):
                pi = cnt % 4; cnt += 1
                pk = 'p1_pp%d' % pi
                for k in range(KT):
                    P.op('pe', lambda e, k=k, pi=pi, ti=ti, h=h: e.matmul(
                        pp[pi][:, :], lhsT=h[:, k, ti * 128:(ti + 1) * 128], rhs=wb[:, k, O_NV:O_NV + 512],
                        start=(k == 0), stop=(k == KT - 1)),
                         reads=['p1_w', hk], writes=[pk])
                evk = 'p1_evb%d' % pi
                P.op('dve', lambda e, pi=pi: e.tensor_copy(out=evb[pi][:], in_=pp[pi][:]), reads=[pk], writes=[evk])
                P.dma('sp', g.NAV[t * 128:(t + 1) * 128, :], evb[pi][:], reads=[evk], writes=['NAV'])


def build(stage="p1", dbg=()):
    nc = bass.Bass("TRN2", target_bir_lowering=False)
    g = declare_io(nc, stage)
    outs = {}
    for name, shape, dt in dbg:
        outs[name] = nc.dram_tensor("o_" + name, list(shape), dt, kind="ExternalOutput").ap()
    if stage == "moe":
        g.dbg_slots = nc.dram_tensor("o_slots", [128, T // 128, 4], U32, kind="ExternalOutput").ap()
        g.dbg_wts = nc.dram_tensor("o_wts", [128, T // 128, 4], F32, kind="ExternalOutput").ap()
    with ExitStack() as es:
        P = Prog(nc, es)
        phase0(nc, P, g, es)
        P.barrier()
        phase1(nc, P, g, 0, g.xin)
        P.barrier()
        if stage in ("rwkv",):
            phase_rwkv(nc, P, g, 0)
            P.barrier()
        if stage in ("s5",):
            phase_s5(nc, P, g, 0, True)
            P.barrier()
            phase_s5_readout(nc, P, g, 0)
            P.barrier()
        if stage in ("na", "merge", "moe"):
            phase_na(nc, P, g, 0, True)
            P.barrier()
        if stage in ("merge", "moe"):
            phase_merge(nc, P, g, 0, g.xin, True)
            P.barrier()
        if stage in ("moe",):
            phase_moe(nc, P, g, 0, True)
            P.barrier()
        for name, shape, dt in dbg:
            src = getattr(g, name)
            P.dma('sp', outs[name], src, reads=[name, 'RWT', 'UB', 'NAQK', 'NAV', 'GT', 'MOD', 'NAO', 'SO', 'AO'] + [('XR', t) for t in range(T // 128)] + [('YS', r) for r in range(0, NE * CAP, 128)] + [('YB', gi) for gi in range(32)], writes=['o_' + name])
        P.wait_all('sp')
        P.emit()
    return nc


def host_inputs(inputs, b):
    f = lambda a: np.ascontiguousarray(a, dtype=np.float32)
    d = {}
    d["xin"] = f(np.concatenate([inputs["ctx"][b], inputs["x"][b]], axis=0))
    cl = inputs["c"][b].reshape(KT, 128).T
    cc = inputs["c_ctx"].reshape(KT, 128).T
    d["cvec"] = f(np.concatenate([cl, cc], axis=1))
    d["ada_w"] = f(inputs["ada_w"])
    d["ada_b"] = f(inputs["ada_b"])
    d["norm1_g"] = f(inputs["norm1_g"].reshape(DEPTH, KT, 128).transpose(0, 2, 1))
    d["norm2_g"] = f(inputs["norm2_g"].reshape(DEPTH, KT, 128).transpose(0, 2, 1))
    d["w_in"] = f(inputs["w_in"])
    d["c_ident"] = np.eye(128, dtype=np.float32)
    d["c_ones"] = np.ones((128, 128), dtype=np.float32)
    d["w_branch"] = f(inputs["w_branch"]); d["w_out"] = f(inputs["w_out"])
    d["c_tri"] = np.triu(np.ones((128, 128), np.float32), 1)
    d["c_iota"] = np.tile(np.arange(NE, dtype=np.float32)[None, :], (128, 1))
    d["router_w"] = f(inputs["router_w"]); d["router_b"] = f(inputs["router_b"])
    d["norm2_row"] = f(inputs["norm2_g"]); d["final_row"] = f(inputs["final_g"].reshape(1, D))
    d["expert_gu_w"] = f(inputs["expert_gu_w"]); d["expert_dn_w"] = f(inputs["expert_dn_w"])
    d["gu_b"] = f(inputs["expert_gu_b"].reshape(DEPTH, NE, 16, 128).transpose(0, 1, 3, 2))
    d["expert_dn_b"] = f(inputs["expert_dn_b"])
    ii = np.zeros((128, 128), np.float32)
    for k in range(128):
        ii[k, k % 64] = 1.0; ii[k, 64 + k % 64] = 1.0
    d["c_ii"] = ii
    def pdup(a):
        a = np.moveaxis(a.reshape((DEPTH, 64, 64) + a.shape[4:]), 2, 1)
        return f(np.concatenate([a, a], axis=1))
    d["s5_lr"] = pdup(inputs["s5_lambda_re"]); d["s5_li"] = pdup(inputs["s5_lambda_im"])
    d["s5_ldt"] = f(np.tile(inputs["s5_log_dt"].reshape(DEPTH, 1, 64), (1, 128, 1)))
    d["s5_br"] = pdup(inputs["s5_b_re"]); d["s5_bi"] = pdup(inputs["s5_b_im"])
    d["s5_cr"] = pdup(np.swapaxes(inputs["s5_c_re"], 3, 4)); d["s5_ci"] = pdup(np.swapaxes(inputs["s5_c_im"], 3, 4))
    dv = inputs["s5_d"].reshape(DEPTH, 32, 16)
    d["s5_dv"] = f(np.tile(np.transpose(dv, (0, 2, 1))[:, None, :, :], (1, 8, 1, 1)).reshape(DEPTH, 128, 32))
    bo = np.zeros((128, 128), np.float32); bo[:64, :64] = 1; bo[64:, 64:] = 1
    d["c_bo"] = bo
    hi_ = np.zeros((128, 2), np.float32); hi_[:64, 0] = 1; hi_[64:, 1] = 1
    d["c_hi"] = hi_
    ii_, jj_ = np.meshgrid(np.arange(128), np.arange(128), indexing="ij")
    bd_ = (ii_ // 32) == (jj_ // 32)
    d["c_masks"] = f(np.stack([(jj_ < ii_), (jj_ > ii_), (jj_ <= ii_), (jj_ >= ii_),
                               (jj_ < ii_) & bd_, (jj_ > ii_) & bd_, (jj_ < ii_) & ~bd_, (jj_ > ii_) & ~bd_], axis=1).astype(np.float32))
    pm = lambda a, n: f(a.reshape(DEPTH, n, 128).transpose(0, 2, 1))
    d["rw_mup"] = pm(inputs["rwkv_mu_prev"], 14); d["rw_mun"] = pm(inputs["rwkv_mu_next"], 14)
    d["rw_pv"] = f(np.stack([pm(inputs["rwkv_k_k"], 4), pm(inputs["rwkv_k_a"], 4), pm(inputs["rwkv_r_k"].reshape(DEPTH, 512), 4)], axis=2))
    w0 = inputs["rwkv_w0"].reshape(DEPTH, 2, 4, 128).transpose(0, 3, 1, 2)
    a0 = inputs["rwkv_a0"].reshape(DEPTH, 2, 4, 128).transpose(0, 3, 1, 2)
    d["rw_wa0"] = f(np.stack([w0, a0], axis=2).reshape(DEPTH, 128, 16))
    for nm in ("rwkv_w2", "rwkv_a2", "rwkv_g2", "rwkv_ln_w", "rwkv_ln_b"):
        d[nm] = f(inputs[nm])
    d["s5_glu_w"] = f(inputs["s5_glu_w"])
    d["s5_glu_bp"] = f(inputs["s5_glu_b"].reshape(DEPTH, 4, 128).transpose(0, 2, 1))
    d["na_tab"] = np.stack([na_tables(inputs["na_rpb"][l]) for l in range(DEPTH)])
    return d


def na_tables(rpb_l):
    out = np.full((5, 8, 576, 128), -30000.0, np.float32)
    for ti, m in enumerate([0, 1, 30, 62, 63]):
        kb = min(max(2 * m - 4, 0), 119)
        qi = np.arange(128); r = 2 * m + qi // 64; c = qi % 64
        rs = np.clip(r - 4, 0, 120); cs = np.clip(c - 8, 0, 48)
        ki = np.arange(576); kr = kb + ki // 64; kc = ki % 64
        ok = ((kr[:, None] >= rs[None, :]) & (kr[:, None] < rs[None, :] + 8) &
              (kc[:, None] >= cs[None, :]) & (kc[:, None] < cs[None, :] + 16))
        ro = np.clip(kr[:, None] - r[None, :] + 7, 0, 14); co = np.clip(kc[:, None] - c[None, :] + 15, 0, 30)
        for h in range(8):
            b = rpb_l[h][ro, co]
            out[ti, h] = np.where(ok, b, -30000.0)
    return out


def phase_na(nc, P, g, l, ctx_out):
    with ExitStack() as es:
        sb, ps = mk_alloc(nc, es)
        NTL = T // 128
        kT = sb("na_kT", [128, T], BF16)
        qT = sb("na_qT", [128, T], BF16)
        v0 = sb("na_v0", [128, NTL, 2, 65], BF16)
        v1 = sb("na_v1", [128, NTL, 2, 65], BF16)
        tbf = sb("na_tbf", [128, 5, 128], F32)
        eb = sb("na_eb", [128, 2, 5, 5, 128], BF16)
        et = [sb("na_et%d" % i, [128, 7, 128], BF16) for i in range(2)]
        ob = sb("na_ob", [128, NTL, 128], BF16)
        rc = sb("na_rc", [128, 2], F32)
        pst = [ps("na_pst%d" % i, [128, 8, 128], F32) for i in range(2)]
        po = [ps("na_po%d" % i, [128, 65], F32) for i in range(2)]
        P.op('pool', lambda e: e.memset(v0[:], 1.0), writes=['na_v0'])
        P.op('pool', lambda e: e.memset(v1[:], 1.0), writes=['na_v1'])
        P.op('pool', lambda e: e.memset(eb[:], 0.0), writes=['na_eb'])
        u = 0
        for hp in range(4):
            P.dma('sp', kT[:], g.NAKT[hp * 128:(hp + 1) * 128, :], reads=['NAQK'], writes=['na_kT'])
            P.dma('sp', qT[:], g.NAQT[hp * 128:(hp + 1) * 128, :], reads=['NAQK'], writes=['na_qT'])
            for hh in range(2):
                P.dma('sp', v1[0:64, NTL - 1, hh, 0:64], g.NAV[T - 64:T, hp * 128 + hh * 64:hp * 128 + hh * 64 + 64],
                      reads=['NAV'], writes=['na_v1'])
                P.dma('sp', v0[:, :, hh, 0:64],
                      g.NAV[:, hp * 128 + hh * 64:hp * 128 + hh * 64 + 64].rearrange("(n p) d -> p n d", p=128),
                      reads=['NAV'], writes=['na_v0'])
                P.dma('sp', v1[:, 0:NTL - 1, hh, 0:64],
                      g.NAV[64:T - 64, hp * 128 + hh * 64:hp * 128 + hh * 64 + 64].rearrange("(n p) d -> p n d", p=128),
                      reads=['NAV'], writes=['na_v1'])
                for tb in range(5):
                    h = hp * 2 + hh
                    for blk in range(5):
                        nk = 128 if blk < 4 else 64
                        P.dma('sp', tbf[:nk, blk, :], g.na_tab[l, tb, h, blk * 128:blk * 128 + nk, :],
                              writes=['na_tbf'])
                    P.op('act', lambda e, hh=hh, tb=tb: e.activation(out=eb[:, hh, tb, 0:4, :], in_=tbf[:, 0:4, :], func=AF.Exp),
                         reads=['na_tbf'], writes=['na_eb'])
                    P.op('act', lambda e, hh=hh, tb=tb: e.activation(out=eb[0:64, hh, tb, 4, :], in_=tbf[0:64, 4, :], func=AF.Exp),
                         reads=['na_tbf'], writes=['na_eb'])
            units = []
            if ctx_out:
                units += [('c', 0), ('c', 1)]
            units += [('l', m) for m in range(64)]
            for (kind, m) in units:
                for hh in range(2):
                    pr = slice(hh * 64, hh * 64 + 64)
                    ui = u % 2; u += 1
                    pk, ek, ok = 'na_pst%d' % ui, 'na_et%d' % ui, 'na_po%d' % ui
                    if kind == 'c':
                        q0 = m * 128
                        blocks = [(0, 128, None), (128, 128, None)]
                        tb = None
                    else:
                        q0 = NCTX + m * 128
                        kb = min(max(2 * m - 4, 0), 119)
                        k0 = NCTX + kb * 64
                        blocks = [(k0 + 128 * j, 128 if j < 4 else 64, j) for j in range(5)] + [(0, 128, None), (128, 128, None)]
                        tb = {0: 0, 1: 1, 62: 3, 63: 4}.get(m, 2)
                    nb = len(blocks)
                    for j, (ks, nk, tj) in enumerate(blocks):
                        P.op('pe', lambda e, ui=ui, j=j, ks=ks, nk=nk, pr=pr, q0=q0: e.matmul(
                            pst[ui][:nk, j, :], lhsT=kT[pr, ks:ks + nk], rhs=qT[pr, q0:q0 + 128], start=True, stop=True),
                             reads=['na_kT', 'na_qT'], writes=[pk])
                    if kind == 'l':
                        P.op('act', lambda e, ui=ui: e.activation(out=et[ui][:, 0:4, :], in_=pst[ui][:, 0:4, :], func=AF.Exp, scale=0.125),
                             reads=[pk], writes=[ek])
                        P.op('act', lambda e, ui=ui: e.activation(out=et[ui][0:64, 4, :], in_=pst[ui][0:64, 4, :], func=AF.Exp, scale=0.125),
                             reads=[pk], writes=[ek])
                        P.op('act', lambda e, ui=ui: e.activation(out=et[ui][:, 5:7, :], in_=pst[ui][:, 5:7, :], func=AF.Exp, scale=0.125),
                             reads=[pk], writes=[ek])
                        P.op('dve', lambda e, ui=ui, hh=hh, tb=tb: e.tensor_tensor(out=et[ui][:, 0:4, :], in0=et[ui][:, 0:4, :],
                                                                                  in1=eb[:, hh, tb, 0:4, :], op=ALU.mult),
                             reads=[ek, 'na_eb'], writes=[ek])
                        P.op('dve', lambda e, ui=ui, hh=hh, tb=tb: e.tensor_tensor(out=et[ui][0:64, 4, :], in0=et[ui][0:64, 4, :],
                                                                                  in1=eb[0:64, hh, tb, 4, :], op=ALU.mult),
                             reads=[ek, 'na_eb'], writes=[ek])
                    else:
                        P.op('act', lambda e, ui=ui: e.activation(out=et[ui][:, 0:2, :], in_=pst[ui][:, 0:2, :], func=AF.Exp, scale=0.125),
                             reads=[pk], writes=[ek])
                    for j, (ks, nk, tj) in enumerate(blocks):
                        if ks % 128 == 0:
                            vv = v0[:nk, ks // 128, hh, :]
                        else:
                            vv = v1[:nk, (ks - 64) // 128, hh, :]
                        P.op('pe', lambda e, ui=ui, j=j, nk=nk, vv=vv, nb=nb: e.matmul(
                            po[ui][:, :], lhsT=et[ui][:nk, j, :], rhs=vv, start=(j == 0), stop=(j == nb - 1)),
                             reads=[ek, 'na_v0', 'na_v1'], writes=[ok])
                    rk = 'na_rc%d' % ui
                    P.op('dve', lambda e, ui=ui: e.reciprocal(out=rc[:, ui:ui + 1], in_=po[ui][:, 64:65]), reads=[ok], writes=[rk])
                    P.op('dve', lambda e, ui=ui, q0=q0, hh=hh: e.tensor_scalar(out=ob[:, q0 // 128, hh * 64:hh * 64 + 64], in0=po[ui][:, 0:64],
                                                                             scalar1=rc[:, ui:ui + 1], scalar2=None, op0=ALU.mult),
                         reads=[ok, rk], writes=['na_ob'])
            t_lo = 0 if ctx_out else 2
            P.dma('sp', g.NAO[t_lo * 128:T, hp * 128:(hp + 1) * 128].rearrange("(n p) d -> p n d", p=128), ob[:, t_lo:, :],
                  reads=['na_ob'], writes=['NAO'])


def phase_merge(nc, P, g, l, x_src, ctx_out):
    with ExitStack() as es:
        sb, ps = mk_alloc(nc, es)
        wbr = sb("mg_wbr", [128, 3, 4, D], BF16)
        wo = sb("mg_wo", [128, KT, D], BF16)
        ident = sb("mg_ident", [128, 128], BF16)
        P.dma('pool', ident[:], g.c_ident[:, :], writes=['mg_ident'])
        for j in range(3):
            P.dma('pool', wbr[:, j, :, :], g.w_branch[l, j, :, :].rearrange("(k p) n -> p k n", p=128), writes=['mg_wbr'])
        P.dma('pool', wo[:], g.w_out[l, :, :].rearrange("(k p) n -> p k n", p=128), writes=['mg_wo'])
        g1bc = sb("mg_g1bc", [128, 2, D], F32)
        for v in range(2):
            P.dma('sp', g1bc[:, v, :], g.MOD[l, v, 2 * D:3 * D].partition_broadcast(128), reads=['MOD'], writes=['mg_g1bc'])
        bt = [sb("mg_bt%d" % i, [128, 512], BF16) for i in range(3)]
        bT = sb("mg_bT", [128, 3, 4, 512], BF16)
        gt = [sb("mg_gt%d" % i, [128, 512], BF16) for i in range(3)]
        tmp = [sb("mg_tmp%d" % i, [128, 512], F32) for i in range(2)]
        acc = sb("mg_acc", [128, 512], F32)
        ymT = sb("mg_ymT", [128, KT, 512], BF16)
        xt = [sb("mg_xt%d" % i, [128, D], F32) for i in range(2)]
        xo = [sb("mg_xo%d" % i, [128, D], F32) for i in range(2)]
        ptr = [ps("mg_ptr%d" % i, [128, 4, 128], BF16) for i in range(2)]
        pm = [ps("mg_pm%d" % i, [128, 512], F32) for i in range(2)]
        po = [ps("mg_po%d" % i, [128, 512], F32) for i in range(2)]
        srcs = [g.AO, g.NAO, g.SO]
        ntile = T // 128
        nblk = (ntile + 3) // 4
        cn = 0
        for b in range(nblk):
            tiles = [t for t in range(4 * b, min(4 * b + 4, ntile))]
            if not ctx_out:
                tiles = [t for t in tiles if t >= 2]
            if not tiles:
                continue
            tA = tiles[0]
            ntok = len(tiles) * 128
            c0 = tA * 128
            for ti, t in enumerate(tiles):
                for j in range(3):
                    P.dma('sp', bt[j][:], srcs[j][t * 128:(t + 1) * 128, :], reads=['AO', 'NAO', 'SO'], writes=['mg_bt%d' % j])
                    pi = cn % 2; cn += 1
                    for k in range(4):
                        P.op('pe', lambda e, j=j, k=k, pi=pi: e.transpose(out=ptr[pi][:, k, :], in_=bt[j][:, k * 128:(k + 1) * 128], identity=ident[:]),
                             reads=['mg_bt%d' % j, 'mg_ident'], writes=['mg_ptr%d' % pi])
                    P.op('act', lambda e, j=j, ti=ti, pi=pi: e.copy(out=bT[:, j, :, ti * 128:(ti + 1) * 128], in_=ptr[pi][:]),
                         reads=['mg_ptr%d' % pi], writes=['mg_bT'])
            for dc in range(KT):
                for j in range(3):
                    pi = cn % 2; cn += 1
                    P.dma('sp', gt[j][:, :ntok], g.GT[j * D + dc * 128:j * D + (dc + 1) * 128, c0:c0 + ntok], reads=['GT'], writes=['mg_gt%d' % j])
                    for k in range(4):
                        P.op('pe', lambda e, j=j, k=k, pi=pi, dc=dc, ntok=ntok: e.matmul(
                            pm[pi][:, :ntok], lhsT=wbr[:, j, k, dc * 128:(dc + 1) * 128], rhs=bT[:, j, k, :ntok], start=(k == 0), stop=(k == 3)),
                             reads=['mg_wbr', 'mg_bT'], writes=['mg_pm%d' % pi])
                    if j == 0:
                        P.op('dve', lambda e, pi=pi, j=j, ntok=ntok: e.tensor_tensor(out=acc[:, :ntok], in0=pm[pi][:, :ntok], in1=gt[j][:, :ntok], op=ALU.mult),
                             reads=['mg_pm%d' % pi, 'mg_gt%d' % j], writes=['mg_acc'])
                    else:
                        tk = j - 1
                        P.op('dve', lambda e, pi=pi, j=j, tk=tk, ntok=ntok: e.tensor_tensor(out=tmp[tk][:, :ntok], in0=pm[pi][:, :ntok], in1=gt[j][:, :ntok], op=ALU.mult),
                             reads=['mg_pm%d' % pi, 'mg_gt%d' % j], writes=['mg_tmp%d' % tk])
                        if j == 1:
                            P.op('pool', lambda e, tk=tk, ntok=ntok: e.tensor_tensor(out=acc[:, :ntok], in0=acc[:, :ntok], in1=tmp[tk][:, :ntok], op=ALU.add),
                                 reads=['mg_tmp%d' % tk, 'mg_acc'], writes=['mg_acc'])
                        else:
                            P.op('pool', lambda e, tk=tk, ntok=ntok, dc=dc: e.tensor_tensor(out=ymT[:, dc, :ntok], in0=acc[:, :ntok], in1=tmp[tk][:, :ntok], op=ALU.add),
                                 reads=['mg_tmp%d' % tk, 'mg_acc'], writes=['mg_ymT'])
            for ti, t in enumerate(tiles):
                v = 1 if t < 2 else 0
                xi = t % 2
                P.dma('sp', xt[xi][:], x_src[t * 128:(t + 1) * 128, :], reads=[('XR', t)], writes=['mg_xt%d' % xi])
                for hf in range(2):
                    pi = cn % 2; cn += 1
                    for k in range(KT):
                        P.op('pe', lambda e, k=k, pi=pi, ti=ti, hf=hf: e.matmul(
                            po[pi][:, :], lhsT=ymT[:, k, ti * 128:(ti + 1) * 128], rhs=wo[:, k, hf * 512:(hf + 1) * 512], start=(k == 0), stop=(k == KT - 1)),
                             reads=['mg_ymT', 'mg_wo'], writes=['mg_po%d' % pi])
                    P.op('dve', lambda e, pi=pi, xi=xi, hf=hf, v=v: e.tensor_tensor(out=xo[xi][:, hf * 512:(hf + 1) * 512], in0=po[pi][:, :],
                                                                                   in1=g1bc[:, v, hf * 512:(hf + 1) * 512], op=ALU.mult),
                         reads=['mg_po%d' % pi, 'mg_g1bc'], writes=['mg_xo%d' % xi])
                P.op('pool', lambda e, xi=xi: e.tensor_tensor(out=xo[xi][:], in0=xo[xi][:], in1=xt[xi][:], op=ALU.add),
                     reads=['mg_xo%d' % xi, 'mg_xt%d' % xi], writes=['mg_xo%d' % xi])
                P.dma('sp', g.XR[t * 128:(t + 1) * 128, :], xo[xi][:], reads=['mg_xo%d' % xi], writes=[('XR', t)])


CAP = 3072
NE = 32
U32 = mybir.dt.uint32


def phase_moe(nc, P, g, l, with_ctx):
    ntile = T // 128
    tiles = list(range(0 if with_ctx else 2, ntile))
    with ExitStack() as es:
        sb, ps = mk_alloc(nc, es)
        ident = sb("mo_ident", [128, 128], BF16)
        tri = sb("mo_tri", [128, 128], BF16)
        ones = sb("mo_ones", [128, 128], BF16)
        iota = sb("mo_iota", [128, NE], F32)
        P.dma('pool', ident[:], g.c_ident[:, :], writes=['mo_ident'])
        P.dma('pool', tri[:], g.c_tri[:, :], writes=['mo_tri'])
        P.dma('pool', ones[:], g.c_ones[:, :], writes=['mo_ones'])
        P.dma('sp', iota[:], g.c_iota[:, :], writes=['mo_iota'])
        rw = sb("mo_rw", [128, KT, NE], BF16)
        P.dma('pool', rw[:], g.router_w[l, :, :].rearrange("(k p) e -> p k e", p=128), writes=['mo_rw'])
        rb = sb("mo_rb", [128, NE], F32)
        P.dma('sp', rb[:], g.router_b[l, :].partition_broadcast(128), writes=['mo_rb'])
        A2 = sb("mo_A2", [128, 2, D], F32)
        S2 = sb("mo_S2", [128, 2, D], F32)
        G2 = sb("mo_G2", [128, 2, D], F32)
        g2 = sb("mo_g2", [128, D], F32)
        P.dma('sp', g2[:], g.norm2_row[l, :].partition_broadcast(128), writes=['mo_g2'])
        for v in range(2):
            P.dma('sp', A2[:, v, :], g.MOD[l, v, 4 * D:5 * D].partition_broadcast(128), reads=['MOD'], writes=['mo_A2'])
            P.dma('sp', S2[:, v, :], g.MOD[l, v, 3 * D:4 * D].partition_broadcast(128), reads=['MOD'], writes=['mo_S2'])
            P.dma('sp', G2[:, v, :], g.MOD[l, v, 5 * D:6 * D].partition_broadcast(128), reads=['MOD'], writes=['mo_G2'])
            P.op('dve', lambda e, v=v: e.scalar_tensor_tensor(out=A2[:, v, :], in0=A2[:, v, :], scalar=1.0, in1=g2[:], op0=ALU.add, op1=ALU.mult),
                 reads=['mo_A2', 'mo_g2'], writes=['mo_A2'])
        slots = sb("mo_slots", [128, ntile, 4], U32)
        wts = sb("mo_wts", [128, ntile, 4], F32)
        base = sb("mo_base", [128, NE], F32)
        P.op('pool', lambda e: e.memset(base[:], 0.0), writes=['mo_base'])
        xt = [sb("mo_xt%d" % i, [128, D], F32) for i in range(2)]
        junk = sb("mo_junk", [128, D], F32)
        hb = [sb("mo_hb%d" % i, [128, D], BF16) for i in range(2)]
        hT = sb("mo_hT", [128, KT, 128], BF16)
        sm = sb("mo_sm", [128, 16], F32)
        lg = sb("mo_lg", [128, NE], F32)
        mx = sb("mo_mx", [128, 8], F32)
        mi = sb("mo_mi", [128, 8], U32)
        mif = sb("mo_mif", [128, 8], F32)
        ex = sb("mo_ex", [128, 8], F32)
        sel = sb("mo_sel", [128, NE], BF16)
        oh = sb("mo_oh", [128, 4, NE], F32)
        pos = sb("mo_pos", [128, NE], F32)
        pk = sb("mo_pk", [128, 4], F32)
        slf = sb("mo_slf", [128, 4], F32)
        ptr = ps("mo_ptr", [128, KT, 128], BF16)
        plg = ps("mo_plg", [128, 3 * NE], F32)
        for t in tiles:
            v = 1 if t < 2 else 0
            xi = t % 2
            xk, hk = 'mo_xt%d' % xi, 'mo_hb%d' % xi
            P.dma('sp', xt[xi][:], g.XR[t * 128:(t + 1) * 128, :], reads=[('XR', t)], writes=[xk])
            P.op('act', lambda e, xi=xi: e.activation(out=junk[:], in_=xt[xi][:], func=AF.Square, accum_out=sm[:, 0:1]),
                 reads=[xk], writes=['mo_junk', 'mo_sm'])
            P.op('dve', lambda e: e.tensor_scalar(out=sm[:, 0:1], in0=sm[:, 0:1], scalar1=1.0 / D, scalar2=1e-6, op0=ALU.mult, op1=ALU.add),
                 reads=['mo_sm'], writes=['mo_sm'])
            P.op('act', lambda e: e.activation(out=sm[:, 0:1], in_=sm[:, 0:1], func=AF.Sqrt), reads=['mo_sm'], writes=['mo_sm'])
            P.op('dve', lambda e: e.reciprocal(out=sm[:, 0:1], in_=sm[:, 0:1]), reads=['mo_sm'], writes=['mo_sm'])
            P.op('dve', lambda e, xi=xi, v=v: e.scalar_tensor_tensor(out=junk[:], in0=xt[xi][:], scalar=sm[:, 0:1], in1=A2[:, v, :], op0=ALU.mult, op1=ALU.mult),
                 reads=[xk, 'mo_sm', 'mo_A2'], writes=['mo_junk'])
            P.op('pool', lambda e, xi=xi, v=v: e.tensor_tensor(out=hb[xi][:], in0=junk[:], in1=S2[:, v, :], op=ALU.add),
                 reads=['mo_junk', 'mo_S2'], writes=[hk])
            for k in range(KT):
                P.op('pe', lambda e, k=k, xi=xi: e.transpose(out=ptr[:, k, :], in_=hb[xi][:, k * 128:(k + 1) * 128], identity=ident[:]),
                     reads=[hk, 'mo_ident'], writes=['mo_ptr'])
            P.op('act', lambda e: e.copy(out=hT[:], in_=ptr[:]), reads=['mo_ptr'], writes=['mo_hT'])
            for k in range(KT):
                P.op('pe', lambda e, k=k: e.matmul(plg[:, 0:NE], lhsT=hT[:, k, :], rhs=rw[:, k, :], start=(k == 0), stop=(k == KT - 1)),
                     reads=['mo_hT', 'mo_rw'], writes=['mo_plg'])
            P.op('dve', lambda e: e.tensor_tensor(out=lg[:], in0=plg[:, 0:NE], in1=rb[:], op=ALU.add), reads=['mo_plg', 'mo_rb'], writes=['mo_lg'])
            P.op('dve', lambda e: e.max(out=mx[:], in_=lg[:]), reads=['mo_lg'], writes=['mo_mx'])
            P.op('dve', lambda e: e.max_index(out=mi[:], in_max=mx[:], in_values=lg[:]), reads=['mo_lg', 'mo_mx'], writes=['mo_mi'])
            P.op('dve', lambda e: e.tensor_copy(out=mif[:], in_=mi[:]), reads=['mo_mi'], writes=['mo_mif'])
            P.op('dve', lambda e: e.tensor_scalar(out=ex[:, 0:4], in0=mx[:, 0:4], scalar1=mx[:, 0:1], scalar2=None, op0=ALU.subtract),
                 reads=['mo_mx'], writes=['mo_ex'])
            P.op('act', lambda e: e.activation(out=ex[:, 0:4], in_=ex[:, 0:4], func=AF.Exp, accum_out=sm[:, 1:2]), reads=['mo_ex'], writes=['mo_ex', 'mo_sm'])
            P.op('dve', lambda e: e.reciprocal(out=sm[:, 1:2], in_=sm[:, 1:2]), reads=['mo_sm'], writes=['mo_sm'])
            P.op('dve', lambda e, t=t: e.tensor_scalar(out=wts[:, t, :], in0=ex[:, 0:4], scalar1=sm[:, 1:2], scalar2=None, op0=ALU.mult),
                 reads=['mo_ex', 'mo_sm'], writes=['mo_wts'])
            for k in range(4):
                P.op('dve', lambda e, k=k: e.tensor_scalar(out=oh[:, k, :], in0=iota[:], scalar1=mif[:, k:k + 1], scalar2=None, op0=ALU.is_equal),
                     reads=['mo_mif', 'mo_iota'], writes=['mo_oh'])
            P.op('dve', lambda e: e.tensor_tensor(out=pos[:], in0=oh[:, 0, :], in1=oh[:, 1, :], op=ALU.add), reads=['mo_oh'], writes=['mo_pos'])
            P.op('dve', lambda e: e.tensor_tensor(out=pos[:], in0=pos[:], in1=oh[:, 2, :], op=ALU.add), reads=['mo_oh', 'mo_pos'], writes=['mo_pos'])
            P.op('dve', lambda e: e.tensor_tensor(out=sel[:], in0=pos[:], in1=oh[:, 3, :], op=ALU.add), reads=['mo_oh', 'mo_pos'], writes=['mo_sel'])
            P.op('pe', lambda e: e.matmul(plg[:, NE:2 * NE], lhsT=tri[:], rhs=sel[:], start=True, stop=True), reads=['mo_tri', 'mo_sel'], writes=['mo_plg2'])
            P.op('pe', lambda e: e.matmul(plg[:, 2 * NE:3 * NE], lhsT=ones[:], rhs=sel[:], start=True, stop=True), reads=['mo_ones', 'mo_sel'], writes=['mo_plg2'])
            P.op('dve', lambda e: e.tensor_tensor(out=pos[:], in0=plg[:, NE:2 * NE], in1=base[:], op=ALU.add), reads=['mo_plg2', 'mo_base'], writes=['mo_pos'])
            P.op('dve', lambda e: e.tensor_tensor(out=base[:], in0=plg[:, 2 * NE:3 * NE], in1=base[:], op=ALU.add), reads=['mo_plg2', 'mo_base'], writes=['mo_base'])
            for k in range(4):
                P.op('dve', lambda e, k=k: e.tensor_tensor(out=oh[:, k, :], in0=oh[:, k, :], in1=pos[:], op=ALU.mult), reads=['mo_oh', 'mo_pos'], writes=['mo_oh'])
            P.op('dve', lambda e: e.tensor_reduce(out=pk[:], in_=oh[:], axis=AX.X, op=ALU.add), reads=['mo_oh'], writes=['mo_pk'])
            P.op('dve', lambda e: e.scalar_tensor_tensor(out=slf[:], in0=mif[:, 0:4], scalar=float(CAP), in1=pk[:], op0=ALU.mult, op1=ALU.add),
                 reads=['mo_mif', 'mo_pk'], writes=['mo_slf'])
            P.op('dve', lambda e, t=t: e.tensor_copy(out=slots[:, t, :], in_=slf[:]), reads=['mo_slf'], writes=['mo_slots'])
            for k in range(4):
                fn = lambda e, t=t, k=k, xi=xi: e.indirect_dma_start(
                    out=g.XS[:, :], out_offset=bass.IndirectOffsetOnAxis(ap=slots[:, t, k:k + 1], axis=0),
                    in_=hb[xi][:], in_offset=None)
                P.dma_fn('pool', fn, reads=['mo_slots', hk], writes=[('XS', t, k)])
        P.barrier()
        gu = sb("mo_gu", [128, KT, 2 * D], BF16)
        dn = sb("mo_dn", [128, KT, D], BF16)
        gub = sb("mo_gub", [128, 16], F32)
        dnb = sb("mo_dnb", [128, D], F32)
        xs = [sb("mo_xs%d" % i, [128, D], BF16) for i in range(2)]
        XeT = sb("mo_XeT", [128, KT, 512], BF16)
        actT = sb("mo_actT", [128, KT, 512], BF16)
        tg = [sb("mo_tg%d" % i, [128, 512], F32) for i in range(2)]
        tsg = [sb("mo_tsg%d" % i, [128, 512], F32) for i in range(2)]
        tl = [sb("mo_tl%d" % i, [128, 512], F32) for i in range(2)]
        ysb = [sb("mo_ysb%d" % i, [128, D], BF16) for i in range(2)]
        pg = ps("mo_pg", [128, 512], F32)
        pl = ps("mo_pl", [128, 512], F32)
        pdn = [ps("mo_pdn%d" % i, [128, 512], F32) for i in range(2)]
        cn = 0
        for e_ in range(NE):
            for k in range(KT):
                P.dma('pool', gu[:, k, :], g.expert_gu_w[l, e_, k * 128:(k + 1) * 128, :], writes=['mo_gu'])
            P.dma('pool', dn[:], g.expert_dn_w[l, e_, :, :].rearrange("(k p) n -> p k n", p=128), writes=['mo_dn'])
            P.dma('sp', gub[:], g.gu_b[l, e_, :, :], writes=['mo_gub'])
            P.dma('sp', dnb[:], g.expert_dn_b[l, e_, :].partition_broadcast(128), writes=['mo_dnb'])
            for (s0, ns) in [(i * 512, 512) for i in range(CAP // 512)]:
                nst = ns // 128
                for st in range(nst):
                    xi = cn % 2; cn += 1
                    r0 = e_ * CAP + s0 + st * 128
                    P.dma('sp', xs[xi][:], g.XS[r0:r0 + 128, :], writes=['mo_xs%d' % xi])
                    for k in range(KT):
                        P.op('pe', lambda e, k=k, xi=xi: e.transpose(out=ptr[:, k, :], in_=xs[xi][:, k * 128:(k + 1) * 128], identity=ident[:]),
                             reads=['mo_xs%d' % xi, 'mo_ident'], writes=['mo_ptr'])
                    P.op('act', lambda e, st=st: e.copy(out=XeT[:, :, st * 128:(st + 1) * 128], in_=ptr[:]), reads=['mo_ptr'], writes=['mo_XeT'])
                for fc in range(KT):
                    i2 = fc % 2
                    for k in range(KT):
                        P.op('pe', lambda e, k=k, fc=fc, ns=ns: e.matmul(pg[:, :ns], lhsT=gu[:, k, fc * 128:(fc + 1) * 128], rhs=XeT[:, k, :ns],
                                                                     start=(k == 0), stop=(k == KT - 1)), reads=['mo_gu', 'mo_XeT'], writes=['mo_pg'])
                    for k in range(KT):
                        P.op('pe', lambda e, k=k, fc=fc, ns=ns: e.matmul(pl[:, :ns], lhsT=gu[:, k, D + fc * 128:D + (fc + 1) * 128], rhs=XeT[:, k, :ns],
                                                                     start=(k == 0), stop=(k == KT - 1)), reads=['mo_gu', 'mo_XeT'], writes=['mo_pl'])
                    P.op('dve', lambda e, fc=fc, i2=i2, ns=ns: e.tensor_scalar(out=tg[i2][:, :ns], in0=pg[:, :ns], scalar1=gub[:, fc:fc + 1], scalar2=7.0,
                                                                            op0=ALU.add, op1=ALU.min), reads=['mo_pg', 'mo_gub'], writes=['mo_tg%d' % i2])
                    P.op('act', lambda e, i2=i2, ns=ns: e.activation(out=tsg[i2][:, :ns], in_=tg[i2][:, :ns], func=AF.Sigmoid, scale=1.702),
                         reads=['mo_tg%d' % i2], writes=['mo_tsg%d' % i2])
                    P.op('dve', lambda e, fc=fc, i2=i2, ns=ns: e.tensor_scalar(out=tl[i2][:, :ns], in0=pl[:, :ns], scalar1=gub[:, 8 + fc:9 + fc], scalar2=7.0,
                                                                            op0=ALU.add, op1=ALU.min), reads=['mo_pl', 'mo_gub'], writes=['mo_tl%d' % i2])
                    P.op('pool', lambda e, i2=i2, ns=ns: e.tensor_scalar(out=tl[i2][:, :ns], in0=tl[i2][:, :ns], scalar1=-7.0, scalar2=1.0,
                                                                      op0=ALU.max, op1=ALU.add), reads=['mo_tl%d' % i2], writes=['mo_tl%d' % i2])
                    P.op('pool', lambda e, i2=i2, ns=ns: e.tensor_tensor(out=tg[i2][:, :ns], in0=tg[i2][:, :ns], in1=tsg[i2][:, :ns], op=ALU.mult),
                         reads=['mo_tg%d' % i2, 'mo_tsg%d' % i2], writes=['mo_tg%d' % i2])
                    P.op('pool', lambda e, i2=i2, fc=fc, ns=ns: e.tensor_tensor(out=actT[:, fc, :ns], in0=tg[i2][:, :ns], in1=tl[i2][:, :ns], op=ALU.mult),
                         reads=['mo_tg%d' % i2, 'mo_tl%d' % i2], writes=['mo_actT'])
                for st in range(nst):
                    yi = cn % 2; cn += 1
                    for hf in range(2):
                        for k in range(KT):
                            P.op('pe', lambda e, k=k, st=st, hf=hf: e.matmul(pdn[hf][:, :], lhsT=actT[:, k, st * 128:(st + 1) * 128], rhs=dn[:, k, hf * 512:(hf + 1) * 512],
                                                                             start=(k == 0), stop=(k == KT - 1)), reads=['mo_actT', 'mo_dn'], writes=['mo_pdn%d' % hf])
                        P.op('dve', lambda e, yi=yi, hf=hf: e.tensor_tensor(out=ysb[yi][:, hf * 512:(hf + 1) * 512], in0=pdn[hf][:, :], in1=dnb[:, hf * 512:(hf + 1) * 512], op=ALU.add),
                             reads=['mo_pdn%d' % hf, 'mo_dnb'], writes=['mo_ysb%d' % yi])
                    r0 = e_ * CAP + s0 + st * 128
                    P.dma('sp', g.YS[r0:r0 + 128, :], ysb[yi][:], reads=['mo_ysb%d' % yi], writes=[('YS', r0)])
        P.barrier()
        if getattr(g, "dbg_slots", None) is not None:
            P.dma('sp', g.dbg_slots, slots[:], reads=['mo_slots'], writes=['dbg_slots'])
            P.dma('sp', g.dbg_wts, wts[:], reads=['mo_wts'], writes=['dbg_wts'])
        yk = [sb("mo_yk%d" % i, [128, D], BF16) for i in range(4)]
        acc = [sb("mo_acc%d" % i, [128, D], F32) for i in range(2)]
        for t in tiles:
            v = 1 if t < 2 else 0
            ai = t % 2
            for k in range(4):
                fn = lambda e, t=t, k=k: e.indirect_dma_start(
                    out=yk[k][:], out_offset=None, in_=g.YS[:, :],
                    in_offset=bass.IndirectOffsetOnAxis(ap=slots[:, t, k:k + 1], axis=0))
                P.dma_fn('pool', fn, reads=['mo_slots'], writes=['mo_yk%d' % k])
            P.dma('sp', xt[ai][:], g.XR[t * 128:(t + 1) * 128, :], reads=[('XR', t)], writes=['mo_xt%d' % ai])
            P.op('dve', lambda e, t=t, ai=ai: e.tensor_scalar(out=acc[ai][:], in0=yk[0][:], scalar1=wts[:, t, 0:1], scalar2=None, op0=ALU.mult),
                 reads=['mo_yk0', 'mo_wts'], writes=['mo_acc%d' % ai])
            for k in range(1, 4):
                eng = 'dve'
                P.op(eng, lambda e, t=t, k=k, ai=ai: e.scalar_tensor_tensor(out=acc[ai][:], in0=yk[k][:], scalar=wts[:, t, k:k + 1], in1=acc[ai][:], op0=ALU.mult, op1=ALU.add),
                     reads=['mo_yk%d' % k, 'mo_wts', 'mo_acc%d' % ai], writes=['mo_acc%d' % ai])
            P.op('dve', lambda e, ai=ai, v=v: e.tensor_tensor(out=acc[ai][:], in0=acc[ai][:], in1=G2[:, v, :], op=ALU.mult), reads=['mo_acc%d' % ai, 'mo_G2'], writes=['mo_acc%d' % ai])
            P.op('pool', lambda e, ai=ai: e.tensor_tensor(out=acc[ai][:], in0=acc[ai][:], in1=xt[ai][:], op=ALU.add), reads=['mo_acc%d' % ai, 'mo_xt%d' % ai], writes=['mo_acc%d' % ai])
            P.dma('sp', g.XR[t * 128:(t + 1) * 128, :], acc[ai][:], reads=['mo_acc%d' % ai], writes=[('XR', t)])


def phase_final(nc, P, g, out_ap):
    with ExitStack() as es:
        sb, ps = mk_alloc(nc, es)
        fg = sb("fn_g", [128, D], F32)
        P.dma('sp', fg[:], g.final_row[0, :].partition_broadcast(128), writes=['fn_g'])
        xt = [sb("fn_xt%d" % i, [128, D], F32) for i in range(2)]
        yo = [sb("fn_yo%d" % i, [128, D], F32) for i in range(2)]
        junk = sb("fn_junk", [128, D], F32)
        sm = sb("fn_sm", [128, 2], F32)
        for t in range(2, T // 128):
            i = t % 2
            P.dma('sp', xt[i][:], g.XR[t * 128:(t + 1) * 128, :], reads=[('XR', t)], writes=['fn_xt%d' % i])
            P.op('act', lambda e, i=i: e.activation(out=junk[:], in_=xt[i][:], func=AF.Square, accum_out=sm[:, i:i + 1]), reads=['fn_xt%d' % i], writes=['fn_junk', 'fn_sm%d' % i])
            P.op('dve', lambda e, i=i: e.tensor_scalar(out=sm[:, i:i + 1], in0=sm[:, i:i + 1], scalar1=1.0 / D, scalar2=1e-6, op0=ALU.mult, op1=ALU.add), reads=['fn_sm%d' % i], writes=['fn_sm%d' % i])
            P.op('act', lambda e, i=i: e.activation(out=sm[:, i:i + 1], in_=sm[:, i:i + 1], func=AF.Sqrt), reads=['fn_sm%d' % i], writes=['fn_sm%d' % i])
            P.op('dve', lambda e, i=i: e.reciprocal(out=sm[:, i:i + 1], in_=sm[:, i:i + 1]), reads=['fn_sm%d' % i], writes=['fn_sm%d' % i])
            P.op('dve', lambda e, i=i: e.scalar_tensor_tensor(out=yo[i][:], in0=xt[i][:], scalar=sm[:, i:i + 1], in1=fg[:], op0=ALU.mult, op1=ALU.mult),
                 reads=['fn_xt%d' % i, 'fn_sm%d' % i, 'fn_g'], writes=['fn_yo%d' % i])
            P.dma('sp', out_ap[(t - 2) * 128:(t - 1) * 128, :], yo[i][:], reads=['fn_yo%d' % i], writes=[('out', t)])


def build_full():
    nc = bass.Bass("TRN2", target_bir_lowering=False)
    g = declare_io(nc, "full")
    out = nc.dram_tensor("out", [SEQ, D], F32, kind="ExternalOutput").ap()
    with ExitStack() as es:
        P = Prog(nc, es)
        phase0(nc, P, g, es)
        P.barrier()
        for l in range(DEPTH):
            last = (l == DEPTH - 1)
            x_src = g.xin if l == 0 else g.XR
            phase1(nc, P, g, l, x_src)
            P.barrier()
            phase_rwkv(nc, P, g, l)
            P.barrier()
            phase_s5(nc, P, g, l, not last)
            P.barrier()
            phase_s5_readout(nc, P, g, l)
            P.barrier()
            phase_na(nc, P, g, l, not last)
            P.barrier()
            phase_merge(nc, P, g, l, x_src, not last)
            P.barrier()
            phase_moe(nc, P, g, l, not last)
            P.barrier()
        phase_final(nc, P, g, out)
        P.wait_all('sp')
        P.emit()
    return nc


def kernel(**inputs):
    inputs = {k: np.asarray(v) for k, v in inputs.items()}
    nc = build_full()
    shared = None
    in_maps = []
    for core in range(8):
        b = core % 4
        d = host_inputs(inputs, b) if shared is None else dict(shared)
        if shared is None:
            shared = dict(d)
        else:
            f = lambda a: np.ascontiguousarray(a, dtype=np.float32)
            d["xin"] = f(np.concatenate([inputs["ctx"][b], inputs["x"][b]], axis=0))
            cl = inputs["c"][b].reshape(KT, 128).T
            cc = inputs["c_ctx"].reshape(KT, 128).T
            d["cvec"] = f(np.concatenate([cl, cc], axis=1))
        in_maps.append(d)
    res = run_bass_kernel_spmd(nc, in_maps, core_ids=list(range(8)))
    out = np.stack([np.asarray(res.results[b]["out"], dtype=np.float32) for b in range(4)], axis=0)
    return out


NJ = T // 8
PI = float(np.pi)


def sl_(start, count, step):
    if step > 0:
        return slice(start, start + step * (count - 1) + 1, step)
    stop = start + step * (count - 1) - 1
    return slice(start, stop if stop >= 0 else None, step)


def phase_s5(nc, P, g, l, ctx_out):
    with ExitStack() as es:
        sb, ps = mk_alloc(nc, es)
        TAUS = list(range(9)) + [64, 256, 768]
        NTAU = len(TAUS)
        LR = sb("s5_LR", [128, 64], F32); LI = sb("s5_LI", [128, 64], F32); DT = sb("s5_DT", [128, 64], F32)
        P.dma('sp', LR[:], g.s5_lr[l], writes=['s5_LR']); P.dma('sp', LI[:], g.s5_li[l], writes=['s5_LI'])
        P.dma('sp', DT[:], g.s5_ldt[l], writes=['s5_DT'])
        P.op('act', lambda e: e.activation(out=DT[:], in_=DT[:], func=AF.Exp), reads=['s5_DT'], writes=['s5_DT'])
        RD = sb("s5_RD", [128, 64], F32); IDt = sb("s5_ID", [128, 64], F32)
        P.op('dve', lambda e: e.tensor_tensor(out=RD[:], in0=LR[:], in1=DT[:], op=ALU.mult), reads=['s5_LR', 's5_DT'], writes=['s5_RD'])
        P.op('dve', lambda e: e.tensor_tensor(out=IDt[:], in0=LI[:], in1=DT[:], op=ALU.mult), reads=['s5_LI', 's5_DT'], writes=['s5_ID'])
        AR = sb("s5_AR", [128, NTAU, 64], F32); AI = sb("s5_AI", [128, NTAU, 64], F32); NAI = sb("s5_NAI", [128, NTAU, 64], F32)
        tmp = sb("s5_tmp", [128, 64], F32); tmp2 = sb("s5_tmp2", [128, 64], F32); mag = sb("s5_mag", [128, 64], F32)
        ki = sb("s5_ki", [128, 64], I32)
        for ti, tau in enumerate(TAUS):
            P.op('act', lambda e, tau=tau: e.activation(out=mag[:], in_=RD[:], func=AF.Exp, scale=float(tau)), reads=['s5_RD'], writes=['s5_mag'])
            for which, shift in (("sin", PI), ("cos", 1.5 * PI)):
                P.op('dve', lambda e, tau=tau, shift=shift: e.tensor_scalar(out=tmp[:], in0=IDt[:], scalar1=float(tau), scalar2=shift - PI, op0=ALU.mult, op1=ALU.add),
                     reads=['s5_ID'], writes=['s5_tmp'])
                P.op('dve', lambda e: e.tensor_scalar(out=tmp2[:], in0=tmp[:], scalar1=1.0 / (2 * PI), scalar2=None, op0=ALU.mult), reads=['s5_tmp'], writes=['s5_tmp2'])
                P.op('dve', lambda e: e.tensor_copy(out=ki[:], in_=tmp2[:]), reads=['s5_tmp2'], writes=['s5_ki'])
                P.op('dve', lambda e: e.tensor_copy(out=tmp2[:], in_=ki[:]), reads=['s5_ki'], writes=['s5_tmp2'])
                P.op('dve', lambda e: e.scalar_tensor_tensor(out=tmp[:], in0=tmp2[:], scalar=-2 * PI, in1=tmp[:], op0=ALU.mult, op1=ALU.add), reads=['s5_tmp2', 's5_tmp'], writes=['s5_tmp'])
                P.op('dve', lambda e: e.tensor_scalar(out=tmp2[:], in0=tmp[:], scalar1=PI, scalar2=-2 * PI, op0=ALU.is_gt, op1=ALU.mult), reads=['s5_tmp'], writes=['s5_tmp2'])
                P.op('dve', lambda e: e.tensor_tensor(out=tmp[:], in0=tmp[:], in1=tmp2[:], op=ALU.add), reads=['s5_tmp', 's5_tmp2'], writes=['s5_tmp'])
                P.op('dve', lambda e: e.tensor_scalar(out=tmp2[:], in0=tmp[:], scalar1=-PI, scalar2=2 * PI, op0=ALU.is_lt, op1=ALU.mult), reads=['s5_tmp'], writes=['s5_tmp2'])
                P.op('dve', lambda e: e.tensor_tensor(out=tmp[:], in0=tmp[:], in1=tmp2[:], op=ALU.add), reads=['s5_tmp', 's5_tmp2'], writes=['s5_tmp'])
                P.op('act', lambda e: e.activation(out=tmp2[:], in_=tmp[:], func=AF.Sin), reads=['s5_tmp'], writes=['s5_tmp2'])
                dst = AI if which == "sin" else AR
                P.op('dve', lambda e, dst=dst, ti=ti: e.tensor_tensor(out=dst[:, ti, :], in0=tmp2[:], in1=mag[:], op=ALU.mult),
                     reads=['s5_tmp2', 's5_mag'], writes=['s5_A'])
        P.op('dve', lambda e: e.tensor_scalar(out=NAI[:], in0=AI[:], scalar1=-1.0, scalar2=None, op0=ALU.mult), reads=['s5_A'], writes=['s5_NAI'])
        S1 = sb("s5_S1", [128, NTAU, 64], F32); S2 = sb("s5_S2", [128, NTAU, 64], F32)
        T1 = sb("s5_T1", [128, NTAU, 64], F32); T2 = sb("s5_T2", [128, NTAU, 64], F32)
        NAR = sb("s5_NAR", [128, NTAU, 64], F32)
        P.op('dve', lambda e: e.tensor_scalar(out=NAR[:], in0=AR[:], scalar1=-1.0, scalar2=None, op0=ALU.mult), reads=['s5_A'], writes=['s5_NAR'])
        T3 = sb("s5_T3", [128, NTAU, 64], F32)
        for (dstt, top, bot) in ((S1, AR, NAI), (S2, NAI, NAR), (T1, AR, AI), (T2, NAI, AR), (T3, AI, AR)):
            P.op('pool', lambda e, dstt=dstt, top=top: e.tensor_copy(out=dstt[0:64], in_=top[0:64]), reads=['s5_A', 's5_NAI', 's5_NAR'], writes=['s5_ST'])
            P.op('pool', lambda e, dstt=dstt, bot=bot: e.tensor_copy(out=dstt[64:128], in_=bot[64:128]), reads=['s5_A', 's5_NAI', 's5_NAR'], writes=['s5_ST'])
        den = sb("s5_den", [128, 64], F32); cr = sb("s5_cr", [128, 64], F32); ci = sb("s5_ci", [128, 64], F32); am1 = sb("s5_am1", [128, 64], F32)
        P.op('dve', lambda e: e.tensor_tensor(out=den[:], in0=LR[:], in1=LR[:], op=ALU.mult), reads=['s5_LR'], writes=['s5_den'])
        P.op('dve', lambda e: e.tensor_tensor(out=tmp[:], in0=LI[:], in1=LI[:], op=ALU.mult), reads=['s5_LI'], writes=['s5_tmp'])
        P.op('dve', lambda e: e.tensor_tensor(out=den[:], in0=den[:], in1=tmp[:], op=ALU.add), reads=['s5_den', 's5_tmp'], writes=['s5_den'])
        P.op('dve', lambda e: e.reciprocal(out=den[:], in_=den[:]), reads=['s5_den'], writes=['s5_den'])
        P.op('dve', lambda e: e.tensor_scalar(out=am1[:], in0=AR[:, 1, :], scalar1=-1.0, scalar2=None, op0=ALU.add), reads=['s5_A'], writes=['s5_am1'])
        P.op('dve', lambda e: e.tensor_tensor(out=cr[:], in0=am1[:], in1=LR[:], op=ALU.mult), reads=['s5_am1', 's5_LR'], writes=['s5_cr'])
        P.op('dve', lambda e: e.tensor_tensor(out=tmp[:], in0=AI[:, 1, :], in1=LI[:], op=ALU.mult), reads=['s5_A', 's5_LI'], writes=['s5_tmp'])
        P.op('dve', lambda e: e.tensor_tensor(out=cr[:], in0=cr[:], in1=tmp[:], op=ALU.add), reads=['s5_cr', 's5_tmp'], writes=['s5_cr'])
        P.op('dve', lambda e: e.tensor_tensor(out=cr[:], in0=cr[:], in1=den[:], op=ALU.mult), reads=['s5_cr', 's5_den'], writes=['s5_cr'])
        P.op('dve', lambda e: e.tensor_tensor(out=ci[:], in0=AI[:, 1, :], in1=LR[:], op=ALU.mult), reads=['s5_A', 's5_LR'], writes=['s5_ci'])
        P.op('dve', lambda e: e.tensor_tensor(out=tmp[:], in0=am1[:], in1=LI[:], op=ALU.mult), reads=['s5_am1', 's5_LI'], writes=['s5_tmp'])
        P.op('dve', lambda e: e.tensor_tensor(out=ci[:], in0=ci[:], in1=tmp[:], op=ALU.subtract), reads=['s5_ci', 's5_tmp'], writes=['s5_ci'])
        P.op('dve', lambda e: e.tensor_tensor(out=ci[:], in0=ci[:], in1=den[:], op=ALU.mult), reads=['s5_ci', 's5_den'], writes=['s5_ci'])
        BR = sb("s5_BR", [128, 64, 16], F32); BI = sb("s5_BI", [128, 64, 16], F32)
        CR = sb("s5_CR", [128, 64, 16], F32); CI = sb("s5_CI", [128, 64, 16], F32)
        P.dma('sp', BR[:], g.s5_br[l], writes=['s5_BR']); P.dma('sp', BI[:], g.s5_bi[l], writes=['s5_BI'])
        P.dma('sp', CR[:], g.s5_cr[l], writes=['s5_CR']); P.dma('sp', CI[:], g.s5_ci[l], writes=['s5_CI'])
        BBR = sb("s5_BBR", [128, 64, 16], F32); BBI = sb("s5_BBI", [128, 64, 16], F32); big = sb("s5_big", [128, 64, 16], F32)
        bc = lambda t2: t2[:].unsqueeze(2).to_broadcast([128, 64, 16])
        P.op('dve', lambda e: e.tensor_tensor(out=BBR[:], in0=BR[:], in1=bc(cr), op=ALU.mult), reads=['s5_BR', 's5_cr'], writes=['s5_BBR'])
        P.op('dve', lambda e: e.tensor_tensor(out=big[:], in0=BI[:], in1=bc(ci), op=ALU.mult), reads=['s5_BI', 's5_ci'], writes=['s5_big'])
        P.op('dve', lambda e: e.tensor_tensor(out=BBR[:], in0=BBR[:], in1=big[:], op=ALU.subtract), reads=['s5_BBR', 's5_big'], writes=['s5_BBR'])
        P.op('dve', lambda e: e.tensor_tensor(out=BBI[:], in0=BI[:], in1=bc(cr), op=ALU.mult), reads=['s5_BI', 's5_cr'], writes=['s5_BBI'])
        P.op('dve', lambda e: e.tensor_tensor(out=big[:], in0=BR[:], in1=bc(ci), op=ALU.mult), reads=['s5_BR', 's5_ci', 's5_BBR'], writes=['s5_big'])
        P.op('dve', lambda e: e.tensor_tensor(out=BBI[:], in0=BBI[:], in1=big[:], op=ALU.add), reads=['s5_BBI', 's5_big'], writes=['s5_BBI'])
        CA = sb("s5_CA", [128, 9, 64, 16], F32); GG = sb("s5_GG", [128, 8, 64, 16], F32)
        bct = lambda tb, ti: tb[:, ti, :].unsqueeze(2).to_broadcast([128, 64, 16])
        for ti in range(9):
            P.op('dve', lambda e, ti=ti: e.tensor_tensor(out=CA[:, ti], in0=CR[:], in1=bct(S1, ti), op=ALU.mult), reads=['s5_CR', 's5_ST'], writes=['s5_CA'])
            P.op('pool', lambda e, ti=ti: e.tensor_tensor(out=big[:], in0=CI[:], in1=bct(S2, ti), op=ALU.mult), reads=['s5_CI', 's5_ST', 's5_BBI', 's5_CA'], writes=['s5_big'])
            P.op('dve', lambda e, ti=ti: e.tensor_tensor(out=CA[:, ti], in0=CA[:, ti], in1=big[:], op=ALU.add), reads=['s5_big', 's5_CA'], writes=['s5_CA'])
        for ti in range(8):
            P.op('dve', lambda e, ti=ti: e.tensor_tensor(out=GG[:, ti], in0=BBR[:], in1=bct(T1, ti), op=ALU.mult), reads=['s5_BBR', 's5_ST'], writes=['s5_GG'])
            P.op('pool', lambda e, ti=ti: e.tensor_tensor(out=big[:], in0=BBI[:], in1=bct(T2, ti), op=ALU.mult), reads=['s5_BBI', 's5_ST', 's5_CA', 's5_GG'], writes=['s5_big'])
            P.op('dve', lambda e, ti=ti: e.tensor_tensor(out=GG[:, ti], in0=GG[:, ti], in1=big[:], op=ALU.add), reads=['s5_big', 's5_GG'], writes=['s5_GG'])
        identf = sb("s5_identf", [128, 128], F32); identb = sb("s5_identb", [128, 128], BF16); II = sb("s5_II", [128, 128], F32)
        DV = sb("s5_DV", [128, 32], F32)
        P.dma('sp', identf[:], g.c_ident[:, :], writes=['s5_identf']); P.dma('pool', identb[:], g.c_ident[:, :], writes=['s5_identb'])
        P.dma('sp', II[:], g.c_ii[:, :], writes=['s5_II']); P.dma('sp', DV[:], g.s5_dv[l], writes=['s5_DV'])
        BP = sb("s5_BP", [128, 15, 16], F32); CP = sb("s5_CP", [128, 15, 16], F32)
        P.op('pool', lambda e: e.memset(BP[:], 0.0), writes=['s5_BP']); P.op('pool', lambda e: e.memset(CP[:], 0.0), writes=['s5_CP'])
        Mi = [sb("s5_Mi%d" % i, [128, 128], BF16) for i in range(2)]
        MV = [sb("s5_MV%d" % i, [128, 128], BF16) for i in range(2)]
        MY = [sb("s5_MY%d" % i, [128, 8, 16], F32) for i in range(2)]
        GT_ = [sb("s5_GTt%d" % i, [128, 8, 16], F32) for i in range(2)]
        Am = [sb("s5_Am%d" % i, [128, 4, 128], F32) for i in range(2)]
        U = [sb("s5_U%d" % i, [128, NJ + 32], BF16) for i in range(2)]
        Z0 = sb("s5_Z0", [128, NJ], F32); Z1 = sb("s5_Z1", [128, 132], F32); Z2 = sb("s5_Z2", [128, 33], F32); Z3 = sb("s5_Z3", [128, 11], F32); P4 = sb("s5_P4", [128, 12], F32)
        acc = [sb("s5_acc%d" % i, [128, 132], F32) for i in range(2)]
        P3 = sb("s5_P3", [128, 34], F32); P2 = sb("s5_P2", [128, 132], F32); P1 = sb("s5_P1", [128, NJ], F32)
        Y = [sb("s5_Y%d" % i, [128, NJ], F32) for i in range(2)]
        Ys = [sb("s5_Ys%d" % i, [128, NJ], F32) for i in range(2)]
        pm = [ps("s5_pm%d" % i, [128, 128], F32) for i in range(2)]
        pz = [ps("s5_pz%d" % i, [128, 352], F32) for i in range(3)]
        pv = ps("s5_pv", [128, 128], F32)
        pt_ = ps("s5_pt", [128, 4], F32)
        it = 0
        for gi in range(32):
            for d in range(2):
                dg = d * 32 + gi
                b = it % 2; it += 1
                kb = '_%d' % b
                P.op('dve', lambda e, dg=dg: e.tensor_copy(out=BP[:, 7, :], in_=GG[:, 0, dg, :]), reads=['s5_GG'], writes=['s5_BP'])
                if d == 0:
                    P.op('dve', lambda e, dg=dg: e.tensor_copy(out=CP[:, 7:15, :], in_=CA[:, 0:8, dg, :]), reads=['s5_CA'], writes=['s5_CP'])
                    P.op('pool', lambda e: e.memset(CP[:, 0:7, :], 0.0), writes=['s5_CP'])
                else:
                    P.op('dve', lambda e, dg=dg: e.tensor_copy(out=CP[:, 0:8, :], in_=CA[:, 7::-1, dg, :]), reads=['s5_CA'], writes=['s5_CP'])
                    P.op('pool', lambda e: e.memset(CP[:, 8:15, :], 0.0), writes=['s5_CP'])
                for s in range(8):
                    P.op('pe', lambda e, s=s, b=b: e.matmul(pm[b][:, :], lhsT=BP[:, 7 - s:15 - s, :], rhs=CP[:, 7 - s:15 - s, :], start=(s == 0), stop=(s == 7)),
                         reads=['s5_BP', 's5_CP'], writes=['s5_pm' + kb])
                if d == 0:
                    P.op('dve', lambda e, b=b, gi=gi: e.scalar_tensor_tensor(out=Mi[b][:], in0=identf[:], scalar=DV[:, gi:gi + 1], in1=pm[b][:], op0=ALU.mult, op1=ALU.add),
                         reads=['s5_pm' + kb, 's5_identf', 's5_DV'], writes=['s5_Mi' + kb])
                else:
                    P.op('dve', lambda e, b=b: e.tensor_copy(out=Mi[b][:], in_=pm[b][:]), reads=['s5_pm' + kb], writes=['s5_Mi' + kb])
                if d == 0:
                    P.op('dve', lambda e, b=b, dg=dg: e.tensor_copy(out=GT_[b][:], in_=GG[:, 7::-1, dg, :]), reads=['s5_GG'], writes=['s5_GTt' + kb])
                else:
                    P.op('dve', lambda e, b=b, dg=dg: e.tensor_copy(out=GT_[b][:], in_=GG[:, 0:8, dg, :]), reads=['s5_GG'], writes=['s5_GTt' + kb])
                P.op('pe', lambda e, b=b: e.matmul(pv[:, :], lhsT=GT_[b][:], rhs=identf[:], start=True, stop=True), reads=['s5_GTt' + kb, 's5_identf'], writes=['s5_pv'])
                P.op('act', lambda e, b=b: e.copy(out=MV[b][:], in_=pv[:]), reads=['s5_pv'], writes=['s5_MV' + kb])
                if d == 0:
                    P.op('pool', lambda e, b=b, dg=dg: e.tensor_copy(out=MY[b][:], in_=CA[:, 1:9, dg, :]), reads=['s5_CA'], writes=['s5_MY' + kb])
                else:
                    P.op('pool', lambda e, b=b, dg=dg: e.tensor_copy(out=MY[b][:], in_=CA[:, 8:0:-1, dg, :]), reads=['s5_CA'], writes=['s5_MY' + kb])
                for li, ti in enumerate((8, 9, 10, 11)):
                    P.op('dve', lambda e, b=b, li=li, ti=ti, dg=dg: e.tensor_scalar(out=Am[b][:, li, 0:64], in0=II[:, 0:64], scalar1=S1[:, ti, dg:dg + 1], scalar2=None, op0=ALU.mult),
                         reads=['s5_II', 's5_ST'], writes=['s5_Am' + kb])
                    P.op('dve', lambda e, b=b, li=li, ti=ti, dg=dg: e.tensor_scalar(out=Am[b][:, li, 64:128], in0=II[:, 64:128], scalar1=T3[:, ti, dg:dg + 1], scalar2=None, op0=ALU.mult),
                         reads=['s5_II', 's5_ST'], writes=['s5_Am' + kb])
                j0 = 0 if d == 0 else 32
                P.dma('pool', U[b][:, 0:NJ], g.UB[gi, :, :, j0:j0 + NJ].rearrange("i c j -> (i c) j"), reads=['UB'], writes=['s5_U' + kb])
                Uv = (lambda c0, n, b=b: U[b][:, c0:c0 + n]) if d == 0 else (lambda c0, n, b=b: U[b][:, sl_(NJ - 1 - c0, n, -1)])
                for pc in range(3):
                    P.op('pe', lambda e, b=b, pc=pc, Uv=Uv: e.matmul(pz[pc][:, :], lhsT=MV[b][:], rhs=Uv(pc * 352, 352), start=True, stop=True),
                         reads=['s5_MV' + kb, 's5_U' + kb], writes=['s5_pz%d' % pc])
                    eng = 'act' if pc == 1 else 'dve'
                    if eng == 'act':
                        P.op('act', lambda e, pc=pc: e.copy(out=Z0[:, pc * 352:(pc + 1) * 352], in_=pz[pc][:, :]), reads=['s5_pz%d' % pc], writes=['s5_Z0'])
                    else:
                        P.op('dve', lambda e, pc=pc: e.tensor_copy(out=Z0[:, pc * 352:(pc + 1) * 352], in_=pz[pc][:, :]), reads=['s5_pz%d' % pc], writes=['s5_Z0'])

                def horner(src, R, M, A, dst, tag):
                    cur = None
                    for n in range(1, R):
                        prev = src[:, sl_(0, M, R)] if n == 1 else cur
                        prevk = tag if n == 1 else 's5_acc%d' % ((n - 1) % 2)
                        last = (n == R - 1)
                        o = dst if last else acc[n % 2][:, 0:M]
                        ok = ('s5_dst' + tag) if last else 's5_acc%d' % (n % 2)
                        P.op('pe', lambda e, prev=prev, A=A, M=M: e.matmul(pz[0][:, 0:M], lhsT=A, rhs=prev, start=True, stop=False),
                             reads=['s5_Am' + kb, prevk], writes=['s5_pz0'])
                        P.op('pe', lambda e, n=n, M=M, R=R, src=src: e.matmul(pz[0][:, 0:M], lhsT=identf[:], rhs=src[:, sl_(n, M, R)], start=False, stop=True),
                             reads=['s5_identf', tag], writes=['s5_pz0'])
                        P.op('dve', lambda e, o=o, M=M: e.tensor_copy(out=o, in_=pz[0][:, 0:M]), reads=['s5_pz0'], writes=[ok])
                        cur = o
                horner(Z0, 8, 132, Am[b][:, 0, :], Z1[:, :], 's5_Z0')
                horner(Z1, 4, 33, Am[b][:, 1, :], Z2[:, :], 's5_dsts5_Z0')
                horner(Z2, 3, 11, Am[b][:, 2, :], Z3[:, :], 's5_dsts5_dsts5_Z0')
                P.op('pool', lambda e: e.memset(P4[:, 0:1], 0.0), writes=['s5_P4'])
                for q in range(10):
                    P.op('pe', lambda e, q=q, b=b: e.matmul(pt_[:, 0:1], lhsT=Am[b][:, 3, :], rhs=P4[:, q:q + 1], start=True, stop=False),
                         reads=['s5_Am' + kb, 's5_P4'], writes=['s5_pt'])
                    P.op('pe', lambda e, q=q: e.matmul(pt_[:, 0:1], lhsT=identf[:], rhs=Z3[:, q:q + 1], start=False, stop=True),
                         reads=['s5_identf', 's5_dsts5_dsts5_dsts5_Z0'], writes=['s5_pt'])
                    P.op('dve', lambda e, q=q: e.tensor_copy(out=P4[:, q + 1:q + 2], in_=pt_[:, 0:1]), reads=['s5_pt'], writes=['s5_P4'])

                def expand(Pc, Zs, R, M, A, Pf, pck, zk, pfk):
                    P.op('pool', lambda e, Pf=Pf, Pc=Pc, R=R, M=M: e.tensor_copy(out=Pf[:, sl_(0, M, R)], in_=Pc[:, 0:M]), reads=[pck], writes=[pfk])
                    for n in range(R - 1):
                        P.op('pe', lambda e, n=n, Pf=Pf, A=A, R=R, M=M: e.matmul(pz[1][:, 0:M], lhsT=A, rhs=Pf[:, sl_(n, M, R)], start=True, stop=False),
                             reads=['s5_Am' + kb, pfk], writes=['s5_pz1'])
                        P.op('pe', lambda e, n=n, Zs=Zs, R=R, M=M: e.matmul(pz[1][:, 0:M], lhsT=identf[:], rhs=Zs[:, sl_(n, M, R)], start=False, stop=True),
                             reads=['s5_identf', zk], writes=['s5_pz1'])
                        P.op('act', lambda e, n=n, Pf=Pf, R=R, M=M: e.copy(out=Pf[:, sl_(n + 1, M, R)], in_=pz[1][:, 0:M]), reads=['s5_pz1'], writes=[pfk])
                expand(P4, Z2, 3, 11, Am[b][:, 2, :], P3, 's5_P4', 's5_dsts5_dsts5_Z0', 's5_P3')
                expand(P3, Z1, 4, 33, Am[b][:, 1, :], P2, 's5_P3', 's5_dsts5_Z0', 's5_P2')
                expand(P2, Z0, 8, 132, Am[b][:, 0, :], P1, 's5_P2', 's5_Z0', 's5_P1')
                for pc in range(3):
                    c0 = pc * 352
                    Pv = P1[:, c0:c0 + 352] if d == 0 else P1[:, sl_(NJ - 1 - c0, 352, -1)]
                    P.op('pe', lambda e, b=b, pc=pc, Pv=Pv: e.matmul(pz[pc][:, :], lhsT=MY[b][:], rhs=Pv, start=True, stop=False),
                         reads=['s5_MY' + kb, 's5_P1'], writes=['s5_pz%d' % pc])
                    P.op('pe', lambda e, b=b, pc=pc, c0=c0: e.matmul(pz[pc][:, :], lhsT=Mi[b][:], rhs=U[b][:, c0:c0 + 352], start=False, stop=True),
                         reads=['s5_Mi' + kb, 's5_U' + kb], writes=['s5_pz%d' % pc])
                    if d == 0:
                        P.op('act', lambda e, pc=pc, c0=c0: e.copy(out=Y[0][:, c0:c0 + 352], in_=pz[pc][:, :]), reads=['s5_pz%d' % pc], writes=['s5_Y0'])
                    else:
                        P.op('dve', lambda e, pc=pc, c0=c0: e.tensor_copy(out=Y[1][:, c0:c0 + 352], in_=pz[pc][:, :]), reads=['s5_pz%d' % pc], writes=['s5_Y1'])
                if d == 1:
                    yi = gi % 2
                    P.op('pool', lambda e, yi=yi: e.tensor_tensor(out=Ys[yi][:, 32:NJ], in0=Y[0][:, 32:NJ], in1=Y[1][:, 0:NJ - 32], op=ALU.add),
                         reads=['s5_Y0', 's5_Y1'], writes=['s5_Ys%d' % yi])
                    P.op('pool', lambda e, yi=yi: e.tensor_tensor(out=Ys[yi][:, 0:32], in0=Y[0][:, 0:32], in1=Y[1][:, NJ - 32:NJ], op=ALU.add),
                         reads=['s5_Y0', 's5_Y1'], writes=['s5_Ys%d' % yi])
                    P.dma('sp', g.YB[gi].rearrange("i c j -> (i c) j"), Ys[yi][:], reads=['s5_Ys%d' % yi], writes=[('YB', gi)])


def phase_s5_readout(nc, P, g, l):
    with ExitStack() as es:
        sb, ps = mk_alloc(nc, es)
        gw = sb("sr_gw", [128, 4, 512], BF16)
        P.dma('pool', gw[:], g.s5_glu_w[l, :, :].rearrange("(k p) n -> p k n", p=128), writes=['sr_gw'])
        gb = sb("sr_gb", [128, 4], F32)
        P.dma('sp', gb[:], g.s5_glu_bp[l], writes=['sr_gb'])
        ident = sb("sr_ident", [128, 128], BF16)
        P.dma('pool', ident[:], g.c_ident[:, :], writes=['sr_ident'])
        yT = [sb("sr_yT%d" % i, [128, 8, 64], F32) for i in range(2)]
        xo = sb("sr_xo", [128, 512], F32); sq = sb("sr_sq", [128, 512], F32); sg = sb("sr_sg", [128, 512], F32)
        glf = sb("sr_glf", [128, 4, 512], F32); glb = sb("sr_glb", [128, 4, 512], BF16)
        soT = sb("sr_soT", [128, 4, 512], BF16)
        so = [sb("sr_so%d" % i, [128, 512], BF16) for i in range(2)]
        pg = [ps("sr_pg%d" % i, [128, 512], F32) for i in range(2)]
        ptr = [ps("sr_ptr%d" % i, [128, 4, 128], BF16) for i in range(2)]
        nblk = (T + 511) // 512
        cn = 0
        for b in range(nblk):
            ntok = min(512, T - b * 512)
            nj = ntok // 8
            j0 = b * 64
            for cc in range(4):
                yi = cn % 2; cn += 1
                for gg in range(8):
                    gi = cc * 8 + gg
                    P.dma('sp', yT[yi][gg * 16:(gg + 1) * 16, :, :nj], g.YB[gi, :, :, j0:j0 + nj].rearrange("i c j -> c i j"),
                          reads=[('YB', gi)], writes=['sr_yT%d' % yi])
                P.op('dve', lambda e, yi=yi, nj=nj, ntok=ntok: e.tensor_copy(out=xo[:, :ntok].rearrange("p (j i) -> p j i", i=8),
                                                                            in_=yT[yi][:, :, :nj].rearrange("p i j -> p j i")),
                     reads=['sr_yT%d' % yi], writes=['sr_xo'])
                P.op('act', lambda e, ntok=ntok: e.activation(out=sq[:, :ntok], in_=xo[:, :ntok], func=AF.Square), reads=['sr_xo'], writes=['sr_sq'])
                P.op('dve', lambda e, ntok=ntok: e.tensor_scalar(out=sq[:, :ntok], in0=sq[:, :ntok], scalar1=0.044715, scalar2=1.0, op0=ALU.mult, op1=ALU.add),
                     reads=['sr_sq'], writes=['sr_sq'])
                P.op('dve', lambda e, ntok=ntok: e.tensor_tensor(out=sq[:, :ntok], in0=sq[:, :ntok], in1=xo[:, :ntok], op=ALU.mult), reads=['sr_sq', 'sr_xo'], writes=['sr_sq'])
                P.op('act', lambda e, ntok=ntok: e.activation(out=sg[:, :ntok], in_=sq[:, :ntok], func=AF.Sigmoid, scale=1.5957691216), reads=['sr_sq'], writes=['sr_sg'])
                P.op('dve', lambda e, cc=cc, ntok=ntok: e.tensor_tensor(out=glf[:, cc, :ntok], in0=xo[:, :ntok], in1=sg[:, :ntok], op=ALU.mult),
                     reads=['sr_xo', 'sr_sg'], writes=['sr_glf'])
                P.op('pool', lambda e, cc=cc, ntok=ntok: e.tensor_copy(out=glb[:, cc, :ntok], in_=glf[:, cc, :ntok]), reads=['sr_glf'], writes=['sr_glb'])
            for oc in range(4):
                pi = cn % 2; cn += 1
                for k in range(4):
                    P.op('pe', lambda e, k=k, oc=oc, pi=pi, ntok=ntok: e.matmul(pg[pi][:, :ntok], lhsT=gw[:, k, oc * 128:(oc + 1) * 128], rhs=glb[:, k, :ntok],
                                                                           start=(k == 0), stop=(k == 3)), reads=['sr_gw', 'sr_glb'], writes=['sr_pg%d' % pi])
                P.op('act', lambda e, oc=oc, pi=pi, ntok=ntok: e.activation(out=sg[:, :ntok], in_=pg[pi][:, :ntok], func=AF.Sigmoid, bias=gb[:, oc:oc + 1]),
                     reads=['sr_pg%d' % pi, 'sr_gb'], writes=['sr_sg'])
                P.op('dve', lambda e, oc=oc, ntok=ntok: e.tensor_tensor(out=soT[:, oc, :ntok], in0=glf[:, oc, :ntok], in1=sg[:, :ntok], op=ALU.mult),
                     reads=['sr_glf', 'sr_sg'], writes=['sr_soT'])
            for ti in range(ntok // 128):
                pi = cn % 2; cn += 1
                for oc in range(4):
                    P.op('pe', lambda e, oc=oc, pi=pi, ti=ti: e.transpose(out=ptr[pi][:, oc, :], in_=soT[:, oc, ti * 128:(ti + 1) * 128], identity=ident[:]),
                         reads=['sr_soT', 'sr_ident'], writes=['sr_ptr%d' % pi])
                P.op('act', lambda e, pi=pi: e.copy(out=so[pi][:].rearrange("p (a b) -> p a b", a=4), in_=ptr[pi][:]), reads=['sr_ptr%d' % pi], writes=['sr_so%d' % pi])
                t = b * 4 + ti
                P.dma('sp', g.SO[t * 128:(t + 1) * 128, :], so[pi][:], reads=['sr_so%d' % pi], writes=['SO'])


import os as _os
RW_DBG_CHUNKS = int(_os.environ.get('RW_DBG_CHUNKS', '0'))
RW_DBG_STOP = int(_os.environ.get('RW_DBG_STOP', '99'))
CDEC = 0.6065306597126334


def phase_rwkv(nc, P, g, l):
    with ExitStack() as es:
        sb, ps = mk_alloc(nc, es)
        MUP = sb("rw_MUP", [128, 14], F32); MUN = sb("rw_MUN", [128, 14], F32); C0 = sb("rw_C0", [128, 14], F32)
        P.dma('sp', MUP[:], g.rw_mup[l], writes=['rw_MUP']); P.dma('sp', MUN[:], g.rw_mun[l], writes=['rw_MUN'])
        P.op('dve', lambda e: e.tensor_tensor(out=C0[:], in0=MUP[:], in1=MUN[:], op=ALU.add), reads=['rw_MUP', 'rw_MUN'], writes=['rw_C0'])
        P.op('dve', lambda e: e.tensor_scalar(out=C0[:], in0=C0[:], scalar1=-1.0, scalar2=1.0, op0=ALU.mult, op1=ALU.add), reads=['rw_C0'], writes=['rw_C0'])
        PV = sb("rw_PV", [128, 4, 4], F32)
        P.dma('sp', PV[:, 0:3, :], g.rw_pv[l], writes=['rw_PV'])
        P.op('dve', lambda e: e.tensor_scalar(out=PV[:, 3, :], in0=PV[:, 1, :], scalar1=-1.0, scalar2=1.0, op0=ALU.mult, op1=ALU.add), reads=['rw_PV'], writes=['rw_PV'])
        WA0 = sb("rw_WA0", [128, 2, 2, 4], F32)
        P.dma('sp', WA0[:].rearrange("p a d h -> p (a d h)"), g.rw_wa0[l], writes=['rw_WA0'])
        LW = sb("rw_LW", [128, 2, 512], BF16)
        for d in range(2):
            P.dma('pool', LW[0:64, d, :], g.rwkv_w2[l, d, :, :], writes=['rw_LW'])
            P.dma('pool', LW[64:128, d, :], g.rwkv_a2[l, d, :, :], writes=['rw_LW'])
        G2 = sb("rw_G2", [128, 512], BF16)
        P.dma('pool', G2[:], g.rwkv_g2[l, :, :], writes=['rw_G2'])
        LNW = sb("rw_LNW", [128, 512], F32); LNB = sb("rw_LNB", [128, 512], F32)
        P.dma('sp', LNW[:], g.rwkv_ln_w[l, :].partition_broadcast(128), writes=['rw_LNW'])
        P.dma('sp', LNB[:], g.rwkv_ln_b[l, :].partition_broadcast(128), writes=['rw_LNB'])
        BO = sb("rw_BO", [128, 128], F32); HI = sb("rw_HI", [128, 2], F32)
        P.dma('sp', BO[:], g.c_bo[:, :], writes=['rw_BO']); P.dma('sp', HI[:], g.c_hi[:, :], writes=['rw_HI'])
        identb = sb("rw_identb", [128, 128], BF16); identf = sb("rw_identf", [128, 128], F32)
        P.dma('pool', identb[:], g.c_ident[:, :], writes=['rw_identb']); P.dma('sp', identf[:], g.c_ident[:, :], writes=['rw_identf'])
        MK = sb("rw_MK", [128, 8, 128], F32)
        P.dma('sp', MK[:], g.c_masks[:, :, :], writes=['rw_MK'])
        RS = sb("rw_RS", [128, 4, 128], F32)
        P.op('pool', lambda e: e.memset(RS[:], 1.0), writes=['rw_RS'])
        P.op('pool', lambda e: e.memset(RS[:, :, 0:1], 0.0), writes=['rw_RS'])
        W4 = [128, 4, 128]
        zraw = [sb("rw_zraw%d" % i, [128, 14, 130], F32) for i in range(2)]
        zT = sb("rw_zT", [128, 14, 128], F32); zt2 = sb("rw_zt2", [128, 14, 128], F32)
        tws = sb("rw_tws", [128, 128], BF16); sgd = sb("rw_sgd", [128, 128], BF16)
        gsb = sb("rw_gsb", [128, 512], F32)
        kk = sb("rw_kk", W4, F32); t1 = sb("rw_t1", W4, F32); t2 = sb("rw_t2", W4, F32)
        sig = sb("rw_sig", W4, F32); cs = sb("rw_cs", W4, F32); ex = sb("rw_ex", W4, F32)
        gi_ = sb("rw_gi", W4, F32); ge_ = sb("rw_ge", W4, F32); d4 = sb("rw_d4", W4, F32)
        e1 = sb("rw_e1", W4, F32); e2 = sb("rw_e2", W4, F32); e3 = sb("rw_e3", W4, F32); e4 = sb("rw_e4", W4, F32)
        av = sb("rw_av", W4, F32); kd = sb("rw_kd", W4, F32); ka_ = sb("rw_ka", W4, F32)
        tot = sb("rw_tot", [128, 4], F32); gamL = sb("rw_gamL", [128, 4], F32)
        ART = sb("rw_ART", [128, 4, 2, 128], BF16); BKT = sb("rw_BKT", [128, 4, 2, 128], BF16)
        TR3 = sb("rw_TR3", [128, 4, 3, 128], BF16); VKB = sb("rw_VKB", [128, 3, 512], BF16)
        bon = sb("rw_bon", [128, 8], F32)
        Sf = sb("rw_Sf", [128, 4, 64], F32); Sb = sb("rw_Sb", [128, 8, 64], BF16)
        BKZ = sb("rw_BKZ", [128, 8, 2, 128], BF16)
        P.op('pool', lambda e: e.memset(BKZ[:], 0.0), writes=['rw_BKZ'])
        XD = [sb("rw_XD%d" % i, [128, 2, 8, 128], BF16) for i in range(2)]
        XO = sb("rw_XO", [128, 8, 128], BF16)
        DD = [sb("rw_DD%d" % i, [128, 2, 8, 128], BF16) for i in range(2)]
        Mb = sb("rw_Mb", [128, 8, 128], BF16); Nb = sb("rw_Nb", [128, 8, 128], BF16); M2b = sb("rw_M2b", [128, 8, 128], BF16); Ssb = sb("rw_Ssb", [128, 8, 128], BF16)
        Ttb = sb("rw_Ttb", [128, 8, 128], BF16)
        ArbT = sb("rw_ArbT", [128, 8, 128], BF16); A3T = sb("rw_A3T", [128, 8, 2, 128], BF16)
        Wb = sb("rw_Wb", [128, 8, 64], BF16); Ub = sb("rw_Ub", [128, 8, 64], BF16)
        Yo = [sb("rw_Yo%d" % i, [128, 512], F32) for i in range(2)]
        y0 = sb("rw_y0", [128, 512], F32); b0 = sb("rw_b0", [128, 8], F32)
        st = sb("rw_st", [128, 4, 8], F32)
        ysq = sb("rw_ysq", [128, 512], F32); ao = [sb("rw_ao%d" % i, [128, 512], BF16) for i in range(2)]
        ptr = ps("rw_ptr", [128, 2, 3, 128], BF16)
        PL = ps("rw_PL", [128, 4, 128], F32)
        PA_ = ps("rw_PA", [128, 2, 256], F32); pA = [PA_[:, 0, :], PA_[:, 1, :]]
        N1 = ps("rw_N1", [128, 4, 128], F32); N2 = ps("rw_N2", [128, 4, 128], F32); N3 = ps("rw_N3", [128, 4, 128], F32)
        PW = ps("rw_PW", [128, 8, 64], F32)
        pY = ps("rw_pY", [128, 512], F32)
        seg_start = {0, 2, 66}; seg_end = {1, 65, 67}
        b14 = lambda t_: t_[:].unsqueeze(2).to_broadcast([128, 14, 128])
        b4 = lambda ap_: ap_.unsqueeze(2).to_broadcast(W4)
        cnt = 0
        for d in range(2):
            P.op('pool', lambda e: e.memset(Sf[:], 0.0), writes=['rw_Sf'])
            P.op('pool', lambda e: e.memset(Sb[:], 0.0), writes=['rw_Sb'])
            chunks = list(range(0, 66)) if d == 0 else [67, 66] + list(range(65, 1, -1))
            if RW_DBG_CHUNKS:
                chunks = chunks[:RW_DBG_CHUNKS]
            m_strict = 0 if d == 0 else 1
            m_T = (1, 3) if d == 0 else (0, 2)
            GI, GE = (cs, ex) if d == 0 else (gi_, ge_)
            gik, gek = ('rw_cs', 'rw_ex') if d == 0 else ('rw_gi', 'rw_ge')
            for c in chunks:
                tt = c if c <= 65 else c - 66
                zi = cnt % 2; cnt += 1
                zr = zraw[zi]; zk = 'rw_zraw%d' % zi
                s0 = c * 128
                lo = s0 - 1 if c not in seg_start else s0
                hi = s0 + 129 if c not in seg_end else s0 + 128
                if c in seg_start:
                    P.op('pool', lambda e, zr=zr: e.memset(zr[:, :, 0:1], 0.0), writes=[zk])
                if c in seg_end:
                    P.op('pool', lambda e, zr=zr: e.memset(zr[:, :, 129:130], 0.0), writes=[zk])
                P.dma('sp', zr[:, :, lo - s0 + 1:hi - s0 + 1], g.RWT[:, lo:hi].rearrange("(r p) t -> p r t", p=128), reads=['RWT'], writes=[zk])
                P.op('dve', lambda e, zr=zr: e.tensor_tensor(out=zT[:], in0=zr[:, :, 1:129], in1=b14(C0), op=ALU.mult), reads=[zk, 'rw_C0'], writes=['rw_zT'])
                P.op('pool', lambda e, zr=zr: e.tensor_tensor(out=zt2[:], in0=zr[:, :, 0:128], in1=b14(MUP), op=ALU.mult), reads=[zk, 'rw_MUP'], writes=['rw_zt2'])
                P.op('dve', lambda e: e.tensor_tensor(out=zT[:], in0=zT[:], in1=zt2[:], op=ALU.add), reads=['rw_zT', 'rw_zt2'], writes=['rw_zT'])
                P.op('pool', lambda e, zr=zr: e.tensor_tensor(out=zt2[:], in0=zr[:, :, 2:130], in1=b14(MUN), op=ALU.mult), reads=[zk, 'rw_MUN', 'rw_zT'], writes=['rw_zt2'])
                P.op('dve', lambda e: e.tensor_tensor(out=zT[:], in0=zT[:], in1=zt2[:], op=ALU.add), reads=['rw_zT', 'rw_zt2'], writes=['rw_zT'])
                r4 = zT[:, 0:4, :]; k4 = zT[:, 4:8, :]; v4 = zT[:, 8:12, :]
                if RW_DBG_STOP <= 1:
                    continue
                P.op('act', lambda e: e.activation(out=tws[0:64, :], in_=zT[0:64, 12, :], func=AF.Tanh), reads=['rw_zT'], writes=['rw_tws'])
                P.op('pool', lambda e: e.tensor_copy(out=tws[64:128, :], in_=zT[64:128, 12, :]), reads=['rw_zT'], writes=['rw_tws'])
                if d == 1:
                    P.op('act', lambda e: e.activation(out=sgd[:], in_=zT[:, 13, :], func=AF.Sigmoid), reads=['rw_zT'], writes=['rw_sgd'])
                    P.op('pe', lambda e: e.matmul(PL[:].rearrange("p a b -> p (a b)"), lhsT=sgd[:], rhs=G2[:], start=True, stop=True), reads=['rw_sgd', 'rw_G2'], writes=['rw_PL'])
                    P.op('act', lambda e: e.copy(out=gsb[:], in_=PL[:].rearrange("p a b -> p (a b)")), reads=['rw_PL'], writes=['rw_gsb'])
                P.op('dve', lambda e: e.tensor_tensor(out=kk[:], in0=k4, in1=b4(PV[:, 0, :]), op=ALU.mult), reads=['rw_zT', 'rw_PV'], writes=['rw_kk'])
                P.op('act', lambda e: e.activation(out=t1[:], in_=kk[:], func=AF.Square), reads=['rw_kk'], writes=['rw_t1'])
                P.op('pe', lambda e: e.matmul(PL[:].rearrange("p a b -> p (a b)"), lhsT=BO[:], rhs=t1[:].rearrange("p a b -> p (a b)"), start=True, stop=True), reads=['rw_BO', 'rw_t1'], writes=['rw_PL'])
                P.op('act', lambda e: e.activation(out=t1[:], in_=PL[:], func=AF.Sqrt), reads=['rw_PL'], writes=['rw_t1'])
                P.op('dve', lambda e: e.tensor_scalar(out=t1[:], in0=t1[:], scalar1=1e-12, scalar2=None, op0=ALU.max), reads=['rw_t1'], writes=['rw_t1'])
                P.op('dve', lambda e: e.reciprocal(out=t1[:], in_=t1[:]), reads=['rw_t1'], writes=['rw_t1'])
                P.op('dve', lambda e: e.tensor_tensor(out=kk[:], in0=kk[:], in1=t1[:], op=ALU.mult), reads=['rw_kk', 'rw_t1'], writes=['rw_kk'])
                for hp in range(4):
                    P.op('pe', lambda e, hp=hp, d=d: e.matmul(PL[:, hp, :], lhsT=LW[0:64, d, hp * 128:(hp + 1) * 128], rhs=tws[0:64, :], start=True, stop=True), reads=['rw_LW', 'rw_tws', 'rw_t1'], writes=['rw_PL'])
                P.op('dve', lambda e, d=d: e.tensor_tensor(out=sig[:], in0=PL[:], in1=b4(WA0[:, 0, d, :]), op=ALU.add), reads=['rw_PL', 'rw_WA0'], writes=['rw_sig'])
                P.op('act', lambda e: e.activation(out=sig[:], in_=sig[:], func=AF.Sigmoid), reads=['rw_sig'], writes=['rw_sig'])
                for hp in range(4):
                    P.op('pe', lambda e, hp=hp, d=d: e.matmul(PL[:, hp, :], lhsT=LW[64:128, d, hp * 128:(hp + 1) * 128], rhs=tws[64:128, :], start=True, stop=True), reads=['rw_LW', 'rw_tws', 'rw_sig'], writes=['rw_PL'])
                P.op('dve', lambda e, d=d: e.tensor_tensor(out=av[:], in0=PL[:], in1=b4(WA0[:, 1, d, :]), op=ALU.add), reads=['rw_PL', 'rw_WA0'], writes=['rw_av'])
                P.op('act', lambda e: e.activation(out=av[:], in_=av[:], func=AF.Sigmoid), reads=['rw_av'], writes=['rw_av'])
                if RW_DBG_STOP <= 2:
                    continue
                fl = lambda t_: t_[:].rearrange("p a b -> p (a b)")
                P.op('dve', lambda e: e.tensor_tensor_scan(out=fl(cs), data0=fl(RS), data1=fl(sig), initial=0.0, op0=ALU.mult, op1=ALU.add), reads=['rw_sig', 'rw_RS'], writes=['rw_cs'])
                P.op('pool', lambda e: e.tensor_copy(out=tot[:], in_=cs[:, :, 127]), reads=['rw_cs'], writes=['rw_tot'])
                P.op('act', lambda e: e.activation(out=gamL[:], in_=cs[:, :, 127], func=AF.Exp, scale=-CDEC), reads=['rw_cs'], writes=['rw_gamL'])
                P.op('dve', lambda e: e.tensor_tensor(out=ex[:], in0=cs[:], in1=sig[:], op=ALU.subtract), reads=['rw_cs', 'rw_sig'], writes=['rw_ex'])
                if d == 1:
                    P.op('dve', lambda e: e.tensor_tensor(out=gi_[:], in0=b4(tot[:, :]), in1=ex[:], op=ALU.subtract), reads=['rw_ex', 'rw_tot'], writes=['rw_gi'])
                    P.op('dve', lambda e: e.tensor_tensor(out=ge_[:], in0=b4(tot[:, :]), in1=cs[:], op=ALU.subtract), reads=['rw_cs', 'rw_tot'], writes=['rw_ge'])
                P.op('dve', lambda e, GI=GI: e.tensor_tensor(out=d4[:], in0=b4(tot[:, :]), in1=GI[:], op=ALU.subtract), reads=[gik, 'rw_tot'], writes=['rw_d4'])
                P.op('act', lambda e, GI=GI: e.activation(out=e1[:], in_=GI[:], func=AF.Exp, scale=-CDEC), reads=[gik], writes=['rw_e1'])
                P.op('act', lambda e, GI=GI: e.activation(out=e2[:], in_=GI[:], func=AF.Exp, scale=CDEC), reads=[gik], writes=['rw_e2'])
                P.op('act', lambda e, GE=GE: e.activation(out=e3[:], in_=GE[:], func=AF.Exp, scale=-CDEC), reads=[gek], writes=['rw_e3'])
                P.op('act', lambda e: e.activation(out=e4[:], in_=d4[:], func=AF.Exp, scale=-CDEC), reads=['rw_d4'], writes=['rw_e4'])
                if RW_DBG_STOP <= 3:
                    continue
                P.op('dve', lambda e: e.tensor_tensor(out=t2[:], in0=av[:], in1=b4(PV[:, 1, :]), op=ALU.mult), reads=['rw_av', 'rw_PV'], writes=['rw_t2'])
                P.op('dve', lambda e: e.tensor_tensor(out=t2[:], in0=t2[:], in1=b4(PV[:, 3, :]), op=ALU.add), reads=['rw_t2', 'rw_PV'], writes=['rw_t2'])
                P.op('dve', lambda e: e.tensor_tensor(out=kd[:], in0=k4, in1=t2[:], op=ALU.mult), reads=['rw_zT', 'rw_t2'], writes=['rw_kd'])
                P.op('pool', lambda e: e.tensor_tensor(out=ka_[:], in0=kk[:], in1=av[:], op=ALU.mult), reads=['rw_kk', 'rw_av'], writes=['rw_ka'])
                P.op('dve', lambda e: e.scalar_tensor_tensor(out=ART[:, :, 0, :], in0=kk[:], scalar=-1.0, in1=e3[:], op0=ALU.mult, op1=ALU.mult), reads=['rw_kk', 'rw_e3'], writes=['rw_ART'])
                P.op('pool', lambda e: e.tensor_tensor(out=ART[:, :, 1, :], in0=r4, in1=e1[:], op=ALU.mult), reads=['rw_zT', 'rw_e1'], writes=['rw_ART'])
                P.op('dve', lambda e: e.tensor_tensor(out=BKT[:, :, 0, :], in0=ka_[:], in1=e2[:], op=ALU.mult), reads=['rw_ka', 'rw_e2'], writes=['rw_BKT'])
                P.op('pool', lambda e: e.tensor_tensor(out=BKT[:, :, 1, :], in0=kd[:], in1=e2[:], op=ALU.mult), reads=['rw_kd', 'rw_e2'], writes=['rw_BKT'])
                P.op('act', lambda e: e.copy(out=TR3[:, :, 0, :], in_=v4), reads=['rw_zT'], writes=['rw_TR3'])
                P.op('dve', lambda e: e.tensor_tensor(out=TR3[:, :, 1, :], in0=kd[:], in1=e4[:], op=ALU.mult), reads=['rw_kd', 'rw_e4'], writes=['rw_TR3'])
                P.op('pool', lambda e: e.tensor_tensor(out=TR3[:, :, 2, :], in0=ka_[:], in1=e4[:], op=ALU.mult), reads=['rw_ka', 'rw_e4'], writes=['rw_TR3'])
                P.op('dve', lambda e: e.tensor_tensor(out=t2[:], in0=r4, in1=kd[:], op=ALU.mult), reads=['rw_zT', 'rw_kd', 'rw_t2'], writes=['rw_t2'])
                P.op('dve', lambda e: e.tensor_tensor(out=t2[:], in0=t2[:], in1=b4(PV[:, 2, :]), op=ALU.mult), reads=['rw_t2', 'rw_PV'], writes=['rw_t2'])
                for hp in range(4):
                    P.op('pe', lambda e, hp=hp: e.matmul(PL[:, 0, 2 * hp:2 * hp + 2], lhsT=t2[:, hp, :], rhs=HI[:], start=True, stop=True), reads=['rw_t2', 'rw_HI', 'rw_av'], writes=['rw_PL'])
                P.op('dve', lambda e: e.tensor_copy(out=bon[:], in_=PL[:, 0, 0:8]), reads=['rw_PL'], writes=['rw_bon'])
                for h2 in range(2):
                    for a_ in range(2):
                        hp = h2 * 2 + a_
                        for j in range(3):
                            P.op('pe', lambda e, hp=hp, a_=a_, j=j: e.transpose(out=ptr[:, a_, j, :], in_=TR3[:, hp, j, :], identity=identb[:]), reads=['rw_TR3', 'rw_identb'], writes=['rw_ptr'])
                    P.op('act', lambda e, h2=h2: e.copy(out=VKB[:, :, h2 * 256:(h2 + 1) * 256].rearrange("p j (a c) -> p j a c", a=2), in_=ptr.rearrange("p a j c -> p j a c")),
                         reads=['rw_ptr'], writes=['rw_VKB'])
                if RW_DBG_STOP <= 4:
                    continue
                hsl = lambda h: (h // 2, slice((h % 2) * 64, (h % 2) * 64 + 64))
                P.op('pool', lambda e: e.tensor_copy(out=BKZ[0:64, 0::2, :, :], in_=BKT[0:64, :, :, :]), reads=['rw_BKT'], writes=['rw_BKZ'])
                P.op('pool', lambda e: e.tensor_copy(out=BKZ[64:128, 1::2, :, :], in_=BKT[64:128, :, :, :]), reads=['rw_BKT'], writes=['rw_BKZ'])
                md_ = 4 if d == 0 else 5
                mo_ = 6 if d == 0 else 7
                mdT_ = 5 if d == 0 else 4
                for grp in range(2):
                    for j in range(4):
                        h = grp * 4 + j; hp, hr = hsl(h)
                        P.op('pe', lambda e, hp=hp, j=j, grp=grp: e.matmul(N1[:, j, :], lhsT=ART[:, hp, 0, :], rhs=BKZ[:, grp * 4 + j, 0, :], start=True, stop=True), reads=['rw_ART', 'rw_BKZ'], writes=['rw_N1'])
                    P.op('dve', lambda e, grp=grp, md_=md_: e.tensor_tensor(out=XD[0][:, 0, grp * 4:(grp + 1) * 4, :], in0=N1[:], in1=MK[:, md_:md_ + 1, :].to_broadcast([128, 4, 128]), op=ALU.mult),
                         reads=['rw_N1', 'rw_MK'], writes=['rw_XD0_g%d' % grp])
                    P.op('dve', lambda e, grp=grp, mo_=mo_: e.tensor_tensor(out=XO[:, grp * 4:(grp + 1) * 4, :], in0=N1[:], in1=MK[:, mo_:mo_ + 1, :].to_broadcast([128, 4, 128]), op=ALU.mult),
                         reads=['rw_N1', 'rw_MK'], writes=['rw_XO_g%d' % grp])
                for h in range(8):
                    hp, hr = hsl(h); ai = h % 2
                    P.op('pe', lambda e, hp=hp, hr=hr, ai=ai, h=h: e.matmul(pA[ai][:, :], lhsT=BKZ[:, h, 0, :], rhs=ART[:, hp, :, :], start=True, stop=True), reads=['rw_ART', 'rw_BKZ'], writes=['rw_pA%d' % ai])
                    P.op('dve', lambda e, ai=ai, h=h, mdT_=mdT_: e.tensor_tensor(out=XD[0][:, 1, h, :], in0=pA[ai][:, 0:128], in1=MK[:, mdT_, :], op=ALU.mult), reads=['rw_pA%d' % ai, 'rw_MK'], writes=['rw_XD0_g%d' % (h // 4)])
                    P.op('dve', lambda e, ai=ai, h=h, m_T=m_T: e.tensor_tensor(out=ArbT[:, h, :], in0=pA[ai][:, 128:256], in1=MK[:, m_T[1], :], op=ALU.mult), reads=['rw_pA%d' % ai, 'rw_MK'], writes=['rw_ArbT'])
                    P.op('pe', lambda e, hp=hp, hr=hr, ai=ai, h=h: e.matmul(pA[ai][:, :], lhsT=BKZ[:, h, 1, :], rhs=ART[:, hp, :, :], start=True, stop=True), reads=['rw_ART', 'rw_BKZ', 'rw_ArbT', 'rw_XD0_g%d' % (h // 4)], writes=['rw_pA%d' % ai])
                    P.op('dve', lambda e, ai=ai, h=h, m_T=m_T: e.tensor_tensor(out=A3T[:, h, 0, :], in0=pA[ai][:, 0:128], in1=MK[:, m_T[0], :], op=ALU.mult), reads=['rw_pA%d' % ai, 'rw_MK'], writes=['rw_A3T'])
                    P.op('dve', lambda e, ai=ai, h=h, m_T=m_T: e.tensor_tensor(out=A3T[:, h, 1, :], in0=pA[ai][:, 128:256], in1=MK[:, m_T[1], :], op=ALU.mult), reads=['rw_pA%d' % ai, 'rw_MK'], writes=['rw_A3T'])
                if RW_DBG_STOP <= 5:
                    continue
                idb4 = identb[:].unsqueeze(1).unsqueeze(1).to_broadcast([128, 2, 4, 128])
                for grp in range(2):
                    hs = slice(grp * 4, grp * 4 + 4)
                    gk = '_g%d' % grp
                    P.op('pool', lambda e, hs=hs: e.tensor_tensor(out=DD[0][:, :, hs, :], in0=XD[0][:, :, hs, :], in1=idb4, op=ALU.add), reads=['rw_XD0' + gk, 'rw_identb'], writes=['rw_DD0' + gk])
                for q_ in range(1, 5):
                    o = (q_ - 1) % 2; n = q_ % 2
                    xo_, xn_ = XD[o], XD[n]; do_, dn_ = DD[o], DD[n]
                    for grp in range(2):
                        hs = slice(grp * 4, grp * 4 + 4)
                        gk = '_g%d' % grp
                        for j in range(4):
                            h = grp * 4 + j
                            P.op('pe', lambda e, xo_=xo_, h=h, j=j: e.matmul(N1[:, j, :], lhsT=xo_[:, 1, h, :], rhs=xo_[:, 0, h, :], start=True, stop=True), reads=['rw_XD%d' % o + gk], writes=['rw_N1'])
                        for j in range(4):
                            h = grp * 4 + j
                            P.op('pe', lambda e, xo_=xo_, h=h, j=j: e.matmul(N2[:, j, :], lhsT=xo_[:, 0, h, :], rhs=xo_[:, 1, h, :], start=True, stop=True), reads=['rw_XD%d' % o + gk], writes=['rw_N2'])
                        P.op('act', lambda e, xn_=xn_, hs=hs: e.copy(out=xn_[:, 0, hs, :], in_=N1[:]), reads=['rw_N1'], writes=['rw_XD%d' % n + gk])
                        P.op('dve', lambda e, xn_=xn_, hs=hs: e.tensor_copy(out=xn_[:, 1, hs, :], in_=N2[:]), reads=['rw_N2'], writes=['rw_XD%d' % n + gk])
                        for j in range(4):
                            h = grp * 4 + j
                            P.op('pe', lambda e, xn_=xn_, do_=do_, h=h, j=j: e.matmul(N3[:, j, :], lhsT=do_[:, 1, h, :], rhs=xn_[:, 0, h, :], start=True, stop=True), reads=['rw_XD%d' % n + gk, 'rw_DD%d' % o + gk], writes=['rw_N3'])
                        for j in range(4):
                            h = grp * 4 + j
                            P.op('pe', lambda e, xn_=xn_, do_=do_, h=h, j=j: e.matmul(N1[:, j, :], lhsT=xn_[:, 0, h, :], rhs=do_[:, 1, h, :], start=True, stop=True), reads=['rw_XD%d' % n + gk, 'rw_DD%d' % o + gk], writes=['rw_N1'])
                        P.op('dve', lambda e, dn_=dn_, do_=do_, hs=hs: e.tensor_tensor(out=dn_[:, 0, hs, :], in0=N3[:], in1=do_[:, 0, hs, :], op=ALU.add), reads=['rw_N3', 'rw_DD%d' % o + gk], writes=['rw_DD%d' % n + gk])
                        P.op('dve', lambda e, dn_=dn_, do_=do_, hs=hs: e.tensor_tensor(out=dn_[:, 1, hs, :], in0=N1[:], in1=do_[:, 1, hs, :], op=ALU.add), reads=['rw_N1', 'rw_DD%d' % o + gk], writes=['rw_DD%d' % n + gk])
                Df = DD[0]
                for grp in range(2):
                    hs = slice(grp * 4, grp * 4 + 4)
                    gk = '_g%d' % grp
                    for j in range(4):
                        h = grp * 4 + j
                        P.op('pe', lambda e, h=h, j=j: e.matmul(N1[:, j, :], lhsT=XO[:, h, :], rhs=Df[:, 1, h, :], start=True, stop=True), reads=['rw_XO' + gk, 'rw_DD0' + gk], writes=['rw_N1'])
                    for j in range(4):
                        h = grp * 4 + j
                        P.op('pe', lambda e, h=h, j=j: e.matmul(N2[:, j, :], lhsT=Df[:, 1, h, :], rhs=XO[:, h, :], start=True, stop=True), reads=['rw_XO' + gk, 'rw_DD0' + gk], writes=['rw_N2'])
                    P.op('act', lambda e, hs=hs: e.copy(out=Mb[:, hs, :], in_=N1[:]), reads=['rw_N1'], writes=['rw_Mb' + gk])
                    P.op('dve', lambda e, hs=hs: e.tensor_copy(out=Nb[:, hs, :], in_=N2[:]), reads=['rw_N2'], writes=['rw_Nb' + gk])
                    for j in range(4):
                        h = grp * 4 + j
                        P.op('pe', lambda e, h=h, j=j: e.matmul(N3[:, j, :], lhsT=Nb[:, h, :], rhs=Mb[:, h, :], start=True, stop=True), reads=['rw_Nb' + gk, 'rw_Mb' + gk], writes=['rw_N3'])
                    P.op('act', lambda e, hs=hs: e.copy(out=M2b[:, hs, :], in_=N3[:]), reads=['rw_N3'], writes=['rw_M2b' + gk])
                    for j in range(4):
                        h = grp * 4 + j
                        P.op('pe', lambda e, h=h, j=j: e.matmul(N1[:, j, :], lhsT=Nb[:, h, :], rhs=M2b[:, h, :], start=True, stop=True), reads=['rw_Nb' + gk, 'rw_M2b' + gk], writes=['rw_N1'])
                    P.op('dve', lambda e, hs=hs: e.tensor_tensor(out=Ssb[:, hs, :], in0=N1[:], in1=Mb[:, hs, :], op=ALU.add), reads=['rw_N1', 'rw_Mb' + gk], writes=['rw_Ssb' + gk])
                    P.op('pool', lambda e, hs=hs: e.tensor_tensor(out=Ssb[:, hs, :], in0=Ssb[:, hs, :], in1=M2b[:, hs, :], op=ALU.add), reads=['rw_Ssb' + gk, 'rw_M2b' + gk], writes=['rw_Ssb' + gk])
                    for j in range(4):
                        h = grp * 4 + j
                        P.op('pe', lambda e, h=h, j=j: e.matmul(N2[:, j, :], lhsT=Df[:, 0, h, :], rhs=Ssb[:, h, :], start=True, stop=True), reads=['rw_DD0' + gk, 'rw_Ssb' + gk], writes=['rw_N2'])
                    P.op('dve', lambda e, hs=hs: e.tensor_tensor(out=Ttb[:, hs, :], in0=N2[:], in1=Df[:, 1, hs, :], op=ALU.add), reads=['rw_N2', 'rw_DD0' + gk], writes=['rw_Ttb'])
                for h in range(8):
                    hp, hr = hsl(h); hc = slice(h * 64, h * 64 + 64)
                    P.op('pe', lambda e, h=h, hc=hc: e.matmul(PW[:, h, :], lhsT=A3T[:, h, 0, :], rhs=VKB[:, 0, hc], start=True, stop=False), reads=['rw_A3T', 'rw_VKB'], writes=['rw_PW'])
                    P.op('pe', lambda e, h=h, hp=hp, hr=hr: e.matmul(PW[:, h, :], lhsT=ART[:, hp, 0, :], rhs=Sb[:, h, :], start=False, stop=True), reads=['rw_ART', 'rw_Sb'], writes=['rw_PW'])
                P.op('act', lambda e: e.copy(out=Wb[:], in_=PW[:]), reads=['rw_PW'], writes=['rw_Wb'])
                for h in range(8):
                    P.op('pe', lambda e, h=h: e.matmul(PW[:, h, :], lhsT=Ttb[:, h, :], rhs=Wb[:, h, :], start=True, stop=True), reads=['rw_Ttb', 'rw_Wb'], writes=['rw_PW'])
                P.op('act', lambda e: e.copy(out=Ub[:], in_=PW[:]), reads=['rw_PW'], writes=['rw_Ub'])
                for h in range(8):
                    hp, hr = hsl(h); hc = slice(h * 64, h * 64 + 64)
                    P.op('pe', lambda e, h=h, hc=hc: e.matmul(pY[:, hc], lhsT=A3T[:, h, 1, :], rhs=VKB[:, 0, hc], start=True, stop=False), reads=['rw_A3T', 'rw_VKB'], writes=['rw_pY'])
                    P.op('pe', lambda e, h=h, hc=hc: e.matmul(pY[:, hc], lhsT=ArbT[:, h, :], rhs=Ub[:, h, :], start=False, stop=False), reads=['rw_ArbT', 'rw_Ub'], writes=['rw_pY'])
                    P.op('pe', lambda e, h=h, hc=hc, hp=hp, hr=hr: e.matmul(pY[:, hc], lhsT=ART[:, hp, 1, :], rhs=Sb[:, h, :], start=False, stop=True), reads=['rw_ART', 'rw_Sb'], writes=['rw_pY'])
                for h in range(8):
                    hp, hr = hsl(h); hc = slice(h * 64, h * 64 + 64)
                    P.op('pe', lambda e, h=h, hp=hp, hc=hc: e.matmul(PW[:, h, :], lhsT=VKB[:, 1, hp * 128:(hp + 1) * 128], rhs=VKB[:, 0, hc], start=True, stop=False), reads=['rw_VKB', 'rw_Ub'], writes=['rw_PW'])
                    P.op('pe', lambda e, h=h, hp=hp: e.matmul(PW[:, h, :], lhsT=VKB[:, 2, hp * 128:(hp + 1) * 128], rhs=Ub[:, h, :], start=False, stop=True), reads=['rw_VKB', 'rw_Ub'], writes=['rw_PW'])
                PW4 = PW[:].rearrange("p (a b) v -> p a b v", b=2)
                for par in range(2):
                    rows = slice(par * 64, par * 64 + 64)
                    P.op('dve', lambda e, rows=rows: e.tensor_tensor(out=Sf[rows], in0=Sf[rows], in1=gamL[rows, :].unsqueeze(2).to_broadcast([64, 4, 64]), op=ALU.mult), reads=['rw_Sf', 'rw_gamL'], writes=['rw_Sf'])
                    P.op('dve', lambda e, rows=rows, par=par: e.tensor_tensor(out=Sf[rows], in0=Sf[rows], in1=PW4[rows, :, par, :], op=ALU.add), reads=['rw_Sf', 'rw_PW'], writes=['rw_Sf'])
                P.op('pool', lambda e: e.tensor_copy(out=Sb[0:64, 0::2, :], in_=Sf[0:64, :, :]), reads=['rw_Sf'], writes=['rw_Sb'])
                P.op('pool', lambda e: e.tensor_copy(out=Sb[64:128, 1::2, :], in_=Sf[64:128, :, :]), reads=['rw_Sf'], writes=['rw_Sb'])
                if RW_DBG_STOP <= 7:
                    continue
                yi = cnt % 2
                if d == 0:
                    P.op('act', lambda e, yi=yi: e.copy(out=Yo[yi][:], in_=pY[:, :]), reads=['rw_pY'], writes=['rw_Yo%d' % yi])
                    P.dma('sp', g.YR[tt * 128:(tt + 1) * 128, :], Yo[yi][:], reads=['rw_Yo%d' % yi], writes=[('YR', tt)])
                    P.dma('sp', g.BON[tt * 128:(tt + 1) * 128, :], bon[:], reads=['rw_bon'], writes=[('BON', tt)])
                else:
                    P.dma('sp', y0[:], g.YR[tt * 128:(tt + 1) * 128, :], reads=[('YR', tt)], writes=['rw_y0'])
                    P.dma('sp', b0[:], g.BON[tt * 128:(tt + 1) * 128, :], reads=[('BON', tt)], writes=['rw_b0'])
                    Y_ = Yo[yi]; yk_ = 'rw_Yo%d' % yi
                    P.op('dve', lambda e, Y_=Y_: e.tensor_tensor(out=Y_[:], in0=pY[:, :], in1=y0[:], op=ALU.add), reads=['rw_pY', 'rw_y0'], writes=[yk_])
                    P.op('pool', lambda e: e.tensor_tensor(out=b0[:], in0=b0[:], in1=bon[:], op=ALU.add), reads=['rw_b0', 'rw_bon'], writes=['rw_b0'])
                    Y3 = Y_[:].rearrange("p (h v) -> p h v", h=8)
                    P.op('dve', lambda e, Y3=Y3: e.tensor_reduce(out=st[:, 0, :], in_=Y3, axis=AX.X, op=ALU.add), reads=[yk_], writes=['rw_st'])
                    P.op('act', lambda e, Y_=Y_: e.activation(out=ysq[:], in_=Y_[:], func=AF.Square), reads=[yk_], writes=['rw_ysq'])
                    P.op('dve', lambda e: e.tensor_reduce(out=st[:, 1, :], in_=ysq[:].rearrange("p (h v) -> p h v", h=8), axis=AX.X, op=ALU.add), reads=['rw_ysq'], writes=['rw_st'])
                    P.op('dve', lambda e: e.tensor_scalar(out=st[:, 0, :], in0=st[:, 0, :], scalar1=1.0 / 64, scalar2=None, op0=ALU.mult), reads=['rw_st'], writes=['rw_st'])
                    P.op('dve', lambda e: e.tensor_tensor(out=st[:, 2, :], in0=st[:, 0, :], in1=st[:, 0, :], op=ALU.mult), reads=['rw_st'], writes=['rw_st'])
                    P.op('dve', lambda e: e.scalar_tensor_tensor(out=st[:, 1, :], in0=st[:, 1, :], scalar=1.0 / 64, in1=st[:, 2, :], op0=ALU.mult, op1=ALU.subtract), reads=['rw_st'], writes=['rw_st'])
                    P.op('dve', lambda e: e.tensor_scalar(out=st[:, 1, :], in0=st[:, 1, :], scalar1=64e-5, scalar2=None, op0=ALU.add), reads=['rw_st'], writes=['rw_st'])
                    P.op('act', lambda e: e.activation(out=st[:, 1, :], in_=st[:, 1, :], func=AF.Sqrt), reads=['rw_st'], writes=['rw_st'])
                    P.op('dve', lambda e: e.reciprocal(out=st[:, 1, :], in_=st[:, 1, :]), reads=['rw_st'], writes=['rw_st'])
                    bcs = lambda col: st[:, col, :].unsqueeze(2).to_broadcast([128, 8, 64])
                    P.op('dve', lambda e, Y3=Y3: e.tensor_tensor(out=Y3, in0=Y3, in1=bcs(0), op=ALU.subtract), reads=[yk_, 'rw_st'], writes=[yk_])
                    P.op('dve', lambda e, Y3=Y3: e.tensor_tensor(out=Y3, in0=Y3, in1=bcs(1), op=ALU.mult), reads=[yk_, 'rw_st'], writes=[yk_])
                    P.op('pool', lambda e, Y_=Y_: e.tensor_tensor(out=Y_[:], in0=Y_[:], in1=LNW[:], op=ALU.mult), reads=[yk_, 'rw_LNW'], writes=[yk_])
                    P.op('pool', lambda e, Y_=Y_: e.tensor_tensor(out=Y_[:], in0=Y_[:], in1=LNB[:], op=ALU.add), reads=[yk_, 'rw_LNB'], writes=[yk_])
                    P.op('dve', lambda e: e.tensor_tensor(out=ysq[:].rearrange("p (h v) -> p h v", h=8), in0=VKB[:, 0, :].rearrange("p (h v) -> p h v", h=8),
                                                        in1=b0[:].unsqueeze(2).to_broadcast([128, 8, 64]), op=ALU.mult), reads=['rw_VKB', 'rw_b0', 'rw_ysq'], writes=['rw_ysq'])
                    P.op('pool', lambda e, Y_=Y_: e.tensor_tensor(out=Y_[:], in0=Y_[:], in1=ysq[:], op=ALU.add), reads=[yk_, 'rw_ysq'], writes=[yk_])
                    P.op('dve', lambda e, Y_=Y_, yi=yi: e.tensor_tensor(out=ao[yi][:], in0=Y_[:], in1=gsb[:], op=ALU.mult), reads=[yk_, 'rw_gsb'], writes=['rw_ao%d' % yi])
                    P.dma('sp', g.AO[tt * 128:(tt + 1) * 128, :], ao[yi][:], reads=['rw_ao%d' % yi], writes=['AO'])
```

```python
import numpy as np
from contextlib import ExitStack
import concourse.bass as bass
import concourse.mybir as mybir
from concourse.bass_utils import run_bass_kernel_spmd

F32 = mybir.dt.float32
BF16 = mybir.dt.bfloat16
I32 = mybir.dt.int32
ALU = mybir.AluOpType
AF = mybir.ActivationFunctionType
AX = mybir.AxisListType

D = 1024
NCTX = 256
SEQ = 8192
T = NCTX + SEQ
TT = T + NCTX
NIN = 6912
DEPTH = 2
KT = D // 128
O_R, O_K, O_V, O_WD, O_AD, O_GD = 0, 512, 1024, 1536, 1600, 1664
O_NQ, O_NK, O_NV = 1792, 2304, 2816
O_S5 = 3328
O_G = 3840


NOSYNC_SAME = ('pe',)


class Prog:
    ENGS = ('pe', 'dve', 'act', 'pool', 'sp')

    def __init__(self, nc, es, n_dma_sems=10):
        self.nc = nc
        self.lists = {e: [] for e in self.ENGS}
        self.sems = {e: es.enter_context(nc.semaphore('s_' + e)) for e in self.ENGS}
        self.cnt = {e: 0 for e in self.ENGS}
        self.seen = {e: {} for e in self.ENGS}
        self.lastw = {}
        self.readers = {}
        self.dma_sems, self.dma_cnt, self.dma_rr = {}, {}, {}
        for q in ('sp', 'act', 'pool'):
            self.dma_sems[q] = [es.enter_context(nc.semaphore('d_%s%d' % (q, i))) for i in range(n_dma_sems)]
            self.dma_cnt[q] = [0] * n_dma_sems
            self.dma_rr[q] = 0
        self.semobj = dict(self.sems)
        for q in self.dma_sems:
            for i, s in enumerate(self.dma_sems[q]):
                self.semobj[(q, i)] = s

    def _deps(self, eng, reads, writes):
        evs = []
        for k in reads:
            if k in self.lastw:
                evs.append(self.lastw[k])
        for k in writes:
            if k in self.lastw:
                evs.append(self.lastw[k])
            evs.extend(self.readers.get(k, ()))
        waits = {}
        for (sk, v) in evs:
            if sk == eng and eng in NOSYNC_SAME:
                continue
            if self.seen[eng].get(sk, 0) >= v:
                continue
            if waits.get(sk, 0) < v:
                waits[sk] = v
        for sk, v in waits.items():
            self.seen[eng][sk] = v
        return list(waits.items())

    def _commit(self, ev, reads, writes):
        for k in writes:
            self.lastw[k] = ev
            self.readers[k] = []
        for k in reads:
            self.readers.setdefault(k, []).append(ev)

    def op(self, eng, fn, reads=(), writes=()):
        waits = self._deps(eng, reads, writes)
        self.cnt[eng] += 1
        ev = (eng, self.cnt[eng])
        self.lists[eng].append((waits, fn, eng, 1))
        self._commit(ev, reads, writes)
        return ev

    def dma(self, q, out, in_, reads=(), writes=(), **kw):
        i = self.dma_rr[q]
        self.dma_rr[q] = (i + 1) % len(self.dma_sems[q])
        sk = (q, i)
        waits = self._deps(q, reads, writes)
        prev = self.dma_cnt[q][i]
        if prev > 0 and self.seen[q].get(sk, 0) < prev:
            waits.append((sk, prev))
            self.seen[q][sk] = prev
        self.dma_cnt[q][i] += 16
        ev = (sk, self.dma_cnt[q][i])
        fn = lambda e, out=out, in_=in_, kw=kw: e.dma_start(out=out, in_=in_, **kw)
        self.lists[q].append((waits, fn, sk, 16))
        self._commit(ev, reads, writes)
        return ev

    def dma_fn(self, q, fn, reads=(), writes=()):
        i = self.dma_rr[q]
        self.dma_rr[q] = (i + 1) % len(self.dma_sems[q])
        sk = (q, i)
        waits = self._deps(q, reads, writes)
        prev = self.dma_cnt[q][i]
        if prev > 0 and self.seen[q].get(sk, 0) < prev:
            waits.append((sk, prev))
            self.seen[q][sk] = prev
        self.dma_cnt[q][i] += 16
        ev = (sk, self.dma_cnt[q][i])
        self.lists[q].append((waits, fn, sk, 16))
        self._commit(ev, reads, writes)
        return ev

    def barrier(self):
        evs = []
        for e in self.ENGS:
            if self.cnt[e] > 0:
                evs.append((e, self.cnt[e]))
        for q in self.dma_sems:
            for i, c in enumerate(self.dma_cnt[q]):
                if c > 0:
                    evs.append(((q, i), c))
        for e in self.ENGS:
            waits = []
            for (sk, v) in evs:
                if sk == e:
                    continue
                if self.seen[e].get(sk, 0) < v:
                    waits.append((sk, v))
                    self.seen[e][sk] = v
            if waits:
                self.lists[e].append((waits, None, None, 0))

    def wait_all(self, eng='sp'):
        waits = []
        for e in self.ENGS:
            if e != eng and self.cnt[e] > 0:
                waits.append((e, self.cnt[e]))
        for q in self.dma_sems:
            for i, c in enumerate(self.dma_cnt[q]):
                if c > 0:
                    waits.append(((q, i), c))
        self.lists[eng].append((waits, None, None, 0))

    def emit(self):
        nc = self.nc
        names = {'pe': 'tensor', 'dve': 'vector', 'act': 'scalar', 'pool': 'gpsimd', 'sp': 'sync'}
        with nc.Block() as block:
            for e in self.ENGS:
                lst = self.lists[e]

                def body(engobj, lst=lst):
                    for (waits, fn, sk, inc) in lst:
                        for (wk, v) in waits:
                            engobj.wait_ge(self.semobj[wk], v)
                        if fn is not None:
                            ins = fn(engobj)
                            ins.then_inc(self.semobj[sk], inc)
                getattr(block, names[e])(body)


class Ctx:
    pass


_UNIQ = [0]


def mk_alloc(nc, es):
    _UNIQ[0] += 1
    sfx = "_u%d" % _UNIQ[0]
    sb = lambda name, shape, dt: es.enter_context(nc.sbuf_tensor(name + sfx, shape, dt))
    ps = lambda name, shape, dt: es.enter_context(nc.psum_tensor(name + sfx, shape, dt))
    return sb, ps


def declare_io(nc, stage):
    g = Ctx()
    def inp(name, shape, dt=F32):
        t = nc.dram_tensor(name, list(shape), dt, kind="ExternalInput").ap()
        setattr(g, name, t)
        return t
    def scr(name, shape, dt=F32):
        t = nc.dram_tensor(name, list(shape), dt, kind="Internal").ap()
        setattr(g, name, t)
        return t
    g.inp, g.scr = inp, scr
    inp("xin", [T, D])
    inp("cvec", [128, 2 * KT])
    inp("ada_w", [DEPTH, D, 6 * D])
    inp("ada_b", [DEPTH, 6 * D])
    inp("norm1_g", [DEPTH, 128, KT])
    inp("norm2_g", [DEPTH, 128, KT])
    inp("w_in", [DEPTH, D, NIN])
    inp("c_ident", [128, 128])
    inp("c_ones", [128, 128])
    scr("XR", [T, D])
    scr("MOD", [DEPTH, 2, 6 * D])
    scr("RWT", [1792, TT])
    scr("UB", [8, 512, TT // 8])
    scr("NAQT", [512, T], BF16)
    scr("NAKT", [512, T], BF16)
    scr("NAV", [T, 512], BF16)
    scr("GT", [3072, T], BF16)
    inp("na_tab", [DEPTH, 5, 8, 576, 128])
    scr("NAO", [T, 512], BF16)
    inp("w_branch", [DEPTH, 3, 512, D])
    inp("w_out", [DEPTH, D, D])
    inp("c_tri", [128, 128]); inp("c_iota", [128, NE])
    inp("router_w", [DEPTH, D, NE]); inp("router_b", [DEPTH, NE])
    inp("norm2_row", [DEPTH, D]); inp("final_row", [1, D])
    inp("expert_gu_w", [DEPTH, NE, D, 2 * D]); inp("expert_dn_w", [DEPTH, NE, D, D])
    inp("gu_b", [DEPTH, NE, 128, 16]); inp("expert_dn_b", [DEPTH, NE, D])
    inp("c_ii", [128, 128])
    inp("c_bo", [128, 128]); inp("c_hi", [128, 2]); inp("c_masks", [128, 8, 128])
    inp("rw_mup", [DEPTH, 128, 14]); inp("rw_mun", [DEPTH, 128, 14]); inp("rw_pv", [DEPTH, 128, 3, 4]); inp("rw_wa0", [DEPTH, 128, 16])
    inp("rwkv_w2", [DEPTH, 2, 64, 512]); inp("rwkv_a2", [DEPTH, 2, 64, 512]); inp("rwkv_g2", [DEPTH, 128, 512])
    inp("rwkv_ln_w", [DEPTH, 512]); inp("rwkv_ln_b", [DEPTH, 512])
    scr("YR", [T, 512]); scr("BON", [T, 8])
    for nm in ("s5_lr", "s5_li", "s5_ldt"):
        inp(nm, [DEPTH, 128, 64])
    for nm in ("s5_br", "s5_bi", "s5_cr", "s5_ci"):
        inp(nm, [DEPTH, 128, 64, 16])
    inp("s5_dv", [DEPTH, 128, 32])
    inp("s5_glu_w", [DEPTH, 512, 512]); inp("s5_glu_bp", [DEPTH, 128, 4])
    scr("YB", [32, 8, 16, T // 8])
    scr("XS", [NE * CAP, D], BF16); scr("YS", [NE * CAP, D], BF16)
    if stage in ("merge", "moe"):
        inp("AO", [T, 512], BF16); inp("SO", [T, 512], BF16)
    else:
        scr("AO", [T, 512], BF16); scr("SO", [T, 512], BF16)
    return g


def phase0(nc, P, g, es0):
    with ExitStack() as es:
        sb, ps = mk_alloc(nc, es)
        cv = sb("p0_cv", [128, 2 * KT], F32)
        cs = sb("p0_cs", [128, 2 * KT], F32)
        aw = [sb("p0_aw%d" % i, [128, KT, 512], F32) for i in range(2)]
        ab = sb("p0_ab", [1, 6 * D], F32)
        row = [sb("p0_row%d" % i, [1, 6 * D], F32) for i in range(2)]
        pr = [ps("p0_pr%d" % i, [1, 512], F32) for i in range(2)]
        P.dma('sp', cv[:], g.cvec[:, :], writes=['p0_cv'])
        P.op('act', lambda e: e.activation(out=cs[:], in_=cv[:], func=AF.Silu), reads=['p0_cv'], writes=['p0_cs'])
        for l in range(DEPTH):
            P.dma('sp', ab[:], g.ada_b[l:l + 1, :], reads=[], writes=['p0_ab'])
            for cch in range(12):
                a = aw[cch % 2]
                ak = 'p0_aw%d' % (cch % 2)
                P.dma('sp', a[:], g.ada_w[l, :, cch * 512:(cch + 1) * 512].rearrange("(k p) n -> p k n", p=128),
                      writes=[ak])
                for v in range(2):
                    for k in range(KT):
                        P.op('pe', lambda e, v=v, k=k, a=a: e.matmul(pr[v][:], lhsT=cs[:, v * KT + k:v * KT + k + 1],
                                                                    rhs=a[:, k, :], start=(k == 0), stop=(k == KT - 1)),
                             reads=['p0_cs', ak], writes=['p0_pr%d' % v])
                    P.op('dve', lambda e, v=v, cch=cch: e.tensor_tensor(out=row[v][:, cch * 512:(cch + 1) * 512],
                                                                        in0=pr[v][:], in1=ab[:, cch * 512:(cch + 1) * 512],
                                                                        op=ALU.add),
                         reads=['p0_pr%d' % v, 'p0_ab'], writes=['p0_row%d' % v])
            for v in range(2):
                P.dma('sp', g.MOD[l, v:v + 1, :], row[v][:], reads=['p0_row%d' % v], writes=['MOD'])


def load_mod_pp(nc, P, g, l, es, sb, tag):
    m = sb(tag + "_modpp", [128, 2, 6, KT], F32)
    for v in range(2):
        for j in range(6):
            P.dma('sp', m[:, v, j, :], g.MOD[l, v, j * D:(j + 1) * D].rearrange("(k p) -> p k", p=128),
                  reads=['MOD'], writes=[tag + '_modpp'], allow_slow_non_contiguous=True)
    return m


def phase1(nc, P, g, l, x_src):
    with ExitStack() as es:
        sb, ps = mk_alloc(nc, es)
        wb = sb("p1_w", [128, KT, NIN], BF16)
        for k in range(KT):
            for c0 in range(0, NIN, 1728):
                P.dma('pool', wb[:, k, c0:c0 + 1728], g.w_in[l, k * 128:(k + 1) * 128, c0:c0 + 1728], writes=['p1_w'])
        ident = sb("p1_ident", [128, 128], BF16)
        P.dma('pool', ident[:], g.c_ident[:, :], writes=['p1_ident'])
        m = load_mod_pp(nc, P, g, l, es, sb, "p1")
        g1 = sb("p1_g1", [128, KT], F32)
        P.dma('sp', g1[:], g.norm1_g[l, :, :], writes=['p1_g1'])
        A1 = sb("p1_A1", [128, 2, KT], F32)
        for v in range(2):
            P.op('dve', lambda e, v=v: e.scalar_tensor_tensor(out=A1[:, v, :], in0=m[:, v, 1, :], scalar=1.0, in1=g1[:],
                                                              op0=ALU.add, op1=ALU.mult),
                 reads=['p1_modpp', 'p1_g1'], writes=['p1_A1'])
        xt = [sb("p1_xt%d" % i, [128, D], F32) for i in range(2)]
        junk = sb("p1_junk", [128, D], F32)
        xb = [sb("p1_xb%d" % i, [128, D], BF16) for i in range(2)]
        ss = sb("p1_ss", [128, 4], F32)
        hT = [sb("p1_hT%d" % i, [128, KT, 512], BF16) for i in range(2)]
        ptr = [ps("p1_ptr%d" % i, [128, KT, 128], BF16) for i in range(2)]
        pp = [ps("p1_pp%d" % i, [128, 512], F32) for i in range(4)]
        ev = [sb("p1_ev%d" % i, [128, 512], F32) for i in range(4)]
        evb = [sb("p1_evb%d" % i, [128, 512], BF16) for i in range(4)]
        evu = [sb("p1_evu%d" % i, [128, 8, 64], F32) for i in range(2)]
        nblk = (T + 511) // 512
        ntile = T // 128
        cnt = 0
        for b in range(nblk):
            tiles = [t for t in range(4 * b, min(4 * b + 4, ntile))]
            nt = len(tiles)
            ntok = nt * 128
            h = hT[b % 2]
            hk = 'p1_hT%d' % (b % 2)
            for ti, t in enumerate(tiles):
                v = 1 if t < 2 else 0
                x_ = xt[t % 2]; xk = 'p1_xt%d' % (t % 2)
                xb_ = xb[t % 2]; xbk = 'p1_xb%d' % (t % 2)
                pt_ = ptr[t % 2]; ptk = 'p1_ptr%d' % (t % 2)
                sc = ss[:, (t % 2) * 2:(t % 2) * 2 + 1]
                sck = 'p1_ss%d' % (t % 2)
                P.dma('sp', x_[:], x_src[t * 128:(t + 1) * 128, :], writes=[xk])
                P.op('act', lambda e, x_=x_, sc=sc: e.activation(out=junk[:], in_=x_[:], func=AF.Square, accum_out=sc),
                     reads=[xk], writes=['p1_junk', sck])
                P.op('dve', lambda e, sc=sc: e.tensor_scalar(out=sc, in0=sc, scalar1=1.0 / D, scalar2=1e-6,
                                                             op0=ALU.mult, op1=ALU.add), reads=[sck], writes=[sck])
                P.op('act', lambda e, sc=sc: e.activation(out=sc, in_=sc, func=AF.Sqrt), reads=[sck], writes=[sck])
                P.op('dve', lambda e, sc=sc: e.reciprocal(out=sc, in_=sc), reads=[sck], writes=[sck])
                P.op('dve', lambda e, x_=x_, xb_=xb_, sc=sc: e.tensor_scalar(out=xb_[:], in0=x_[:], scalar1=sc, scalar2=None,
                                                                            op0=ALU.mult), reads=[xk, sck], writes=[xbk])
                for k in range(KT):
                    P.op('pe', lambda e, k=k, xb_=xb_, pt_=pt_: e.transpose(out=pt_[:, k, :], in_=xb_[:, k * 128:(k + 1) * 128],
                                                                           identity=ident[:]),
                         reads=[xbk, 'p1_ident'], writes=[ptk])
                hs = h[:, :, ti * 128:(ti + 1) * 128]
                P.op('dve', lambda e, hs=hs, pt_=pt_, v=v: e.tensor_tensor(out=hs, in0=pt_[:],
                                                                           in1=A1[:, v, :].unsqueeze(2).to_broadcast([128, KT, 128]),
                                                                           op=ALU.mult),
                     reads=[ptk, 'p1_A1'], writes=[hk])
                P.op('pool', lambda e, hs=hs, v=v: e.tensor_tensor(out=hs, in0=hs,
                                                                   in1=m[:, v, 0, :].unsqueeze(2).to_broadcast([128, KT, 128]),
                                                                   op=ALU.add),
                     reads=[hk, 'p1_modpp'], writes=[hk])
            chunks = [c for c in range(NIN // 128) if not (O_NV <= c * 128 < O_NV + 512)]
            for c in chunks:
                col = c * 128
                pi = cnt % 4; cnt += 1
                pk = 'p1_pp%d' % pi
                for k in range(KT):
                    P.op('pe', lambda e, k=k, col=col, pi=pi, ntok=ntok, h=h: e.matmul(
                        pp[pi][:, :ntok], lhsT=wb[:, k, col:col + 128], rhs=h[:, k, :ntok],
                        start=(k == 0), stop=(k == KT - 1)),
                         reads=['p1_w', hk], writes=[pk])
                t0 = b * 512
                if col < O_NQ:
                    eng = 'act' if c % 2 == 0 else 'dve'
                    evk = 'p1_ev%d' % pi
                    if eng == 'act':
                        P.op('act', lambda e, pi=pi, ntok=ntok: e.copy(out=ev[pi][:, :ntok], in_=pp[pi][:, :ntok]),
                             reads=[pk], writes=[evk])
                    else:
                        P.op('dve', lambda e, pi=pi, ntok=ntok: e.tensor_copy(out=ev[pi][:, :ntok], in_=pp[pi][:, :ntok]),
                             reads=[pk], writes=[evk])
                    P.dma('sp', g.RWT[col:col + 128, t0:t0 + ntok], ev[pi][:, :ntok], reads=[evk], writes=['RWT'])
                    if b == 0:
                        P.dma('sp', g.RWT[col:col + 128, T:T + NCTX], ev[pi][:, :NCTX], reads=[evk], writes=['RWT'])
                elif col < O_S5:
                    evk = 'p1_evb%d' % pi
                    P.op('act', lambda e, pi=pi, ntok=ntok: e.copy(out=evb[pi][:, :ntok], in_=pp[pi][:, :ntok]),
                         reads=[pk], writes=[evk])
                    dst = g.NAQT if col < O_NK else g.NAKT
                    r0 = col - (O_NQ if col < O_NK else O_NK)
                    P.dma('sp', dst[r0:r0 + 128, t0:t0 + ntok], evb[pi][:, :ntok], reads=[evk], writes=['NAQK'])
                elif col < O_G:
                    ui = cnt % 2
                    evk = 'p1_evu%d' % ui
                    nj = ntok // 8
                    P.op('dve', lambda e, pi=pi, ui=ui, ntok=ntok, nj=nj: e.tensor_copy(
                        out=evu[ui][:, :, :nj], in_=pp[pi][:, :ntok].rearrange("p (j i) -> p i j", i=8)),
                         reads=[pk], writes=[evk])
                    ch0 = col - O_S5
                    j0 = t0 // 8
                    for i in range(8):
                        P.dma('sp', g.UB[i, ch0:ch0 + 128, j0:j0 + nj], evu[ui][:, i, :nj], reads=[evk], writes=['UB'])
                        if b == 0:
                            P.dma('sp', g.UB[i, ch0:ch0 + 128, T // 8:T // 8 + 32], evu[ui][:, i, :32], reads=[evk], writes=['UB'])
                else:
                    evk = 'p1_evb%d' % pi
                    P.op('act', lambda e, pi=pi, ntok=ntok: e.activation(out=evb[pi][:, :ntok], in_=pp[pi][:, :ntok],
                                                                         func=AF.Sigmoid),
                         reads=[pk], writes=[evk])
                    r0 = col - O_G
                    P.dma('sp', g.GT[r0:r0 + 128, t0:t0 + ntok], evb[pi][:, :ntok], reads=[evk], writes=['GT'])
            for ti, t in enumerate(tiles):
                pi = cnt % 4; cnt += 1
                pk = 'p1_pp%d' % pi
                for k in range(KT):
                    P.op('pe', lambda e, k=k, pi=pi, ti=ti, h=h: e.matmul(
                        pp[pi][:, :], lhsT=h[:, k, ti * 128:(ti + 1) * 128], rhs=wb[:, k, O_NV:O_NV + 512],
                        start=(k == 0), stop=(k == KT - 1)),
                         reads=['p1_w', hk], writes=[pk])
                evk = 'p1_evb%d' % pi
                P.op('dve', lambda e, pi=pi: e.tensor_copy(out=evb[pi][:], in_=pp[pi][:]), reads=[pk], writes=[evk])
                P.dma('sp', g.NAV[t * 128:(t + 1) * 128, :], evb[pi][:], reads=[evk], writes=['NAV'])


def build(stage="p1", dbg=()):
    nc = bass.Bass("TRN2", target_bir_lowering=False)
    g = declare_io(nc, stage)
    outs = {}
    for name, shape, dt in dbg:
        outs[name] = nc.dram_tensor("o_" + name, list(shape), dt, kind="ExternalOutput").ap()
    if stage == "moe":
        g.dbg_slots = nc.dram_tensor("o_slots", [128, T // 128, 4], U32, kind="ExternalOutput").ap()
        g.dbg_wts = nc.dram_tensor("o_wts", [128, T // 128, 4], F32, kind="ExternalOutput").ap()
    with ExitStack() as es:
        P = Prog(nc, es)
        phase0(nc, P, g, es)
        P.barrier()
        phase1(nc, P, g, 0, g.xin)
        P.barrier()
        if stage in ("rwkv",):
            phase_rwkv(nc, P, g, 0)
            P.barrier()
        if stage in ("s5",):
            phase_s5(nc, P, g, 0, True)
            P.barrier()
            phase_s5_readout(nc, P, g, 0)
            P.barrier()
        if stage in ("na", "merge", "moe"):
            phase_na(nc, P, g, 0, True)
            P.barrier()
        if stage in ("merge", "moe"):
            phase_merge(nc, P, g, 0, g.xin, True)
            P.barrier()
        if stage in ("moe",):
            phase_moe(nc, P, g, 0, True)
            P.barrier()
        for name, shape, dt in dbg:
            src = getattr(g, name)
            P.dma('sp', outs[name], src, reads=[name, 'RWT', 'UB', 'NAQK', 'NAV', 'GT', 'MOD', 'NAO', 'SO', 'AO'] + [('XR', t) for t in range(T // 128)] + [('YS', r) for r in range(0, NE * CAP, 128)] + [('YB', gi) for gi in range(32)], writes=['o_' + name])
        P.wait_all('sp')
        P.emit()
    return nc


def host_inputs(inputs, b):
    f = lambda a: np.ascontiguousarray(a, dtype=np.float32)
    d = {}
    d["xin"] = f(np.concatenate([inputs["ctx"][b], inputs["x"][b]], axis=0))
    cl = inputs["c"][b].reshape(KT, 128).T
    cc = inputs["c_ctx"].reshape(KT, 128).T
    d["cvec"] = f(np.concatenate([cl, cc], axis=1))
    d["ada_w"] = f(inputs["ada_w"])
    d["ada_b"] = f(inputs["ada_b"])
    d["norm1_g"] = f(inputs["norm1_g"].reshape(DEPTH, KT, 128).transpose(0, 2, 1))
    d["norm2_g"] = f(inputs["norm2_g"].reshape(DEPTH, KT, 128).transpose(0, 2, 1))
    d["w_in"] = f(inputs["w_in"])
    d["c_ident"] = np.eye(128, dtype=np.float32)
    d["c_ones"] = np.ones((128, 128), dtype=np.float32)
    d["w_branch"] = f(inputs["w_branch"]); d["w_out"] = f(inputs["w_out"])
    d["c_tri"] = np.triu(np.ones((128, 128), np.float32), 1)
    d["c_iota"] = np.tile(np.arange(NE, dtype=np.float32)[None, :], (128, 1))
    d["router_w"] = f(inputs["router_w"]); d["router_b"] = f(inputs["router_b"])
    d["norm2_row"] = f(inputs["norm2_g"]); d["final_row"] = f(inputs["final_g"].reshape(1, D))
    d["expert_gu_w"] = f(inputs["expert_gu_w"]); d["expert_dn_w"] = f(inputs["expert_dn_w"])
    d["gu_b"] = f(inputs["expert_gu_b"].reshape(DEPTH, NE, 16, 128).transpose(0, 1, 3, 2))
    d["expert_dn_b"] = f(inputs["expert_dn_b"])
    ii = np.zeros((128, 128), np.float32)
    for k in range(128):
        ii[k, k % 64] = 1.0; ii[k, 64 + k % 64] = 1.0
    d["c_ii"] = ii
    def pdup(a):
        a = np.moveaxis(a.reshape((DEPTH, 64, 64) + a.shape[4:]), 2, 1)
        return f(np.concatenate([a, a], axis=1))
    d["s5_lr"] = pdup(inputs["s5_lambda_re"]); d["s5_li"] = pdup(inputs["s5_lambda_im"])
    d["s5_ldt"] = f(np.tile(inputs["s5_log_dt"].reshape(DEPTH, 1, 64), (1, 128, 1)))
    d["s5_br"] = pdup(inputs["s5_b_re"]); d["s5_bi"] = pdup(inputs["s5_b_im"])
    d["s5_cr"] = pdup(np.swapaxes(inputs["s5_c_re"], 3, 4)); d["s5_ci"] = pdup(np.swapaxes(inputs["s5_c_im"], 3, 4))
    dv = inputs["s5_d"].reshape(DEPTH, 32, 16)
    d["s5_dv"] = f(np.tile(np.transpose(dv, (0, 2, 1))[:, None, :, :], (1, 8, 1, 1)).reshape(DEPTH, 128, 32))
    bo = np.zeros((128, 128), np.float32); bo[:64, :64] = 1; bo[64:, 64:] = 1
    d["c_bo"] = bo
    hi_ = np.zeros((128, 2), np.float32); hi_[:64, 0] = 1; hi_[64:, 1] = 1
    d["c_hi"] = hi_
    ii_, jj_ = np.meshgrid(np.arange(128), np.arange(128), indexing="ij")
    bd_ = (ii_ // 32) == (jj_ // 32)
    d["c_masks"] = f(np.stack([(jj_ < ii_), (jj_ > ii_), (jj_ <= ii_), (jj_ >= ii_),
                               (jj_ < ii_) & bd_, (jj_ > ii_) & bd_, (jj_ < ii_) & ~bd_, (jj_ > ii_) & ~bd_], axis=1).astype(np.float32))
    pm = lambda a, n: f(a.reshape(DEPTH, n, 128).transpose(0, 2, 1))
    d["rw_mup"] = pm(inputs["rwkv_mu_prev"], 14); d["rw_mun"] = pm(inputs["rwkv_mu_next"], 14)
    d["rw_pv"] = f(np.stack([pm(inputs["rwkv_k_k"], 4), pm(inputs["rwkv_k_a"], 4), pm(inputs["rwkv_r_k"].reshape(DEPTH, 512), 4)], axis=2))
    w0 = inputs["rwkv_w0"].reshape(DEPTH, 2, 4, 128).transpose(0, 3, 1, 2)
    a0 = inputs["rwkv_a0"].reshape(DEPTH, 2, 4, 128).transpose(0, 3, 1, 2)
    d["rw_wa0"] = f(np.stack([w0, a0], axis=2).reshape(DEPTH, 128, 16))
    for nm in ("rwkv_w2", "rwkv_a2", "rwkv_g2", "rwkv_ln_w", "rwkv_ln_b"):
        d[nm] = f(inputs[nm])
    d["s5_glu_w"] = f(inputs["s5_glu_w"])
    d["s5_glu_bp"] = f(inputs["s5_glu_b"].reshape(DEPTH, 4, 128).transpose(0, 2, 1))
    d["na_tab"] = np.stack([na_tables(inputs["na_rpb"][l]) for l in range(DEPTH)])
    return d


def na_tables(rpb_l):
    out = np.full((5, 8, 576, 128), -30000.0, np.float32)
    for ti, m in enumerate([0, 1, 30, 62, 63]):
        kb = min(max(2 * m - 4, 0), 119)
        qi = np.arange(128); r = 2 * m + qi // 64; c = qi % 64
        rs = np.clip(r - 4, 0, 120); cs = np.clip(c - 8, 0, 48)
        ki = np.arange(576); kr = kb + ki // 64; kc = ki % 64
        ok = ((kr[:, None] >= rs[None, :]) & (kr[:, None] < rs[None, :] + 8) &
              (kc[:, None] >= cs[None, :]) & (kc[:, None] < cs[None, :] + 16))
        ro = np.clip(kr[:, None] - r[None, :] + 7, 0, 14); co = np.clip(kc[:, None] - c[None, :] + 15, 0, 30)
        for h in range(8):
            b = rpb_l[h][ro, co]
            out[ti, h] = np.where(ok, b, -30000.0)
    return out


def phase_na(nc, P, g, l, ctx_out):
    with ExitStack() as es:
        sb, ps = mk_alloc(nc, es)
        NTL = T // 128
        kT = sb("na_kT", [128, T], BF16)
        qT = sb("na_qT", [128, T], BF16)
        v0 = sb("na_v0", [128, NTL, 2, 65], BF16)
        v1 = sb("na_v1", [128, NTL, 2, 65], BF16)
        tbf = sb("na_tbf", [128, 5, 128], F32)
        eb = sb("na_eb", [128, 2, 5, 5, 128], BF16)
        et = [sb("na_et%d" % i, [128, 7, 128], BF16) for i in range(2)]
        ob = sb("na_ob", [128, NTL, 128], BF16)
        rc = sb("na_rc", [128, 2], F32)
        pst = [ps("na_pst%d" % i, [128, 8, 128], F32) for i in range(2)]
        po = [ps("na_po%d" % i, [128, 65], F32) for i in range(2)]
        P.op('pool', lambda e: e.memset(v0[:], 1.0), writes=['na_v0'])
        P.op('pool', lambda e: e.memset(v1[:], 1.0), writes=['na_v1'])
        P.op('pool', lambda e: e.memset(eb[:], 0.0), writes=['na_eb'])
        u = 0
        for hp in range(4):
            P.dma('sp', kT[:], g.NAKT[hp * 128:(hp + 1) * 128, :], reads=['NAQK'], writes=['na_kT'])
            P.dma('sp', qT[:], g.NAQT[hp * 128:(hp + 1) * 128, :], reads=['NAQK'], writes=['na_qT'])
            for hh in range(2):
                P.dma('sp', v1[0:64, NTL - 1, hh, 0:64], g.NAV[T - 64:T, hp * 128 + hh * 64:hp * 128 + hh * 64 + 64],
                      reads=['NAV'], writes=['na_v1'])
                P.dma('sp', v0[:, :, hh, 0:64],
                      g.NAV[:, hp * 128 + hh * 64:hp * 128 + hh * 64 + 64].rearrange("(n p) d -> p n d", p=128),
                      reads=['NAV'], writes=['na_v0'])
                P.dma('sp', v1[:, 0:NTL - 1, hh, 0:64],
                      g.NAV[64:T - 64, hp * 128 + hh * 64:hp * 128 + hh * 64 + 64].rearrange("(n p) d -> p n d", p=128),
                      reads=['NAV'], writes=['na_v1'])
                for tb in range(5):
                    h = hp * 2 + hh
                    for blk in range(5):
                        nk = 128 if blk < 4 else 64
                        P.dma('sp', tbf[:nk, blk, :], g.na_tab[l, tb, h, blk * 128:blk * 128 + nk, :],
                              writes=['na_tbf'])
                    P.op('act', lambda e, hh=hh, tb=tb: e.activation(out=eb[:, hh, tb, 0:4, :], in_=tbf[:, 0:4, :], func=AF.Exp),
                         reads=['na_tbf'], writes=['na_eb'])
                    P.op('act', lambda e, hh=hh, tb=tb: e.activation(out=eb[0:64, hh, tb, 4, :], in_=tbf[0:64, 4, :], func=AF.Exp),
                         reads=['na_tbf'], writes=['na_eb'])
            units = []
            if ctx_out:
                units += [('c', 0), ('c', 1)]
            units += [('l', m) for m in range(64)]
            for (kind, m) in units:
                for hh in range(2):
                    pr = slice(hh * 64, hh * 64 + 64)
                    ui = u % 2; u += 1
                    pk, ek, ok = 'na_pst%d' % ui, 'na_et%d' % ui, 'na_po%d' % ui
                    if kind == 'c':
                        q0 = m * 128
                        blocks = [(0, 128, None), (128, 128, None)]
                        tb = None
                    else:
                        q0 = NCTX + m * 128
                        kb = min(max(2 * m - 4, 0), 119)
                        k0 = NCTX + kb * 64
                        blocks = [(k0 + 128 * j, 128 if j < 4 else 64, j) for j in range(5)] + [(0, 128, None), (128, 128, None)]
                        tb = {0: 0, 1: 1, 62: 3, 63: 4}.get(m, 2)
                    nb = len(blocks)
                    for j, (ks, nk, tj) in enumerate(blocks):
                        P.op('pe', lambda e, ui=ui, j=j, ks=ks, nk=nk, pr=pr, q0=q0: e.matmul(
                            pst[ui][:nk, j, :], lhsT=kT[pr, ks:ks + nk], rhs=qT[pr, q0:q0 + 128], start=True, stop=True),
                             reads=['na_kT', 'na_qT'], writes=[pk])
                    if kind == 'l':
                        P.op('act', lambda e, ui=ui: e.activation(out=et[ui][:, 0:4, :], in_=pst[ui][:, 0:4, :], func=AF.Exp, scale=0.125),
                             reads=[pk], writes=[ek])
                        P.op('act', lambda e, ui=ui: e.activation(out=et[ui][0:64, 4, :], in_=pst[ui][0:64, 4, :], func=AF.Exp, scale=0.125),
                             reads=[pk], writes=[ek])
                        P.op('act', lambda e, ui=ui: e.activation(out=et[ui][:, 5:7, :], in_=pst[ui][:, 5:7, :], func=AF.Exp, scale=0.125),
                             reads=[pk], writes=[ek])
                        P.op('dve', lambda e, ui=ui, hh=hh, tb=tb: e.tensor_tensor(out=et[ui][:, 0:4, :], in0=et[ui][:, 0:4, :],
                                                                                  in1=eb[:, hh, tb, 0:4, :], op=ALU.mult),
                             reads=[ek, 'na_eb'], writes=[ek])
                        P.op('dve', lambda e, ui=ui, hh=hh, tb=tb: e.tensor_tensor(out=et[ui][0:64, 4, :], in0=et[ui][0:64, 4, :],
                                                                                  in1=eb[0:64, hh, tb, 4, :], op=ALU.mult),
                             reads=[ek, 'na_eb'], writes=[ek])
                    else:
                        P.op('act', lambda e, ui=ui: e.activation(out=et[ui][:, 0:2, :], in_=pst[ui][:, 0:2, :], func=AF.Exp, scale=0.125),
                             reads=[pk], writes=[ek])
                    for j, (ks, nk, tj) in enumerate(blocks):
                        if ks % 128 == 0:
                            vv = v0[:nk, ks // 128, hh, :]
                        else:
                            vv = v1[:nk, (ks - 64) // 128, hh, :]
                        P.op('pe', lambda e, ui=ui, j=j, nk=nk, vv=vv, nb=nb: e.matmul(
                            po[ui][:, :], lhsT=et[ui][:nk, j, :], rhs=vv, start=(j == 0), stop=(j == nb - 1)),
                             reads=[ek, 'na_v0', 'na_v1'], writes=[ok])
                    rk = 'na_rc%d' % ui
                    P.op('dve', lambda e, ui=ui: e.reciprocal(out=rc[:, ui:ui + 1], in_=po[ui][:, 64:65]), reads=[ok], writes=[rk])
                    P.op('dve', lambda e, ui=ui, q0=q0, hh=hh: e.tensor_scalar(out=ob[:, q0 // 128, hh * 64:hh * 64 + 64], in0=po[ui][:, 0:64],
                                                                             scalar1=rc[:, ui:ui + 1], scalar2=None, op0=ALU.mult),
                         reads=[ok, rk], writes=['na_ob'])
            t_lo = 0 if ctx_out else 2
            P.dma('sp', g.NAO[t_lo * 128:T, hp * 128:(hp + 1) * 128].rearrange("(n p) d -> p n d", p=128), ob[:, t_lo:, :],
                  reads=['na_ob'], writes=['NAO'])


def phase_merge(nc, P, g, l, x_src, ctx_out):
    with ExitStack() as es:
        sb, ps = mk_alloc(nc, es)
        wbr = sb("mg_wbr", [128, 3, 4, D], BF16)
        wo = sb("mg_wo", [128, KT, D], BF16)
        ident = sb("mg_ident", [128, 128], BF16)
        P.dma('pool', ident[:], g.c_ident[:, :], writes=['mg_ident'])
        for j in range(3):
            P.dma('pool', wbr[:, j, :, :], g.w_branch[l, j, :, :].rearrange("(k p) n -> p k n", p=128), writes=['mg_wbr'])
        P.dma('pool', wo[:], g.w_out[l, :, :].rearrange("(k p) n -> p k n", p=128), writes=['mg_wo'])
        g1bc = sb("mg_g1bc", [128, 2, D], F32)
        for v in range(2):
            P.dma('sp', g1bc[:, v, :], g.MOD[l, v, 2 * D:3 * D].partition_broadcast(128), reads=['MOD'], writes=['mg_g1bc'])
        bt = [sb("mg_bt%d" % i, [128, 512], BF16) for i in range(3)]
        bT = sb("mg_bT", [128, 3, 4, 512], BF16)
        gt = [sb("mg_gt%d" % i, [128, 512], BF16) for i in range(3)]
        tmp = [sb("mg_tmp%d" % i, [128, 512], F32) for i in range(2)]
        acc = sb("mg_acc", [128, 512], F32)
        ymT = sb("mg_ymT", [128, KT, 512], BF16)
        xt = [sb("mg_xt%d" % i, [128, D], F32) for i in range(2)]
        xo = [sb("mg_xo%d" % i, [128, D], F32) for i in range(2)]
        ptr = [ps("mg_ptr%d" % i, [128, 4, 128], BF16) for i in range(2)]
        pm = [ps("mg_pm%d" % i, [128, 512], F32) for i in range(2)]
        po = [ps("mg_po%d" % i, [128, 512], F32) for i in range(2)]
        srcs = [g.AO, g.NAO, g.SO]
        ntile = T // 128
        nblk = (ntile + 3) // 4
        cn = 0
        for b in range(nblk):
            tiles = [t for t in range(4 * b, min(4 * b + 4, ntile))]
            if not ctx_out:
                tiles = [t for t in tiles if t >= 2]
            if not tiles:
                continue
            tA = tiles[0]
            ntok = len(tiles) * 128
            c0 = tA * 128
            for ti, t in enumerate(tiles):
                for j in range(3):
                    P.dma('sp', bt[j][:], srcs[j][t * 128:(t + 1) * 128, :], reads=['AO', 'NAO', 'SO'], writes=['mg_bt%d' % j])
                    pi = cn % 2; cn += 1
                    for k in range(4):
                        P.op('pe', lambda e, j=j, k=k, pi=pi: e.transpose(out=ptr[pi][:, k, :], in_=bt[j][:, k * 128:(k + 1) * 128], identity=ident[:]),
                             reads=['mg_bt%d' % j, 'mg_ident'], writes=['mg_ptr%d' % pi])
                    P.op('act', lambda e, j=j, ti=ti, pi=pi: e.copy(out=bT[:, j, :, ti * 128:(ti + 1) * 128], in_=ptr[pi][:]),
                         reads=['mg_ptr%d' % pi], writes=['mg_bT'])
            for dc in range(KT):
                for j in range(3):
                    pi = cn % 2; cn += 1
                    P.dma('sp', gt[j][:, :ntok], g.GT[j * D + dc * 128:j * D + (dc + 1) * 128, c0:c0 + ntok], reads=['GT'], writes=['mg_gt%d' % j])
                    for k in range(4):
                        P.op('pe', lambda e, j=j, k=k, pi=pi, dc=dc, ntok=ntok: e.matmul(
                            pm[pi][:, :ntok], lhsT=wbr[:, j, k, dc * 128:(dc + 1) * 128], rhs=bT[:, j, k, :ntok], start=(k == 0), stop=(k == 3)),
                             reads=['mg_wbr', 'mg_bT'], writes=['mg_pm%d' % pi])
                    if j == 0:
                        P.op('dve', lambda e, pi=pi, j=j, ntok=ntok: e.tensor_tensor(out=acc[:, :ntok], in0=pm[pi][:, :ntok], in1=gt[j][:, :ntok], op=ALU.mult),
                             reads=['mg_pm%d' % pi, 'mg_gt%d' % j], writes=['mg_acc'])
                    else:
                        tk = j - 1
                        P.op('dve', lambda e, pi=pi, j=j, tk=tk, ntok=ntok: e.tensor_tensor(out=tmp[tk][:, :ntok], in0=pm[pi][:, :ntok], in1=gt[j][:, :ntok], op=ALU.mult),
                             reads=['mg_pm%d' % pi, 'mg_gt%d' % j], writes=['mg_tmp%d' % tk])
                        if j == 1:
                            P.op('pool', lambda e, tk=tk, ntok=ntok: e.tensor_tensor(out=acc[:, :ntok], in0=acc[:, :ntok], in1=tmp[tk][:, :ntok], op=ALU.add),
                                 reads=['mg_tmp%d' % tk, 'mg_acc'], writes=['mg_acc'])
                        else:
                            P.op('pool', lambda e, tk=tk, ntok=ntok, dc=dc: e.tensor_tensor(out=ymT[:, dc, :ntok], in0=acc[:, :ntok], in1=tmp[tk][:, :ntok], op=ALU.add),
                                 reads=['mg_tmp%d' % tk, 'mg_acc'], writes=['mg_ymT'])
            for ti, t in enumerate(tiles):
                v = 1 if t < 2 else 0
                xi = t % 2
                P.dma('sp', xt[xi][:], x_src[t * 128:(t + 1) * 128, :], reads=[('XR', t)], writes=['mg_xt%d' % xi])
                for hf in range(2):
                    pi = cn % 2; cn += 1
                    for k in range(KT):
                        P.op('pe', lambda e, k=k, pi=pi, ti=ti, hf=hf: e.matmul(
                            po[pi][:, :], lhsT=ymT[:, k, ti * 128:(ti + 1) * 128], rhs=wo[:, k, hf * 512:(hf + 1) * 512], start=(k == 0), stop=(k == KT - 1)),
                             reads=['mg_ymT', 'mg_wo'], writes=['mg_po%d' % pi])
                    P.op('dve', lambda e, pi=pi, xi=xi, hf=hf, v=v: e.tensor_tensor(out=xo[xi][:, hf * 512:(hf + 1) * 512], in0=po[pi][:, :],
                                                                                   in1=g1bc[:, v, hf * 512:(hf + 1) * 512], op=ALU.mult),
                         reads=['mg_po%d' % pi, 'mg_g1bc'], writes=['mg_xo%d' % xi])
                P.op('pool', lambda e, xi=xi: e.tensor_tensor(out=xo[xi][:], in0=xo[xi][:], in1=xt[xi][:], op=ALU.add),
                     reads=['mg_xo%d' % xi, 'mg_xt%d' % xi], writes=['mg_xo%d' % xi])
                P.dma('sp', g.XR[t * 128:(t + 1) * 128, :], xo[xi][:], reads=['mg_xo%d' % xi], writes=[('XR', t)])


CAP = 3072
NE = 32
U32 = mybir.dt.uint32


def phase_moe(nc, P, g, l, with_ctx):
    ntile = T // 128
    tiles = list(range(0 if with_ctx else 2, ntile))
    with ExitStack() as es:
        sb, ps = mk_alloc(nc, es)
        ident = sb("mo_ident", [128, 128], BF16)
        tri = sb("mo_tri", [128, 128], BF16)
        ones = sb("mo_ones", [128, 128], BF16)
        iota = sb("mo_iota", [128, NE], F32)
        P.dma('pool', ident[:], g.c_ident[:, :], writes=['mo_ident'])
        P.dma('pool', tri[:], g.c_tri[:, :], writes=['mo_tri'])
        P.dma('pool', ones[:], g.c_ones[:, :], writes=['mo_ones'])
        P.dma('sp', iota[:], g.c_iota[:, :], writes=['mo_iota'])
        rw = sb("mo_rw", [128, KT, NE], BF16)
        P.dma('pool', rw[:], g.router_w[l, :, :].rearrange("(k p) e -> p k e", p=128), writes=['mo_rw'])
        rb = sb("mo_rb", [128, NE], F32)
        P.dma('sp', rb[:], g.router_b[l, :].partition_broadcast(128), writes=['mo_rb'])
        A2 = sb("mo_A2", [128, 2, D], F32)
        S2 = sb("mo_S2", [128, 2, D], F32)
        G2 = sb("mo_G2", [128, 2, D], F32)
        g2 = sb("mo_g2", [128, D], F32)
        P.dma('sp', g2[:], g.norm2_row[l, :].partition_broadcast(128), writes=['mo_g2'])
        for v in range(2):
            P.dma('sp', A2[:, v, :], g.MOD[l, v, 4 * D:5 * D].partition_broadcast(128), reads=['MOD'], writes=['mo_A2'])
            P.dma('sp', S2[:, v, :], g.MOD[l, v, 3 * D:4 * D].partition_broadcast(128), reads=['MOD'], writes=['mo_S2'])
            P.dma('sp', G2[:, v, :], g.MOD[l, v, 5 * D:6 * D].partition_broadcast(128), reads=['MOD'], writes=['mo_G2'])
            P.op('dve', lambda e, v=v: e.scalar_tensor_tensor(out=A2[:, v, :], in0=A2[:, v, :], scalar=1.0, in1=g2[:], op0=ALU.add, op1=ALU.mult),
                 reads=['mo_A2', 'mo_g2'], writes=['mo_A2'])
        slots = sb("mo_slots", [128, ntile, 4], U32)
        wts = sb("mo_wts", [128, ntile, 4], F32)
        base = sb("mo_base", [128, NE], F32)
        P.op('pool', lambda e: e.memset(base[:], 0.0), writes=['mo_base'])
        xt = [sb("mo_xt%d" % i, [128, D], F32) for i in range(2)]
        junk = sb("mo_junk", [128, D], F32)
        hb = [sb("mo_hb%d" % i, [128, D], BF16) for i in range(2)]
        hT = sb("mo_hT", [128, KT, 128], BF16)
        sm = sb("mo_sm", [128, 16], F32)
        lg = sb("mo_lg", [128, NE], F32)
        mx = sb("mo_mx", [128, 8], F32)
        mi = sb("mo_mi", [128, 8], U32)
        mif = sb("mo_mif", [128, 8], F32)
        ex = sb("mo_ex", [128, 8], F32)
        sel = sb("mo_sel", [128, NE], BF16)
        oh = sb("mo_oh", [128, 4, NE], F32)
        pos = sb("mo_pos", [128, NE], F32)
        pk = sb("mo_pk", [128, 4], F32)
        slf = sb("mo_slf", [128, 4], F32)
        ptr = ps("mo_ptr", [128, KT, 128], BF16)
        plg = ps("mo_plg", [128, 3 * NE], F32)
        for t in tiles:
            v = 1 if t < 2 else 0
            xi = t % 2
            xk, hk = 'mo_xt%d' % xi, 'mo_hb%d' % xi
            P.dma('sp', xt[xi][:], g.XR[t * 128:(t + 1) * 128, :], reads=[('XR', t)], writes=[xk])
            P.op('act', lambda e, xi=xi: e.activation(out=junk[:], in_=xt[xi][:], func=AF.Square, accum_out=sm[:, 0:1]),
                 reads=[xk], writes=['mo_junk', 'mo_sm'])
            P.op('dve', lambda e: e.tensor_scalar(out=sm[:, 0:1], in0=sm[:, 0:1], scalar1=1.0 / D, scalar2=1e-6, op0=ALU.mult, op1=ALU.add),
                 reads=['mo_sm'], writes=['mo_sm'])
            P.op('act', lambda e: e.activation(out=sm[:, 0:1], in_=sm[:, 0:1], func=AF.Sqrt), reads=['mo_sm'], writes=['mo_sm'])
            P.op('dve', lambda e: e.reciprocal(out=sm[:, 0:1], in_=sm[:, 0:1]), reads=['mo_sm'], writes=['mo_sm'])
            P.op('dve', lambda e, xi=xi, v=v: e.scalar_tensor_tensor(out=junk[:], in0=xt[xi][:], scalar=sm[:, 0:1], in1=A2[:, v, :], op0=ALU.mult, op1=ALU.mult),
                 reads=[xk, 'mo_sm', 'mo_A2'], writes=['mo_junk'])
            P.op('pool', lambda e, xi=xi, v=v: e.tensor_tensor(out=hb[xi][:], in0=junk[:], in1=S2[:, v, :], op=ALU.add),
                 reads=['mo_junk', 'mo_S2'], writes=[hk])
            for k in range(KT):
                P.op('pe', lambda e, k=k, xi=xi: e.transpose(out=ptr[:, k, :], in_=hb[xi][:, k * 128:(k + 1) * 128], identity=ident[:]),
                     reads=[hk, 'mo_ident'], writes=['mo_ptr'])
            P.op('act', lambda e: e.copy(out=hT[:], in_=ptr[:]), reads=['mo_ptr'], writes=['mo_hT'])
            for k in range(KT):
                P.op('pe', lambda e, k=k: e.matmul(plg[:, 0:NE], lhsT=hT[:, k, :], rhs=rw[:, k, :], start=(k == 0), stop=(k == KT - 1)),
                     reads=['mo_hT', 'mo_rw'], writes=['mo_plg'])
            P.op('dve', lambda e: e.tensor_tensor(out=lg[:], in0=plg[:, 0:NE], in1=rb[:], op=ALU.add), reads=['mo_plg', 'mo_rb'], writes=['mo_lg'])
            P.op('dve', lambda e: e.max(out=mx[:], in_=lg[:]), reads=['mo_lg'], writes=['mo_mx'])
            P.op('dve', lambda e: e.max_index(out=mi[:], in_max=mx[:], in_values=lg[:]), reads=['mo_lg', 'mo_mx'], writes=['mo_mi'])
            P.op('dve', lambda e: e.tensor_copy(out=mif[:], in_=mi[:]), reads=['mo_mi'], writes=['mo_mif'])
            P.op('dve', lambda e: e.tensor_scalar(out=ex[:, 0:4], in0=mx[:, 0:4], scalar1=mx[:, 0:1], scalar2=None, op0=ALU.subtract),
                 reads=['mo_mx'], writes=['mo_ex'])
            P.op('act', lambda e: e.activation(out=ex[:, 0:4], in_=ex[:, 0:4], func=AF.Exp, accum_out=sm[:, 1:2]), reads=['mo_ex'], writes=['mo_ex', 'mo_sm'])
            P.op('dve', lambda e: e.reciprocal(out=sm[:, 1:2], in_=sm[:, 1:2]), reads=['mo_sm'], writes=['mo_sm'])
            P.op('dve', lambda e, t=t: e.tensor_scalar(out=wts[:, t, :], in0=ex[:, 0:4], scalar1=sm[:, 1:2], scalar2=None, op0=ALU.mult),
                 reads=['mo_ex', 'mo_sm'], writes=['mo_wts'])
            for k in range(4):
                P.op('dve', lambda e, k=k: e.tensor_scalar(out=oh[:, k, :], in0=iota[:], scalar1=mif[:, k:k + 1], scalar2=None, op0=ALU.is_equal),
                     reads=['mo_mif', 'mo_iota'], writes=['mo_oh'])
            P.op('dve', lambda e: e.tensor_tensor(out=pos[:], in0=oh[:, 0, :], in1=oh[:, 1, :], op=ALU.add), reads=['mo_oh'], writes=['mo_pos'])
            P.op('dve', lambda e: e.tensor_tensor(out=pos[:], in0=pos[:], in1=oh[:, 2, :], op=ALU.add), reads=['mo_oh', 'mo_pos'], writes=['mo_pos'])
            P.op('dve', lambda e: e.tensor_tensor(out=sel[:], in0=pos[:], in1=oh[:, 3, :], op=ALU.add), reads=['mo_oh', 'mo_pos'], writes=['mo_sel'])
            P.op('pe', lambda e: e.matmul(plg[:, NE:2 * NE], lhsT=tri[:], rhs=sel[:], start=True, stop=True), reads=['mo_tri', 'mo_sel'], writes=['mo_plg2'])
            P.op('pe', lambda e: e.matmul(plg[:, 2 * NE:3 * NE], lhsT=ones[:], rhs=sel[:], start=True, stop=True), reads=['mo_ones', 'mo_sel'], writes=['mo_plg2'])
            P.op('dve', lambda e: e.tensor_tensor(out=pos[:], in0=plg[:, NE:2 * NE], in1=base[:], op=ALU.add), reads=['mo_plg2', 'mo_base'], writes=['mo_pos'])
            P.op('dve', lambda e: e.tensor_tensor(out=base[:], in0=plg[:, 2 * NE:3 * NE], in1=base[:], op=ALU.add), reads=['mo_plg2', 'mo_base'], writes=['mo_base'])
            for k in range(4):
                P.op('dve', lambda e, k=k: e.tensor_tensor(out=oh[:, k, :], in0=oh[:, k, :], in1=pos[:], op=ALU.mult), reads=['mo_oh', 'mo_pos'], writes=['mo_oh'])
            P.op('dve', lambda e: e.tensor_reduce(out=pk[:], in_=oh[:], axis=AX.X, op=ALU.add), reads=['mo_oh'], writes=['mo_pk'])
            P.op('dve', lambda e: e.scalar_tensor_tensor(out=slf[:], in0=mif[:, 0:4], scalar=float(CAP), in1=pk[:], op0=ALU.mult, op1=ALU.add),
                 reads=['mo_mif', 'mo_pk'], writes=['mo_slf'])
            P.op('dve', lambda e, t=t: e.tensor_copy(out=slots[:, t, :], in_=slf[:]), reads=['mo_slf'], writes=['mo_slots'])
            for k in range(4):
                fn = lambda e, t=t, k=k, xi=xi: e.indirect_dma_start(
                    out=g.XS[:, :], out_offset=bass.IndirectOffsetOnAxis(ap=slots[:, t, k:k + 1], axis=0),
                    in_=hb[xi][:], in_offset=None)
                P.dma_fn('pool', fn, reads=['mo_slots', hk], writes=[('XS', t, k)])
        P.barrier()
        gu = sb("mo_gu", [128, KT, 2 * D], BF16)
        dn = sb("mo_dn", [128, KT, D], BF16)
        gub = sb("mo_gub", [128, 16], F32)
        dnb = sb("mo_dnb", [128, D], F32)
        xs = [sb("mo_xs%d" % i, [128, D], BF16) for i in range(2)]
        XeT = sb("mo_XeT", [128, KT, 512], BF16)
        actT = sb("mo_actT", [128, KT, 512], BF16)
        tg = [sb("mo_tg%d" % i, [128, 512], F32) for i in range(2)]
        tsg = [sb("mo_tsg%d" % i, [128, 512], F32) for i in range(2)]
        tl = [sb("mo_tl%d" % i, [128, 512], F32) for i in range(2)]
        ysb = [sb("mo_ysb%d" % i, [128, D], BF16) for i in range(2)]
        pg = ps("mo_pg", [128, 512], F32)
        pl = ps("mo_pl", [128, 512], F32)
        pdn = [ps("mo_pdn%d" % i, [128, 512], F32) for i in range(2)]
        cn = 0
        for e_ in range(NE):
            for k in range(KT):
                P.dma('pool', gu[:, k, :], g.expert_gu_w[l, e_, k * 128:(k + 1) * 128, :], writes=['mo_gu'])
            P.dma('pool', dn[:], g.expert_dn_w[l, e_, :, :].rearrange("(k p) n -> p k n", p=128), writes=['mo_dn'])
            P.dma('sp', gub[:], g.gu_b[l, e_, :, :], writes=['mo_gub'])
            P.dma('sp', dnb[:], g.expert_dn_b[l, e_, :].partition_broadcast(128), writes=['mo_dnb'])
            for (s0, ns) in [(i * 512, 512) for i in range(CAP // 512)]:
                nst = ns // 128
                for st in range(nst):
                    xi = cn % 2; cn += 1
                    r0 = e_ * CAP + s0 + st * 128
                    P.dma('sp', xs[xi][:], g.XS[r0:r0 + 128, :], writes=['mo_xs%d' % xi])
                    for k in range(KT):
                        P.op('pe', lambda e, k=k, xi=xi: e.transpose(out=ptr[:, k, :], in_=xs[xi][:, k * 128:(k + 1) * 128], identity=ident[:]),
                             reads=['mo_xs%d' % xi, 'mo_ident'], writes=['mo_ptr'])
                    P.op('act', lambda e, st=st: e.copy(out=XeT[:, :, st * 128:(st + 1) * 128], in_=ptr[:]), reads=['mo_ptr'], writes=['mo_XeT'])
                for fc in range(KT):
                    i2 = fc % 2
                    for k in range(KT):
                        P.op('pe', lambda e, k=k, fc=fc, ns=ns: e.matmul(pg[:, :ns], lhsT=gu[:, k, fc * 128:(fc + 1) * 128], rhs=XeT[:, k, :ns],
                                                                     start=(k == 0), stop=(k == KT - 1)), reads=['mo_gu', 'mo_XeT'], writes=['mo_pg'])
                    for k in range(KT):
                        P.op('pe', lambda e, k=k, fc=fc, ns=ns: e.matmul(pl[:, :ns], lhsT=gu[:, k, D + fc * 128:D + (fc + 1) * 128], rhs=XeT[:, k, :ns],
                                                                     start=(k == 0), stop=(k == KT - 1)), reads=['mo_gu', 'mo_XeT'], writes=['mo_pl'])
                    P.op('dve', lambda e, fc=fc, i2=i2, ns=ns: e.tensor_scalar(out=tg[i2][:, :ns], in0=pg[:, :ns], scalar1=gub[:, fc:fc + 1], scalar2=7.0,
                                                                            op0=ALU.add, op1=ALU.min), reads=['mo_pg', 'mo_gub'], writes=['mo_tg%d' % i2])
                    P.op('act', lambda e, i2=i2, ns=ns: e.activation(out=tsg[i2][:, :ns], in_=tg[i2][:, :ns], func=AF.Sigmoid, scale=1.702),
                         reads=['mo_tg%d' % i2], writes=['mo_tsg%d' % i2])
                    P.op('dve', lambda e, fc=fc, i2=i2, ns=ns: e.tensor_scalar(out=tl[i2][:, :ns], in0=pl[:, :ns], scalar1=gub[:, 8 + fc:9 + fc], scalar2=7.0,
                                                                            op0=ALU.add, op1=ALU.min), reads=['mo_pl', 'mo_gub'], writes=['mo_tl%d' % i2])
                    P.op('pool', lambda e, i2=i2, ns=ns: e.tensor_scalar(out=tl[i2][:, :ns], in0=tl[i2][:, :ns], scalar1=-7.0, scalar2=1.0,
                                                                      op0=ALU.max, op1=ALU.add), reads=['mo_tl%d' % i2], writes=['mo_tl%d' % i2])
                    P.op('pool', lambda e, i2=i2, ns=ns: e.tensor_tensor(out=tg[i2][:, :ns], in0=tg[i2][:, :ns], in1=tsg[i2][:, :ns], op=ALU.mult),
                         reads=['mo_tg%d' % i2, 'mo_tsg%d' % i2], writes=['mo_tg%d' % i2])
                    P.op('pool', lambda e, i2=i2, fc=fc, ns=ns: e.tensor_tensor(out=actT[:, fc, :ns], in0=tg[i2][:, :ns], in1=tl[i2][:, :ns], op=ALU.mult),
                         reads=['mo_tg%d' % i2, 'mo_tl%d' % i2], writes=['mo_actT'])
                for st in range(nst):
                    yi = cn % 2; cn += 1
                    for hf in range(2):
                        for k in range(KT):
                            P.op('pe', lambda e, k=k, st=st, hf=hf: e.matmul(pdn[hf][:, :], lhsT=actT[:, k, st * 128:(st + 1) * 128], rhs=dn[:, k, hf * 512:(hf + 1) * 512],
                                                                             start=(k == 0), stop=(k == KT - 1)), reads=['mo_actT', 'mo_dn'], writes=['mo_pdn%d' % hf])
                        P.op('dve', lambda e, yi=yi, hf=hf: e.tensor_tensor(out=ysb[yi][:, hf * 512:(hf + 1) * 512], in0=pdn[hf][:, :], in1=dnb[:, hf * 512:(hf + 1) * 512], op=ALU.add),
                             reads=['mo_pdn%d' % hf, 'mo_dnb'], writes=['mo_ysb%d' % yi])
                    r0 = e_ * CAP + s0 + st * 128
                    P.dma('sp', g.YS[r0:r0 + 128, :], ysb[yi][:], reads=['mo_ysb%d' % yi], writes=[('YS', r0)])
        P.barrier()
        if getattr(g, "dbg_slots", None) is not None:
            P.dma('sp', g.dbg_slots, slots[:], reads=['mo_slots'], writes=['dbg_slots'])
            P.dma('sp', g.dbg_wts, wts[:], reads=['mo_wts'], writes=['dbg_wts'])
        yk = [sb("mo_yk%d" % i, [128, D], BF16) for i in range(4)]
        acc = [sb("mo_acc%d" % i, [128, D], F32) for i in range(2)]
        for t in tiles:
            v = 1 if t < 2 else 0
            ai = t % 2
            for k in range(4):
                fn = lambda e, t=t, k=k: e.indirect_dma_start(
                    out=yk[k][:], out_offset=None, in_=g.YS[:, :],
                    in_offset=bass.IndirectOffsetOnAxis(ap=slots[:, t, k:k + 1], axis=0))
                P.dma_fn('pool', fn, reads=['mo_slots'], writes=['mo_yk%d' % k])
            P.dma('sp', xt[ai][:], g.XR[t * 128:(t + 1) * 128, :], reads=[('XR', t)], writes=['mo_xt%d' % ai])
            P.op('dve', lambda e, t=t, ai=ai: e.tensor_scalar(out=acc[ai][:], in0=yk[0][:], scalar1=wts[:, t, 0:1], scalar2=None, op0=ALU.mult),
                 reads=['mo_yk0', 'mo_wts'], writes=['mo_acc%d' % ai])
            for k in range(1, 4):
                eng = 'dve'
                P.op(eng, lambda e, t=t, k=k, ai=ai: e.scalar_tensor_tensor(out=acc[ai][:], in0=yk[k][:], scalar=wts[:, t, k:k + 1], in1=acc[ai][:], op0=ALU.mult, op1=ALU.add),
                     reads=['mo_yk%d' % k, 'mo_wts', 'mo_acc%d' % ai], writes=['mo_acc%d' % ai])
            P.op('dve', lambda e, ai=ai, v=v: e.tensor_tensor(out=acc[ai][:], in0=acc[ai][:], in1=G2[:, v, :], op=ALU.mult), reads=['mo_acc%d' % ai, 'mo_G2'], writes=['mo_acc%d' % ai])
            P.op('pool', lambda e, ai=ai: e.tensor_tensor(out=acc[ai][:], in0=acc[ai][:], in1=xt[ai][:], op=ALU.add), reads=['mo_acc%d' % ai, 'mo_xt%d' % ai], writes=['mo_acc%d' % ai])
            P.dma('sp', g.XR[t * 128:(t + 1) * 128, :], acc[ai][:], reads=['mo_acc%d' % ai], writes=[('XR', t)])


def phase_final(nc, P, g, out_ap):
    with ExitStack() as es:
        sb, ps = mk_alloc(nc, es)
        fg = sb("fn_g", [128, D], F32)
        P.dma('sp', fg[:], g.final_row[0, :].partition_broadcast(128), writes=['fn_g'])
        xt = [sb("fn_xt%d" % i, [128, D], F32) for i in range(2)]
        yo = [sb("fn_yo%d" % i, [128, D], F32) for i in range(2)]
        junk = sb("fn_junk", [128, D], F32)
        sm = sb("fn_sm", [128, 2], F32)
        for t in range(2, T // 128):
            i = t % 2
            P.dma('sp', xt[i][:], g.XR[t * 128:(t + 1) * 128, :], reads=[('XR', t)], writes=['fn_xt%d' % i])
            P.op('act', lambda e, i=i: e.activation(out=junk[:], in_=xt[i][:], func=AF.Square, accum_out=sm[:, i:i + 1]), reads=['fn_xt%d' % i], writes=['fn_junk', 'fn_sm%d' % i])
            P.op('dve', lambda e, i=i: e.tensor_scalar(out=sm[:, i:i + 1], in0=sm[:, i:i + 1], scalar1=1.0 / D, scalar2=1e-6, op0=ALU.mult, op1=ALU.add), reads=['fn_sm%d' % i], writes=['fn_sm%d' % i])
            P.op('act', lambda e, i=i: e.activation(out=sm[:, i:i + 1], in_=sm[:, i:i + 1], func=AF.Sqrt), reads=['fn_sm%d' % i], writes=['fn_sm%d' % i])
            P.op('dve', lambda e, i=i: e.reciprocal(out=sm[:, i:i + 1], in_=sm[:, i:i + 1]), reads=['fn_sm%d' % i], writes=['fn_sm%d' % i])
            P.op('dve', lambda e, i=i: e.scalar_tensor_tensor(out=yo[i][:], in0=xt[i][:], scalar=sm[:, i:i + 1], in1=fg[:], op0=ALU.mult, op1=ALU.mult),
                 reads=['fn_xt%d' % i, 'fn_sm%d' % i, 'fn_g'], writes=['fn_yo%d' % i])
            P.dma('sp', out_ap[(t - 2) * 128:(t - 1) * 128, :], yo[i][:], reads=['fn_yo%d' % i], writes=[('out', t)])


def build_full():
    nc = bass.Bass("TRN2", target_bir_lowering=False)
    g = declare_io(nc, "full")
    out = nc.dram_tensor("out", [SEQ, D], F32, kind="ExternalOutput").ap()
    with ExitStack() as es:
        P = Prog(nc, es)
        phase0(nc, P, g, es)
        P.barrier()
        for l in range(DEPTH):
            last = (l == DEPTH - 1)
            x_src = g.xin if l == 0 else g.XR
            phase1(nc, P, g, l, x_src)
            P.barrier()
            phase_rwkv(nc, P, g, l)
            P.barrier()
            phase_s5(nc, P, g, l, not last)
            P.barrier()
            phase_s5_readout(nc, P, g, l)
            P.barrier()
            phase_na(nc, P, g, l, not last)
            P.barrier()
            phase_merge(nc, P, g, l, x_src, not last)
            P.barrier()
            phase_moe(nc, P, g, l, not last)
            P.barrier()
        phase_final(nc, P, g, out)
        P.wait_all('sp')
        P.emit()
    return nc


def kernel(**inputs):
    inputs = {k: np.asarray(v) for k, v in inputs.items()}
    nc = build_full()
    shared = None
    in_maps = []
    for core in range(8):
        b = core % 4
        d = host_inputs(inputs, b) if shared is None else dict(shared)
        if shared is None:
            shared = dict(d)
        else:
            f = lambda a: np.ascontiguousarray(a, dtype=np.float32)
            d["xin"] = f(np.concatenate([inputs["ctx"][b], inputs["x"][b]], axis=0))
            cl = inputs["c"][b].reshape(KT, 128).T
            cc = inputs["c_ctx"].reshape(KT, 128).T
            d["cvec"] = f(np.concatenate([cl, cc], axis=1))
        in_maps.append(d)
    res = run_bass_kernel_spmd(nc, in_maps, core_ids=list(range(8)))
    out = np.stack([np.asarray(res.results[b]["out"], dtype=np.float32) for b in range(4)], axis=0)
    return out


NJ = T // 8
PI = float(np.pi)


def sl_(start, count, step):
    if step > 0:
        return slice(start, start + step * (count - 1) + 1, step)
    stop = start + step * (count - 1) - 1
    return slice(start, stop if stop >= 0 else None, step)


def phase_s5(nc, P, g, l, ctx_out):
    with ExitStack() as es:
        sb, ps = mk_alloc(nc, es)
        TAUS = list(range(9)) + [64, 256, 768]
        NTAU = len(TAUS)
        LR = sb("s5_LR", [128, 64], F32); LI = sb("s5_LI", [128, 64], F32); DT = sb("s5_DT", [128, 64], F32)
        P.dma('sp', LR[:], g.s5_lr[l], writes=['s5_LR']); P.dma('sp', LI[:], g.s5_li[l], writes=['s5_LI'])
        P.dma('sp', DT[:], g.s5_ldt[l], writes=['s5_DT'])
        P.op('act', lambda e: e.activation(out=DT[:], in_=DT[:], func=AF.Exp), reads=['s5_DT'], writes=['s5_DT'])
        RD = sb("s5_RD", [128, 64], F32); IDt = sb("s5_ID", [128, 64], F32)
        P.op('dve', lambda e: e.tensor_tensor(out=RD[:], in0=LR[:], in1=DT[:], op=ALU.mult), reads=['s5_LR', 's5_DT'], writes=['s5_RD'])
        P.op('dve', lambda e: e.tensor_tensor(out=IDt[:], in0=LI[:], in1=DT[:], op=ALU.mult), reads=['s5_LI', 's5_DT'], writes=['s5_ID'])
        AR = sb("s5_AR", [128, NTAU, 64], F32); AI = sb("s5_AI", [128, NTAU, 64], F32); NAI = sb("s5_NAI", [128, NTAU, 64], F32)
        tmp = sb("s5_tmp", [128, 64], F32); tmp2 = sb("s5_tmp2", [128, 64], F32); mag = sb("s5_mag", [128, 64], F32)
        ki = sb("s5_ki", [128, 64], I32)
        for ti, tau in enumerate(TAUS):
            P.op('act', lambda e, tau=tau: e.activation(out=mag[:], in_=RD[:], func=AF.Exp, scale=float(tau)), reads=['s5_RD'], writes=['s5_mag'])
            for which, shift in (("sin", PI), ("cos", 1.5 * PI)):
                P.op('dve', lambda e, tau=tau, shift=shift: e.tensor_scalar(out=tmp[:], in0=IDt[:], scalar1=float(tau), scalar2=shift - PI, op0=ALU.mult, op1=ALU.add),
                     reads=['s5_ID'], writes=['s5_tmp'])
                P.op('dve', lambda e: e.tensor_scalar(out=tmp2[:], in0=tmp[:], scalar1=1.0 / (2 * PI), scalar2=None, op0=ALU.mult), reads=['s5_tmp'], writes=['s5_tmp2'])
                P.op('dve', lambda e: e.tensor_copy(out=ki[:], in_=tmp2[:]), reads=['s5_tmp2'], writes=['s5_ki'])
                P.op('dve', lambda e: e.tensor_copy(out=tmp2[:], in_=ki[:]), reads=['s5_ki'], writes=['s5_tmp2'])
                P.op('dve', lambda e: e.scalar_tensor_tensor(out=tmp[:], in0=tmp2[:], scalar=-2 * PI, in1=tmp[:], op0=ALU.mult, op1=ALU.add), reads=['s5_tmp2', 's5_tmp'], writes=['s5_tmp'])
                P.op('dve', lambda e: e.tensor_scalar(out=tmp2[:], in0=tmp[:], scalar1=PI, scalar2=-2 * PI, op0=ALU.is_gt, op1=ALU.mult), reads=['s5_tmp'], writes=['s5_tmp2'])
                P.op('dve', lambda e: e.tensor_tensor(out=tmp[:], in0=tmp[:], in1=tmp2[:], op=ALU.add), reads=['s5_tmp', 's5_tmp2'], writes=['s5_tmp'])
                P.op('dve', lambda e: e.tensor_scalar(out=tmp2[:], in0=tmp[:], scalar1=-PI, scalar2=2 * PI, op0=ALU.is_lt, op1=ALU.mult), reads=['s5_tmp'], writes=['s5_tmp2'])
                P.op('dve', lambda e: e.tensor_tensor(out=tmp[:], in0=tmp[:], in1=tmp2[:], op=ALU.add), reads=['s5_tmp', 's5_tmp2'], writes=['s5_tmp'])
                P.op('act', lambda e: e.activation(out=tmp2[:], in_=tmp[:], func=AF.Sin), reads=['s5_tmp'], writes=['s5_tmp2'])
                dst = AI if which == "sin" else AR
                P.op('dve', lambda e, dst=dst, ti=ti: e.tensor_tensor(out=dst[:, ti, :], in0=tmp2[:], in1=mag[:], op=ALU.mult),
                     reads=['s5_tmp2', 's5_mag'], writes=['s5_A'])
        P.op('dve', lambda e: e.tensor_scalar(out=NAI[:], in0=AI[:], scalar1=-1.0, scalar2=None, op0=ALU.mult), reads=['s5_A'], writes=['s5_NAI'])
        S1 = sb("s5_S1", [128, NTAU, 64], F32); S2 = sb("s5_S2", [128, NTAU, 64], F32)
        T1 = sb("s5_T1", [128, NTAU, 64], F32); T2 = sb("s5_T2", [128, NTAU, 64], F32)
        NAR = sb("s5_NAR", [128, NTAU, 64], F32)
        P.op('dve', lambda e: e.tensor_scalar(out=NAR[:], in0=AR[:], scalar1=-1.0, scalar2=None, op0=ALU.mult), reads=['s5_A'], writes=['s5_NAR'])
        T3 = sb("s5_T3", [128, NTAU, 64], F32)
        for (dstt, top, bot) in ((S1, AR, NAI), (S2, NAI, NAR), (T1, AR, AI), (T2, NAI, AR), (T3, AI, AR)):
            P.op('pool', lambda e, dstt=dstt, top=top: e.tensor_copy(out=dstt[0:64], in_=top[0:64]), reads=['s5_A', 's5_NAI', 's5_NAR'], writes=['s5_ST'])
            P.op('pool', lambda e, dstt=dstt, bot=bot: e.tensor_copy(out=dstt[64:128], in_=bot[64:128]), reads=['s5_A', 's5_NAI', 's5_NAR'], writes=['s5_ST'])
        den = sb("s5_den", [128, 64], F32); cr = sb("s5_cr", [128, 64], F32); ci = sb("s5_ci", [128, 64], F32); am1 = sb("s5_am1", [128, 64], F32)
        P.op('dve', lambda e: e.tensor_tensor(out=den[:], in0=LR[:], in1=LR[:], op=ALU.mult), reads=['s5_LR'], writes=['s5_den'])
        P.op('dve', lambda e: e.tensor_tensor(out=tmp[:], in0=LI[:], in1=LI[:], op=ALU.mult), reads=['s5_LI'], writes=['s5_tmp'])
        P.op('dve', lambda e: e.tensor_tensor(out=den[:], in0=den[:], in1=tmp[:], op=ALU.add), reads=['s5_den', 's5_tmp'], writes=['s5_den'])
        P.op('dve', lambda e: e.reciprocal(out=den[:], in_=den[:]), reads=['s5_den'], writes=['s5_den'])
        P.op('dve', lambda e: e.tensor_scalar(out=am1[:], in0=AR[:, 1, :], scalar1=-1.0, scalar2=None, op0=ALU.add), reads=['s5_A'], writes=['s5_am1'])
        P.op('dve', lambda e: e.tensor_tensor(out=cr[:], in0=am1[:], in1=LR[:], op=ALU.mult), reads=['s5_am1', 's5_LR'], writes=['s5_cr'])
        P.op('dve', lambda e: e.tensor_tensor(out=tmp[:], in0=AI[:, 1, :], in1=LI[:], op=ALU.mult), reads=['s5_A', 's5_LI'], writes=['s5_tmp'])
        P.op('dve', lambda e: e.tensor_tensor(out=cr[:], in0=cr[:], in1=tmp[:], op=ALU.add), reads=['s5_cr', 's5_tmp'], writes=['s5_cr'])
        P.op('dve', lambda e: e.tensor_tensor(out=cr[:], in0=cr[:], in1=den[:], op=ALU.mult), reads=['s5_cr', 's5_den'], writes=['s5_cr'])
        P.op('dve', lambda e: e.tensor_tensor(out=ci[:], in0=AI[:, 1, :], in1=LR[:], op=ALU.mult), reads=['s5_A', 's5_LR'], writes=['s5_ci'])
        P.op('dve', lambda e: e.tensor_tensor(out=tmp[:], in0=am1[:], in1=LI[:], op=ALU.mult), reads=['s5_am1', 's5_LI'], writes=['s5_tmp'])
        P.op('dve', lambda e: e.tensor_tensor(out=ci[:], in0=ci[:], in1=tmp[:], op=ALU.subtract), reads=['s5_ci', 's5_tmp'], writes=['s5_ci'])
        P.op('dve', lambda e: e.tensor_tensor(out=ci[:], in0=ci[:], in1=den[:], op=ALU.mult), reads=['s5_ci', 's5_den'], writes=['s5_ci'])
        BR = sb("s5_BR", [128, 64, 16], F32); BI = sb("s5_BI", [128, 64, 16], F32)
        CR = sb("s5_CR", [128, 64, 16], F32); CI = sb("s5_CI", [128, 64, 16], F32)
        P.dma('sp', BR[:], g.s5_br[l], writes=['s5_BR']); P.dma('sp', BI[:], g.s5_bi[l], writes=['s5_BI'])
        P.dma('sp', CR[:], g.s5_cr[l], writes=['s5_CR']); P.dma('sp', CI[:], g.s5_ci[l], writes=['s5_CI'])
        BBR = sb("s5_BBR", [128, 64, 16], F32); BBI = sb("s5_BBI", [128, 64, 16], F32); big = sb("s5_big", [128, 64, 16], F32)
        bc = lambda t2: t2[:].unsqueeze(2).to_broadcast([128, 64, 16])
        P.op('dve', lambda e: e.tensor_tensor(out=BBR[:], in0=BR[:], in1=bc(cr), op=ALU.mult), reads=['s5_BR', 's5_cr'], writes=['s5_BBR'])
        P.op('dve', lambda e: e.tensor_tensor(out=big[:], in0=BI[:], in1=bc(ci), op=ALU.mult), reads=['s5_BI', 's5_ci'], writes=['s5_big'])
        P.op('dve', lambda e: e.tensor_tensor(out=BBR[:], in0=BBR[:], in1=big[:], op=ALU.subtract), reads=['s5_BBR', 's5_big'], writes=['s5_BBR'])
        P.op('dve', lambda e: e.tensor_tensor(out=BBI[:], in0=BI[:], in1=bc(cr), op=ALU.mult), reads=['s5_BI', 's5_cr'], writes=['s5_BBI'])
        P.op('dve', lambda e: e.tensor_tensor(out=big[:], in0=BR[:], in1=bc(ci), op=ALU.mult), reads=['s5_BR', 's5_ci', 's5_BBR'], writes=['s5_big'])
        P.op('dve', lambda e: e.tensor_tensor(out=BBI[:], in0=BBI[:], in1=big[:], op=ALU.add), reads=['s5_BBI', 's5_big'], writes=['s5_BBI'])
        CA = sb("s5_CA", [128, 9, 64, 16], F32); GG = sb("s5_GG", [128, 8, 64, 16], F32)
        bct = lambda tb, ti: tb[:, ti, :].unsqueeze(2).to_broadcast([128, 64, 16])
        for ti in range(9):
            P.op('dve', lambda e, ti=ti: e.tensor_tensor(out=CA[:, ti], in0=CR[:], in1=bct(S1, ti), op=ALU.mult), reads=['s5_CR', 's5_ST'], writes=['s5_CA'])
            P.op('pool', lambda e, ti=ti: e.tensor_tensor(out=big[:], in0=CI[:], in1=bct(S2, ti), op=ALU.mult), reads=['s5_CI', 's5_ST', 's5_BBI', 's5_CA'], writes=['s5_big'])
            P.op('dve', lambda e, ti=ti: e.tensor_tensor(out=CA[:, ti], in0=CA[:, ti], in1=big[:], op=ALU.add), reads=['s5_big', 's5_CA'], writes=['s5_CA'])
        for ti in range(8):
            P.op('dve', lambda e, ti=ti: e.tensor_tensor(out=GG[:, ti], in0=BBR[:], in1=bct(T1, ti), op=ALU.mult), reads=['s5_BBR', 's5_ST'], writes=['s5_GG'])
            P.op('pool', lambda e, ti=ti: e.tensor_tensor(out=big[:], in0=BBI[:], in1=bct(T2, ti), op=ALU.mult), reads=['s5_BBI', 's5_ST', 's5_CA', 's5_GG'], writes=['s5_big'])
            P.op('dve', lambda e, ti=ti: e.tensor_tensor(out=GG[:, ti], in0=GG[:, ti], in1=big[:], op=ALU.add), reads=['s5_big', 's5_GG'], writes=['s5_GG'])
        identf = sb("s5_identf", [128, 128], F32); identb = sb("s5_identb", [128, 128], BF16); II = sb("s5_II", [128, 128], F32)
        DV = sb("s5_DV", [128, 32], F32)
        P.dma('sp', identf[:], g.c_ident[:, :], writes=['s5_identf']); P.dma('pool', identb[:], g.c_ident[:, :], writes=['s5_identb'])
        P.dma('sp', II[:], g.c_ii[:, :], writes=['s5_II']); P.dma('sp', DV[:], g.s5_dv[l], writes=['s5_DV'])
        BP = sb("s5_BP", [128, 15, 16], F32); CP = sb("s5_CP", [128, 15, 16], F32)
        P.op('pool', lambda e: e.memset(BP[:], 0.0), writes=['s5_BP']); P.op('pool', lambda e: e.memset(CP[:], 0.0), writes=['s5_CP'])
        Mi = [sb("s5_Mi%d" % i, [128, 128], BF16) for i in range(2)]
        MV = [sb("s5_MV%d" % i, [128, 128], BF16) for i in range(2)]
        MY = [sb("s5_MY%d" % i, [128, 8, 16], F32) for i in range(2)]
        GT_ = [sb("s5_GTt%d" % i, [128, 8, 16], F32) for i in range(2)]
        Am = [sb("s5_Am%d" % i, [128, 4, 128], F32) for i in range(2)]
        U = [sb("s5_U%d" % i, [128, NJ + 32], BF16) for i in range(2)]
        Z0 = sb("s5_Z0", [128, NJ], F32); Z1 = sb("s5_Z1", [128, 132], F32); Z2 = sb("s5_Z2", [128, 33], F32); Z3 = sb("s5_Z3", [128, 11], F32); P4 = sb("s5_P4", [128, 12], F32)
        acc = [sb("s5_acc%d" % i, [128, 132], F32) for i in range(2)]
        P3 = sb("s5_P3", [128, 34], F32); P2 = sb("s5_P2", [128, 132], F32); P1 = sb("s5_P1", [128, NJ], F32)
        Y = [sb("s5_Y%d" % i, [128, NJ], F32) for i in range(2)]
        Ys = [sb("s5_Ys%d" % i, [128, NJ], F32) for i in range(2)]
        pm = [ps("s5_pm%d" % i, [128, 128], F32) for i in range(2)]
        pz = [ps("s5_pz%d" % i, [128, 352], F32) for i in range(3)]
        pv = ps("s5_pv", [128, 128], F32)
        pt_ = ps("s5_pt", [128, 4], F32)
        it = 0
        for gi in range(32):
            for d in range(2):
                dg = d * 32 + gi
                b = it % 2; it += 1
                kb = '_%d' % b
                P.op('dve', lambda e, dg=dg: e.tensor_copy(out=BP[:, 7, :], in_=GG[:, 0, dg, :]), reads=['s5_GG'], writes=['s5_BP'])
                if d == 0:
                    P.op('dve', lambda e, dg=dg: e.tensor_copy(out=CP[:, 7:15, :], in_=CA[:, 0:8, dg, :]), reads=['s5_CA'], writes=['s5_CP'])
                    P.op('pool', lambda e: e.memset(CP[:, 0:7, :], 0.0), writes=['s5_CP'])
                else:
                    P.op('dve', lambda e, dg=dg: e.tensor_copy(out=CP[:, 0:8, :], in_=CA[:, 7::-1, dg, :]), reads=['s5_CA'], writes=['s5_CP'])
                    P.op('pool', lambda e: e.memset(CP[:, 8:15, :], 0.0), writes=['s5_CP'])
                for s in range(8):
                    P.op('pe', lambda e, s=s, b=b: e.matmul(pm[b][:, :], lhsT=BP[:, 7 - s:15 - s, :], rhs=CP[:, 7 - s:15 - s, :], start=(s == 0), stop=(s == 7)),
                         reads=['s5_BP', 's5_CP'], writes=['s5_pm' + kb])
                if d == 0:
                    P.op('dve', lambda e, b=b, gi=gi: e.scalar_tensor_tensor(out=Mi[b][:], in0=identf[:], scalar=DV[:, gi:gi + 1], in1=pm[b][:], op0=ALU.mult, op1=ALU.add),
                         reads=['s5_pm' + kb, 's5_identf', 's5_DV'], writes=['s5_Mi' + kb])
                else:
                    P.op('dve', lambda e, b=b: e.tensor_copy(out=Mi[b][:], in_=pm[b][:]), reads=['s5_pm' + kb], writes=['s5_Mi' + kb])
                if d == 0:
                    P.op('dve', lambda e, b=b, dg=dg: e.tensor_copy(out=GT_[b][:], in_=GG[:, 7::-1, dg, :]), reads=['s5_GG'], writes=['s5_GTt' + kb])
                else:
                    P.op('dve', lambda e, b=b, dg=dg: e.tensor_copy(out=GT_[b][:], in_=GG[:, 0:8, dg, :]), reads=['s5_GG'], writes=['s5_GTt' + kb])
                P.op('pe', lambda e, b=b: e.matmul(pv[:, :], lhsT=GT_[b][:], rhs=identf[:], start=True, stop=True), reads=['s5_GTt' + kb, 's5_identf'], writes=['s5_pv'])
                P.op('act', lambda e, b=b: e.copy(out=MV[b][:], in_=pv[:]), reads=['s5_pv'], writes=['s5_MV' + kb])
                if d == 0:
                    P.op('pool', lambda e, b=b, dg=dg: e.tensor_copy(out=MY[b][:], in_=CA[:, 1:9, dg, :]), reads=['s5_CA'], writes=['s5_MY' + kb])
                else:
                    P.op('pool', lambda e, b=b, dg=dg: e.tensor_copy(out=MY[b][:], in_=CA[:, 8:0:-1, dg, :]), reads=['s5_CA'], writes=['s5_MY' + kb])
                for li, ti in enumerate((8, 9, 10, 11)):
                    P.op('dve', lambda e, b=b, li=li, ti=ti, dg=dg: e.tensor_scalar(out=Am[b][:, li, 0:64], in0=II[:, 0:64], scalar1=S1[:, ti, dg:dg + 1], scalar2=None, op0=ALU.mult),
                         reads=['s5_II', 's5_ST'], writes=['s5_Am' + kb])
                    P.op('dve', lambda e, b=b, li=li, ti=ti, dg=dg: e.tensor_scalar(out=Am[b][:, li, 64:128], in0=II[:, 64:128], scalar1=T3[:, ti, dg:dg + 1], scalar2=None, op0=ALU.mult),
                         reads=['s5_II', 's5_ST'], writes=['s5_Am' + kb])
                j0 = 0 if d == 0 else 32
                for i8 in range(8):
                    P.dma('pool', U[b][i8 * 16:(i8 + 1) * 16, 0:NJ], g.UB[i8, gi * 16:(gi + 1) * 16, j0:j0 + NJ], reads=['UB'], writes=['s5_U' + kb])
                Uv = (lambda c0, n, b=b: U[b][:, c0:c0 + n]) if d == 0 else (lambda c0, n, b=b: U[b][:, sl_(NJ - 1 - c0, n, -1)])
                for pc in range(3):
                    P.op('pe', lambda e, b=b, pc=pc, Uv=Uv: e.matmul(pz[pc][:, :], lhsT=MV[b][:], rhs=Uv(pc * 352, 352), start=True, stop=True),
                         reads=['s5_MV' + kb, 's5_U' + kb], writes=['s5_pz%d' % pc])
                    eng = 'act' if pc == 1 else 'dve'
                    if eng == 'act':
                        P.op('act', lambda e, pc=pc: e.copy(out=Z0[:, pc * 352:(pc + 1) * 352], in_=pz[pc][:, :]), reads=['s5_pz%d' % pc], writes=['s5_Z0'])
                    else:
                        P.op('dve', lambda e, pc=pc: e.tensor_copy(out=Z0[:, pc * 352:(pc + 1) * 352], in_=pz[pc][:, :]), reads=['s5_pz%d' % pc], writes=['s5_Z0'])

                def horner(src, R, M, A, dst, tag):
                    cur = None
                    for n in range(1, R):
                        prev = src[:, sl_(0, M, R)] if n == 1 else cur
                        prevk = tag if n == 1 else 's5_acc%d' % ((n - 1) % 2)
                        last = (n == R - 1)
                        o = dst if last else acc[n % 2][:, 0:M]
                        ok = ('s5_dst' + tag) if last else 's5_acc%d' % (n % 2)
                        P.op('pe', lambda e, prev=prev, A=A, M=M: e.matmul(pz[0][:, 0:M], lhsT=A, rhs=prev, start=True, stop=False),
                             reads=['s5_Am' + kb, prevk], writes=['s5_pz0'])
                        P.op('pe', lambda e, n=n, M=M, R=R, src=src: e.matmul(pz[0][:, 0:M], lhsT=identf[:], rhs=src[:, sl_(n, M, R)], start=False, stop=True),
                             reads=['s5_identf', tag], writes=['s5_pz0'])
                        P.op('dve', lambda e, o=o, M=M: e.tensor_copy(out=o, in_=pz[0][:, 0:M]), reads=['s5_pz0'], writes=[ok])
                        cur = o
                horner(Z0, 8, 132, Am[b][:, 0, :], Z1[:, :], 's5_Z0')
                horner(Z1, 4, 33, Am[b][:, 1, :], Z2[:, :], 's5_dsts5_Z0')
                horner(Z2, 3, 11, Am[b][:, 2, :], Z3[:, :], 's5_dsts5_dsts5_Z0')
                P.op('pool', lambda e: e.memset(P4[:, 0:1], 0.0), writes=['s5_P4'])
                for q in range(10):
                    P.op('pe', lambda e, q=q, b=b: e.matmul(pt_[:, 0:1], lhsT=Am[b][:, 3, :], rhs=P4[:, q:q + 1], start=True, stop=False),
                         reads=['s5_Am' + kb, 's5_P4'], writes=['s5_pt'])
                    P.op('pe', lambda e, q=q: e.matmul(pt_[:, 0:1], lhsT=identf[:], rhs=Z3[:, q:q + 1], start=False, stop=True),
                         reads=['s5_identf', 's5_dsts5_dsts5_dsts5_Z0'], writes=['s5_pt'])
                    P.op('dve', lambda e, q=q: e.tensor_copy(out=P4[:, q + 1:q + 2], in_=pt_[:, 0:1]), reads=['s5_pt'], writes=['s5_P4'])

                def expand(Pc, Zs, R, M, A, Pf, pck, zk, pfk):
                    P.op('pool', lambda e, Pf=Pf, Pc=Pc, R=R, M=M: e.tensor_copy(out=Pf[:, sl_(0, M, R)], in_=Pc[:, 0:M]), reads=[pck], writes=[pfk])
                    for n in range(R - 1):
                        P.op('pe', lambda e, n=n, Pf=Pf, A=A, R=R, M=M: e.matmul(pz[1][:, 0:M], lhsT=A, rhs=Pf[:, sl_(n, M, R)], start=True, stop=False),
                             reads=['s5_Am' + kb, pfk], writes=['s5_pz1'])
                        P.op('pe', lambda e, n=n, Zs=Zs, R=R, M=M: e.matmul(pz[1][:, 0:M], lhsT=identf[:], rhs=Zs[:, sl_(n, M, R)], start=False, stop=True),
                             reads=['s5_identf', zk], writes=['s5_pz1'])
                        P.op('act', lambda e, n=n, Pf=Pf, R=R, M=M: e.copy(out=Pf[:, sl_(n + 1, M, R)], in_=pz[1][:, 0:M]), reads=['s5_pz1'], writes=[pfk])
                expand(P4, Z2, 3, 11, Am[b][:, 2, :], P3, 's5_P4', 's5_dsts5_dsts5_Z0', 's5_P3')
                expand(P3, Z1, 4, 33, Am[b][:, 1, :], P2, 's5_P3', 's5_dsts5_Z0', 's5_P2')
                expand(P2, Z0, 8, 132, Am[b][:, 0, :], P1, 's5_P2', 's5_Z0', 's5_P1')
                for pc in range(3):
                    c0 = pc * 352
                    Pv = P1[:, c0:c0 + 352] if d == 0 else P1[:, sl_(NJ - 1 - c0, 352, -1)]
                    P.op('pe', lambda e, b=b, pc=pc, Pv=Pv: e.matmul(pz[pc][:, :], lhsT=MY[b][:], rhs=Pv, start=True, stop=False),
                         reads=['s5_MY' + kb, 's5_P1'], writes=['s5_pz%d' % pc])
                    P.op('pe', lambda e, b=b, pc=pc, c0=c0: e.matmul(pz[pc][:, :], lhsT=Mi[b][:], rhs=U[b][:, c0:c0 + 352], start=False, stop=True),
                         reads=['s5_Mi' + kb, 's5_U' + kb], writes=['s5_pz%d' % pc])
                    if d == 0:
                        P.op('act', lambda e, pc=pc, c0=c0: e.copy(out=Y[0][:, c0:c0 + 352], in_=pz[pc][:, :]), reads=['s5_pz%d' % pc], writes=['s5_Y0'])
                    else:
                        P.op('dve', lambda e, pc=pc, c0=c0: e.tensor_copy(out=Y[1][:, c0:c0 + 352], in_=pz[pc][:, :]), reads=['s5_pz%d' % pc], writes=['s5_Y1'])
                if d == 1:
                    yi = gi % 2
                    P.op('pool', lambda e, yi=yi: e.tensor_tensor(out=Ys[yi][:, 32:NJ], in0=Y[0][:, 32:NJ], in1=Y[1][:, 0:NJ - 32], op=ALU.add),
                         reads=['s5_Y0', 's5_Y1'], writes=['s5_Ys%d' % yi])
                    P.op('pool', lambda e, yi=yi: e.tensor_tensor(out=Ys[yi][:, 0:32], in0=Y[0][:, 0:32], in1=Y[1][:, NJ - 32:NJ], op=ALU.add),
                         reads=['s5_Y0', 's5_Y1'], writes=['s5_Ys%d' % yi])
                    P.dma('sp', g.YB[gi].rearrange("i c j -> (i c) j"), Ys[yi][:], reads=['s5_Ys%d' % yi], writes=[('YB', gi)])


def phase_s5_readout(nc, P, g, l):
    with ExitStack() as es:
        sb, ps = mk_alloc(nc, es)
        gw = sb("sr_gw", [128, 4, 512], BF16)
        P.dma('pool', gw[:], g.s5_glu_w[l, :, :].rearrange("(k p) n -> p k n", p=128), writes=['sr_gw'])
        gb = sb("sr_gb", [128, 4], F32)
        P.dma('sp', gb[:], g.s5_glu_bp[l], writes=['sr_gb'])
        ident = sb("sr_ident", [128, 128], BF16)
        P.dma('pool', ident[:], g.c_ident[:, :], writes=['sr_ident'])
        yT = [sb("sr_yT%d" % i, [128, 8, 64], F32) for i in range(2)]
        xo = sb("sr_xo", [128, 512], F32); sq = sb("sr_sq", [128, 512], F32); sg = sb("sr_sg", [128, 512], F32)
        glf = sb("sr_glf", [128, 4, 512], F32); glb = sb("sr_glb", [128, 4, 512], BF16)
        soT = sb("sr_soT", [128, 4, 512], BF16)
        so = [sb("sr_so%d" % i, [128, 512], BF16) for i in range(2)]
        pg = [ps("sr_pg%d" % i, [128, 512], F32) for i in range(2)]
        ptr = [ps("sr_ptr%d" % i, [128, 4, 128], BF16) for i in range(2)]
        nblk = (T + 511) // 512
        cn = 0
        for b in range(nblk):
            ntok = min(512, T - b * 512)
            nj = ntok // 8
            j0 = b * 64
            for cc in range(4):
                yi = cn % 2; cn += 1
                for gg in range(8):
                    gi = cc * 8 + gg
                    P.dma('sp', yT[yi][gg * 16:(gg + 1) * 16, :, :nj], g.YB[gi, :, :, j0:j0 + nj].rearrange("i c j -> c i j"),
                          reads=[('YB', gi)], writes=['sr_yT%d' % yi])
                P.op('dve', lambda e, yi=yi, nj=nj, ntok=ntok: e.tensor_copy(out=xo[:, :ntok].rearrange("p (j i) -> p j i", i=8),
                                                                            in_=yT[yi][:, :, :nj].rearrange("p i j -> p j i")),
                     reads=['sr_yT%d' % yi], writes=['sr_xo'])
                P.op('act', lambda e, ntok=ntok: e.activation(out=sq[:, :ntok], in_=xo[:, :ntok], func=AF.Square), reads=['sr_xo'], writes=['sr_sq'])
                P.op('dve', lambda e, ntok=ntok: e.tensor_scalar(out=sq[:, :ntok], in0=sq[:, :ntok], scalar1=0.044715, scalar2=1.0, op0=ALU.mult, op1=ALU.add),
                     reads=['sr_sq'], writes=['sr_sq'])
                P.op('dve', lambda e, ntok=ntok: e.tensor_tensor(out=sq[:, :ntok], in0=sq[:, :ntok], in1=xo[:, :ntok], op=ALU.mult), reads=['sr_sq', 'sr_xo'], writes=['sr_sq'])
                P.op('act', lambda e, ntok=ntok: e.activation(out=sg[:, :ntok], in_=sq[:, :ntok], func=AF.Sigmoid, scale=1.5957691216), reads=['sr_sq'], writes=['sr_sg'])
                P.op('dve', lambda e, cc=cc, ntok=ntok: e.tensor_tensor(out=glf[:, cc, :ntok], in0=xo[:, :ntok], in1=sg[:, :ntok], op=ALU.mult),
                     reads=['sr_xo', 'sr_sg'], writes=['sr_glf'])
                P.op('pool', lambda e, cc=cc, ntok=ntok: e.tensor_copy(out=glb[:, cc, :ntok], in_=glf[:, cc, :ntok]), reads=['sr_glf'], writes=['sr_glb'])
            for oc in range(4):
                pi = cn % 2; cn += 1
                for k in range(4):
                    P.op('pe', lambda e, k=k, oc=oc, pi=pi, ntok=ntok: e.matmul(pg[pi][:, :ntok], lhsT=gw[:, k, oc * 128:(oc + 1) * 128], rhs=glb[:, k, :ntok],
                                                                           start=(k == 0), stop=(k == 3)), reads=['sr_gw', 'sr_glb'], writes=['sr_pg%d' % pi])
                P.op('act', lambda e, oc=oc, pi=pi, ntok=ntok: e.activation(out=sg[:, :ntok], in_=pg[pi][:, :ntok], func=AF.Sigmoid, bias=gb[:, oc:oc + 1]),
                     reads=['sr_pg%d' % pi, 'sr_gb'], writes=['sr_sg'])
                P.op('dve', lambda e, oc=oc, ntok=ntok: e.tensor_tensor(out=soT[:, oc, :ntok], in0=glf[:, oc, :ntok], in1=sg[:, :ntok], op=ALU.mult),
                     reads=['sr_glf', 'sr_sg'], writes=['sr_soT'])
            for ti in range(ntok // 128):
                pi = cn % 2; cn += 1
                for oc in range(4):
                    P.op('pe', lambda e, oc=oc, pi=pi, ti=ti: e.transpose(out=ptr[pi][:, oc, :], in_=soT[:, oc, ti * 128:(ti + 1) * 128], identity=ident[:]),
                         reads=['sr_soT', 'sr_ident'], writes=['sr_ptr%d' % pi])
                P.op('act', lambda e, pi=pi: e.copy(out=so[pi][:].rearrange("p (a b) -> p a b", a=4), in_=ptr[pi][:]), reads=['sr_ptr%d' % pi], writes=['sr_so%d' % pi])
                t = b * 4 + ti
                P.dma('sp', g.SO[t * 128:(t + 1) * 128, :], so[pi][:], reads=['sr_so%d' % pi], writes=['SO'])


import os as _os
RW_DBG_CHUNKS = int(_os.environ.get('RW_DBG_CHUNKS', '0'))
RW_DBG_STOP = int(_os.environ.get('RW_DBG_STOP', '99'))
CDEC = 0.6065306597126334


def phase_rwkv(nc, P, g, l):
    with ExitStack() as es:
        sb, ps = mk_alloc(nc, es)
        MUP = sb("rw_MUP", [128, 14], F32); MUN = sb("rw_MUN", [128, 14], F32); C0 = sb("rw_C0", [128, 14], F32)
        P.dma('sp', MUP[:], g.rw_mup[l], writes=['rw_MUP']); P.dma('sp', MUN[:], g.rw_mun[l], writes=['rw_MUN'])
        P.op('dve', lambda e: e.tensor_tensor(out=C0[:], in0=MUP[:], in1=MUN[:], op=ALU.add), reads=['rw_MUP', 'rw_MUN'], writes=['rw_C0'])
        P.op('dve', lambda e: e.tensor_scalar(out=C0[:], in0=C0[:], scalar1=-1.0, scalar2=1.0, op0=ALU.mult, op1=ALU.add), reads=['rw_C0'], writes=['rw_C0'])
        PV = sb("rw_PV", [128, 4, 4], F32)
        P.dma('sp', PV[:, 0:3, :], g.rw_pv[l], writes=['rw_PV'])
        P.op('dve', lambda e: e.tensor_scalar(out=PV[:, 3, :], in0=PV[:, 1, :], scalar1=-1.0, scalar2=1.0, op0=ALU.mult, op1=ALU.add), reads=['rw_PV'], writes=['rw_PV'])
        WA0 = sb("rw_WA0", [128, 2, 2, 4], F32)
        P.dma('sp', WA0[:].rearrange("p a d h -> p (a d h)"), g.rw_wa0[l], writes=['rw_WA0'])
        LW = sb("rw_LW", [128, 2, 512], BF16)
        for d in range(2):
            P.dma('pool', LW[0:64, d, :], g.rwkv_w2[l, d, :, :], writes=['rw_LW'])
            P.dma('pool', LW[64:128, d, :], g.rwkv_a2[l, d, :, :], writes=['rw_LW'])
        G2 = sb("rw_G2", [128, 512], BF16)
        P.dma('pool', G2[:], g.rwkv_g2[l, :, :], writes=['rw_G2'])
        LNW = sb("rw_LNW", [128, 512], F32); LNB = sb("rw_LNB", [128, 512], F32)
        P.dma('sp', LNW[:], g.rwkv_ln_w[l, :].partition_broadcast(128), writes=['rw_LNW'])
        P.dma('sp', LNB[:], g.rwkv_ln_b[l, :].partition_broadcast(128), writes=['rw_LNB'])
        BO = sb("rw_BO", [128, 128], F32); HI = sb("rw_HI", [128, 2], F32)
        P.dma('sp', BO[:], g.c_bo[:, :], writes=['rw_BO']); P.dma('sp', HI[:], g.c_hi[:, :], writes=['rw_HI'])
        identb = sb("rw_identb", [128, 128], BF16); identf = sb("rw_identf", [128, 128], F32)
        P.dma('pool', identb[:], g.c_ident[:, :], writes=['rw_identb']); P.dma('sp', identf[:], g.c_ident[:, :], writes=['rw_identf'])
        MK = sb("rw_MK", [128, 8, 128], F32)
        P.dma('sp', MK[:], g.c_masks[:, :, :], writes=['rw_MK'])
        RS = sb("rw_RS", [128, 4, 128], F32)
        P.op('pool', lambda e: e.memset(RS[:], 1.0), writes=['rw_RS'])
        P.op('pool', lambda e: e.memset(RS[:, :, 0:1], 0.0), writes=['rw_RS'])
        W4 = [128, 4, 128]
        zraw = [sb("rw_zraw%d" % i, [128, 14, 130], F32) for i in range(2)]
        zT = sb("rw_zT", [128, 14, 128], F32); zt2 = sb("rw_zt2", [128, 14, 128], F32)
        tws = sb("rw_tws", [128, 128], BF16); sgd = sb("rw_sgd", [128, 128], BF16)
        gsb = sb("rw_gsb", [128, 512], F32)
        kk = sb("rw_kk", W4, F32); t1 = sb("rw_t1", W4, F32); t2 = sb("rw_t2", W4, F32)
        sig = sb("rw_sig", W4, F32); cs = sb("rw_cs", W4, F32); ex = sb("rw_ex", W4, F32)
        gi_ = sb("rw_gi", W4, F32); ge_ = sb("rw_ge", W4, F32); d4 = sb("rw_d4", W4, F32)
        e1 = sb("rw_e1", W4, F32); e2 = sb("rw_e2", W4, F32); e3 = sb("rw_e3", W4, F32); e4 = sb("rw_e4", W4, F32)
        av = sb("rw_av", W4, F32); kd = sb("rw_kd", W4, F32); ka_ = sb("rw_ka", W4, F32)
        tot = sb("rw_tot", [128, 4], F32); gamL = sb("rw_gamL", [128, 4], F32)
        ART = sb("rw_ART", [128, 4, 2, 128], BF16); BKT = sb("rw_BKT", [128, 4, 2, 128], BF16)
        TR3 = sb("rw_TR3", [128, 4, 3, 128], BF16); VKB = sb("rw_VKB", [128, 3, 512], BF16)
        bon = sb("rw_bon", [128, 8], F32)
        Sf = sb("rw_Sf", [128, 4, 64], F32); Sb = sb("rw_Sb", [128, 8, 64], BF16)
        BKZ = sb("rw_BKZ", [128, 8, 2, 128], BF16)
        P.op('pool', lambda e: e.memset(BKZ[:], 0.0), writes=['rw_BKZ'])
        XD = [sb("rw_XD%d" % i, [128, 2, 8, 128], BF16) for i in range(2)]
        XO = sb("rw_XO", [128, 8, 128], BF16)
        DD = [sb("rw_DD%d" % i, [128, 2, 8, 128], BF16) for i in range(2)]
        Mb = sb("rw_Mb", [128, 8, 128], BF16); Nb = sb("rw_Nb", [128, 8, 128], BF16); M2b = sb("rw_M2b", [128, 8, 128], BF16); Ssb = sb("rw_Ssb", [128, 8, 128], BF16)
        Ttb = sb("rw_Ttb", [128, 8, 128], BF16)
        ArbT = sb("rw_ArbT", [128, 8, 128], BF16); A3T = sb("rw_A3T", [128, 8, 2, 128], BF16)
        Wb = sb("rw_Wb", [128, 8, 64], BF16); Ub = sb("rw_Ub", [128, 8, 64], BF16)
        Yo = [sb("rw_Yo%d" % i, [128, 512], F32) for i in range(2)]
        y0 = sb("rw_y0", [128, 512], F32); b0 = sb("rw_b0", [128, 8], F32)
        st = sb("rw_st", [128, 4, 8], F32)
        ysq = sb("rw_ysq", [128, 512], F32); ao = [sb("rw_ao%d" % i, [128, 512], BF16) for i in range(2)]
        ptr = ps("rw_ptr", [128, 2, 3, 128], BF16)
        PL = ps("rw_PL", [128, 4, 128], F32)
        PA_ = ps("rw_PA", [128, 2, 256], F32); pA = [PA_[:, 0, :], PA_[:, 1, :]]
        N1 = ps("rw_N1", [128, 4, 128], F32); N2 = ps("rw_N2", [128, 4, 128], F32); N3 = ps("rw_N3", [128, 4, 128], F32)
        PW = ps("rw_PW", [128, 8, 64], F32)
        pY = ps("rw_pY", [128, 512], F32)
        seg_start = {0, 2, 66}; seg_end = {1, 65, 67}
        b14 = lambda t_: t_[:].unsqueeze(2).to_broadcast([128, 14, 128])
        b4 = lambda ap_: ap_.unsqueeze(2).to_broadcast(W4)
        cnt = 0
        for d in range(2):
            P.op('pool', lambda e: e.memset(Sf[:], 0.0), writes=['rw_Sf'])
            P.op('pool', lambda e: e.memset(Sb[:], 0.0), writes=['rw_Sb'])
            chunks = list(range(0, 66)) if d == 0 else [67, 66] + list(range(65, 1, -1))
            if RW_DBG_CHUNKS:
                chunks = chunks[:RW_DBG_CHUNKS]
            m_strict = 0 if d == 0 else 1
            m_T = (1, 3) if d == 0 else (0, 2)
            GI, GE = (cs, ex) if d == 0 else (gi_, ge_)
            gik, gek = ('rw_cs', 'rw_ex') if d == 0 else ('rw_gi', 'rw_ge')
            for c in chunks:
                tt = c if c <= 65 else c - 66
                zi = cnt % 2; cnt += 1
                zr = zraw[zi]; zk = 'rw_zraw%d' % zi
                s0 = c * 128
                lo = s0 - 1 if c not in seg_start else s0
                hi = s0 + 129 if c not in seg_end else s0 + 128
                if c in seg_start:
                    P.op('pool', lambda e, zr=zr: e.memset(zr[:, :, 0:1], 0.0), writes=[zk])
                if c in seg_end:
                    P.op('pool', lambda e, zr=zr: e.memset(zr[:, :, 129:130], 0.0), writes=[zk])
                P.dma('sp', zr[:, :, lo - s0 + 1:hi - s0 + 1], g.RWT[:, lo:hi].rearrange("(r p) t -> p r t", p=128), reads=['RWT'], writes=[zk])
                P.op('dve', lambda e, zr=zr: e.tensor_tensor(out=zT[:], in0=zr[:, :, 1:129], in1=b14(C0), op=ALU.mult), reads=[zk, 'rw_C0'], writes=['rw_zT'])
                P.op('pool', lambda e, zr=zr: e.tensor_tensor(out=zt2[:], in0=zr[:, :, 0:128], in1=b14(MUP), op=ALU.mult), reads=[zk, 'rw_MUP'], writes=['rw_zt2'])
                P.op('dve', lambda e: e.tensor_tensor(out=zT[:], in0=zT[:], in1=zt2[:], op=ALU.add), reads=['rw_zT', 'rw_zt2'], writes=['rw_zT'])
                P.op('pool', lambda e, zr=zr: e.tensor_tensor(out=zt2[:], in0=zr[:, :, 2:130], in1=b14(MUN), op=ALU.mult), reads=[zk, 'rw_MUN', 'rw_zT'], writes=['rw_zt2'])
                P.op('dve', lambda e: e.tensor_tensor(out=zT[:], in0=zT[:], in1=zt2[:], op=ALU.add), reads=['rw_zT', 'rw_zt2'], writes=['rw_zT'])
                r4 = zT[:, 0:4, :]; k4 = zT[:, 4:8, :]; v4 = zT[:, 8:12, :]
                if RW_DBG_STOP <= 1:
                    continue
                P.op('act', lambda e: e.activation(out=tws[0:64, :], in_=zT[0:64, 12, :], func=AF.Tanh), reads=['rw_zT'], writes=['rw_tws'])
                P.op('pool', lambda e: e.tensor_copy(out=tws[64:128, :], in_=zT[64:128, 12, :]), reads=['rw_zT'], writes=['rw_tws'])
                if d == 1:
                    P.op('act', lambda e: e.activation(out=sgd[:], in_=zT[:, 13, :], func=AF.Sigmoid), reads=['rw_zT'], writes=['rw_sgd'])
                    P.op('pe', lambda e: e.matmul(PL[:].rearrange("p a b -> p (a b)"), lhsT=sgd[:], rhs=G2[:], start=True, stop=True), reads=['rw_sgd', 'rw_G2'], writes=['rw_PL'])
                    P.op('act', lambda e: e.copy(out=gsb[:], in_=PL[:].rearrange("p a b -> p (a b)")), reads=['rw_PL'], writes=['rw_gsb'])
                P.op('dve', lambda e: e.tensor_tensor(out=kk[:], in0=k4, in1=b4(PV[:, 0, :]), op=ALU.mult), reads=['rw_zT', 'rw_PV'], writes=['rw_kk'])
                P.op('act', lambda e: e.activation(out=t1[:], in_=kk[:], func=AF.Square), reads=['rw_kk'], writes=['rw_t1'])
                P.op('pe', lambda e: e.matmul(PL[:].rearrange("p a b -> p (a b)"), lhsT=BO[:], rhs=t1[:].rearrange("p a b -> p (a b)"), start=True, stop=True), reads=['rw_BO', 'rw_t1'], writes=['rw_PL'])
                P.op('act', lambda e: e.activation(out=t1[:], in_=PL[:], func=AF.Sqrt), reads=['rw_PL'], writes=['rw_t1'])
                P.op('dve', lambda e: e.tensor_scalar(out=t1[:], in0=t1[:], scalar1=1e-12, scalar2=None, op0=ALU.max), reads=['rw_t1'], writes=['rw_t1'])
                P.op('dve', lambda e: e.reciprocal(out=t1[:], in_=t1[:]), reads=['rw_t1'], writes=['rw_t1'])
                P.op('dve', lambda e: e.tensor_tensor(out=kk[:], in0=kk[:], in1=t1[:], op=ALU.mult), reads=['rw_kk', 'rw_t1'], writes=['rw_kk'])
                for hp in range(4):
                    P.op('pe', lambda e, hp=hp, d=d: e.matmul(PL[:, hp, :], lhsT=LW[0:64, d, hp * 128:(hp + 1) * 128], rhs=tws[0:64, :], start=True, stop=True), reads=['rw_LW', 'rw_tws', 'rw_t1'], writes=['rw_PL'])
                P.op('dve', lambda e, d=d: e.tensor_tensor(out=sig[:], in0=PL[:], in1=b4(WA0[:, 0, d, :]), op=ALU.add), reads=['rw_PL', 'rw_WA0'], writes=['rw_sig'])
                P.op('act', lambda e: e.activation(out=sig[:], in_=sig[:], func=AF.Sigmoid), reads=['rw_sig'], writes=['rw_sig'])
                for hp in range(4):
                    P.op('pe', lambda e, hp=hp, d=d: e.matmul(PL[:, hp, :], lhsT=LW[64:128, d, hp * 128:(hp + 1) * 128], rhs=tws[64:128, :], start=True, stop=True), reads=['rw_LW', 'rw_tws', 'rw_sig'], writes=['rw_PL'])
                P.op('dve', lambda e, d=d: e.tensor_tensor(out=av[:], in0=PL[:], in1=b4(WA0[:, 1, d, :]), op=ALU.add), reads=['rw_PL', 'rw_WA0'], writes=['rw_av'])
                P.op('act', lambda e: e.activation(out=av[:], in_=av[:], func=AF.Sigmoid), reads=['rw_av'], writes=['rw_av'])
                if RW_DBG_STOP <= 2:
                    continue
                fl = lambda t_: t_[:].rearrange("p a b -> p (a b)")
                P.op('dve', lambda e: e.tensor_tensor_scan(out=fl(cs), data0=fl(RS), data1=fl(sig), initial=0.0, op0=ALU.mult, op1=ALU.add), reads=['rw_sig', 'rw_RS'], writes=['rw_cs'])
                P.op('pool', lambda e: e.tensor_copy(out=tot[:], in_=cs[:, :, 127]), reads=['rw_cs'], writes=['rw_tot'])
                P.op('act', lambda e: e.activation(out=gamL[:], in_=cs[:, :, 127], func=AF.Exp, scale=-CDEC), reads=['rw_cs'], writes=['rw_gamL'])
                P.op('dve', lambda e: e.tensor_tensor(out=ex[:], in0=cs[:], in1=sig[:], op=ALU.subtract), reads=['rw_cs', 'rw_sig'], writes=['rw_ex'])
                if d == 1:
                    P.op('dve', lambda e: e.tensor_tensor(out=gi_[:], in0=b4(tot[:, :]), in1=ex[:], op=ALU.subtract), reads=['rw_ex', 'rw_tot'], writes=['rw_gi'])
                    P.op('dve', lambda e: e.tensor_tensor(out=ge_[:], in0=b4(tot[:, :]), in1=cs[:], op=ALU.subtract), reads=['rw_cs', 'rw_tot'], writes=['rw_ge'])
                P.op('dve', lambda e, GI=GI: e.tensor_tensor(out=d4[:], in0=b4(tot[:, :]), in1=GI[:], op=ALU.subtract), reads=[gik, 'rw_tot'], writes=['rw_d4'])
                P.op('act', lambda e, GI=GI: e.activation(out=e1[:], in_=GI[:], func=AF.Exp, scale=-CDEC), reads=[gik], writes=['rw_e1'])
                P.op('act', lambda e, GI=GI: e.activation(out=e2[:], in_=GI[:], func=AF.Exp, scale=CDEC), reads=[gik], writes=['rw_e2'])
                P.op('act', lambda e, GE=GE: e.activation(out=e3[:], in_=GE[:], func=AF.Exp, scale=-CDEC), reads=[gek], writes=['rw_e3'])
                P.op('act', lambda e: e.activation(out=e4[:], in_=d4[:], func=AF.Exp, scale=-CDEC), reads=['rw_d4'], writes=['rw_e4'])
                if RW_DBG_STOP <= 3:
                    continue
                P.op('dve', lambda e: e.tensor_tensor(out=t2[:], in0=av[:], in1=b4(PV[:, 1, :]), op=ALU.mult), reads=['rw_av', 'rw_PV'], writes=['rw_t2'])
                P.op('dve', lambda e: e.tensor_tensor(out=t2[:], in0=t2[:], in1=b4(PV[:, 3, :]), op=ALU.add), reads=['rw_t2', 'rw_PV'], writes=['rw_t2'])
                P.op('dve', lambda e: e.tensor_tensor(out=kd[:], in0=k4, in1=t2[:], op=ALU.mult), reads=['rw_zT', 'rw_t2'], writes=['rw_kd'])
                P.op('pool', lambda e: e.tensor_tensor(out=ka_[:], in0=kk[:], in1=av[:], op=ALU.mult), reads=['rw_kk', 'rw_av'], writes=['rw_ka'])
                P.op('dve', lambda e: e.scalar_tensor_tensor(out=ART[:, :, 0, :], in0=kk[:], scalar=-1.0, in1=e3[:], op0=ALU.mult, op1=ALU.mult), reads=['rw_kk', 'rw_e3'], writes=['rw_ART'])
                P.op('pool', lambda e: e.tensor_tensor(out=ART[:, :, 1, :], in0=r4, in1=e1[:], op=ALU.mult), reads=['rw_zT', 'rw_e1'], writes=['rw_ART'])
                P.op('dve', lambda e: e.tensor_tensor(out=BKT[:, :, 0, :], in0=ka_[:], in1=e2[:], op=ALU.mult), reads=['rw_ka', 'rw_e2'], writes=['rw_BKT'])
                P.op('pool', lambda e: e.tensor_tensor(out=BKT[:, :, 1, :], in0=kd[:], in1=e2[:], op=ALU.mult), reads=['rw_kd', 'rw_e2'], writes=['rw_BKT'])
                P.op('act', lambda e: e.copy(out=TR3[:, :, 0, :], in_=v4), reads=['rw_zT'], writes=['rw_TR3'])
                P.op('dve', lambda e: e.tensor_tensor(out=TR3[:, :, 1, :], in0=kd[:], in1=e4[:], op=ALU.mult), reads=['rw_kd', 'rw_e4'], writes=['rw_TR3'])
                P.op('pool', lambda e: e.tensor_tensor(out=TR3[:, :, 2, :], in0=ka_[:], in1=e4[:], op=ALU.mult), reads=['rw_ka', 'rw_e4'], writes=['rw_TR3'])
                P.op('dve', lambda e: e.tensor_tensor(out=t2[:], in0=r4, in1=kd[:], op=ALU.mult), reads=['rw_zT', 'rw_kd', 'rw_t2'], writes=['rw_t2'])
                P.op('dve', lambda e: e.tensor_tensor(out=t2[:], in0=t2[:], in1=b4(PV[:, 2, :]), op=ALU.mult), reads=['rw_t2', 'rw_PV'], writes=['rw_t2'])
                for hp in range(4):
                    P.op('pe', lambda e, hp=hp: e.matmul(PL[:, 0, 2 * hp:2 * hp + 2], lhsT=t2[:, hp, :], rhs=HI[:], start=True, stop=True), reads=['rw_t2', 'rw_HI', 'rw_av'], writes=['rw_PL'])
                P.op('dve', lambda e: e.tensor_copy(out=bon[:], in_=PL[:, 0, 0:8]), reads=['rw_PL'], writes=['rw_bon'])
                for h2 in range(2):
                    for a_ in range(2):
                        hp = h2 * 2 + a_
                        for j in range(3):
                            P.op('pe', lambda e, hp=hp, a_=a_, j=j: e.transpose(out=ptr[:, a_, j, :], in_=TR3[:, hp, j, :], identity=identb[:]), reads=['rw_TR3', 'rw_identb'], writes=['rw_ptr'])
                    P.op('act', lambda e, h2=h2: e.copy(out=VKB[:, :, h2 * 256:(h2 + 1) * 256].rearrange("p j (a c) -> p j a c", a=2), in_=ptr.rearrange("p a j c -> p j a c")),
                         reads=['rw_ptr'], writes=['rw_VKB'])
                if RW_DBG_STOP <= 4:
                    continue
                hsl = lambda h: (h // 2, slice((h % 2) * 64, (h % 2) * 64 + 64))
                P.op('pool', lambda e: e.tensor_copy(out=BKZ[0:64, 0::2, :, :], in_=BKT[0:64, :, :, :]), reads=['rw_BKT'], writes=['rw_BKZ'])
                P.op('pool', lambda e: e.tensor_copy(out=BKZ[64:128, 1::2, :, :], in_=BKT[64:128, :, :, :]), reads=['rw_BKT'], writes=['rw_BKZ'])
                md_ = 4 if d == 0 else 5
                mo_ = 6 if d == 0 else 7
                mdT_ = 5 if d == 0 else 4
                for grp in range(2):
                    for j in range(4):
                        h = grp * 4 + j; hp, hr = hsl(h)
                        P.op('pe', lambda e, hp=hp, j=j, grp=grp: e.matmul(N1[:, j, :], lhsT=ART[:, hp, 0, :], rhs=BKZ[:, grp * 4 + j, 0, :], start=True, stop=True), reads=['rw_ART', 'rw_BKZ'], writes=['rw_N1'])
                    P.op('dve', lambda e, grp=grp, md_=md_: e.tensor_tensor(out=XD[0][:, 0, grp * 4:(grp + 1) * 4, :], in0=N1[:], in1=MK[:, md_:md_ + 1, :].to_broadcast([128, 4, 128]), op=ALU.mult),
                         reads=['rw_N1', 'rw_MK'], writes=['rw_XD0_g%d' % grp])
                    P.op('dve', lambda e, grp=grp, mo_=mo_: e.tensor_tensor(out=XO[:, grp * 4:(grp + 1) * 4, :], in0=N1[:], in1=MK[:, mo_:mo_ + 1, :].to_broadcast([128, 4, 128]), op=ALU.mult),
                         reads=['rw_N1', 'rw_MK'], writes=['rw_XO_g%d' % grp])
                for h in range(8):
                    hp, hr = hsl(h); ai = h % 2
                    P.op('pe', lambda e, hp=hp, hr=hr, ai=ai, h=h: e.matmul(pA[ai][:, :], lhsT=BKZ[:, h, 0, :], rhs=ART[:, hp, :, :], start=True, stop=True), reads=['rw_ART', 'rw_BKZ'], writes=['rw_pA%d' % ai])
                    P.op('dve', lambda e, ai=ai, h=h, mdT_=mdT_: e.tensor_tensor(out=XD[0][:, 1, h, :], in0=pA[ai][:, 0:128], in1=MK[:, mdT_, :], op=ALU.mult), reads=['rw_pA%d' % ai, 'rw_MK'], writes=['rw_XD0_g%d' % (h // 4)])
                    P.op('dve', lambda e, ai=ai, h=h, m_T=m_T: e.tensor_tensor(out=ArbT[:, h, :], in0=pA[ai][:, 128:256], in1=MK[:, m_T[1], :], op=ALU.mult), reads=['rw_pA%d' % ai, 'rw_MK'], writes=['rw_ArbT'])
                    P.op('pe', lambda e, hp=hp, hr=hr, ai=ai, h=h: e.matmul(pA[ai][:, :], lhsT=BKZ[:, h, 1, :], rhs=ART[:, hp, :, :], start=True, stop=True), reads=['rw_ART', 'rw_BKZ', 'rw_ArbT', 'rw_XD0_g%d' % (h // 4)], writes=['rw_pA%d' % ai])
                    P.op('dve', lambda e, ai=ai, h=h, m_T=m_T: e.tensor_tensor(out=A3T[:, h, 0, :], in0=pA[ai][:, 0:128], in1=MK[:, m_T[0], :], op=ALU.mult), reads=['rw_pA%d' % ai, 'rw_MK'], writes=['rw_A3T'])
                    P.op('dve', lambda e, ai=ai, h=h, m_T=m_T: e.tensor_tensor(out=A3T[:, h, 1, :], in0=pA[ai][:, 128:256], in1=MK[:, m_T[1], :], op=ALU.mult), reads=['rw_pA%d' % ai, 'rw_MK'], writes=['rw_A3T'])
                if RW_DBG_STOP <= 5:
                    continue
                idb4 = identb[:].unsqueeze(1).unsqueeze(1).to_broadcast([128, 2, 4, 128])
                for grp in range(2):
                    hs = slice(grp * 4, grp * 4 + 4)
                    gk = '_g%d' % grp
                    P.op('pool', lambda e, hs=hs: e.tensor_tensor(out=DD[0][:, :, hs, :], in0=XD[0][:, :, hs, :], in1=idb4, op=ALU.add), reads=['rw_XD0' + gk, 'rw_identb'], writes=['rw_DD0' + gk])
                for q_ in range(1, 5):
                    o = (q_ - 1) % 2; n = q_ % 2
                    xo_, xn_ = XD[o], XD[n]; do_, dn_ = DD[o], DD[n]
                    for grp in range(2):
                        hs = slice(grp * 4, grp * 4 + 4)
                        gk = '_g%d' % grp
                        for j in range(4):
                            h = grp * 4 + j
                            P.op('pe', lambda e, xo_=xo_, h=h, j=j: e.matmul(N1[:, j, :], lhsT=xo_[:, 1, h, :], rhs=xo_[:, 0, h, :], start=True, stop=True), reads=['rw_XD%d' % o + gk], writes=['rw_N1'])
                        for j in range(4):
                            h = grp * 4 + j
                            P.op('pe', lambda e, xo_=xo_, h=h, j=j: e.matmul(N2[:, j, :], lhsT=xo_[:, 0, h, :], rhs=xo_[:, 1, h, :], start=True, stop=True), reads=['rw_XD%d' % o + gk], writes=['rw_N2'])
                        P.op('act', lambda e, xn_=xn_, hs=hs: e.copy(out=xn_[:, 0, hs, :], in_=N1[:]), reads=['rw_N1'], writes=['rw_XD%d' % n + gk])
                        P.op('dve', lambda e, xn_=xn_, hs=hs: e.tensor_copy(out=xn_[:, 1, hs, :], in_=N2[:]), reads=['rw_N2'], writes=['rw_XD%d' % n + gk])
                        for j in range(4):
                            h = grp * 4 + j
                            P.op('pe', lambda e, xn_=xn_, do_=do_, h=h, j=j: e.matmul(N3[:, j, :], lhsT=do_[:, 1, h, :], rhs=xn_[:, 0, h, :], start=True, stop=True), reads=['rw_XD%d' % n + gk, 'rw_DD%d' % o + gk], writes=['rw_N3'])
                        for j in range(4):
                            h = grp * 4 + j
                            P.op('pe', lambda e, xn_=xn_, do_=do_, h=h, j=j: e.matmul(N1[:, j, :], lhsT=xn_[:, 0, h, :], rhs=do_[:, 1, h, :], start=True, stop=True), reads=['rw_XD%d' % n + gk, 'rw_DD%d' % o + gk], writes=['rw_N1'])
                        P.op('dve', lambda e, dn_=dn_, do_=do_, hs=hs: e.tensor_tensor(out=dn_[:, 0, hs, :], in0=N3[:], in1=do_[:, 0, hs, :], op=ALU.add), reads=['rw_N3', 'rw_DD%d' % o + gk], writes=['rw_DD%d' % n + gk])
                        P.op('dve', lambda e, dn_=dn_, do_=do_, hs=hs: e.tensor_tensor(out=dn_[:, 1, hs, :], in0=N1[:], in1=do_[:, 1, hs, :], op=ALU.add), reads=['rw_N1', 'rw_DD%d' % o + gk], writes=['rw_DD%d' % n + gk])
                Df = DD[0]
                for grp in range(2):
                    hs = slice(grp * 4, grp * 4 + 4)
                    gk = '_g%d' % grp
                    for j in range(4):
                        h = grp * 4 + j
                        P.op('pe', lambda e, h=h, j=j: e.matmul(N1[:, j, :], lhsT=XO[:, h, :], rhs=Df[:, 1, h, :], start=True, stop=True), reads=['rw_XO' + gk, 'rw_DD0' + gk], writes=['rw_N1'])
                    for j in range(4):
                        h = grp * 4 + j
                        P.op('pe', lambda e, h=h, j=j: e.matmul(N2[:, j, :], lhsT=Df[:, 1, h, :], rhs=XO[:, h, :], start=True, stop=True), reads=['rw_XO' + gk, 'rw_DD0' + gk], writes=['rw_N2'])
                    P.op('act', lambda e, hs=hs: e.copy(out=Mb[:, hs, :], in_=N1[:]), reads=['rw_N1'], writes=['rw_Mb' + gk])
                    P.op('dve', lambda e, hs=hs: e.tensor_copy(out=Nb[:, hs, :], in_=N2[:]), reads=['rw_N2'], writes=['rw_Nb' + gk])
                    for j in range(4):
                        h = grp * 4 + j
                        P.op('pe', lambda e, h=h, j=j: e.matmul(N3[:, j, :], lhsT=Nb[:, h, :], rhs=Mb[:, h, :], start=True, stop=True), reads=['rw_Nb' + gk, 'rw_Mb' + gk], writes=['rw_N3'])
                    P.op('act', lambda e, hs=hs: e.copy(out=M2b[:, hs, :], in_=N3[:]), reads=['rw_N3'], writes=['rw_M2b' + gk])
                    for j in range(4):
                        h = grp * 4 + j
                        P.op('pe', lambda e, h=h, j=j: e.matmul(N1[:, j, :], lhsT=Nb[:, h, :], rhs=M2b[:, h, :], start=True, stop=True), reads=['rw_Nb' + gk, 'rw_M2b' + gk], writes=['rw_N1'])
                    P.op('dve', lambda e, hs=hs: e.tensor_tensor(out=Ssb[:, hs, :], in0=N1[:], in1=Mb[:, hs, :], op=ALU.add), reads=['rw_N1', 'rw_Mb' + gk], writes=['rw_Ssb' + gk])
                    P.op('pool', lambda e, hs=hs: e.tensor_tensor(out=Ssb[:, hs, :], in0=Ssb[:, hs, :], in1=M2b[:, hs, :], op=ALU.add), reads=['rw_Ssb' + gk, 'rw_M2b' + gk], writes=['rw_Ssb' + gk])
                    for j in range(4):
                        h = grp * 4 + j
                        P.op('pe', lambda e, h=h, j=j: e.matmul(N2[:, j, :], lhsT=Df[:, 0, h, :], rhs=Ssb[:, h, :], start=True, stop=True), reads=['rw_DD0' + gk, 'rw_Ssb' + gk], writes=['rw_N2'])
                    P.op('dve', lambda e, hs=hs: e.tensor_tensor(out=Ttb[:, hs, :], in0=N2[:], in1=Df[:, 1, hs, :], op=ALU.add), reads=['rw_N2', 'rw_DD0' + gk], writes=['rw_Ttb'])
                for h in range(8):
                    hp, hr = hsl(h); hc = slice(h * 64, h * 64 + 64)
                    P.op('pe', lambda e, h=h, hc=hc: e.matmul(PW[:, h, :], lhsT=A3T[:, h, 0, :], rhs=VKB[:, 0, hc], start=True, stop=False), reads=['rw_A3T', 'rw_VKB'], writes=['rw_PW'])
                    P.op('pe', lambda e, h=h, hp=hp, hr=hr: e.matmul(PW[:, h, :], lhsT=ART[:, hp, 0, :], rhs=Sb[:, h, :], start=False, stop=True), reads=['rw_ART', 'rw_Sb'], writes=['rw_PW'])
                P.op('act', lambda e: e.copy(out=Wb[:], in_=PW[:]), reads=['rw_PW'], writes=['rw_Wb'])
                for h in range(8):
                    P.op('pe', lambda e, h=h: e.matmul(PW[:, h, :], lhsT=Ttb[:, h, :], rhs=Wb[:, h, :], start=True, stop=True), reads=['rw_Ttb', 'rw_Wb'], writes=['rw_PW'])
                P.op('act', lambda e: e.copy(out=Ub[:], in_=PW[:]), reads=['rw_PW'], writes=['rw_Ub'])
                for h in range(8):
                    hp, hr = hsl(h); hc = slice(h * 64, h * 64 + 64)
                    P.op('pe', lambda e, h=h, hc=hc: e.matmul(pY[:, hc], lhsT=A3T[:, h, 1, :], rhs=VKB[:, 0, hc], start=True, stop=False), reads=['rw_A3T', 'rw_VKB'], writes=['rw_pY'])
                    P.op('pe', lambda e, h=h, hc=hc: e.matmul(pY[:, hc], lhsT=ArbT[:, h, :], rhs=Ub[:, h, :], start=False, stop=False), reads=['rw_ArbT', 'rw_Ub'], writes=['rw_pY'])
                    P.op('pe', lambda e, h=h, hc=hc, hp=hp, hr=hr: e.matmul(pY[:, hc], lhsT=ART[:, hp, 1, :], rhs=Sb[:, h, :], start=False, stop=True), reads=['rw_ART', 'rw_Sb'], writes=['rw_pY'])
                for h in range(8):
                    hp, hr = hsl(h); hc = slice(h * 64, h * 64 + 64)
                    P.op('pe', lambda e, h=h, hp=hp, hc=hc: e.matmul(PW[:, h, :], lhsT=VKB[:, 1, hp * 128:(hp + 1) * 128], rhs=VKB[:, 0, hc], start=True, stop=False), reads=['rw_VKB', 'rw_Ub'], writes=['rw_PW'])
                    P.op('pe', lambda e, h=h, hp=hp: e.matmul(PW[:, h, :], lhsT=VKB[:, 2, hp * 128:(hp + 1) * 128], rhs=Ub[:, h, :], start=False, stop=True), reads=['rw_VKB', 'rw_Ub'], writes=['rw_PW'])
                PW4 = PW[:].rearrange("p (a b) v -> p a b v", b=2)
                for par in range(2):
                    rows = slice(par * 64, par * 64 + 64)
                    P.op('dve', lambda e, rows=rows: e.tensor_tensor(out=Sf[rows], in0=Sf[rows], in1=gamL[rows, :].unsqueeze(2).to_broadcast([64, 4, 64]), op=ALU.mult), reads=['rw_Sf', 'rw_gamL'], writes=['rw_Sf'])
                    P.op('dve', lambda e, rows=rows, par=par: e.tensor_tensor(out=Sf[rows], in0=Sf[rows], in1=PW4[rows, :, par, :], op=ALU.add), reads=['rw_Sf', 'rw_PW'], writes=['rw_Sf'])
                P.op('pool', lambda e: e.tensor_copy(out=Sb[0:64, 0::2, :], in_=Sf[0:64, :, :]), reads=['rw_Sf'], writes=['rw_Sb'])
                P.op('pool', lambda e: e.tensor_copy(out=Sb[64:128, 1::2, :], in_=Sf[64:128, :, :]), reads=['rw_Sf'], writes=['rw_Sb'])
                if RW_DBG_STOP <= 7:
                    continue
                yi = cnt % 2
                if d == 0:
                    P.op('act', lambda e, yi=yi: e.copy(out=Yo[yi][:], in_=pY[:, :]), reads=['rw_pY'], writes=['rw_Yo%d' % yi])
                    P.dma('sp', g.YR[tt * 128:(tt + 1) * 128, :], Yo[yi][:], reads=['rw_Yo%d' % yi], writes=[('YR', tt)])
                    P.dma('sp', g.BON[tt * 128:(tt + 1) * 128, :], bon[:], reads=['rw_bon'], writes=[('BON', tt)])
                else:
                    P.dma('sp', y0[:], g.YR[tt * 128:(tt + 1) * 128, :], reads=[('YR', tt)], writes=['rw_y0'])
                    P.dma('sp', b0[:], g.BON[tt * 128:(tt + 1) * 128, :], reads=[('BON', tt)], writes=['rw_b0'])
                    Y_ = Yo[yi]; yk_ = 'rw_Yo%d' % yi
                    P.op('dve', lambda e, Y_=Y_: e.tensor_tensor(out=Y_[:], in0=pY[:, :], in1=y0[:], op=ALU.add), reads=['rw_pY', 'rw_y0'], writes=[yk_])
                    P.op('pool', lambda e: e.tensor_tensor(out=b0[:], in0=b0[:], in1=bon[:], op=ALU.add), reads=['rw_b0', 'rw_bon'], writes=['rw_b0'])
                    Y3 = Y_[:].rearrange("p (h v) -> p h v", h=8)
                    P.op('dve', lambda e, Y3=Y3: e.tensor_reduce(out=st[:, 0, :], in_=Y3, axis=AX.X, op=ALU.add), reads=[yk_], writes=['rw_st'])
                    P.op('act', lambda e, Y_=Y_: e.activation(out=ysq[:], in_=Y_[:], func=AF.Square), reads=[yk_], writes=['rw_ysq'])
                    P.op('dve', lambda e: e.tensor_reduce(out=st[:, 1, :], in_=ysq[:].rearrange("p (h v) -> p h v", h=8), axis=AX.X, op=ALU.add), reads=['rw_ysq'], writes=['rw_st'])
                    P.op('dve', lambda e: e.tensor_scalar(out=st[:, 0, :], in0=st[:, 0, :], scalar1=1.0 / 64, scalar2=None, op0=ALU.mult), reads=['rw_st'], writes=['rw_st'])
                    P.op('dve', lambda e: e.tensor_tensor(out=st[:, 2, :], in0=st[:, 0, :], in1=st[:, 0, :], op=ALU.mult), reads=['rw_st'], writes=['rw_st'])
                    P.op('dve', lambda e: e.scalar_tensor_tensor(out=st[:, 1, :], in0=st[:, 1, :], scalar=1.0 / 64, in1=st[:, 2, :], op0=ALU.mult, op1=ALU.subtract), reads=['rw_st'], writes=['rw_st'])
                    P.op('dve', lambda e: e.tensor_scalar(out=st[:, 1, :], in0=st[:, 1, :], scalar1=64e-5, scalar2=None, op0=ALU.add), reads=['rw_st'], writes=['rw_st'])
                    P.op('act', lambda e: e.activation(out=st[:, 1, :], in_=st[:, 1, :], func=AF.Sqrt), reads=['rw_st'], writes=['rw_st'])
                    P.op('dve', lambda e: e.reciprocal(out=st[:, 1, :], in_=st[:, 1, :]), reads=['rw_st'], writes=['rw_st'])
                    bcs = lambda col: st[:, col, :].unsqueeze(2).to_broadcast([128, 8, 64])
                    P.op('dve', lambda e, Y3=Y3: e.tensor_tensor(out=Y3, in0=Y3, in1=bcs(0), op=ALU.subtract), reads=[yk_, 'rw_st'], writes=[yk_])
                    P.op('dve', lambda e, Y3=Y3: e.tensor_tensor(out=Y3, in0=Y3, in1=bcs(1), op=ALU.mult), reads=[yk_, 'rw_st'], writes=[yk_])
                    P.op('pool', lambda e, Y_=Y_: e.tensor_tensor(out=Y_[:], in0=Y_[:], in1=LNW[:], op=ALU.mult), reads=[yk_, 'rw_LNW'], writes=[yk_])
                    P.op('pool', lambda e, Y_=Y_: e.tensor_tensor(out=Y_[:], in0=Y_[:], in1=LNB[:], op=ALU.add), reads=[yk_, 'rw_LNB'], writes=[yk_])
                    P.op('dve', lambda e: e.tensor_tensor(out=ysq[:].rearrange("p (h v) -> p h v", h=8), in0=VKB[:, 0, :].rearrange("p (h v) -> p h v", h=8),
                                                        in1=b0[:].unsqueeze(2).to_broadcast([128, 8, 64]), op=ALU.mult), reads=['rw_VKB', 'rw_b0', 'rw_ysq'], writes=['rw_ysq'])
                    P.op('pool', lambda e, Y_=Y_: e.tensor_tensor(out=Y_[:], in0=Y_[:], in1=ysq[:], op=ALU.add), reads=[yk_, 'rw_ysq'], writes=[yk_])
                    P.op('dve', lambda e, Y_=Y_, yi=yi: e.tensor_tensor(out=ao[yi][:], in0=Y_[:], in1=gsb[:], op=ALU.mult), reads=[yk_, 'rw_gsb'], writes=['rw_ao%d' % yi])
                    P.dma('sp', g.AO[tt * 128:(tt + 1) * 128, :], ao[yi][:], reads=['rw_ao%d' % yi], writes=['AO'])
```
